# Optimizing a Trainium2 kernel written in Bass

```python
import jax, jax.numpy as jnp
from jax import lax
import numpy as np

D_MODEL = 1024
BATCH = 8
SEQ = 2048
DEPTH = 4

CTX_LEN = 256
GRID_W = 64

N_MIXERS = 3
N_RG_LAYERS = (DEPTH + 2) // 3
N_MLA_LAYERS = (DEPTH + 1) // 3
N_MLSTM_LAYERS = DEPTH // 3

RG_WIDTH = D_MODEL
RG_BLOCKS = 16
RG_BLOCK_W = RG_WIDTH // RG_BLOCKS
RG_CONV_W = 4
RG_C = 8.0

MLA_HEADS = 16
MLA_Q_RANK = D_MODEL // 4
MLA_KV_RANK = D_MODEL // 8
MLA_NOPE = 64
MLA_ROPE = 32
MLA_V = 64
MLA_QK = MLA_NOPE + MLA_ROPE
ATTN_SCALE = MLA_QK ** -0.5
ROPE_AXIS_DIM = MLA_ROPE // 2
ROPE_BASE = 10000.0
Q_BLOCK = 128

ML_HEADS = 4
ML_DV = D_MODEL // ML_HEADS
ML_DQK = ML_DV // 2
ML_IN = 2 * ML_HEADS * ML_DQK + 2 * ML_HEADS * ML_DV + 4 * ML_HEADS
ML_CHUNK = 64
ML_M_INIT = -1e30

MOE_GROUPS = 8
MOE_PER_GROUP = 8
MOE_EXPERTS = MOE_GROUPS * MOE_PER_GROUP
MOE_TOPK = 2
MOE_FF = D_MODEL // 4
MOE_BLOCK = 128

RMS_EPS = 1e-6

kernel_name = "hybrid_flow_backbone_rglru_mla_mlstm_hmoe"


def rms_norm(x, g):
    xf = x.astype(jnp.float32)
    y = xf * lax.rsqrt(jnp.mean(xf * xf, axis=-1, keepdims=True) + RMS_EPS)
    return (y * g.astype(jnp.float32)).astype(x.dtype)


def depthwise_conv(u, w, b):
    left = RG_CONV_W // 2
    y = lax.conv_general_dilated(u, w[:, None, :].astype(u.dtype), window_strides=(1,),
                                 padding=[(left, RG_CONV_W - 1 - left)],
                                 dimension_numbers=('NWC', 'WIO', 'NWC'),
                                 feature_group_count=u.shape[-1])
    return y + b


def _lin_combine(left, right):
    a1, b1 = left
    a2, b2 = right
    return a1 * a2, a2 * b1 + b2


def rglru_scan(u, w_gate, b_gate, lam, h0):
    B, L, _ = u.shape
    ub = u.reshape(B, L, RG_BLOCKS, RG_BLOCK_W)
    g = jnp.einsum('blni,gnij->gblnj', ub, w_gate.astype(jnp.float32)).reshape(2, B, L, RG_WIDTH)
    g = g + b_gate.astype(jnp.float32)[:, None, None, :]
    r = jax.nn.sigmoid(g[0])
    i_g = jax.nn.sigmoid(g[1])
    log_a = -RG_C * r * jax.nn.softplus(-lam.astype(jnp.float32))
    a = jnp.exp(log_a)
    b = jnp.sqrt(-jnp.expm1(2.0 * log_a)) * (i_g * u)
    a_cum, b_cum = lax.associative_scan(_lin_combine, (a, b), axis=1)
    return a_cum * h0[:, None, :] + b_cum


def rglru_mixer(h_ctx, h_lat, w_in, conv_w, conv_b, gate_w, gate_b, lam, w_out):
    def branches(h):
        gate, u = jnp.split(h @ w_in, 2, axis=-1)
        return jax.nn.gelu(gate), depthwise_conv(u, conv_w, conv_b).astype(jnp.float32)

    g_ctx, u_ctx = branches(h_ctx)
    g_lat, u_lat = branches(h_lat)
    zero = jnp.zeros((h_lat.shape[0], RG_WIDTH), jnp.float32)
    hc_f = rglru_scan(u_ctx, gate_w[0], gate_b[0], lam[0], zero)
    hl_f = rglru_scan(u_lat, gate_w[0], gate_b[0], lam[0], hc_f[:, -1])
    hc_b = rglru_scan(jnp.flip(u_ctx, 1), gate_w[1], gate_b[1], lam[1], zero)
    hl_b = rglru_scan(jnp.flip(u_lat, 1), gate_w[1], gate_b[1], lam[1], hc_b[:, -1])
    y_ctx = (g_ctx * (hc_f + jnp.flip(hc_b, 1)).astype(g_ctx.dtype)) @ w_out
    y_lat = (g_lat * (hl_f + jnp.flip(hl_b, 1)).astype(g_lat.dtype)) @ w_out
    return y_ctx, y_lat


def rope_2d(t, ang_row, ang_col):
    def rot(z, ang):
        z1, z2 = jnp.split(z, 2, axis=-1)
        cs = jnp.cos(ang)[None, :, None, :].astype(z.dtype)
        sn = jnp.sin(ang)[None, :, None, :].astype(z.dtype)
        return jnp.concatenate([z1 * cs - z2 * sn, z2 * cs + z1 * sn], axis=-1)
    t_row, t_col = jnp.split(t, 2, axis=-1)
    return jnp.concatenate([rot(t_row, ang_row), rot(t_col, ang_col)], axis=-1)


def mla_project(h, w_down, q_norm, kv_norm, w_uq, w_ukv, qk_norm, angles):
    B, L, _ = h.shape
    cq, ckv, k_rope = jnp.split(h @ w_down, [MLA_Q_RANK, MLA_Q_RANK + MLA_KV_RANK], axis=-1)
    q = (rms_norm(cq, q_norm) @ w_uq).reshape(B, L, MLA_HEADS, MLA_QK)
    kv = (rms_norm(ckv, kv_norm) @ w_ukv).reshape(B, L, MLA_HEADS, MLA_NOPE + MLA_V)
    k_nope, v = jnp.split(kv, [MLA_NOPE], axis=-1)
    k = jnp.concatenate([k_nope, jnp.broadcast_to(k_rope[:, :, None, :], (B, L, MLA_HEADS, MLA_ROPE))], axis=-1)
    q = rms_norm(q, qk_norm[0])
    k = rms_norm(k, qk_norm[1])
    if angles is not None:
        ang_row, ang_col = angles
        q = jnp.concatenate([q[..., :MLA_NOPE], rope_2d(q[..., MLA_NOPE:], ang_row, ang_col)], axis=-1)
        k = jnp.concatenate([k[..., :MLA_NOPE], rope_2d(k[..., MLA_NOPE:], ang_row, ang_col)], axis=-1)
    return q, k, v


def block_attention(q, k, v):
    B, Lq, H, dq = q.shape
    qb = jnp.moveaxis(q.reshape(B, Lq // Q_BLOCK, Q_BLOCK, H, dq), 1, 0)

    def one_block(qi):
        s = jnp.einsum('bqhd,bkhd->bhqk', qi, k).astype(jnp.float32) * ATTN_SCALE
        p = jax.nn.softmax(s, axis=-1).astype(v.dtype)
        return jnp.einsum('bhqk,bkhd->bqhd', p, v)

    o = lax.map(one_block, qb)
    return jnp.moveaxis(o, 0, 1).reshape(B, Lq, H * v.shape[-1])


def mla_mixer(h_ctx, h_lat, w_down, q_norm, kv_norm, w_uq, w_ukv, qk_norm, w_o, angles):
    qc, kc, vc = mla_project(h_ctx, w_down, q_norm, kv_norm, w_uq, w_ukv, qk_norm, None)
    ql, kl, vl = mla_project(h_lat, w_down, q_norm, kv_norm, w_uq, w_ukv, qk_norm, angles)
    o_ctx = block_attention(qc, kc, vc)
    o_lat = block_attention(ql, jnp.concatenate([kc, kl], axis=1), jnp.concatenate([vc, vl], axis=1))
    return o_ctx @ w_o, o_lat @ w_o


def mlstm_chunked(q, k, v, ig, lf, state):
    B, H, L, _ = q.shape
    n_chunks = L // ML_CHUNK

    def to_chunks(t):
        return jnp.moveaxis(t.reshape(B, H, n_chunks, ML_CHUNK, *t.shape[3:]), 2, 0)

    causal = jnp.tril(jnp.ones((ML_CHUNK, ML_CHUNK), dtype=bool))

    def step(carry, inp):
        C, n, m = carry
        qc, kc, vc, igc, lfc = inp
        b = jnp.cumsum(lfc, axis=-1)
        d_log = jnp.where(causal, b[..., :, None] - b[..., None, :] + igc[..., None, :], -jnp.inf)
        inter_log = b + m[..., None]
        m_t = jnp.maximum(inter_log, jnp.max(d_log, axis=-1))
        s = jnp.einsum('bhtd,bhsd->bhts', qc, kc) * jnp.exp(d_log - m_t[..., None])
        inter = jnp.exp(inter_log - m_t)
        num = jnp.einsum('bhts,bhse->bhte', s, vc) + inter[..., None] * jnp.einsum('bhtd,bhde->bhte', qc, C)
        den = jnp.sum(s, axis=-1) + inter * jnp.einsum('bhtd,bhd->bht', qc, n)
        h = num / jnp.maximum(jnp.abs(den), jnp.exp(-m_t))[..., None]
        b_last = b[..., -1]
        w_log = b_last[..., None] - b + igc
        m_new = jnp.maximum(b_last + m, jnp.max(w_log, axis=-1))
        w = jnp.exp(w_log - m_new[..., None])
        decay = jnp.exp(b_last + m - m_new)
        C_new = decay[..., None, None] * C + jnp.einsum('bhs,bhsd,bhse->bhde', w, kc, vc)
        n_new = decay[..., None] * n + jnp.einsum('bhs,bhsd->bhd', w, kc)
        return (C_new, n_new, m_new), h

    state, h = lax.scan(step, state, tuple(to_chunks(t) for t in (q, k, v, ig, lf)))
    return jnp.moveaxis(h, 0, 2).reshape(B, H, L, v.shape[-1]), state


def mlstm_mixer(h_ctx, h_lat, w_in, gate_b, out_norm, w_out):
    def project(h):
        B, L, _ = h.shape
        q, k, v, og, gates = jnp.split(h @ w_in, [ML_HEADS * ML_DQK, 2 * ML_HEADS * ML_DQK,
                                                  2 * ML_HEADS * ML_DQK + ML_HEADS * ML_DV,
                                                  2 * ML_HEADS * ML_DQK + 2 * ML_HEADS * ML_DV], axis=-1)
        heads = lambda t: jnp.moveaxis(t.reshape(B, L, ML_HEADS, -1), 1, 2).astype(jnp.float32)
        gates = jnp.moveaxis((gates.reshape(B, L, 4, ML_HEADS) + gate_b).astype(jnp.float32), 1, 3)
        return heads(q) * ML_DQK ** -0.5, heads(k), heads(v), og, gates

    qc, kc, vc, ogc, gc = project(h_ctx)
    ql, kl, vl, ogl, gl = project(h_lat)
    B = h_lat.shape[0]
    init = (jnp.zeros((B, ML_HEADS, ML_DQK, ML_DV), jnp.float32),
            jnp.zeros((B, ML_HEADS, ML_DQK), jnp.float32),
            jnp.full((B, ML_HEADS), ML_M_INIT, jnp.float32))
    flip = lambda t: jnp.flip(t, axis=2)
    lsig = jax.nn.log_sigmoid
    hc_f, st_f = mlstm_chunked(qc, kc, vc, gc[:, 0], lsig(gc[:, 1]), init)
    hl_f, _ = mlstm_chunked(ql, kl, vl, gl[:, 0], lsig(gl[:, 1]), st_f)
    hc_b, st_b = mlstm_chunked(flip(qc), flip(kc), flip(vc), flip(gc[:, 2]), flip(lsig(gc[:, 3])), init)
    hl_b, _ = mlstm_chunked(flip(ql), flip(kl), flip(vl), flip(gl[:, 2]), flip(lsig(gl[:, 3])), st_b)

    def readout(h_sum, og):
        B_, H, L, _ = h_sum.shape
        hn = rms_norm(jnp.moveaxis(h_sum, 1, 2), out_norm.reshape(ML_HEADS, ML_DV)).reshape(B_, L, H * ML_DV)
        return (hn.astype(og.dtype) * jax.nn.sigmoid(og)) @ w_out

    return readout(hc_f + flip(hc_b), ogc), readout(hl_f + flip(hl_b), ogl)


def grouped_expert_ffn(h, expert_id, weights, w_gate_up, w_down):
    N, D = h.shape
    NK = N * MOE_TOPK
    n_blocks = -(-NK // MOE_BLOCK) + MOE_EXPERTS
    e_flat = expert_id.reshape(NK)
    order = jnp.argsort(e_flat)
    e_sorted = e_flat[order]
    tok_sorted = (order // MOE_TOPK).astype(jnp.int32)
    w_sorted = weights.reshape(NK)[order]
    counts = jnp.bincount(e_flat, length=MOE_EXPERTS)
    starts = jnp.cumsum(counts) - counts
    padded = (counts + MOE_BLOCK - 1) // MOE_BLOCK * MOE_BLOCK
    pad_end = jnp.cumsum(padded)
    pad_start = pad_end - padded
    dest = pad_start[e_sorted] + jnp.arange(NK, dtype=jnp.int32) - starts[e_sorted]
    slot_token = jnp.full((n_blocks * MOE_BLOCK,), N, jnp.int32).at[dest].set(tok_sorted)
    block_expert = jnp.minimum(jnp.searchsorted(pad_end, jnp.arange(n_blocks, dtype=jnp.int32) * MOE_BLOCK,
                                                side='right'), MOE_EXPERTS - 1)
    h_pad = jnp.concatenate([h, jnp.zeros((1, D), h.dtype)], axis=0)

    def run_block(args):
        idx, e = args
        gate, up = jnp.split(h_pad[idx] @ w_gate_up[e], 2, axis=-1)
        return (jax.nn.silu(gate) * up) @ w_down[e]

    y_slots = lax.map(run_block, (slot_token.reshape(n_blocks, MOE_BLOCK), block_expert)).reshape(-1, D)
    contrib = y_slots[dest] * w_sorted[:, None].astype(y_slots.dtype)
    return jax.ops.segment_sum(contrib, tok_sorted, num_segments=N)


def hier_moe(h, w_group, b_group, w_expert, b_expert, w_gate_up, w_down):
    N = h.shape[0]
    g_prob = jax.nn.softmax((h @ w_group + b_group).astype(jnp.float32), axis=-1)
    p_top, g_sel = lax.top_k(g_prob, 1)
    e_logits = (h @ w_expert + b_expert).astype(jnp.float32).reshape(N, MOE_GROUPS, MOE_PER_GROUP)
    e_in_group = jnp.take_along_axis(e_logits, g_sel[:, :, None], axis=1)[:, 0]
    e_top, e_sel = lax.top_k(e_in_group, MOE_TOPK)
    weights = jax.nn.softmax(e_top, axis=-1) * p_top
    expert_id = g_sel * MOE_PER_GROUP + e_sel
    return grouped_expert_ffn(h, expert_id, weights, w_gate_up, w_down)


def setup_inputs(seed: int = 0) -> dict:
    key = jax.random.key(seed)
    keys = iter(list(jax.random.split(key, 40)))
    f32 = jnp.float32
    nrm = lambda shape, scale: jax.random.normal(next(keys), shape, f32) * scale
    gain = lambda shape: 1.0 + nrm(shape, 0.02)
    D = D_MODEL
    x = nrm((BATCH, SEQ, D), 1.0)
    c = nrm((BATCH, D), 1.0)
    ctx = nrm((BATCH, CTX_LEN, D), 1.0)
    c_ctx = nrm((D,), 1.0)
    ada_w = nrm((DEPTH, D, 6 * D), 0.5 * D ** -0.5)
    ada_b = nrm((DEPTH, 6 * D), 0.02)
    norm_mix = gain((DEPTH, D))
    norm_ffn = gain((DEPTH, D))
    rg_w_in = nrm((N_RG_LAYERS, D, 2 * RG_WIDTH), D ** -0.5)
    rg_conv_w = nrm((N_RG_LAYERS, RG_CONV_W, RG_WIDTH), RG_CONV_W ** -0.5)
    rg_conv_b = nrm((N_RG_LAYERS, RG_WIDTH), 0.02)
    rg_gate_w = nrm((N_RG_LAYERS, 2, 2, RG_BLOCKS, RG_BLOCK_W, RG_BLOCK_W), RG_BLOCK_W ** -0.5)
    rg_gate_b = nrm((N_RG_LAYERS, 2, 2, RG_WIDTH), 0.02)
    a0 = jax.random.uniform(next(keys), (N_RG_LAYERS, 2, RG_WIDTH), f32, 0.9, 0.999)
    rg_lambda = jnp.log(a0) - jnp.log1p(-a0)
    rg_w_out = nrm((N_RG_LAYERS, RG_WIDTH, D), RG_WIDTH ** -0.5)
    mla_w_down = nrm((N_MLA_LAYERS, D, MLA_Q_RANK + MLA_KV_RANK + MLA_ROPE), D ** -0.5)
    mla_q_norm = gain((N_MLA_LAYERS, MLA_Q_RANK))
    mla_kv_norm = gain((N_MLA_LAYERS, MLA_KV_RANK))
    mla_w_uq = nrm((N_MLA_LAYERS, MLA_Q_RANK, MLA_HEADS * MLA_QK), MLA_Q_RANK ** -0.5)
    mla_w_ukv = nrm((N_MLA_LAYERS, MLA_KV_RANK, MLA_HEADS * (MLA_NOPE + MLA_V)), MLA_KV_RANK ** -0.5)
    mla_qk_norm = gain((N_MLA_LAYERS, 2, MLA_QK))
    mla_w_o = nrm((N_MLA_LAYERS, MLA_HEADS * MLA_V, D), (MLA_HEADS * MLA_V) ** -0.5)
    ml_w_in = nrm((N_MLSTM_LAYERS, D, ML_IN), D ** -0.5)
    ml_gate_b = nrm((N_MLSTM_LAYERS, 4, ML_HEADS), 0.1) + jnp.array([0.0, 3.0, 0.0, 3.0], f32)[None, :, None]
    ml_out_norm = gain((N_MLSTM_LAYERS, ML_HEADS * ML_DV))
    ml_w_out = nrm((N_MLSTM_LAYERS, ML_HEADS * ML_DV, D), (ML_HEADS * ML_DV) ** -0.5)
    moe_w_group = nrm((DEPTH, D, MOE_GROUPS), D ** -0.5)
    moe_b_group = nrm((DEPTH, MOE_GROUPS), 0.01)
    moe_w_expert = nrm((DEPTH, D, MOE_EXPERTS), D ** -0.5)
    moe_b_expert = nrm((DEPTH, MOE_EXPERTS), 0.01)
    moe_w_gate_up = nrm((DEPTH, MOE_EXPERTS, D, 2 * MOE_FF), D ** -0.5)
    moe_w_down = nrm((DEPTH, MOE_EXPERTS, MOE_FF, D), MOE_FF ** -0.5)
    return {"x": x, "c": c, "ctx": ctx, "c_ctx": c_ctx,
            "ada_w": ada_w, "ada_b": ada_b, "norm_mix": norm_mix, "norm_ffn": norm_ffn,
            "rg_w_in": rg_w_in, "rg_conv_w": rg_conv_w, "rg_conv_b": rg_conv_b, "rg_gate_w": rg_gate_w,
            "rg_gate_b": rg_gate_b, "rg_lambda": rg_lambda, "rg_w_out": rg_w_out,
            "mla_w_down": mla_w_down, "mla_q_norm": mla_q_norm, "mla_kv_norm": mla_kv_norm,
            "mla_w_uq": mla_w_uq, "mla_w_ukv": mla_w_ukv, "mla_qk_norm": mla_qk_norm, "mla_w_o": mla_w_o,
            "ml_w_in": ml_w_in, "ml_gate_b": ml_gate_b, "ml_out_norm": ml_out_norm, "ml_w_out": ml_w_out,
            "moe_w_group": moe_w_group, "moe_b_group": moe_b_group, "moe_w_expert": moe_w_expert,
            "moe_b_expert": moe_b_expert, "moe_w_gate_up": moe_w_gate_up, "moe_w_down": moe_w_down}


def reference(x, c, ctx, c_ctx, ada_w, ada_b, norm_mix, norm_ffn,
              rg_w_in, rg_conv_w, rg_conv_b, rg_gate_w, rg_gate_b, rg_lambda, rg_w_out,
              mla_w_down, mla_q_norm, mla_kv_norm, mla_w_uq, mla_w_ukv, mla_qk_norm, mla_w_o,
              ml_w_in, ml_gate_b, ml_out_norm, ml_w_out,
              moe_w_group, moe_b_group, moe_w_expert, moe_b_expert, moe_w_gate_up, moe_w_down):
    B, L, D = x.shape
    ROWS = L // GRID_W
    row = jnp.broadcast_to(jnp.arange(ROWS, dtype=jnp.float32)[:, None], (ROWS, GRID_W)).reshape(L)
    col = jnp.broadcast_to(jnp.arange(GRID_W, dtype=jnp.float32)[None, :], (ROWS, GRID_W)).reshape(L)
    inv_freq = ROPE_BASE ** (-jnp.arange(0, ROPE_AXIS_DIM, 2, dtype=jnp.float32) / ROPE_AXIS_DIM)
    angles = (row[:, None] * inv_freq, col[:, None] * inv_freq)

    for i in range(DEPTH):
        last = i == DEPTH - 1
        mod_lat = (jax.nn.silu(c) @ ada_w[i] + ada_b[i]).reshape(B, 6, 1, D)
        mod_ctx = (jax.nn.silu(c_ctx) @ ada_w[i] + ada_b[i]).reshape(6, D)
        h_lat = rms_norm(x, norm_mix[i]) * (1 + mod_lat[:, 1]) + mod_lat[:, 0]
        h_ctx = rms_norm(ctx, norm_mix[i]) * (1 + mod_ctx[1]) + mod_ctx[0]
        kind, j = i % N_MIXERS, i // N_MIXERS
        if kind == 0:
            y_ctx, y_lat = rglru_mixer(h_ctx, h_lat, rg_w_in[j], rg_conv_w[j], rg_conv_b[j], rg_gate_w[j],
                                       rg_gate_b[j], rg_lambda[j], rg_w_out[j])
        elif kind == 1:
            y_ctx, y_lat = mla_mixer(h_ctx, h_lat, mla_w_down[j], mla_q_norm[j], mla_kv_norm[j], mla_w_uq[j],
                                     mla_w_ukv[j], mla_qk_norm[j], mla_w_o[j], angles)
        else:
            y_ctx, y_lat = mlstm_mixer(h_ctx, h_lat, ml_w_in[j], ml_gate_b[j], ml_out_norm[j], ml_w_out[j])
        x = x + mod_lat[:, 2] * y_lat
        f_lat = rms_norm(x, norm_ffn[i]) * (1 + mod_lat[:, 4]) + mod_lat[:, 3]
        moe_params = (moe_w_group[i], moe_b_group[i], moe_w_expert[i], moe_b_expert[i],
                      moe_w_gate_up[i], moe_w_down[i])
        if last:
            x = x + mod_lat[:, 5] * hier_moe(f_lat.reshape(B * L, D), *moe_params).reshape(B, L, D)
        else:
            ctx = ctx + mod_ctx[2] * y_ctx
            f_ctx = rms_norm(ctx, norm_ffn[i]) * (1 + mod_ctx[4]) + mod_ctx[3]
            y = hier_moe(jnp.concatenate([f_lat.reshape(B * L, D), f_ctx.reshape(-1, D)], axis=0), *moe_params)
            x = x + mod_lat[:, 5] * y[:B * L].reshape(B, L, D)
            ctx = ctx + mod_ctx[5] * y[B * L:].reshape(B, -1, D)
    return x
```

```python
import numpy as np
from contextlib import ExitStack
import concourse.bass as bass
import concourse.mybir as mybir
from concourse.bass_utils import run_bass_kernel_spmd

F32 = mybir.dt.float32
BF16 = mybir.dt.bfloat16
I32 = mybir.dt.int32
AF = mybir.ActivationFunctionType
ALU = mybir.AluOpType
AX = mybir.AxisListType

NDMA_SEM = 32
D = 1024
NT = 2304
NCTX = 256
NLAT = 2048
TT = [(0, 256), (256, 768), (768, 1280), (1280, 1792), (1792, 2304)]
DEPTH = 4
BIG = 1.0e30


class Prog:
    ENGS = ("pe", "act", "dve", "pool", "sp")

    def __init__(self, nc, stack):
        self.nc = nc
        self.stack = stack
        self.sem = {e: stack.enter_context(nc.semaphore("s_" + e)) for e in self.ENGS}
        self.dsem = [stack.enter_context(nc.semaphore("d%d" % i)) for i in range(NDMA_SEM)]
        self.ops = {e: [] for e in self.ENGS}
        self.cnt = {e: 0 for e in self.ENGS}
        self.ndma = 0
        self.lw = {}
        self.rd = {}
        self.known = {e: {} for e in self.ENGS}
        self.final_waits = []
        self.pending = {e: [] for e in self.ENGS}

    def barrier(self):
        toks = [(e, self.cnt[e]) for e in self.ENGS if self.cnt[e] > 0]
        for j in range(NDMA_SEM):
            n = (self.ndma - j + NDMA_SEM - 1) // NDMA_SEM
            if n > 0:
                toks.append((("d", j), 16 * n))
        for e in self.ENGS:
            self.pending[e] = list(toks)

    def _semobj(self, sk):
        return self.sem[sk] if isinstance(sk, str) else self.dsem[sk[1]]

    def _need(self, eng, tok, waits):
        sk, v = tok
        if sk == "pe" and eng == "pe":
            return
        if self.known[eng].get(sk, 0) >= v:
            return
        if waits.get(sk, 0) < v:
            waits[sk] = v

    def op(self, eng, fn, reads=(), writes=(), dma=False):
        waits = {}
        for k in reads:
            t = self.lw.get(k)
            if t is not None:
                self._need(eng, t, waits)
        for k in writes:
            t = self.lw.get(k)
            if t is not None:
                self._need(eng, t, waits)
            for t in self.rd.get(k, ()):
                self._need(eng, t, waits)
        for tok in self.pending[eng]:
            self._need(eng, tok, waits)
        self.pending[eng] = []
        if dma:
            j = self.ndma % NDMA_SEM
            rnd = self.ndma // NDMA_SEM
            self.ndma += 1
            sk = ("d", j)
            if rnd > 0:
                self._need(eng, (sk, 16 * rnd), waits)
            tok = (sk, 16 * (rnd + 1))
        else:
            self.cnt[eng] += 1
            tok = (eng, self.cnt[eng])
        for sk, v in waits.items():
            self.known[eng][sk] = v
        self.ops[eng].append((list(waits.items()), fn, tok))
        for k in writes:
            self.lw[k] = tok
            self.rd[k] = []
        for k in reads:
            if k in writes:
                continue
            self.rd.setdefault(k, []).append(tok)
        return tok

    def finish(self, keys):
        waits = {}
        for k in keys:
            t = self.lw.get(k)
            if t is not None:
                self._need("sp", t, waits)
        self.final_waits = list(waits.items())

    def emit(self):
        nc = self.nc
        with nc.Block() as block:
            def mk(e):
                def body(engobj):
                    for waits, fn, tok in self.ops[e]:
                        for sk, v in waits:
                            engobj.wait_ge(self._semobj(sk), v)
                        ins = fn(engobj)
                        ins.then_inc(self._semobj(tok[0]), 1 if isinstance(tok[0], str) else 16)
                    if e == "sp":
                        for sk, v in self.final_waits:
                            engobj.wait_ge(self._semobj(sk), v)
                return body
            block.tensor(mk("pe"))
            block.scalar(mk("act"))
            block.vector(mk("dve"))
            block.gpsimd(mk("pool"))
            block.sync(mk("sp"))

    def sb(self, name, shape, dt):
        return self.stack.enter_context(self.nc.sbuf_tensor(name, shape, dt))

    def ps(self, name, shape, dt):
        return self.stack.enter_context(self.nc.psum_tensor(name, shape, dt))


class PV:
    def __init__(self):
        self.cols = []
        self.off = {}
        self.n = 0

    def add(self, name, v):
        v = np.asarray(v, np.float32).reshape(-1)
        assert v.size % 128 == 0
        a = v.reshape(-1, 128).T
        self.off[name] = self.n
        self.cols.append(a)
        self.n += a.shape[1]

    def table(self):
        return np.ascontiguousarray(np.concatenate(self.cols, axis=1))


def pv_layout(inp):
    pv = PV()
    for i in range(DEPTH):
        pv.add("nmix%d" % i, inp["norm_mix"][i])
        pv.add("nffn%d" % i, inp["norm_ffn"][i])
        ab = inp["ada_b"][i].reshape(48, 128)
        ab2 = np.repeat(ab[:, None, :], 2, axis=1)
        pv.add("adab%d" % i, ab2.reshape(-1))
    for j in range(2):
        for k in range(4):
            pv.add("rgcw%d_%d" % (j, k), inp["rg_conv_w"][j, k])
        pv.add("rgcb%d" % j, inp["rg_conv_b"][j])
        for dr in range(2):
            for g in range(2):
                pv.add("rggb%d_%d_%d" % (j, dr, g), inp["rg_gate_b"][j, dr, g])
            pv.add("rglam%d_%d" % (j, dr), inp["rg_lambda"][j, dr])
    pad = lambda v: np.concatenate([np.asarray(v, np.float32).reshape(-1), np.zeros(128 - np.asarray(v).size % 128 if np.asarray(v).size % 128 else 0, np.float32)])
    pv.add("mla_qn", inp["mla_q_norm"][0])
    pv.add("mla_kvn", inp["mla_kv_norm"][0])
    pv.add("mla_qkn0", pad(inp["mla_qk_norm"][0, 0]))
    pv.add("mla_qkn1", pad(inp["mla_qk_norm"][0, 1]))
    pv.add("ml_gb", pad(inp["ml_gate_b"][0].reshape(-1)))
    return pv


def rope_tables():
    L = NLAT
    rows = L // 64
    row = np.broadcast_to(np.arange(rows, dtype=np.float32)[:, None], (rows, 64)).reshape(L)
    col = np.broadcast_to(np.arange(64, dtype=np.float32)[None, :], (rows, 64)).reshape(L)
    inv_freq = (np.float32(10000.0) ** (-np.arange(0, 16, 2, dtype=np.float32) / np.float32(16))).astype(np.float32)
    ar = (row[:, None] * inv_freq).astype(np.float32)
    ac = (col[:, None] * inv_freq).astype(np.float32)
    C = np.ones((96, NT), np.float32)
    S = np.zeros((96, NT), np.float32)
    for base, ang in ((64, ar), (80, ac)):
        C[base:base + 8, NCTX:] = np.cos(ang).T
        C[base + 8:base + 16, NCTX:] = np.cos(ang).T
        S[base:base + 8, NCTX:] = np.sin(ang).T
        S[base + 8:base + 16, NCTX:] = np.sin(ang).T
    R = np.zeros((96, 96), np.float32)
    for base in (64, 80):
        for j in range(8):
            R[base + j, base + 8 + j] = -1.0
            R[base + 8 + j, base + j] = 1.0
    return C, S, np.ascontiguousarray(R.T)


def build(pvoff, npv, n_layers=DEPTH, stop_after_mixer=False):
    nc = bass.Bass("TRN2", target_bir_lowering=False)

    def din(name, shape, dt=F32):
        return nc.dram_tensor(name, list(shape), dt, kind="ExternalInput").ap()

    x_d = din("x", [NLAT, D])
    ctx_d = din("ctx", [NCTX, D])
    cc_d = din("cc", [128, 16])
    pv_d = din("pv", [128, npv])
    ident_d = din("ident", [128, 128])
    ada_w = din("ada_w", [DEPTH, D, 6 * D])
    rg_w_in = din("rg_w_in", [2, D, 2 * D])
    rg_gate_w = din("rg_gate_w", [2, 2, 2, 16, 64, 64])
    rg_w_out = din("rg_w_out", [2, D, D])
    mla_wdn = din("mla_w_down", [D, 416])
    mla_wuq = din("mla_w_uq", [256, 1536])
    mla_wukv = din("mla_w_ukv", [128, 2048])
    mla_wo = din("mla_w_o", [D, D])
    ropeC_d = din("ropeC", [96, NT])
    ropeS_d = din("ropeS", [96, NT])
    ropeRT_d = din("ropeRT", [96, 96])
    ml_win = din("ml_w_in", [D, 3088])
    ml_wout = din("ml_w_out", [D, D])
    ml_gain = din("ml_gain", [128, D])
    ml_selc = din("ml_selc", [16, 2, 16, 64])
    ml_selh = din("ml_selh", [16, 2, 4])
    ml_maskc = din("ml_maskc", [64, 2, 4, 64])
    hdir = nc.dram_tensor("hdir_scratch", [2, NT, D], BF16).ap()
    moec_d = din("moec", [128, 128 + 64 + 1 + 36])
    NSLOT = 12800
    xslots = nc.dram_tensor("xslots_scratch", [NSLOT, D], BF16).ap()
    yslots = nc.dram_tensor("yslots_scratch", [NSLOT, D], BF16).ap()
    moe_wr = din("moe_wr", [DEPTH, D, 72])
    moe_br = din("moe_br", [DEPTH, 128, 72])
    moe_wgu = din("moe_w_gate_up", [DEPTH, 64, D, 512])
    moe_wd = din("moe_w_down", [DEPTH, 64, 256, D])
    out_d = nc.dram_tensor("out", [NLAT, D], F32, kind="ExternalOutput").ap()
    octx_d = nc.dram_tensor("octx", [NCTX, D], F32, kind="ExternalOutput").ap()

    with ExitStack() as st:
        P = Prog(nc, st)
        xT = P.sb("xT", [128, 8, NT], F32)
        hT = P.sb("hT", [128, 8, NT], BF16)
        mT = P.sb("mT", [128, 8, NT], BF16)
        pvt = P.sb("pvt", [128, npv], F32)
        ident = P.sb("ident_sb", [128, 128], F32)
        onesm = P.sb("onesm", [128, 128], F32)
        cst = P.sb("cst", [128, 4], F32)
        cct = P.sb("cct", [128, 16], F32)
        modt = P.sb("modt", [128, 48, 2], F32)
        mA = P.sb("mA", [128, 2, 8, 2], F32)
        tmp = [P.sb("tmp%d" % i, [128, 512], F32) for i in range(6)]
        rstd = P.sb("rstd", [128, 512], F32)
        SCRW = 11400
        scr = P.sb("scr", [128, SCRW], F32)
        mTflat = mT[:].rearrange("p c t -> p (c t)")
        hTflat = hT[:].rearrange("p c t -> p (c t)")
        PSB = [P.ps("psb%d" % i, [128, 512], F32) for i in range(8)]

        class Arena:
            def __init__(self, kind):
                self.kind = kind
                self.off = 0

            def reset(self):
                self.off = 0

            def alloc(self, shape, dt):
                n = 1
                for d_ in shape[1:]:
                    n *= d_
                nf32 = n if dt in (F32, I32) else (n + 1) // 2
                o = self.off
                self.off += nf32
                if self.kind == "A":
                    assert self.off <= SCRW, ("arena A overflow", self.off)
                    v = scr[0:shape[0], o:o + nf32]
                    if dt != F32:
                        v = v.bitcast(dt)[:, 0:n]
                else:
                    assert self.off * 2 <= 8 * NT, ("arena B/C overflow", self.off)
                    flat = mTflat if self.kind == "B" else hTflat
                    v = flat[0:shape[0], 2 * o:2 * o + 2 * nf32]
                    if dt == F32:
                        v = v.bitcast(F32)
                    else:
                        v = v[:, 0:n]
                if len(shape) == 3:
                    v = v.rearrange("p (a b) -> p a b", a=shape[1])
                elif len(shape) == 4:
                    v = v.rearrange("p (a b c) -> p a b c", a=shape[1], b=shape[2])
                return v

        arA, arB, arC = Arena("A"), Arena("B"), Arena("C")
        _regs = {}

        def getreg(e, v):
            if v not in _regs:
                _regs[v] = e.to_reg(v)
            return _regs[v]

        def phase():
            P.barrier()
            arA.reset()
            arB.reset()
            arC.reset()

        def pvc(name, k=0, n=1):
            o = pvoff[name] + k
            return pvt[:, o:o + n]

        P.op("sp", lambda e: e.dma_start(out=pvt[:], in_=pv_d), writes=["pvt"], dma=True)
        P.op("sp", lambda e: e.dma_start(out=ident[:], in_=ident_d), writes=["ident"], dma=True)
        P.op("sp", lambda e: e.dma_start(out=cct[:], in_=cc_d), writes=["cct"], dma=True)
        P.op("pool", lambda e: e.memset(onesm[:], 1.0 / 1024.0), writes=["onesm"])
        P.op("pool", lambda e: e.memset(cst[:, 0:1], 1e-6), writes=["cst0"])
        P.op("pool", lambda e: e.memset(cst[:, 1:2], 1.0), writes=["cst1"])
        P.op("act", lambda e: e.activation(out=cct[:], in_=cct[:], func=AF.Silu), reads=["cct"], writes=["cct"])

        stage = arA.alloc([128, 2, 1024], F32)
        for ti in range(18):
            src = ctx_d[ti * 128:(ti + 1) * 128, :] if ti < 2 else x_d[(ti - 2) * 128:(ti - 1) * 128, :]
            sbuf = ti % 2
            P.op("sp", lambda e, src=src, sbuf=sbuf: e.dma_start(out=stage[:, sbuf, :], in_=src), writes=[("stage", sbuf)], dma=True)
            jt = [j for j, (a, b) in enumerate(TT) if a <= ti * 128 < b][0]
            for half in range(2):
                pb = PSB[half]

                def trfn(e, pb=pb, half=half, sbuf=sbuf):
                    ins = None
                    for q in range(4):
                        c = half * 4 + q
                        ins = e.transpose(pb[:, q * 128:(q + 1) * 128], stage[:, sbuf, c * 128:(c + 1) * 128], ident[:])
                    return ins
                P.op("pe", trfn, reads=[("stage", sbuf), "ident"], writes=[("psb", half)])
                P.op("dve" if half == 0 else "act",
                     lambda e, pb=pb, half=half, ti=ti: (e.tensor_copy if half == 0 else e.copy)(
                         out=xT[:, half * 4:half * 4 + 4, ti * 128:(ti + 1) * 128], in_=pb[:].rearrange("p (q t) -> p q t", q=4)),
                     reads=[("psb", half)], writes=[("x", c, jt) for c in range(half * 4, half * 4 + 4)])

        def compute_mod(i):
            phase()
            adaw = arA.alloc([128, 5, 8, 256], F32)
            pm = PSB[7]
            for piece in range(24):
                bsel = piece % 5
                P.op("sp", lambda e, piece=piece, bsel=bsel: e.dma_start(
                    out=adaw[:, bsel, :, :], in_=ada_w[i, :, piece * 256:(piece + 1) * 256].rearrange("(k p) n -> p k n", p=128)),
                    writes=[("adaw", bsel)], dma=True)

                def mmfn(e, bsel=bsel, piece=piece):
                    ins = None
                    for sub in range(2):
                        oc = piece * 2 + sub
                        for k in range(8):
                            ins = e.matmul(pm[:, oc * 2:oc * 2 + 2], lhsT=adaw[:, bsel, k, sub * 128:(sub + 1) * 128],
                                           rhs=cct[:, k * 2:k * 2 + 2], start=(k == 0), stop=(k == 7))
                    return ins
                P.op("pe", mmfn, reads=[("adaw", bsel), "cct"], writes=[("psb", 7)])
            ab = pvt[:, pvoff["adab%d" % i]:pvoff["adab%d" % i] + 96]
            P.op("dve", lambda e: e.tensor_tensor(out=modt[:].rearrange("p a b -> p (a b)"), in0=pm[:, 0:96], in1=ab, op=ALU.add),
                 reads=[("psb", 7), "pvt"], writes=["modt"])
            for w, (nm, m) in enumerate((("nmix%d" % i, 1), ("nffn%d" % i, 4))):
                for j2 in range(2):
                    P.op("dve", lambda e, w=w, nm=nm, m=m, j2=j2: e.scalar_tensor_tensor(
                        out=mA[:, w, :, j2], in0=modt[:, m * 8:m * 8 + 8, j2], scalar=1.0, in1=pvc(nm, 0, 8),
                        op0=ALU.add, op1=ALU.mult),
                        reads=["modt", "pvt"], writes=["mA"])

        def modcol(m, c, j):
            jj = 1 if j == 0 else 0
            return modt[:, m * 8 + c, jj:jj + 1]

        def norm_mod(w, tiles, extra=None):
            mshift = 0 if w == 0 else 3
            for j in tiles:
                a, b = TT[j]
                n = b - a
                jj = 1 if j == 0 else 0
                pst = PSB[6]
                for c in range(8):
                    tb = tmp[c % 2]
                    P.op("act", lambda e, tb=tb, c=c, a=a, b=b, n=n: e.activation(out=tb[:, :n], in_=xT[:, c, a:b], func=AF.Square),
                         reads=[("x", c, j)], writes=[("tmp", c % 2)])
                    P.op("pe", lambda e, tb=tb, c=c, n=n: e.matmul(pst[:, :n], lhsT=onesm[:], rhs=tb[:, :n], start=(c == 0), stop=(c == 7)),
                         reads=[("tmp", c % 2), "onesm"], writes=[("psb", 6)])
                P.op("act", lambda e, n=n: e.activation(out=rstd[:, :n], in_=pst[:, :n], func=AF.Ln, bias=cst[:, 0:1], scale=1.0),
                     reads=[("psb", 6), "cst0"], writes=["rstd"])
                P.op("act", lambda e, n=n: e.activation(out=rstd[:, :n], in_=rstd[:, :n], func=AF.Exp, scale=-0.5), reads=["rstd"], writes=["rstd"])
                for c in range(8):
                    tb = tmp[2 + c % 2]
                    P.op("dve", lambda e, tb=tb, c=c, a=a, b=b, n=n: e.tensor_tensor(out=tb[:, :n], in0=xT[:, c, a:b], in1=rstd[:, :n], op=ALU.mult),
                         reads=[("x", c, j), "rstd"], writes=[("tmp", 2 + c % 2)])
                    P.op("act", lambda e, tb=tb, c=c, a=a, b=b, n=n, jj=jj: e.activation(
                        out=hT[:, c, a:b], in_=tb[:, :n], func=AF.Identity,
                        bias=modt[:, mshift * 8 + c, jj:jj + 1], scale=mA[:, w, c, jj:jj + 1]),
                        reads=[("tmp", 2 + c % 2), "modt", "mA"], writes=[("h", c, j)])
                    if extra is not None:
                        extra(j, c, tb)

        def rglru(jl, tiles_all):
            phase()
            k = "rg%d" % jl
            win = arA.alloc([128, 2, 8, 256], BF16)
            wout = arA.alloc([128, 2, 8, 128], BF16)
            gw = arA.alloc([128, 4, 128], BF16)
            gwf = arA.alloc([128, 4, 128], F32)
            clam = arA.alloc([128, 2, 2, 8], F32)
            uh = arA.alloc([128, NT], F32)
            ucv = arA.alloc([128, NT], F32)
            ub = arA.alloc([128, NT], BF16)
            hb = arA.alloc([128, 2, 512], F32)
            for dr in range(2):
                lam = pvc("rglam%d_%d" % (jl, dr), 0, 8)
                P.op("act", lambda e, dr=dr, lam=lam: e.activation(out=clam[:, dr, 0, :], in_=lam, func=AF.Exp, scale=-1.0),
                     reads=["pvt"], writes=[(k, "clam")])
                P.op("act", lambda e, dr=dr: e.activation(out=clam[:, dr, 0, :], in_=clam[:, dr, 0, :], func=AF.Ln, bias=cst[:, 1:2], scale=1.0),
                     reads=[(k, "clam"), "cst1"], writes=[(k, "clam")])
                P.op("dve", lambda e, dr=dr: e.tensor_scalar(out=clam[:, dr, 1, :], in0=clam[:, dr, 0, :], scalar1=-16.0, scalar2=None, op0=ALU.mult),
                     reads=[(k, "clam")], writes=[(k, "clam")])
                P.op("dve", lambda e, dr=dr: e.tensor_scalar(out=clam[:, dr, 0, :], in0=clam[:, dr, 0, :], scalar1=-8.0, scalar2=None, op0=ALU.mult),
                     reads=[(k, "clam")], writes=[(k, "clam")])
            for cc in range(8):
                wb_ = cc % 2
                for part in range(2):
                    P.op("pool", lambda e, part=part, wb_=wb_, cc=cc: e.dma_start(
                        out=win[:, wb_, :, part * 128:(part + 1) * 128],
                        in_=rg_w_in[jl, :, part * 1024 + cc * 128: part * 1024 + (cc + 1) * 128].rearrange("(k p) n -> p k n", p=128)),
                        writes=[(k, "win", wb_, part)], dma=True)
                subk = [(k, "gwf", q) for q in range(8)]
                P.op("pool", lambda e: e.memset(gwf[:, :, :], 0.0), writes=subk)
                for dr in range(2):
                    for g in range(2):
                        for blk in range(2):
                            P.op("sp", lambda e, dr=dr, g=g, blk=blk, cc=cc: e.dma_start(
                                out=gwf[blk * 64:(blk + 1) * 64, dr * 2 + g, blk * 64:(blk + 1) * 64],
                                in_=rg_gate_w[jl, dr, g, cc * 2 + blk, :, :]),
                                writes=[(k, "gwf", dr * 4 + g * 2 + blk)], dma=True)
                P.op("pool", lambda e: e.tensor_copy(out=gw[:, :, :], in_=gwf[:, :, :]), reads=subk, writes=[(k, "gw")])
                for j in tiles_all:
                    a, b = TT[j]
                    n = b - a
                    pg, pu = PSB[0 + j % 2], PSB[2 + j % 2]
                    for part, pp, pk in ((0, pg, ("psb", 0 + j % 2)), (1, pu, ("psb", 2 + j % 2))):
                        def mmfn(e, part=part, pp=pp, a=a, b=b, n=n, wb_=wb_):
                            ins = None
                            for kk in range(8):
                                ins = e.matmul(pp[:, :n], lhsT=win[:, wb_, kk, part * 128:(part + 1) * 128], rhs=hT[:, kk, a:b],
                                               start=(kk == 0), stop=(kk == 7))
                            return ins
                        P.op("pe", mmfn, reads=[(k, "win", wb_, part)] + [("h", kk, j) for kk in range(8)], writes=[pk])
                    t0, t1 = tmp[0], tmp[1]
                    pgk = ("psb", 0 + j % 2)
                    P.op("act", lambda e, pg=pg, n=n: e.activation(out=t0[:, :n], in_=pg[:, :n], func=AF.Square),
                         reads=[pgk], writes=[("tmp", 0)])
                    P.op("dve", lambda e, n=n: e.tensor_scalar(out=t0[:, :n], in0=t0[:, :n], scalar1=0.044715, scalar2=1.0, op0=ALU.mult, op1=ALU.add),
                         reads=[("tmp", 0)], writes=[("tmp", 0)])
                    P.op("dve", lambda e, pg=pg, n=n: e.tensor_tensor(out=t0[:, :n], in0=t0[:, :n], in1=pg[:, :n], op=ALU.mult),
                         reads=[("tmp", 0), pgk], writes=[("tmp", 0)])
                    P.op("act", lambda e, n=n: e.activation(out=t1[:, :n], in_=t0[:, :n], func=AF.Sigmoid, scale=1.5957691216),
                         reads=[("tmp", 0)], writes=[("tmp", 1)])
                    P.op("dve", lambda e, pg=pg, n=n, a=a, b=b, cc=cc: e.tensor_tensor(out=mT[:, cc, a:b], in0=t1[:, :n], in1=pg[:, :n], op=ALU.mult),
                         reads=[("tmp", 1), pgk], writes=[("m", cc, j)])
                    P.op("act", lambda e, pu=pu, n=n, a=a, b=b: e.copy(out=uh[:, a:b], in_=pu[:, :n]),
                         reads=[("psb", 2 + j % 2)], writes=[(k, "uh")])
                P.op("act", lambda e, cc=cc: e.activation(out=ucv[:, :], in_=uh[:, :], func=AF.Identity,
                                                           bias=pvc("rgcb%d" % jl, cc), scale=pvc("rgcw%d_2" % jl, cc)),
                     reads=[(k, "uh"), "pvt"], writes=[(k, "ucv")])
                for (s0, s1) in ((0, NCTX), (NCTX, NT)):
                    for tap, off in ((0, -2), (1, -1), (3, 1)):
                        lo = max(s0, s0 - off)
                        hi = min(s1, s1 - off)
                        P.op("dve", lambda e, lo=lo, hi=hi, off=off, tap=tap, cc=cc: e.scalar_tensor_tensor(
                            out=ucv[:, lo:hi], in0=uh[:, lo + off:hi + off], scalar=pvc("rgcw%d_%d" % (jl, tap), cc),
                            in1=ucv[:, lo:hi], op0=ALU.mult, op1=ALU.add),
                            reads=[(k, "uh"), (k, "ucv"), "pvt"], writes=[(k, "ucv")])
                P.op("pool", lambda e: e.tensor_copy(out=ub[:, :], in_=ucv[:, :]), reads=[(k, "ucv")], writes=[(k, "ub")])
                for dr in range(2):
                    order = tiles_all if dr == 0 else [tiles_all[0]] + list(reversed(tiles_all[1:]))
                    prev = None
                    groups = [order[g:g + 2] for g in range(0, len(order), 2)]
                    oi = 0
                    for grp in groups:
                        info = []
                        for gi, j in enumerate(grp):
                            a, b = TT[j]
                            n = b - a
                            tr, ta2, ti_ = tmp[3 * gi + 0], tmp[3 * gi + 1], tmp[3 * gi + 2]
                            kr, k2, ki_ = ("tmp", 3 * gi + 0), ("tmp", 3 * gi + 1), ("tmp", 3 * gi + 2)
                            pr = PSB[4 + gi]
                            pi = PSB[6 + gi]
                            prk = ("psb", 4 + gi)
                            pik = ("psb", 6 + gi)
                            P.op("pe", lambda e, pr=pr, dr=dr, a=a, b=b, n=n: e.matmul(pr[:, :n], lhsT=gw[:, dr * 2 + 0, :], rhs=ub[:, a:b], start=True, stop=True),
                                 reads=[(k, "gw"), (k, "ub")], writes=[prk])
                            P.op("pe", lambda e, pi=pi, dr=dr, a=a, b=b, n=n: e.matmul(pi[:, :n], lhsT=gw[:, dr * 2 + 1, :], rhs=ub[:, a:b], start=True, stop=True),
                                 reads=[(k, "gw"), (k, "ub")], writes=[pik])
                            P.op("act", lambda e, pr=pr, n=n, dr=dr, cc=cc, tr=tr: e.activation(out=tr[:, :n], in_=pr[:, :n], func=AF.Sigmoid, bias=pvc("rggb%d_%d_0" % (jl, dr), cc), scale=1.0),
                                 reads=[prk, "pvt"], writes=[kr])
                            P.op("act", lambda e, pi=pi, n=n, dr=dr, cc=cc, ti_=ti_: e.activation(out=ti_[:, :n], in_=pi[:, :n], func=AF.Sigmoid, bias=pvc("rggb%d_%d_1" % (jl, dr), cc), scale=1.0),
                                 reads=[pik, "pvt"], writes=[ki_])
                            info.append((j, a, b, n, tr, ta2, ti_, kr, k2, ki_))
                        for (j, a, b, n, tr, ta2, ti_, kr, k2, ki_) in info:
                            P.op("act", lambda e, n=n, dr=dr, cc=cc, tr=tr: e.activation(out=tr[:, :n], in_=tr[:, :n], func=AF.Exp, scale=clam[:, dr, 0, cc:cc + 1]),
                                 reads=[kr, (k, "clam")], writes=[kr])
                            P.op("act", lambda e, n=n, tr=tr, ta2=ta2: e.activation(out=ta2[:, :n], in_=tr[:, :n], func=AF.Square), reads=[kr], writes=[k2])
                            P.op("act", lambda e, n=n, ta2=ta2: e.activation(out=ta2[:, :n], in_=ta2[:, :n], func=AF.Ln, bias=cst[:, 1:2], scale=-1.0), reads=[k2, "cst1"], writes=[k2])
                            P.op("act", lambda e, n=n, ta2=ta2: e.activation(out=ta2[:, :n], in_=ta2[:, :n], func=AF.Exp, scale=0.5), reads=[k2], writes=[k2])
                        for (j, a, b, n, tr, ta2, ti_, kr, k2, ki_) in info:
                            P.op("dve", lambda e, n=n, a=a, b=b, ti_=ti_: e.tensor_tensor(out=ti_[:, :n], in0=ti_[:, :n], in1=ucv[:, a:b], op=ALU.mult),
                                 reads=[ki_, (k, "ucv")], writes=[ki_])
                            P.op("dve", lambda e, n=n, ti_=ti_, ta2=ta2: e.tensor_tensor(out=ti_[:, :n], in0=ti_[:, :n], in1=ta2[:, :n], op=ALU.mult),
                                 reads=[ki_, k2], writes=[ki_])
                            if dr == 0:
                                init = 0.0 if prev is None else uh[:, a - 1:a]
                                P.op("dve", lambda e, n=n, a=a, b=b, init=init, tr=tr, ti_=ti_: e.tensor_tensor_scan(out=uh[:, a:b], data0=tr[:, :n], data1=ti_[:, :n], initial=init, op0=ALU.mult, op1=ALU.add),
                                     reads=[kr, ki_, (k, "uh"), (k, "ucv")], writes=[(k, "uh")])
                            else:
                                hbb = oi % 2
                                init = 0.0 if prev is None else hb[:, 1 - hbb, 0:1]
                                P.op("dve", lambda e, n=n, hbb=hbb, init=init, tr=tr, ti_=ti_: e.tensor_tensor_scan(out=hb[:, hbb, 0:n][:, ::-1], data0=tr[:, 0:n][:, ::-1], data1=ti_[:, 0:n][:, ::-1], initial=init, op0=ALU.mult, op1=ALU.add),
                                     reads=[kr, ki_, (k, "hb", 1 - hbb)], writes=[(k, "hb", hbb)])
                                P.op("dve", lambda e, n=n, a=a, b=b, hbb=hbb, tr=tr: e.tensor_tensor(out=tr[:, :n], in0=hb[:, hbb, 0:n], in1=uh[:, a:b], op=ALU.add),
                                     reads=[(k, "hb", hbb), (k, "uh")], writes=[kr])
                                P.op("dve", lambda e, n=n, a=a, b=b, cc=cc, tr=tr: e.tensor_tensor(out=mT[:, cc, a:b], in0=tr[:, :n], in1=mT[:, cc, a:b], op=ALU.mult),
                                     reads=[kr, ("m", cc, j)], writes=[("m", cc, j)])
                            prev = j
                            oi += 1
            for oc in range(8):
                ob = oc % 2
                P.op("pool", lambda e, oc=oc, ob=ob: e.dma_start(out=wout[:, ob, :, :], in_=rg_w_out[jl, :, oc * 128:(oc + 1) * 128].rearrange("(k p) n -> p k n", p=128)),
                     writes=[(k, "wout", ob)], dma=True)
                for j in tiles_all:
                    a, b = TT[j]
                    n = b - a
                    py = PSB[j % 4]

                    def mmfn(e, py=py, ob=ob, a=a, b=b, n=n):
                        ins = None
                        for cc in range(8):
                            ins = e.matmul(py[:, :n], lhsT=wout[:, ob, cc, :], rhs=mT[:, cc, a:b], start=(cc == 0), stop=(cc == 7))
                        return ins
                    P.op("pe", mmfn, reads=[(k, "wout", ob)] + [("m", cc, j) for cc in range(8)], writes=[("psb", j % 4)])
                    P.op("dve", lambda e, py=py, oc=oc, a=a, b=b, n=n, j=j: e.scalar_tensor_tensor(
                        out=xT[:, oc, a:b], in0=py[:, :n], scalar=modcol(2, oc, j), in1=xT[:, oc, a:b], op0=ALU.mult, op1=ALU.add),
                        reads=[("psb", j % 4), "modt", ("x", oc, j)], writes=[("x", oc, j)])

        def mla(tiles_all):
            phase()
            k = "mla"
            SC = 96 ** -0.5
            cqn = arA.alloc([128, 2, NT], BF16)
            ckvn = arA.alloc([128, NT], BF16)
            krope = arA.alloc([96, NT], F32)
            wuq = arA.alloc([128, 2, 1536], BF16)
            wukv = arA.alloc([128, 2048], BF16)
            ones96 = arA.alloc([96, 96], BF16)
            RTf = arA.alloc([96, 96], F32)
            RT = arA.alloc([96, 96], BF16)
            ones1r = arA.alloc([65, 64], F32)
            rr = arA.alloc([65, 512], F32)
            onesb = arA.alloc([128, 64], BF16)
            wdn = arB.alloc([128, 8, 416], BF16)
            wo = arB.alloc([64, 2, 1024], BF16)
            tC = arB.alloc([96, NT], F32)
            tS = arB.alloc([96, NT], F32)
            P.op("pool", lambda e: e.dma_start(out=wdn, in_=mla_wdn.rearrange("(k p) n -> p k n", p=128)), writes=[(k, "wdn")], dma=True)
            P.op("pool", lambda e: e.dma_start(out=wuq, in_=mla_wuq.rearrange("(k p) n -> p k n", p=128)), writes=[(k, "wuq")], dma=True)
            P.op("pool", lambda e: e.dma_start(out=wukv, in_=mla_wukv), writes=[(k, "wukv")], dma=True)
            P.op("sp", lambda e: e.dma_start(out=tC, in_=ropeC_d), writes=[(k, "tC")], dma=True)
            P.op("sp", lambda e: e.dma_start(out=tS, in_=ropeS_d), writes=[(k, "tS")], dma=True)
            P.op("sp", lambda e: e.dma_start(out=RTf, in_=ropeRT_d), writes=[(k, "RTf")], dma=True)
            P.op("pool", lambda e: e.tensor_copy(out=RT, in_=RTf), reads=[(k, "RTf")], writes=[(k, "RT")])
            P.op("pool", lambda e: e.memset(ones1r, 1.0), writes=[(k, "ones1r")])
            P.op("pool", lambda e: e.memset(ones96, 1.0 / 96.0), writes=[(k, "ones96")])
            P.op("pool", lambda e: e.memset(onesb, 1.0), writes=[(k, "onesb")])
            for j in tiles_all:
                a, b = TT[j]
                n = b - a
                specs = ((0, 0, 128), (1, 128, 128), (2, 256, 128), (3, 320, 96))
                for bi, c0, m in specs:
                    def mmfn(e, bi=bi, c0=c0, m=m, a=a, b=b, n=n):
                        ins = None
                        for kk in range(8):
                            ins = e.matmul(PSB[bi][0:m, :n], lhsT=wdn[:, kk, c0:c0 + m], rhs=hT[:, kk, a:b], start=(kk == 0), stop=(kk == 7))
                        return ins
                    P.op("pe", mmfn, reads=[(k, "wdn")] + [("h", kk, j) for kk in range(8)], writes=[("psb", bi)])
                P.op("act", lambda e, a=a, b=b, n=n: e.copy(out=krope[64:96, a:b], in_=PSB[3][64:96, :n]), reads=[("psb", 3)], writes=[(k, "krope", j)])
                for c in range(2):
                    P.op("act", lambda e, c=c, n=n: e.activation(out=tmp[c][:, :n], in_=PSB[c][:, :n], func=AF.Square), reads=[("psb", c)], writes=[("tmp", c)])
                    P.op("pe", lambda e, c=c, n=n: e.matmul(PSB[4][:, :n], lhsT=onesm[:], rhs=tmp[c][:, :n], start=(c == 0), stop=(c == 1)),
                         reads=[("tmp", c), "onesm"], writes=[("psb", 4)])
                P.op("act", lambda e, n=n: e.activation(out=rstd[:, :n], in_=PSB[4][:, :n], func=AF.Ln, bias=cst[:, 0:1], scale=4.0), reads=[("psb", 4), "cst0"], writes=["rstd"])
                P.op("act", lambda e, n=n: e.activation(out=rstd[:, :n], in_=rstd[:, :n], func=AF.Exp, scale=-0.5), reads=["rstd"], writes=["rstd"])
                for c in range(2):
                    P.op("dve", lambda e, c=c, n=n: e.tensor_tensor(out=tmp[2 + c][:, :n], in0=PSB[c][:, :n], in1=rstd[:, :n], op=ALU.mult), reads=[("psb", c), "rstd"], writes=[("tmp", 2 + c)])
                    P.op("act", lambda e, c=c, a=a, b=b, n=n: e.activation(out=cqn[:, c, a:b], in_=tmp[2 + c][:, :n], func=AF.Identity, scale=pvc("mla_qn", c)), reads=[("tmp", 2 + c), "pvt"], writes=[(k, "cqn", j)])
                P.op("act", lambda e, n=n: e.activation(out=tmp[4][:, :n], in_=PSB[2][:, :n], func=AF.Square), reads=[("psb", 2)], writes=[("tmp", 4)])
                P.op("pe", lambda e, n=n: e.matmul(PSB[5][:, :n], lhsT=onesm[:], rhs=tmp[4][:, :n], start=True, stop=True), reads=[("tmp", 4), "onesm"], writes=[("psb", 5)])
                P.op("act", lambda e, n=n: e.activation(out=tmp[5][:, :n], in_=PSB[5][:, :n], func=AF.Ln, bias=cst[:, 0:1], scale=8.0), reads=[("psb", 5), "cst0"], writes=[("tmp", 5)])
                P.op("act", lambda e, n=n: e.activation(out=tmp[5][:, :n], in_=tmp[5][:, :n], func=AF.Exp, scale=-0.5), reads=[("tmp", 5)], writes=[("tmp", 5)])
                P.op("dve", lambda e, n=n: e.tensor_tensor(out=tmp[4][:, :n], in0=PSB[2][:, :n], in1=tmp[5][:, :n], op=ALU.mult), reads=[("psb", 2), ("tmp", 5)], writes=[("tmp", 4)])
                P.op("act", lambda e, a=a, b=b, n=n: e.activation(out=ckvn[:, a:b], in_=tmp[4][:, :n], func=AF.Identity, scale=pvc("mla_kvn", 0)), reads=[("tmp", 4), "pvt"], writes=[(k, "ckvn", j)])
            P.barrier()
            qTs = [arC.alloc([96, NT], BF16) for _ in range(2)]
            kTs = [arC.alloc([96, NT], BF16) for _ in range(2)]
            vhs = [arC.alloc([128, 18, 65], BF16) for _ in range(2)]
            for vv in range(2):
                P.op("pool", lambda e, vv=vv: e.memset(vhs[vv], 1.0), writes=[(k, "vh", vv)])
            PT = arC.alloc([128, 2, 512], BF16)
            xf = arC.alloc([96, 512], F32)
            xn = arC.alloc([96, 512], BF16)
            rs = arC.alloc([96, 512], F32)
            t1 = arC.alloc([96, 512], F32)
            t1b = arC.alloc([96, 512], BF16)
            t2 = arC.alloc([96, 512], F32)
            rden = tmp[0][0:64, :]
            oT = tmp[1][0:64, :].bitcast(BF16)[:, 0:512]

            def proj_gen(h):
                hp = h % 2
                qT, kT, vh = qTs[hp], kTs[hp], vhs[hp]
                P.op("pool", lambda e: e.dma_start(out=wo[:, hp, :], in_=mla_wo[h * 64:(h + 1) * 64, :]), writes=[(k, "wo", hp)], dma=True)
                for j in tiles_all:
                    a, b = TT[j]
                    n = b - a

                    def mmq(e, a=a, b=b, n=n):
                        ins = None
                        for kk in range(2):
                            ins = e.matmul(PSB[0][0:96, :n], lhsT=wuq[:, kk, h * 96:(h + 1) * 96], rhs=cqn[:, kk, a:b], start=(kk == 0), stop=(kk == 1))
                        return ins
                    P.op("pe", mmq, reads=[(k, "wuq"), (k, "cqn", j)], writes=[("psb", 0)])
                    P.op("pe", lambda e, a=a, b=b, n=n: e.matmul(PSB[1][0:64, :n], lhsT=wukv[:, h * 128:h * 128 + 64], rhs=ckvn[:, a:b], start=True, stop=True),
                         reads=[(k, "wukv"), (k, "ckvn", j)], writes=[("psb", 1)])
                    for which in range(2):
                        gname = "mla_qkn%d" % which
                        if which == 0:
                            P.op("act", lambda e, n=n: e.copy(out=xf[:, :n], in_=PSB[0][0:96, :n]), reads=[("psb", 0)], writes=[(k, "xf")])
                        else:
                            P.op("act", lambda e, n=n: e.copy(out=xf[0:64, :n], in_=PSB[1][0:64, :n]), reads=[("psb", 1)], writes=[(k, "xf")])
                            P.op("pool", lambda e, a=a, b=b, n=n: e.tensor_copy(out=xf[64:96, :n], in_=krope[64:96, a:b]), reads=[(k, "krope", j), (k, "xf")], writes=[(k, "xf")])
                        P.op("act", lambda e, n=n: e.activation(out=t1b[:, :n], in_=xf[:, :n], func=AF.Square), reads=[(k, "xf")], writes=[(k, "t1b")])
                        P.op("pe", lambda e, n=n: e.matmul(PSB[2][0:96, :n], lhsT=ones96, rhs=t1b[:, :n], start=True, stop=True), reads=[(k, "t1b"), (k, "ones96")], writes=[("psb", 2)])
                        yield
                        P.op("act", lambda e, n=n: e.activation(out=rs[:, :n], in_=PSB[2][0:96, :n], func=AF.Ln, bias=cst[0:96, 0:1], scale=1.0), reads=[("psb", 2), "cst0"], writes=[(k, "rs")])
                        P.op("act", lambda e, n=n: e.activation(out=rs[:, :n], in_=rs[:, :n], func=AF.Exp, scale=-0.5), reads=[(k, "rs")], writes=[(k, "rs")])
                        P.op("dve", lambda e, n=n, gname=gname: e.scalar_tensor_tensor(out=xn[:, :n], in0=xf[:, :n], scalar=pvt[0:96, pvoff[gname]:pvoff[gname] + 1], in1=rs[:, :n], op0=ALU.mult, op1=ALU.mult),
                             reads=[(k, "xf"), (k, "rs"), "pvt"], writes=[(k, "xn")])
                        P.op("pe", lambda e, n=n: e.matmul(PSB[3][0:96, :n], lhsT=RT, rhs=xn[:, :n], start=True, stop=True), reads=[(k, "xn"), (k, "RT")], writes=[("psb", 3)])
                        yield
                        P.op("pool", lambda e, a=a, b=b, n=n: e.tensor_tensor(out=t1[:, :n], in0=xn[:, :n], in1=tC[:, a:b], op=ALU.mult), reads=[(k, "xn"), (k, "tC")], writes=[(k, "t1")])
                        P.op("dve", lambda e, a=a, b=b, n=n: e.tensor_tensor(out=t2[:, :n], in0=PSB[3][0:96, :n], in1=tS[:, a:b], op=ALU.mult), reads=[("psb", 3), (k, "tS")], writes=[(k, "t2")])
                        dst = qT if which == 0 else kT
                        P.op("pool", lambda e, a=a, b=b, n=n, dst=dst: e.tensor_tensor(out=dst[:, a:b], in0=t1[:, :n], in1=t2[:, :n], op=ALU.add),
                             reads=[(k, "t1"), (k, "t2")], writes=[(k, "qk", hp, which, j)])
                        yield
                for g3 in range(3):
                    kts = list(range(g3 * 8, min(18, g3 * 8 + 8)))

                    def mmv(e, kts=kts):
                        ins = None
                        for qi, kt in enumerate(kts):
                            ins = e.matmul(PSB[3][:, qi * 64:(qi + 1) * 64], lhsT=ckvn[:, kt * 128:(kt + 1) * 128], rhs=wukv[:, h * 128 + 64:h * 128 + 128], start=True, stop=True)
                        return ins
                    P.op("pe", mmv, reads=[(k, "wukv")] + [(k, "ckvn", j) for j in tiles_all], writes=[("psb", 3)])
                    P.op("act", lambda e, kts=kts: e.copy(out=vh[:, kts[0]:kts[-1] + 1, 0:64], in_=PSB[3][:, 0:64 * len(kts)].rearrange("p (a b) -> p a b", b=64)),
                         reads=[("psb", 3)], writes=[(k, "vh", hp)])
                    yield

            def attn(h, gen):
                hp = h % 2
                qT, kT, vh = qTs[hp], kTs[hp], vhs[hp]

                def pump(cnt=1):
                    if gen is None:
                        return
                    for _ in range(cnt):
                        try:
                            next(gen)
                        except StopIteration:
                            return
                for j in tiles_all:
                    a, b = TT[j]
                    n = b - a
                    keyt = [0, 1] if j == 0 else list(range(18))

                    def emitS(ki, kt, a=a, b=b, n=n, j=j):
                        pb_ = ki % 2
                        jk = [jj for jj, (aa, bb) in enumerate(TT) if aa <= kt * 128 < bb][0]
                        P.op("pe", lambda e, kt=kt, pb_=pb_, n=n, a=a, b=b: e.matmul(PSB[4 + pb_][:, :n], lhsT=kT[:, kt * 128:(kt + 1) * 128], rhs=qT[:, a:b], start=True, stop=True),
                             reads=[(k, "qk", hp, 1, jk), (k, "qk", hp, 0, j)], writes=[("psb", 4 + pb_)])
                    emitS(0, keyt[0])
                    for ki, kt in enumerate(keyt):
                        pb_ = ki % 2
                        if ki + 1 < len(keyt):
                            emitS(ki + 1, keyt[ki + 1])
                        P.op("act", lambda e, n=n, pb_=pb_: e.activation(out=PT[:, pb_, :n], in_=PSB[4 + pb_][:, :n], func=AF.Exp, scale=SC), reads=[("psb", 4 + pb_)], writes=[(k, "PT", pb_)])
                        P.op("pe", lambda e, kt=kt, n=n, pb_=pb_, ki=ki, nk=len(keyt): e.matmul(PSB[6][0:65, :n], lhsT=vh[:, kt, :], rhs=PT[:, pb_, :n], start=(ki == 0), stop=(ki == nk - 1)),
                             reads=[(k, "vh", hp), (k, "PT", pb_)], writes=[("psb", 6)])
                        if ki % 2 == 1:
                            pump(1)
                    P.op("act", lambda e, n=n: e.activation(out=rr[64:65, :n], in_=PSB[6][64:65, :n], func=AF.Ln), reads=[("psb", 6)], writes=[(k, "rr")])
                    P.op("act", lambda e, n=n: e.activation(out=rr[64:65, :n], in_=rr[64:65, :n], func=AF.Exp, scale=-1.0), reads=[(k, "rr")], writes=[(k, "rr")])
                    P.op("pe", lambda e, n=n: e.matmul(PSB[7][0:64, :n], lhsT=ones1r[64:65, :], rhs=rr[64:65, :n], start=True, stop=True), reads=[(k, "rr"), (k, "ones1r")], writes=[("psb", 7)])
                    P.op("act", lambda e, n=n: e.copy(out=rden[:, :n], in_=PSB[7][0:64, :n]), reads=[("psb", 7)], writes=[("tmp", 0)])
                    P.op("dve", lambda e, n=n: e.tensor_tensor(out=oT[:, :n], in0=PSB[6][0:64, :n], in1=rden[:, :n], op=ALU.mult), reads=[("psb", 6), ("tmp", 0)], writes=[("tmp", 1)])
                    for oc in range(8):
                        pyb = 4 + oc % 2
                        P.op("pe", lambda e, oc=oc, n=n, pyb=pyb: e.matmul(PSB[pyb][:, :n], lhsT=wo[:, hp, oc * 128:(oc + 1) * 128], rhs=oT[:, :n], start=True, stop=True),
                             reads=[(k, "wo", hp), ("tmp", 1)], writes=[("psb", pyb)])
                        P.op("dve", lambda e, oc=oc, a=a, b=b, n=n, j=j, pyb=pyb: e.scalar_tensor_tensor(
                            out=xT[:, oc, a:b], in0=PSB[pyb][:, :n], scalar=modcol(2, oc, j), in1=xT[:, oc, a:b], op0=ALU.mult, op1=ALU.add),
                            reads=[("psb", pyb), "modt", ("x", oc, j)], writes=[("x", oc, j)])
                    pump(2)
                if gen is not None:
                    for _ in gen:
                        pass

            g0 = proj_gen(0)
            for _ in g0:
                pass
            for h in range(16):
                attn(h, proj_gen(h + 1) if h + 1 < 16 else None)

        def mlstm(tiles_all):
            phase()
            k = "ml"
            wqkv = arA.alloc([128, 8, 2048], BF16)
            selc = arA.alloc([16, 2, 16, 64], F32)
            maskc = arA.alloc([64, 2, 256], F32)
            selh = arA.alloc([16, 2, 4], F32)
            wg = arA.alloc([128, 8, 16], BF16)
            Cbf = arA.alloc([128, 4, 257], BF16)
            G = arB.alloc([16, NT], F32)
            Bd = [arB.alloc([16, NT], F32), arB.alloc([16, NT], F32)]
            Vp = arB.alloc([64, 4, 257], BF16)
            Cst = arB.alloc([128, 4, 257], F32)
            qkT = tmp[0][:, :].bitcast(BF16)[:, 0:512]
            Kw = tmp[1][0:64, :].bitcast(BF16)[:, 0:512].rearrange("p (a b) -> p a b", a=4)
            Em = tmp[2][0:64, 0:256]
            St = tmp[2][0:64, 256:384].bitcast(BF16)
            pis = tmp[3][0:64, 0:257]
            nd = tmp[4][0:64, 0:257]
            hout = tmp[5][0:64, :].bitcast(BF16)
            inter = rstd[0:64, 0:4]
            decay = rstd[:, 8:12]
            rdn = rstd[0:64, 16:17]
            P.op("pool", lambda e: e.dma_start(out=wqkv, in_=ml_win[:, 0:2048].rearrange("(k p) n -> p k n", p=128)), writes=[(k, "wqkv")], dma=True)
            P.op("pool", lambda e: e.dma_start(out=wg, in_=ml_win[:, 3072:3088].rearrange("(k p) n -> p k n", p=128)), writes=[(k, "wg")], dma=True)
            P.op("sp", lambda e: e.dma_start(out=selc, in_=ml_selc), writes=[(k, "selc")], dma=True)
            P.op("sp", lambda e: e.dma_start(out=selh, in_=ml_selh), writes=[(k, "selh")], dma=True)
            P.op("sp", lambda e: e.dma_start(out=maskc, in_=ml_maskc.rearrange("p a b c -> p a (b c)")), writes=[(k, "maskc")], dma=True)
            P.op("pool", lambda e: e.memset(Vp, 1.0), writes=[(k, "Vp")])
            for j in tiles_all:
                a, b = TT[j]
                n = b - a

                def mmg(e, a=a, b=b, n=n):
                    ins = None
                    for kk in range(8):
                        ins = e.matmul(PSB[0][0:16, :n], lhsT=wg[:, kk, :], rhs=hT[:, kk, a:b], start=(kk == 0), stop=(kk == 7))
                    return ins
                P.op("pe", mmg, reads=[(k, "wg")] + [("h", kk, j) for kk in range(8)], writes=[("psb", 0)])
                P.op("act", lambda e, a=a, b=b, n=n: e.activation(out=G[:, a:b], in_=PSB[0][0:16, :n], func=AF.Identity, bias=pvt[0:16, pvoff["ml_gb"]:pvoff["ml_gb"] + 1], scale=1.0),
                     reads=[("psb", 0), "pvt"], writes=[(k, "G")])
            lf = tmp[3][0:16, :]
            for j in tiles_all:
                a, b = TT[j]
                n = b - a
                P.op("act", lambda e, a=a, b=b, n=n: e.activation(out=lf[:, :n], in_=G[:, a:b], func=AF.Exp, scale=-1.0), reads=[(k, "G")], writes=[("tmp", 3)])
                P.op("act", lambda e, n=n: e.activation(out=lf[:, :n], in_=lf[:, :n], func=AF.Ln, bias=cst[0:16, 1:2], scale=1.0), reads=[("tmp", 3), "cst1"], writes=[("tmp", 3)])
                P.op("dve", lambda e, n=n: e.tensor_scalar(out=lf[:, :n], in0=lf[:, :n], scalar1=-1.0, scalar2=None, op0=ALU.mult), reads=[("tmp", 3)], writes=[("tmp", 3)])
                for ci in range(n // 64):
                    c0 = ci * 64
                    P.op("dve", lambda e, a=a, c0=c0: e.tensor_tensor_scan(out=Bd[0][:, a + c0:a + c0 + 64], data0=cst[0:16, 1:2].to_broadcast([16, 64]), data1=lf[:, c0:c0 + 64], initial=0.0, op0=ALU.mult, op1=ALU.add),
                         reads=[("tmp", 3), "cst1"], writes=[(k, "B0")])
                    P.op("dve", lambda e, a=a, c0=c0: e.tensor_tensor_scan(out=Bd[1][:, a + c0:a + c0 + 64][:, ::-1], data0=cst[0:16, 1:2].to_broadcast([16, 64]), data1=lf[:, c0:c0 + 64][:, ::-1], initial=0.0, op0=ALU.mult, op1=ALU.add),
                         reads=[("tmp", 3), "cst1"], writes=[(k, "B1")])
            SQ = 128 ** -0.5
            for d in range(2):
                P.op("pool", lambda e: e.memset(Cst, 0.0), reads=[(k, "C", h) for h in range(4)], writes=[(k, "C", h) for h in range(4)])
                P.op("pool", lambda e: e.memset(Cbf, 0.0), reads=[(k, "Cbf", h) for h in range(4)], writes=[(k, "Cbf", h) for h in range(4)])
                order = list(range(36)) if d == 0 else [3, 2, 1, 0] + list(range(35, 3, -1))
                for ch in order:
                    t0 = ch * 64
                    j = [jj for jj, (aa, bb) in enumerate(TT) if aa <= t0 < bb][0]
                    tend = t0 + 63 if d == 0 else t0
                    tl = 63 if d == 0 else 0
                    hr = [("h", kk, j) for kk in range(8)]

                    def mmk(e, t0=t0):
                        ins = None
                        for kk in range(8):
                            ins = e.matmul(PSB[0][0:64, 0:512], lhsT=hT[:, kk, t0:t0 + 64], rhs=wqkv[:, kk, 512:1024], start=(kk == 0), stop=(kk == 7))
                        return ins
                    P.op("pe", mmk, reads=hr + [(k, "wqkv")], writes=[("psb", 0)])
                    for half in range(2):
                        def mmv(e, t0=t0, half=half):
                            ins = None
                            for kk in range(8):
                                ins = e.matmul(PSB[1 + half][0:64, 0:512], lhsT=hT[:, kk, t0:t0 + 64], rhs=wqkv[:, kk, 1024 + half * 512:1536 + half * 512], start=(kk == 0), stop=(kk == 7))
                            return ins
                        P.op("pe", mmv, reads=hr + [(k, "wqkv")], writes=[("psb", 1 + half)])

                    def mmqk(e, t0=t0):
                        ins = None
                        for qk in range(2):
                            for h in range(4):
                                for kk in range(8):
                                    ins = e.matmul(PSB[3][:, qk * 256 + h * 64:qk * 256 + (h + 1) * 64], lhsT=wqkv[:, kk, qk * 512 + h * 128:qk * 512 + (h + 1) * 128],
                                                   rhs=hT[:, kk, t0:t0 + 64], start=(kk == 0), stop=(kk == 7))
                        return ins
                    P.op("pe", mmqk, reads=hr + [(k, "wqkv")], writes=[("psb", 3)])
                    P.op("act", lambda e: e.activation(out=qkT[:, 0:256], in_=PSB[3][:, 0:256], func=AF.Identity, scale=SQ), reads=[("psb", 3)], writes=[(k, "qT")])
                    P.op("act", lambda e: e.copy(out=qkT[:, 256:512], in_=PSB[3][:, 256:512]), reads=[("psb", 3)], writes=[(k, "kT")])
                    for half in range(2):
                        P.op("act", lambda e, half=half: e.copy(out=Vp[:, 2 * half:2 * half + 2, 0:256], in_=PSB[1 + half][0:64, :].rearrange("p (a b) -> p a b", a=2)),
                             reads=[("psb", 1 + half)], writes=[(k, "Vp")])
                    def mme(e, t0=t0, tend=tend, d=d):
                        ins = None
                        for h in range(4):
                            rb = (4 if d == 0 else 12) + h
                            ri = (0 if d == 0 else 8) + h
                            o = PSB[4][0:64, h * 64:(h + 1) * 64]
                            e.matmul(o, lhsT=selc[:, 0, rb, :], rhs=Bd[d][:, t0:t0 + 64], start=True, stop=False)
                            e.matmul(o, lhsT=Bd[d][:, t0:t0 + 64], rhs=selc[:, 1, rb, :], start=False, stop=False)
                            e.matmul(o, lhsT=G[:, t0:t0 + 64], rhs=selc[:, 0, ri, :], start=False, stop=True)
                        e.matmul(PSB[4][0:64, 256:260], lhsT=Bd[d][:, t0:t0 + 64], rhs=selh[:, d, :], start=True, stop=True)
                        ins = e.matmul(PSB[4][:, 260:264], lhsT=Bd[d][:, tend:tend + 1].to_broadcast([16, 128]), rhs=selh[:, d, :], start=True, stop=True)
                        return ins
                    P.op("pe", mme, reads=[(k, "selc"), (k, "selh"), (k, "B0"), (k, "B1"), (k, "G")], writes=[("psb", 4)])
                    P.op("dve", lambda e, d=d: e.tensor_tensor(out=Em, in0=PSB[4][0:64, 0:256], in1=maskc[:, d, :], op=ALU.add), reads=[("psb", 4), (k, "maskc")], writes=[(k, "Em")])
                    P.op("act", lambda e: e.activation(out=Em, in_=Em, func=AF.Exp), reads=[(k, "Em")], writes=[(k, "Em")])
                    P.op("act", lambda e: e.activation(out=inter, in_=PSB[4][0:64, 256:260], func=AF.Exp), reads=[("psb", 4)], writes=[(k, "inter")])
                    P.op("act", lambda e: e.activation(out=decay, in_=PSB[4][:, 260:264], func=AF.Exp), reads=[("psb", 4)], writes=[(k, "decay")])
                    def mms(e):
                        ins = None
                        for h in range(4):
                            ins = e.matmul(PSB[5][0:64, h * 64:(h + 1) * 64], lhsT=qkT[:, 256 + h * 64:256 + (h + 1) * 64], rhs=qkT[:, h * 64:(h + 1) * 64], start=True, stop=True)
                        return ins
                    P.op("pe", mms, reads=[(k, "qT"), (k, "kT")], writes=[("psb", 5)])
                    P.op("dve", lambda e: e.tensor_tensor(out=St, in0=PSB[5][0:64, 0:256], in1=Em, op=ALU.mult), reads=[("psb", 5), (k, "Em")], writes=[(k, "St")])
                    for h in range(4):
                        P.op("pe", lambda e, h=h: e.matmul(PSB[6][0:64, 0:257], lhsT=St[:, h * 64:(h + 1) * 64], rhs=Vp[:, h, :], start=True, stop=True),
                             reads=[(k, "St"), (k, "Vp")], writes=[("psb", 6)])
                        P.op("pe", lambda e, h=h: e.matmul(PSB[7][0:64, 0:257], lhsT=qkT[:, h * 64:(h + 1) * 64], rhs=Cbf[:, h, :], start=True, stop=True),
                             reads=[(k, "qT"), (k, "Cbf", h)], writes=[("psb", 7)])
                        P.op("act", lambda e: e.copy(out=pis, in_=PSB[6][0:64, 0:257]), reads=[("psb", 6)], writes=[(k, "pis")])
                        P.op("dve", lambda e, h=h: e.scalar_tensor_tensor(out=nd, in0=PSB[7][0:64, 0:257], scalar=inter[:, h:h + 1], in1=pis, op0=ALU.mult, op1=ALU.add),
                             reads=[("psb", 7), (k, "inter"), (k, "pis")], writes=[(k, "nd")])
                        P.op("dve", lambda e: e.tensor_scalar(out=rdn, in0=nd[:, 256:257], scalar1=-1.0, scalar2=None, op0=ALU.mult), reads=[(k, "nd")], writes=[(k, "rdn")])
                        P.op("dve", lambda e: e.tensor_tensor(out=rdn, in0=rdn, in1=nd[:, 256:257], op=ALU.max), reads=[(k, "nd"), (k, "rdn")], writes=[(k, "rdn")])
                        P.op("dve", lambda e: e.tensor_scalar(out=rdn, in0=rdn, scalar1=1.0, scalar2=None, op0=ALU.max), reads=[(k, "rdn")], writes=[(k, "rdn")])
                        P.op("dve", lambda e: e.reciprocal(out=rdn, in_=rdn), reads=[(k, "rdn")], writes=[(k, "rdn")])
                        P.op("dve", lambda e, h=h: e.tensor_scalar(out=hout[:, h * 256:(h + 1) * 256], in0=nd[:, 0:256], scalar1=rdn, scalar2=None, op0=ALU.mult),
                             reads=[(k, "nd"), (k, "rdn")], writes=[(k, "hout")])
                        P.op("dve", lambda e, h=h, tl=tl: e.tensor_scalar(out=Kw[:, h, :], in0=PSB[0][0:64, h * 128:(h + 1) * 128], scalar1=Em[:, h * 64 + tl:h * 64 + tl + 1], scalar2=None, op0=ALU.mult),
                             reads=[("psb", 0), (k, "Em")], writes=[(k, "Kw", h)])
                        ub_ = 1 + h % 2
                        P.op("pe", lambda e, h=h, ub_=ub_: e.matmul(PSB[ub_][:, 0:257], lhsT=Kw[:, h, :], rhs=Vp[:, h, :], start=True, stop=True),
                             reads=[(k, "Kw", h), (k, "Vp")], writes=[("psb", ub_)])
                        P.op("dve", lambda e, h=h, ub_=ub_: e.scalar_tensor_tensor(out=Cst[:, h, :], in0=Cst[:, h, :], scalar=decay[:, h:h + 1], in1=PSB[ub_][:, 0:257], op0=ALU.mult, op1=ALU.add),
                             reads=[(k, "C", h), (k, "decay"), ("psb", ub_)], writes=[(k, "C", h)])
                        P.op("act", lambda e, h=h: e.copy(out=Cbf[:, h, :], in_=Cst[:, h, :]), reads=[(k, "C", h)], writes=[(k, "Cbf", h)])
                    P.op("sp", lambda e, d=d, t0=t0: e.dma_start(out=hdir[d, t0:t0 + 64, :], in_=hout), reads=[(k, "hout")], writes=[(k, "hdir", d, ch // 2)], dma=True)
            phase()
            wog = arA.alloc([128, 8, 1024], BF16)
            gain = arA.alloc([128, 1024], F32)
            hfb = arA.alloc([128, 2, 1024], BF16)
            hs = arA.alloc([128, 1024], F32)
            sq = arA.alloc([128, 1024], F32)
            wout = arA.alloc([128, 2, 8, 128], BF16)
            ss = rstd[:, 0:4]
            P.op("pool", lambda e: e.dma_start(out=wog, in_=ml_win[:, 2048:3072].rearrange("(k p) n -> p k n", p=128)), writes=[(k, "wog")], dma=True)
            P.op("sp", lambda e: e.dma_start(out=gain, in_=ml_gain), writes=[(k, "gain")], dma=True)
            for ti in range(18):
                jt = [jj for jj, (aa, bb) in enumerate(TT) if aa <= ti * 128 < bb][0]
                for d in range(2):
                    P.op("sp", lambda e, d=d, ti=ti: e.dma_start(out=hfb[:, d, :], in_=hdir[d, ti * 128:(ti + 1) * 128, :]), writes=[(k, "hfb", d)], dma=True)
                for half in range(2):
                    def mmo(e, ti=ti, half=half):
                        ins = None
                        for kk in range(8):
                            ins = e.matmul(PSB[2 + half][:, :], lhsT=hT[:, kk, ti * 128:(ti + 1) * 128], rhs=wog[:, kk, half * 512:(half + 1) * 512], start=(kk == 0), stop=(kk == 7))
                        return ins
                    P.op("pe", mmo, reads=[("h", kk, jt) for kk in range(8)] + [(k, "wog")], writes=[("psb", 2 + half)])
                P.op("dve", lambda e: e.tensor_tensor(out=hs, in0=hfb[:, 0, :], in1=hfb[:, 1, :], op=ALU.add), reads=[(k, "hfb", 0), (k, "hfb", 1)], writes=[(k, "hs")])
                P.op("act", lambda e: e.activation(out=sq, in_=hs, func=AF.Square), reads=[(k, "hs")], writes=[(k, "sq")])
                P.op("dve", lambda e: e.tensor_reduce(out=ss, in_=sq[:, :].rearrange("p (a b) -> p a b", a=4), axis=AX.X, op=ALU.add), reads=[(k, "sq")], writes=[(k, "ss")])
                P.op("act", lambda e: e.activation(out=ss, in_=ss, func=AF.Sqrt, bias=cst[:, 0:1], scale=1.0 / 256.0), reads=[(k, "ss"), "cst0"], writes=[(k, "ss")])
                P.op("dve", lambda e: e.reciprocal(out=ss, in_=ss), reads=[(k, "ss")], writes=[(k, "ss")])
                for h in range(4):
                    P.op("dve", lambda e, h=h: e.scalar_tensor_tensor(out=hs[:, h * 256:(h + 1) * 256], in0=hs[:, h * 256:(h + 1) * 256], scalar=ss[:, h:h + 1], in1=gain[:, h * 256:(h + 1) * 256], op0=ALU.mult, op1=ALU.mult),
                         reads=[(k, "hs"), (k, "ss"), (k, "gain")], writes=[(k, "hs")])
                for half in range(2):
                    P.op("act", lambda e, half=half: e.activation(out=sq[:, half * 512:(half + 1) * 512], in_=PSB[2 + half][:, :], func=AF.Sigmoid), reads=[("psb", 2 + half), (k, "sq")], writes=[(k, "sq")])
                P.op("dve", lambda e: e.tensor_tensor(out=hs, in0=hs, in1=sq, op=ALU.mult), reads=[(k, "hs"), (k, "sq")], writes=[(k, "hs")])
                for half in range(2):
                    def trf(e, half=half):
                        ins = None
                        for q in range(4):
                            c = half * 4 + q
                            ins = e.transpose(PSB[half][:, q * 128:(q + 1) * 128], hs[:, c * 128:(c + 1) * 128], ident[:])
                        return ins
                    P.op("pe", trf, reads=[(k, "hs"), "ident"], writes=[("psb", half)])
                    P.op("act" if half else "dve", lambda e, half=half, ti=ti: (e.copy if half else e.tensor_copy)(out=mT[:, half * 4:half * 4 + 4, ti * 128:(ti + 1) * 128], in_=PSB[half][:, :].rearrange("p (q t) -> p q t", q=4)),
                         reads=[("psb", half)], writes=[("m", c, jt) for c in range(half * 4, half * 4 + 4)])
            for oc in range(8):
                ob = oc % 2
                P.op("pool", lambda e, oc=oc, ob=ob: e.dma_start(out=wout[:, ob, :, :], in_=ml_wout[:, oc * 128:(oc + 1) * 128].rearrange("(k p) n -> p k n", p=128)),
                     writes=[(k, "wout", ob)], dma=True)
                for j in tiles_all:
                    a, b = TT[j]
                    n = b - a
                    py = PSB[4 + j % 4]

                    def mmfn(e, py=py, ob=ob, a=a, b=b, n=n):
                        ins = None
                        for cc in range(8):
                            ins = e.matmul(py[:, :n], lhsT=wout[:, ob, cc, :], rhs=mT[:, cc, a:b], start=(cc == 0), stop=(cc == 7))
                        return ins
                    P.op("pe", mmfn, reads=[(k, "wout", ob)] + [("m", cc, j) for cc in range(8)], writes=[("psb", 4 + j % 4)])
                    P.op("dve", lambda e, py=py, oc=oc, a=a, b=b, n=n, j=j: e.scalar_tensor_tensor(
                        out=xT[:, oc, a:b], in0=py[:, :n], scalar=modcol(2, oc, j), in1=xT[:, oc, a:b], op0=ALU.mult, op1=ALU.add),
                        reads=[("psb", 4 + j % 4), "modt", ("x", oc, j)], writes=[("x", oc, j)])

        def moe_sparse(i, tiles):
            phase()
            k = "moe%d" % i
            subs = [t for j in tiles for t in range(TT[j][0] // 128, TT[j][1] // 128)]
            NOV = 36
            wr = arA.alloc([128, 8, 72], F32)
            brb = arA.alloc([128, 72], F32)
            f32t = arA.alloc([128, 8, 512], F32)
            o4 = arA.off
            lgs4 = arA.alloc([128, 4, 72], F32)
            sm4 = arA.alloc([128, 8, 4], F32)
            oh4 = arA.alloc([128, 4, 8], F32)
            eg4 = arA.alloc([128, 4, 8], F32)
            em4 = arA.alloc([128, 4, 64], F32)
            as4 = arA.alloc([128, 4, 64], F32)
            RK = arA.alloc([128, 18, 64], F32)
            assert arA.off - o4 >= 2048
            stg4 = scr[:, o4:o4 + 2048]
            OH = arA.alloc([128, 18, 2, 64], F32)
            WP = arA.alloc([128, 18, 2], F32)
            acum = arA.alloc([128, 64], F32)
            asum = arA.alloc([128, 64], F32)
            mc = arA.alloc([128, 229], F32)
            ones1 = arA.alloc([128, 128], F32)
            identb = arA.alloc([128, 128], BF16)
            cntb = arA.alloc([128, 64], F32)
            ovn = arA.alloc([128, 64], F32)
            ove = arA.alloc([128, 64], F32)
            ovsp = arA.alloc([128, 64], F32)
            dlt = arA.alloc([128, 64], F32)
            t64a = arA.alloc([128, 64], F32)
            t64b = arA.alloc([128, 64], F32)
            EB = arA.alloc([128, NOV], F32)
            DEST = arA.alloc([128, 18, 2], F32)
            DESTI = arA.alloc([128, 18, 2], I32)
            idxi = arA.alloc([128, NOV, 4], I32)
            idxf = arA.alloc([128, NOV, 4], F32)
            Ltri = mc[:, 0:128]
            e128 = mc[:, 128:192]
            iop = mc[:, 192:193]
            thr36 = mc[:, 193:229]
            P.op("sp", lambda e: e.dma_start(out=wr, in_=moe_wr[i].rearrange("(k p) n -> p k n", p=128)), writes=[(k, "wr")], dma=True)
            P.op("sp", lambda e: e.dma_start(out=brb, in_=moe_br[i]), writes=[(k, "br")], dma=True)
            P.op("sp", lambda e: e.dma_start(out=mc, in_=moec_d), writes=[(k, "mc")], dma=True)
            P.op("pool", lambda e: e.memset(ones1, 1.0), writes=[(k, "ones1")])
            P.op("pool", lambda e: e.memset(acum, 0.0), writes=[(k, "rk")])
            P.op("pool", lambda e: e.tensor_copy(out=identb, in_=ident[:]), reads=["ident"], writes=[(k, "identb")])
            RKK = [(k, "rk")]

            def extra(j, c, tb):
                a, b = TT[j]
                n = b - a
                jj = 1 if j == 0 else 0
                P.op("pool", lambda e, c=c, tb=tb, n=n, jj=jj: e.tensor_scalar(out=f32t[:, c, :n], in0=tb[:, :n], scalar1=mA[:, 1, c, jj:jj + 1],
                                                                               scalar2=modt[:, 3 * 8 + c, jj:jj + 1], op0=ALU.mult, op1=ALU.add),
                     reads=[("tmp", 2 + c % 2), "modt", "mA"], writes=[(k, "f32", c)])
                if c == 7:
                    S = n // 128
                    g0 = a // 128
                    pl = PSB[5]

                    def mmfn(e, S=S):
                        ins = None
                        for s_ in range(S):
                            for kk in range(8):
                                ins = e.matmul(pl[:, s_ * 72:(s_ + 1) * 72], lhsT=f32t[:, kk, s_ * 128:(s_ + 1) * 128], rhs=wr[:, kk, :], start=(kk == 0), stop=(kk == 7))
                        return ins
                    P.op("pe", mmfn, reads=[(k, "f32", cc) for cc in range(8)] + [(k, "wr")], writes=[("psb", 5)])
                    R = [(k, "rt")]
                    V = lambda fn, extra_r=(), extra_w=(): P.op("dve", fn, reads=R + list(extra_r), writes=R + list(extra_w))
                    L4 = lgs4[:, 0:S, :]
                    G4 = lgs4[:, 0:S, 0:8]
                    E4 = lgs4[:, 0:S, 8:72]
                    E44 = E4.rearrange("p s (g j) -> p s g j", g=8)
                    o8 = oh4[:, 0:S, :]
                    oh1 = OH[:, g0:g0 + S, 0, :]
                    oh2 = OH[:, g0:g0 + S, 1, :]
                    em_ = em4[:, 0:S, :]
                    sc = lambda i_: sm4[:, i_, 0:S]
                    bc8 = lambda v: v.unsqueeze(2).to_broadcast([128, S, 8])
                    bc64 = lambda v: v.unsqueeze(2).to_broadcast([128, S, 64])
                    V(lambda e: e.tensor_tensor(out=L4, in0=pl[:, 0:S * 72].rearrange("p (s c) -> p s c", s=S), in1=brb.unsqueeze(1).to_broadcast([128, S, 72]), op=ALU.add), [("psb", 5), (k, "br")])
                    V(lambda e: e.tensor_reduce(out=sc(0), in_=G4, axis=AX.X, op=ALU.max))
                    V(lambda e: e.tensor_tensor(out=o8, in0=G4, in1=bc8(sc(0)), op=ALU.is_equal))
                    V(lambda e: e.tensor_tensor(out=eg4[:, 0:S, :], in0=G4, in1=bc8(sc(0)), op=ALU.subtract))
                    P.op("act", lambda e: e.activation(out=eg4[:, 0:S, :], in_=eg4[:, 0:S, :], func=AF.Exp), reads=R, writes=R)
                    V(lambda e: e.tensor_reduce(out=sc(2), in_=eg4[:, 0:S, :], axis=AX.X, op=ALU.add))
                    V(lambda e: e.reciprocal(out=sc(3), in_=sc(2)))
                    V(lambda e: e.tensor_scalar(out=o8, in0=o8, scalar1=BIG, scalar2=-BIG, op0=ALU.mult, op1=ALU.add))
                    V(lambda e: e.tensor_tensor(out=em_.rearrange("p s (g j) -> p s g j", g=8), in0=E44, in1=o8.unsqueeze(3).to_broadcast([128, S, 8, 8]), op=ALU.add))
                    V(lambda e: e.tensor_reduce(out=sc(4), in_=em_, axis=AX.X, op=ALU.max))
                    V(lambda e: e.tensor_tensor(out=oh1, in0=em_, in1=bc64(sc(4)), op=ALU.is_equal), (), RKK)
                    V(lambda e: e.scalar_tensor_tensor(out=em_, in0=oh1, scalar=-BIG, in1=em_, op0=ALU.mult, op1=ALU.add))
                    V(lambda e: e.tensor_reduce(out=sc(5), in_=em_, axis=AX.X, op=ALU.max))
                    V(lambda e: e.tensor_tensor(out=oh2, in0=em_, in1=bc64(sc(5)), op=ALU.is_equal), (), RKK)
                    V(lambda e: e.tensor_tensor(out=sc(6), in0=sc(5), in1=sc(4), op=ALU.subtract))
                    P.op("act", lambda e: e.activation(out=sc(6), in_=sc(6), func=AF.Exp), reads=R, writes=R)
                    V(lambda e: e.tensor_scalar(out=sc(6), in0=sc(6), scalar1=1.0, scalar2=None, op0=ALU.add))
                    V(lambda e: e.reciprocal(out=sc(6), in_=sc(6)))
                    V(lambda e: e.tensor_tensor(out=WP[:, g0:g0 + S, 0], in0=sc(6), in1=sc(3), op=ALU.mult), (), RKK)
                    V(lambda e: e.tensor_tensor(out=WP[:, g0:g0 + S, 1], in0=sc(3), in1=WP[:, g0:g0 + S, 0], op=ALU.subtract), (), RKK)
                    V(lambda e: e.tensor_tensor(out=as4[:, 0:S, :], in0=oh1, in1=oh2, op=ALU.add), (), RKK)

                    def mmr(e, S=S):
                        ins = None
                        for s_ in range(S):
                            o = PSB[4][:, s_ * 64:(s_ + 1) * 64]
                            e.matmul(o, lhsT=Ltri, rhs=as4[:, s_, :], start=True, stop=False)
                            for sp_ in range(s_):
                                e.matmul(o, lhsT=ones1, rhs=as4[:, sp_, :], start=False, stop=False)
                            ins = e.matmul(o, lhsT=ones1, rhs=acum, start=False, stop=True)
                        return ins
                    P.op("pe", mmr, reads=RKK + R + [(k, "mc"), (k, "ones1")], writes=[("psb", 4)])
                    P.op("act", lambda e: e.copy(out=RK[:, g0:g0 + S, :], in_=PSB[4][:, 0:S * 64].rearrange("p (s c) -> p s c", s=S)), reads=[("psb", 4)] + RKK, writes=RKK)
                    V(lambda e: e.tensor_reduce(out=asum, in_=as4[:, 0:S, :].rearrange("p s c -> p c s"), axis=AX.X, op=ALU.add), [("psb", 4)], RKK)
                    V(lambda e: e.tensor_tensor(out=acum, in0=acum, in1=asum, op=ALU.add), [("psb", 4)], RKK)

            norm_mod(1, tiles, extra=extra)

            P.op("pe", lambda e: e.matmul(PSB[4][:, 0:64], lhsT=ones1, rhs=acum, start=True, stop=True), reads=RKK + [(k, "ones1")], writes=[("psb", 4)])
            V2 = lambda fn: P.op("dve", fn, reads=RKK + [("psb", 4), (k, "mc")], writes=RKK)
            V2(lambda e: e.tensor_scalar(out=cntb, in0=PSB[4][:, 0:64], scalar1=-128.0, scalar2=0.0, op0=ALU.add, op1=ALU.max))
            f32flat0 = f32t[:, :, :].rearrange("p a b -> p (a b)")
            tQ = f32flat0[:, 0:64 * NOV].rearrange("p (c q) -> p c q", q=NOV)
            F3a = [(k, "f32", cc) for cc in range(8)]
            V2b = lambda fn: P.op("dve", fn, reads=RKK + F3a + [("psb", 4), (k, "mc")], writes=RKK + F3a)
            V2b(lambda e: e.tensor_tensor(out=tQ, in0=cntb.unsqueeze(2).to_broadcast([128, 64, NOV]), in1=thr36.unsqueeze(1).to_broadcast([128, 64, NOV]), op=ALU.is_gt))
            V2b(lambda e: e.tensor_reduce(out=ovn, in_=tQ, axis=AX.X, op=ALU.add))
            V2(lambda e: e.tensor_scalar(out=ovn, in0=ovn, scalar1=128.0, scalar2=None, op0=ALU.mult))
            V2(lambda e: e.tensor_tensor_scan(out=ove, data0=ones1[:, 0:64], data1=ovn, initial=0.0, op0=ALU.mult, op1=ALU.add))
            V2(lambda e: e.tensor_tensor(out=ovsp, in0=ove, in1=ovn, op=ALU.subtract))
            V2(lambda e: e.tensor_scalar(out=ovsp, in0=ovsp, scalar1=8064.0, scalar2=None, op0=ALU.add))
            V2(lambda e: e.tensor_tensor(out=dlt, in0=e128, in1=ovsp, op=ALU.subtract))
            tQ2 = f32flat0[:, 0:64 * NOV].rearrange("p (q c) -> p q c", q=NOV)
            V2b(lambda e: e.tensor_tensor(out=tQ2, in0=ove.unsqueeze(1).to_broadcast([128, NOV, 64]), in1=thr36.unsqueeze(2).to_broadcast([128, NOV, 64]), op=ALU.is_le))
            V2b(lambda e: e.tensor_reduce(out=EB, in_=tQ2, axis=AX.X, op=ALU.add))
            V2(lambda e: e.tensor_scalar(out=t64a[:, 0:NOV], in0=EB, scalar1=64.0, scalar2=1.0e6, op0=ALU.is_ge, op1=ALU.mult))
            V2(lambda e: e.scalar_tensor_tensor(out=t64a[:, 0:NOV], in0=EB, scalar=256.0, in1=t64a[:, 0:NOV], op0=ALU.mult, op1=ALU.add))
            V2(lambda e: e.tensor_scalar(out=t64b[:, 0:1], in0=iop, scalar1=2.0, scalar2=float(i * 16384), op0=ALU.mult, op1=ALU.add))
            V2(lambda e: e.tensor_scalar(out=idxf[:, :, 0], in0=t64a[:, 0:NOV], scalar1=t64b[:, 0:1], scalar2=None, op0=ALU.add))
            V2(lambda e: e.tensor_scalar(out=idxf[:, :, 1], in0=idxf[:, :, 0], scalar1=1.0, scalar2=None, op0=ALU.add))
            V2(lambda e: e.tensor_scalar(out=t64a[:, 0:NOV], in0=t64a[:, 0:NOV], scalar1=0.5, scalar2=None, op0=ALU.mult))
            V2(lambda e: e.tensor_scalar(out=t64b[:, 1:2], in0=iop, scalar1=float(i * 8192), scalar2=None, op0=ALU.add))
            V2(lambda e: e.tensor_scalar(out=idxf[:, :, 2], in0=t64a[:, 0:NOV], scalar1=t64b[:, 1:2], scalar2=None, op0=ALU.add))
            V2(lambda e: e.tensor_copy(out=idxf[:, :, 3], in_=idxf[:, :, 2]))
            V2(lambda e: e.tensor_copy(out=idxi, in_=idxf))
            sg0, SG = subs[0], len(subs)
            f32flat = f32t[:, :, :].rearrange("p a b -> p (a b)")
            tA = f32flat[:, 0:SG * 64].rearrange("p (s c) -> p s c", s=SG)
            tB = f32flat[:, 2048:2048 + SG * 64].rearrange("p (s c) -> p s c", s=SG)
            RKs = RK[:, sg0:sg0 + SG, :]
            F3 = [(k, "f32", cc) for cc in range(8)]
            V3 = lambda fn: P.op("dve", fn, reads=RKK + F3 + [(k, "mc")], writes=RKK + F3)
            V3(lambda e: e.tensor_scalar(out=tA, in0=RKs, scalar1=128.0, scalar2=None, op0=ALU.is_lt))
            V3(lambda e: e.tensor_tensor(out=tA, in0=tA, in1=dlt.unsqueeze(1).to_broadcast([128, SG, 64]), op=ALU.mult))
            V3(lambda e: e.tensor_tensor(out=tB, in0=RKs, in1=ovsp.unsqueeze(1).to_broadcast([128, SG, 64]), op=ALU.add))
            V3(lambda e: e.tensor_tensor(out=tA, in0=tA, in1=tB, op=ALU.add))
            for kk in range(2):
                V3(lambda e, kk=kk: e.tensor_tensor(out=tB, in0=OH[:, sg0:sg0 + SG, kk, :], in1=tA, op=ALU.mult))
                V3(lambda e, kk=kk: e.tensor_reduce(out=DEST[:, sg0:sg0 + SG, kk], in_=tB, axis=AX.X, op=ALU.add))
            V2(lambda e: e.tensor_copy(out=DESTI, in_=DEST))
            P.barrier()
            ftok = arB.alloc([128, 2, 1024], BF16)
            for si, gt in enumerate(subs):
                fb = si % 2
                jt = [jj for jj, (aa, bb) in enumerate(TT) if aa <= gt * 128 < bb][0]
                pbf = PSB[fb][:, :].bitcast(BF16)

                def trf(e, gt=gt, pbf=pbf):
                    ins = None
                    for c in range(8):
                        ins = e.transpose(pbf[:, c * 128:(c + 1) * 128], hT[:, c, gt * 128:(gt + 1) * 128], identb)
                    return ins
                P.op("pe", trf, reads=[("h", c, jt) for c in range(8)] + [(k, "identb")], writes=[("psb", fb)])
                P.op("act", lambda e, fb=fb, pbf=pbf: e.copy(out=ftok[:, fb, :], in_=pbf), reads=[("psb", fb)], writes=[(k, "ftok", fb)])
                for kk in range(2):
                    P.op("pool", lambda e, fb=fb, gt=gt, kk=kk: e.indirect_dma_start(
                        out=xslots[:, :], out_offset=bass.IndirectOffsetOnAxis(ap=DESTI[:, gt, kk:kk + 1], axis=0), in_=ftok[:, fb, :], in_offset=None),
                        reads=[(k, "ftok", fb)] + RKK, writes=[(k, "xs", gt, kk)], dma=True)
            P.barrier()
            arB.reset()
            stg = f32t[:, :, :].rearrange("p a b -> p (a b)").rearrange("p (s n) -> p s n", s=2)
            xb = arB.alloc([128, 2, 1024], BF16)
            xbT = arB.alloc([128, 2, 8, 128], BF16)
            wgu = arB.alloc([128, 2, 8, 512], BF16)
            wd = arB.alloc([128, 2, 2, 1024], BF16)
            sg = arB.alloc([128, 256], F32)
            actb = arB.alloc([128, 2, 2, 128], BF16)
            wgu_rows = moe_wgu.rearrange("l e (p h q) n -> (l e p h) (q n)", p=128, h=2)
            wd_rows = moe_wd.rearrange("l e (p q) n -> (l e p) (q n)", p=128)
            OHflat = OH[:, :, :, :].rearrange("p a b c -> p (a b c)")
            stgs = [stg[:, 0, :], stg[:, 1, :], OHflat[:, 0:2048], stg4]
            nstg = [0]

            def emit_weights(b, si):
                wb = si % 2
                ov = b - 64
                for piece in range(3):
                    sb_ = nstg[0] % 4
                    nstg[0] += 1
                    sv = stgs[sb_]
                    if b < 64:
                        if piece < 2:
                            src = moe_wgu[i, b].rearrange("(p q) n -> p (q n)", p=128)[:, piece * 2048:(piece + 1) * 2048]
                        else:
                            src = moe_wd[i, b].rearrange("(p q) n -> p (q n)", p=128)
                        P.op("sp", lambda e, src=src, sv=sv: e.dma_start(out=sv, in_=src), writes=[(k, "stg", sb_)], dma=True)
                    else:
                        if piece < 2:
                            P.op("pool", lambda e, ov=ov, sv=sv, piece=piece: e.indirect_dma_start(
                                out=sv, out_offset=None, in_=wgu_rows[:, :],
                                in_offset=bass.IndirectOffsetOnAxis(ap=idxi[:, ov, piece:piece + 1], axis=0), bounds_check=getreg(e, 65535), oob_is_err=False),
                                reads=RKK, writes=[(k, "stg", sb_)], dma=True)
                        else:
                            P.op("pool", lambda e, ov=ov, sv=sv: e.indirect_dma_start(
                                out=sv, out_offset=None, in_=wd_rows[:, :],
                                in_offset=bass.IndirectOffsetOnAxis(ap=idxi[:, ov, 2:3], axis=0), bounds_check=getreg(e, 32767), oob_is_err=False),
                                reads=RKK, writes=[(k, "stg", sb_)], dma=True)
                    ceng = "act" if piece != 1 else "dve"
                    if piece < 2:
                        P.op(ceng, lambda e, sv=sv, wb=wb, piece=piece, ceng=ceng: (e.copy if ceng == "act" else e.tensor_copy)(out=wgu[:, wb, piece * 4:(piece + 1) * 4, :], in_=sv.rearrange("p (q n) -> p q n", q=4)),
                             reads=[(k, "stg", sb_)], writes=[(k, "wgu", wb, piece)])
                    else:
                        P.op(ceng, lambda e, sv=sv, wb=wb: e.copy(out=wd[:, wb, :, :], in_=sv.rearrange("p (q n) -> p q n", q=2)),
                             reads=[(k, "stg", sb_)], writes=[(k, "wd", wb)])

            def emit_compute(b, si):
                wb = si % 2
                xbuf = si % 2
                P.op("sp", lambda e, b=b, xbuf=xbuf: e.dma_start(out=xb[:, xbuf, :], in_=xslots[b * 128:(b + 1) * 128, :]), writes=[(k, "xb", xbuf)], dma=True)
                pbf = PSB[xbuf][:, :].bitcast(BF16)

                def trx(e, xbuf=xbuf, pbf=pbf):
                    ins = None
                    for q in range(8):
                        ins = e.transpose(pbf[:, q * 128:(q + 1) * 128], xb[:, xbuf, q::8], identb)
                    return ins
                P.op("pe", trx, reads=[(k, "xb", xbuf), (k, "identb")], writes=[("psb", xbuf)])
                P.op("dve", lambda e, xbuf=xbuf, pbf=pbf: e.tensor_copy(out=xbT[:, xbuf, :, :], in_=pbf.rearrange("p (q s) -> p q s", q=8)), reads=[("psb", xbuf)], writes=[(k, "xbT", xbuf)])
                pgu = PSB[2 + xbuf]

                def mmgu(e, xbuf=xbuf, wb=wb, pgu=pgu):
                    ins = None
                    for oc in range(4):
                        for q in range(8):
                            ins = e.matmul(pgu[:, oc * 128:(oc + 1) * 128], lhsT=wgu[:, wb, q, (oc // 2) * 256 + (oc % 2):(oc // 2) * 256 + 256:2], rhs=xbT[:, xbuf, q, :], start=(q == 0), stop=(q == 7))
                    return ins
                P.op("pe", mmgu, reads=[(k, "wgu", wb, 0), (k, "wgu", wb, 1), (k, "xbT", xbuf)], writes=[("psb", 2 + xbuf)])
                P.op("act", lambda e, pgu=pgu: e.activation(out=sg, in_=pgu[:, 0:256], func=AF.Silu), reads=[("psb", 2 + xbuf)], writes=[(k, "sg")])
                P.op("dve", lambda e, pgu=pgu, xbuf=xbuf: e.tensor_tensor(out=actb[:, xbuf, :, :], in0=sg[:, :].rearrange("p (a b) -> p a b", a=2), in1=pgu[:, 256:512].rearrange("p (a b) -> p a b", a=2), op=ALU.mult),
                     reads=[(k, "sg"), ("psb", 2 + xbuf)], writes=[(k, "actb", xbuf)])
                for half in range(2):
                    pyb = 4 + 2 * xbuf + half
                    tb_ = 2 * xbuf + half

                    def mmd(e, half=half, pyb=pyb, xbuf=xbuf, wb=wb):
                        ins = None
                        for fc in range(2):
                            ins = e.matmul(PSB[pyb][:, :], lhsT=actb[:, xbuf, fc, :], rhs=wd[:, wb, fc, half * 512:(half + 1) * 512], start=(fc == 0), stop=(fc == 1))
                        return ins
                    P.op("pe", mmd, reads=[(k, "actb", xbuf), (k, "wd", wb)], writes=[("psb", pyb)])
                    P.op("dve", lambda e, pyb=pyb, tb_=tb_: e.tensor_copy(out=tmp[tb_][:, :].bitcast(BF16)[:, 0:512], in_=PSB[pyb][:, :]),
                         reads=[("psb", pyb)], writes=[("tmp", tb_)])
                    P.op("pool", lambda e, b=b, half=half, tb_=tb_: e.dma_start(out=yslots[b * 128:(b + 1) * 128, half * 512:(half + 1) * 512], in_=tmp[tb_][:, :].bitcast(BF16)[:, 0:512]),
                         reads=[("tmp", tb_)], writes=[(k, "ys", b, half)], dma=True)

            order = []
            ovl = list(range(64, 64 + NOV))
            for b in range(64):
                order.append(b)
                if b % 2 == 1 and ovl:
                    order.append(ovl.pop(0))
            order += ovl
            emit_weights(order[0], 0)
            for si, b in enumerate(order):
                if si + 1 < len(order):
                    emit_weights(order[si + 1], si + 1)
                emit_compute(b, si)
            P.barrier()
            arB.reset()
            yg = arB.alloc([128, 2, 2, 1024], BF16)
            yt = arB.alloc([128, 1024], F32)
            for si, gt in enumerate(subs):
                gb = si % 2
                jt = [jj for jj, (aa, bb) in enumerate(TT) if aa <= gt * 128 < bb][0]
                for kk in range(2):
                    P.op("pool", lambda e, gb=gb, gt=gt, kk=kk: e.indirect_dma_start(
                        out=yg[:, gb, kk, :], out_offset=None, in_=yslots[:, :], in_offset=bass.IndirectOffsetOnAxis(ap=DESTI[:, gt, kk:kk + 1], axis=0)),
                        reads=RKK, writes=[(k, "yg", gb, kk)], dma=True)
                P.op("dve", lambda e, gb=gb, gt=gt: e.tensor_scalar(out=yt, in0=yg[:, gb, 0, :], scalar1=WP[:, gt, 0:1], scalar2=None, op0=ALU.mult),
                     reads=[(k, "yg", gb, 0)] + RKK, writes=[(k, "yt")])
                P.op("dve", lambda e, gb=gb, gt=gt: e.scalar_tensor_tensor(out=yt, in0=yg[:, gb, 1, :], scalar=WP[:, gt, 1:2], in1=yt, op0=ALU.mult, op1=ALU.add),
                     reads=[(k, "yg", gb, 1), (k, "yt")] + RKK, writes=[(k, "yt")])
                for half in range(2):
                    pb = PSB[4 + half]

                    def trf(e, pb=pb, half=half):
                        ins = None
                        for q in range(4):
                            c = half * 4 + q
                            ins = e.transpose(pb[:, q * 128:(q + 1) * 128], yt[:, c * 128:(c + 1) * 128], ident[:])
                        return ins
                    P.op("pe", trf, reads=[(k, "yt"), "ident"], writes=[("psb", 4 + half)])
                    for q in range(4):
                        c = half * 4 + q
                        P.op("dve", lambda e, pb=pb, q=q, c=c, gt=gt, jt=jt: e.scalar_tensor_tensor(
                            out=xT[:, c, gt * 128:(gt + 1) * 128], in0=pb[:, q * 128:(q + 1) * 128], scalar=modcol(5, c, jt), in1=xT[:, c, gt * 128:(gt + 1) * 128], op0=ALU.mult, op1=ALU.add),
                            reads=[("psb", 4 + half), "modt", ("x", c, jt)], writes=[("x", c, jt)])

        def moe(i, tiles):
            phase()
            k = "moe%d" % i
            wr = arA.alloc([128, 8, 72], F32)
            brb = arA.alloc([128, 72], F32)
            f32t = arA.alloc([128, 8, 512], F32)
            lgs = arA.alloc([128, 72], F32)
            sm = arA.alloc([128, 16], F32)
            oh = arA.alloc([128, 8], F32)
            em = arA.alloc([128, 64], F32)
            oh1 = arA.alloc([128, 64], F32)
            oh2 = arA.alloc([128, 64], F32)
            wt = arA.alloc([128, 64], F32)
            wtT = arA.alloc([64, NT], F32)
            wgu = arB.alloc([128, 2, 8, 512], BF16)
            wd = arB.alloc([128, 2, 2, 1024], BF16)
            sgt = arB.alloc([128, 2, 512], F32)
            actt = arB.alloc([128, 2, 2, 512], BF16)
            wbc = arB.alloc([128, 512], F32)
            P.op("sp", lambda e: e.dma_start(out=wr, in_=moe_wr[i].rearrange("(k p) n -> p k n", p=128)), writes=[(k, "wr")], dma=True)
            P.op("sp", lambda e: e.dma_start(out=brb, in_=moe_br[i]), writes=[(k, "br")], dma=True)

            def extra(j, c, tb):
                a, b = TT[j]
                n = b - a
                jj = 1 if j == 0 else 0
                P.op("pool", lambda e, c=c, tb=tb, n=n, jj=jj: e.tensor_scalar(out=f32t[:, c, :n], in0=tb[:, :n], scalar1=mA[:, 1, c, jj:jj + 1],
                                                                               scalar2=modt[:, 3 * 8 + c, jj:jj + 1], op0=ALU.mult, op1=ALU.add),
                     reads=[("tmp", 2 + c % 2), "modt", "mA"], writes=[(k, "f32", c)])
                if c == 7:
                    for s in range(n // 128):
                        gt = a // 128 + s
                        pl = PSB[5]

                        def mmfn(e, s=s):
                            ins = None
                            for kk in range(8):
                                ins = e.matmul(pl[:, 0:72], lhsT=f32t[:, kk, s * 128:(s + 1) * 128], rhs=wr[:, kk, :], start=(kk == 0), stop=(kk == 7))
                            return ins
                        P.op("pe", mmfn, reads=[(k, "f32", cc) for cc in range(8)] + [(k, "wr")], writes=[("psb", 5)])
                        R = [(k, "rt")]
                        V = lambda fn, extra_r=(): P.op("dve", fn, reads=R + list(extra_r), writes=R)
                        V(lambda e: e.tensor_tensor(out=lgs, in0=pl[:, 0:72], in1=brb, op=ALU.add), [("psb", 5), (k, "br")])
                        V(lambda e: e.tensor_reduce(out=sm[:, 0:1], in_=lgs[:, 0:8], axis=AX.X, op=ALU.max))
                        V(lambda e: e.tensor_scalar(out=oh, in0=lgs[:, 0:8], scalar1=sm[:, 0:1], scalar2=None, op0=ALU.is_equal))
                        V(lambda e: e.tensor_scalar(out=sm[:, 1:2], in0=sm[:, 0:1], scalar1=-1.0, scalar2=None, op0=ALU.mult))
                        P.op("act", lambda e: e.activation(out=sm[:, 8:16], in_=lgs[:, 0:8], func=AF.Exp, bias=sm[:, 1:2], scale=1.0), reads=R, writes=R)
                        V(lambda e: e.tensor_reduce(out=sm[:, 2:3], in_=sm[:, 8:16], axis=AX.X, op=ALU.add))
                        V(lambda e: e.reciprocal(out=sm[:, 3:4], in_=sm[:, 2:3]))
                        V(lambda e: e.tensor_scalar(out=oh, in0=oh, scalar1=BIG, scalar2=-BIG, op0=ALU.mult, op1=ALU.add))
                        for g in range(8):
                            V(lambda e, g=g: e.tensor_scalar(out=em[:, g * 8:(g + 1) * 8], in0=lgs[:, 8 + g * 8:16 + g * 8], scalar1=oh[:, g:g + 1], scalar2=None, op0=ALU.add))
                        V(lambda e: e.tensor_reduce(out=sm[:, 4:5], in_=em, axis=AX.X, op=ALU.max))
                        V(lambda e: e.tensor_scalar(out=oh1, in0=em, scalar1=sm[:, 4:5], scalar2=None, op0=ALU.is_equal))
                        V(lambda e: e.scalar_tensor_tensor(out=em, in0=oh1, scalar=-BIG, in1=em, op0=ALU.mult, op1=ALU.add))
                        V(lambda e: e.tensor_reduce(out=sm[:, 5:6], in_=em, axis=AX.X, op=ALU.max))
                        V(lambda e: e.tensor_scalar(out=oh2, in0=em, scalar1=sm[:, 5:6], scalar2=None, op0=ALU.is_equal))
                        V(lambda e: e.tensor_tensor(out=sm[:, 6:7], in0=sm[:, 5:6], in1=sm[:, 4:5], op=ALU.subtract))
                        P.op("act", lambda e: e.activation(out=sm[:, 6:7], in_=sm[:, 6:7], func=AF.Exp), reads=R, writes=R)
                        V(lambda e: e.tensor_scalar(out=sm[:, 6:7], in0=sm[:, 6:7], scalar1=1.0, scalar2=None, op0=ALU.add))
                        V(lambda e: e.reciprocal(out=sm[:, 6:7], in_=sm[:, 6:7]))
                        V(lambda e: e.tensor_tensor(out=sm[:, 6:7], in0=sm[:, 6:7], in1=sm[:, 3:4], op=ALU.mult))
                        V(lambda e: e.tensor_tensor(out=sm[:, 7:8], in0=sm[:, 3:4], in1=sm[:, 6:7], op=ALU.subtract))
                        V(lambda e: e.tensor_scalar(out=wt, in0=oh1, scalar1=sm[:, 6:7], scalar2=None, op0=ALU.mult))
                        V(lambda e: e.scalar_tensor_tensor(out=wt, in0=oh2, scalar=sm[:, 7:8], in1=wt, op0=ALU.mult, op1=ALU.add))
                        pt = PSB[4]
                        P.op("pe", lambda e: e.transpose(pt[0:64, 0:128], wt, ident[:]), reads=R + ["ident"], writes=[("psb", 4)])
                        P.op("act", lambda e, gt=gt: e.copy(out=wtT[:, gt * 128:(gt + 1) * 128], in_=pt[0:64, 0:128]), reads=[("psb", 4)], writes=[(k, "wtT", j)])

            norm_mod(1, tiles, extra=extra)

            for ex in range(64):
                eb = ex % 2
                P.op("pool", lambda e, ex=ex, eb=eb: e.dma_start(out=wgu[:, eb, :, :], in_=moe_wgu[i, ex].rearrange("(k p) n -> p k n", p=128)),
                     writes=[(k, "wgu", eb)], dma=True)
                P.op("pool", lambda e, ex=ex, eb=eb: e.dma_start(out=wd[:, eb, :, :], in_=moe_wd[i, ex].rearrange("(k p) n -> p k n", p=128)),
                     writes=[(k, "wd", eb)], dma=True)
                for j in tiles:
                    a, b = TT[j]
                    n = b - a
                    pw = PSB[6]
                    P.op("pe", lambda e, ex=ex, a=a, b=b, n=n: e.matmul(pw[:, :n], lhsT=ident[0:64, ex:ex + 1].to_broadcast([64, 128]), rhs=wtT[:, a:b], start=True, stop=True),
                         reads=["ident", (k, "wtT", j)], writes=[("psb", 6)])
                    P.op("act", lambda e, n=n: e.copy(out=wbc[:, :n], in_=pw[:, :n]), reads=[("psb", 6)], writes=[(k, "wbc")])
                    for fc in range(2):
                        pg, pu = PSB[0 + fc], PSB[2 + fc]
                        for part, pp, pk in ((0, pg, ("psb", 0 + fc)), (1, pu, ("psb", 2 + fc))):
                            def mmfn(e, part=part, pp=pp, a=a, b=b, n=n, eb=eb, fc=fc):
                                ins = None
                                for kk in range(8):
                                    ins = e.matmul(pp[:, :n], lhsT=wgu[:, eb, kk, part * 256 + fc * 128: part * 256 + (fc + 1) * 128], rhs=hT[:, kk, a:b],
                                                   start=(kk == 0), stop=(kk == 7))
                                return ins
                            P.op("pe", mmfn, reads=[(k, "wgu", eb)] + [("h", kk, j) for kk in range(8)], writes=[pk])
                        P.op("act", lambda e, pg=pg, fc=fc, n=n: e.activation(out=sgt[:, fc, :n], in_=pg[:, :n], func=AF.Silu), reads=[("psb", 0 + fc)], writes=[(k, "sgt", fc)])
                        P.op("dve", lambda e, pu=pu, fc=fc, n=n: e.tensor_tensor(out=sgt[:, fc, :n], in0=sgt[:, fc, :n], in1=pu[:, :n], op=ALU.mult), reads=[(k, "sgt", fc), ("psb", 2 + fc)], writes=[(k, "sgt", fc)])
                        P.op("pool", lambda e, fc=fc, n=n, eb=eb: e.tensor_tensor(out=actt[:, eb, fc, :n], in0=sgt[:, fc, :n], in1=wbc[:, :n], op=ALU.mult), reads=[(k, "sgt", fc), (k, "wbc")], writes=[(k, "actt", eb, fc)])
                    for oc in range(8):
                        py = PSB[4 + oc % 2]

                        def mmfn(e, py=py, oc=oc, n=n, eb=eb):
                            ins = None
                            for fc in range(2):
                                ins = e.matmul(py[:, :n], lhsT=wd[:, eb, fc, oc * 128:(oc + 1) * 128], rhs=actt[:, eb, fc, :n], start=(fc == 0), stop=(fc == 1))
                            return ins
                        P.op("pe", mmfn, reads=[(k, "wd", eb), (k, "actt", eb, 0), (k, "actt", eb, 1)], writes=[("psb", 4 + oc % 2)])
                        P.op("dve", lambda e, py=py, oc=oc, a=a, b=b, n=n, j=j: e.scalar_tensor_tensor(
                            out=xT[:, oc, a:b], in0=py[:, :n], scalar=modcol(5, oc, j), in1=xT[:, oc, a:b], op0=ALU.mult, op1=ALU.add),
                            reads=[("psb", 4 + oc % 2), "modt", ("x", oc, j)], writes=[("x", oc, j)])

        all_tiles = [0, 1, 2, 3, 4]
        for i in range(n_layers):
            last = i == DEPTH - 1
            compute_mod(i)
            phase()
            norm_mod(0, all_tiles)
            kind, jl = i % 3, i // 3
            if kind == 0:
                rglru(jl, all_tiles)
            elif kind == 1:
                mla(all_tiles)
            else:
                mlstm(all_tiles)
            if stop_after_mixer and i == n_layers - 1:
                break
            moe_sparse(i, [1, 2, 3, 4] if last else all_tiles)

        phase()
        stage = arA.alloc([128, 2, 1024], F32)
        outkeys = []
        for ti in range(18):
            jt = [j for j, (a, b) in enumerate(TT) if a <= ti * 128 < b][0]
            dst = octx_d[ti * 128:(ti + 1) * 128, :] if ti < 2 else out_d[(ti - 2) * 128:(ti - 1) * 128, :]
            sbuf = ti % 2
            for half in range(2):
                pb = PSB[half]

                def trfn(e, pb=pb, half=half, ti=ti):
                    ins = None
                    for q in range(4):
                        c = half * 4 + q
                        ins = e.transpose(pb[:, q * 128:(q + 1) * 128], xT[:, c, ti * 128:(ti + 1) * 128], ident[:])
                    return ins
                P.op("pe", trfn, reads=[("x", c, jt) for c in range(half * 4, half * 4 + 4)] + ["ident"], writes=[("psb", half)])
                P.op("dve" if half == 0 else "act",
                     lambda e, pb=pb, half=half, sbuf=sbuf: (e.tensor_copy if half == 0 else e.copy)(out=stage[:, sbuf, half * 512:(half + 1) * 512], in_=pb[:, :]),
                     reads=[("psb", half)], writes=[("ostage", sbuf, half)])
            P.op("sp", lambda e, dst=dst, sbuf=sbuf: e.dma_start(out=dst, in_=stage[:, sbuf, :]),
                 reads=[("ostage", sbuf, 0), ("ostage", sbuf, 1)], writes=[("out", ti)], dma=True)
            outkeys.append(("out", ti))
        P.finish(outkeys)
        P.emit()
    return nc


_CACHE = {}


def host_prep(inp, n_layers=DEPTH, stop_after_mixer=False):
    pv = pv_layout(inp)
    pvt = pv.table()
    key = (pvt.shape[1], n_layers, stop_after_mixer)
    if key not in _CACHE:
        _CACHE[key] = build(pv.off, pvt.shape[1], n_layers, stop_after_mixer)
    nc = _CACHE[key]
    f32 = lambda a: np.ascontiguousarray(np.asarray(a, np.float32))
    rC, rS, rRT = rope_tables()
    moec = np.zeros((128, 229), np.float32)
    moec[:, 193:229] = (np.arange(36, dtype=np.float32) * 128.0)[None, :]
    tp_, tt_ = np.meshgrid(np.arange(128), np.arange(128), indexing="ij")
    moec[:, 0:128] = (tp_ < tt_).astype(np.float32)
    moec[:, 128:192] = (np.arange(64, dtype=np.float32) * 128.0)[None, :]
    moec[:, 192] = np.arange(128, dtype=np.float32)
    selc = np.zeros((16, 2, 16, 64), np.float32)
    for r in range(16):
        selc[r, 0, r, :] = 1.0
        selc[r, 1, r, :] = -1.0
    selh = np.zeros((16, 2, 4), np.float32)
    for h in range(4):
        selh[4 + h, 0, h] = 1.0
        selh[12 + h, 1, h] = 1.0
    maskc = np.zeros((64, 2, 4, 64), np.float32)
    si, ti_ = np.meshgrid(np.arange(64), np.arange(64), indexing="ij")
    maskc[:, 0, :, :] = np.where(si <= ti_, 0.0, -30000.0)[:, None, :]
    maskc[:, 1, :, :] = np.where(si >= ti_, 0.0, -30000.0)[:, None, :]
    moe_wr = f32(np.concatenate([inp["moe_w_group"], inp["moe_w_expert"]], axis=2))
    br = np.concatenate([inp["moe_b_group"], inp["moe_b_expert"]], axis=1)
    moe_br = f32(np.repeat(br[:, None, :], 128, axis=1))
    shared = {
        "pv": pvt, "ident": np.eye(128, dtype=np.float32), "ada_w": f32(inp["ada_w"]),
        "rg_w_in": f32(inp["rg_w_in"]), "rg_gate_w": f32(inp["rg_gate_w"]), "rg_w_out": f32(inp["rg_w_out"]),
        "moe_wr": moe_wr, "moe_br": moe_br, "moec": moec,
        "mla_w_down": f32(inp["mla_w_down"][0]), "mla_w_uq": f32(inp["mla_w_uq"][0]), "mla_w_ukv": f32(inp["mla_w_ukv"][0]),
        "ml_w_in": f32(inp["ml_w_in"][0]), "ml_w_out": f32(inp["ml_w_out"][0]),
        "ml_gain": f32(np.repeat(np.asarray(inp["ml_out_norm"][0], np.float32)[None, :], 128, axis=0)),
        "ml_selc": selc, "ml_selh": selh, "ml_maskc": maskc,
        "mla_w_o": f32(inp["mla_w_o"][0]), "ropeC": rC, "ropeS": rS, "ropeRT": rRT,
        "moe_w_gate_up": f32(inp["moe_w_gate_up"]), "moe_w_down": f32(inp["moe_w_down"]),
    }
    in_maps = []
    for b in range(8):
        cc = np.zeros((128, 16), np.float32)
        cc[:, 0::2] = np.asarray(inp["c"][b], np.float32).reshape(8, 128).T
        cc[:, 1::2] = np.asarray(inp["c_ctx"], np.float32).reshape(8, 128).T
        m = dict(shared)
        m["x"] = f32(inp["x"][b])
        m["ctx"] = f32(inp["ctx"][b])
        m["cc"] = cc
        in_maps.append(m)
    return nc, in_maps


def kernel(**inputs):
    nc, in_maps = host_prep(inputs)
    res = run_bass_kernel_spmd(nc, in_maps, core_ids=list(range(8)))
    return np.stack([np.asarray(r["out"]) for r in res.results], axis=0).astype(np.float32)
```

```python
import numpy as np
from contextlib import ExitStack
import concourse.bass as bass
import concourse.mybir as mybir
from concourse.bass_utils import run_bass_kernel_spmd

F32 = mybir.dt.float32
BF16 = mybir.dt.bfloat16
I32 = mybir.dt.int32
AF = mybir.ActivationFunctionType
ALU = mybir.AluOpType
AX = mybir.AxisListType

NDMA_SEM = 32
D = 1024
NT = 2304
NCTX = 256
NLAT = 2048
TT = [(0, 256), (256, 768), (768, 1280), (1280, 1792), (1792, 2304)]
DEPTH = 4
BIG = 1.0e30


class Prog:
    ENGS = ("pe", "act", "dve", "pool", "sp")

    def __init__(self, nc, stack):
        self.nc = nc
        self.stack = stack
        self.sem = {e: stack.enter_context(nc.semaphore("s_" + e)) for e in self.ENGS}
        self.dsem = [stack.enter_context(nc.semaphore("d%d" % i)) for i in range(NDMA_SEM)]
        self.ops = {e: [] for e in self.ENGS}
        self.cnt = {e: 0 for e in self.ENGS}
        self.ndma = 0
        self.lw = {}
        self.rd = {}
        self.known = {e: {} for e in self.ENGS}
        self.final_waits = []
        self.pending = {e: [] for e in self.ENGS}

    def barrier(self):
        toks = [(e, self.cnt[e]) for e in self.ENGS if self.cnt[e] > 0]
        for j in range(NDMA_SEM):
            n = (self.ndma - j + NDMA_SEM - 1) // NDMA_SEM
            if n > 0:
                toks.append((("d", j), 16 * n))
        for e in self.ENGS:
            self.pending[e] = list(toks)

    def _semobj(self, sk):
        return self.sem[sk] if isinstance(sk, str) else self.dsem[sk[1]]

    def _need(self, eng, tok, waits):
        sk, v = tok
        if sk == "pe" and eng == "pe":
            return
        if self.known[eng].get(sk, 0) >= v:
            return
        if waits.get(sk, 0) < v:
            waits[sk] = v

    def op(self, eng, fn, reads=(), writes=(), dma=False):
        waits = {}
        for k in reads:
            t = self.lw.get(k)
            if t is not None:
                self._need(eng, t, waits)
        for k in writes:
            t = self.lw.get(k)
            if t is not None:
                self._need(eng, t, waits)
            for t in self.rd.get(k, ()):
                self._need(eng, t, waits)
        for tok in self.pending[eng]:
            self._need(eng, tok, waits)
        self.pending[eng] = []
        if dma:
            j = self.ndma % NDMA_SEM
            rnd = self.ndma // NDMA_SEM
            self.ndma += 1
            sk = ("d", j)
            if rnd > 0:
                self._need(eng, (sk, 16 * rnd), waits)
            tok = (sk, 16 * (rnd + 1))
        else:
            self.cnt[eng] += 1
            tok = (eng, self.cnt[eng])
        for sk, v in waits.items():
            self.known[eng][sk] = v
        self.ops[eng].append((list(waits.items()), fn, tok))
        for k in writes:
            self.lw[k] = tok
            self.rd[k] = []
        for k in reads:
            if k in writes:
                continue
            self.rd.setdefault(k, []).append(tok)
        return tok

    def finish(self, keys):
        waits = {}
        for k in keys:
            t = self.lw.get(k)
            if t is not None:
                self._need("sp", t, waits)
        self.final_waits = list(waits.items())

    def emit(self):
        nc = self.nc
        with nc.Block() as block:
            def mk(e):
                def body(engobj):
                    for waits, fn, tok in self.ops[e]:
                        for sk, v in waits:
                            engobj.wait_ge(self._semobj(sk), v)
                        ins = fn(engobj)
                        ins.then_inc(self._semobj(tok[0]), 1 if isinstance(tok[0], str) else 16)
                    if e == "sp":
                        for sk, v in self.final_waits:
                            engobj.wait_ge(self._semobj(sk), v)
                return body
            block.tensor(mk("pe"))
            block.scalar(mk("act"))
            block.vector(mk("dve"))
            block.gpsimd(mk("pool"))
            block.sync(mk("sp"))

    def sb(self, name, shape, dt):
        return self.stack.enter_context(self.nc.sbuf_tensor(name, shape, dt))

    def ps(self, name, shape, dt):
        return self.stack.enter_context(self.nc.psum_tensor(name, shape, dt))


class PV:
    def __init__(self):
        self.cols = []
        self.off = {}
        self.n = 0

    def add(self, name, v):
        v = np.asarray(v, np.float32).reshape(-1)
        assert v.size % 128 == 0
        a = v.reshape(-1, 128).T
        self.off[name] = self.n
        self.cols.append(a)
        self.n += a.shape[1]

    def table(self):
        return np.ascontiguousarray(np.concatenate(self.cols, axis=1))


def pv_layout(inp):
    pv = PV()
    for i in range(DEPTH):
        pv.add("nmix%d" % i, inp["norm_mix"][i])
        pv.add("nffn%d" % i, inp["norm_ffn"][i])
        ab = inp["ada_b"][i].reshape(48, 128)
        ab2 = np.repeat(ab[:, None, :], 2, axis=1)
        pv.add("adab%d" % i, ab2.reshape(-1))
    for j in range(2):
        for k in range(4):
            pv.add("rgcw%d_%d" % (j, k), inp["rg_conv_w"][j, k])
        pv.add("rgcb%d" % j, inp["rg_conv_b"][j])
        for dr in range(2):
            for g in range(2):
                pv.add("rggb%d_%d_%d" % (j, dr, g), inp["rg_gate_b"][j, dr, g])
            pv.add("rglam%d_%d" % (j, dr), inp["rg_lambda"][j, dr])
    pad = lambda v: np.concatenate([np.asarray(v, np.float32).reshape(-1), np.zeros(128 - np.asarray(v).size % 128 if np.asarray(v).size % 128 else 0, np.float32)])
    pv.add("mla_qn", inp["mla_q_norm"][0])
    pv.add("mla_kvn", inp["mla_kv_norm"][0])
    pv.add("mla_qkn0", pad(inp["mla_qk_norm"][0, 0]))
    pv.add("mla_qkn1", pad(inp["mla_qk_norm"][0, 1]))
    pv.add("ml_gb", pad(inp["ml_gate_b"][0].reshape(-1)))
    return pv


def rope_tables():
    L = NLAT
    rows = L // 64
    row = np.broadcast_to(np.arange(rows, dtype=np.float32)[:, None], (rows, 64)).reshape(L)
    col = np.broadcast_to(np.arange(64, dtype=np.float32)[None, :], (rows, 64)).reshape(L)
    inv_freq = (np.float32(10000.0) ** (-np.arange(0, 16, 2, dtype=np.float32) / np.float32(16))).astype(np.float32)
    ar = (row[:, None] * inv_freq).astype(np.float32)
    ac = (col[:, None] * inv_freq).astype(np.float32)
    C = np.ones((96, NT), np.float32)
    S = np.zeros((96, NT), np.float32)
    for base, ang in ((64, ar), (80, ac)):
        C[base:base + 8, NCTX:] = np.cos(ang).T
        C[base + 8:base + 16, NCTX:] = np.cos(ang).T
        S[base:base + 8, NCTX:] = np.sin(ang).T
        S[base + 8:base + 16, NCTX:] = np.sin(ang).T
    R = np.zeros((96, 96), np.float32)
    for base in (64, 80):
        for j in range(8):
            R[base + j, base + 8 + j] = -1.0
            R[base + 8 + j, base + j] = 1.0
    return C, S, np.ascontiguousarray(R.T)


def build(pvoff, npv, n_layers=DEPTH, stop_after_mixer=False):
    nc = bass.Bass("TRN2", target_bir_lowering=False)

    def din(name, shape, dt=F32):
        return nc.dram_tensor(name, list(shape), dt, kind="ExternalInput").ap()

    x_d = din("x", [NLAT, D])
    ctx_d = din("ctx", [NCTX, D])
    cc_d = din("cc", [128, 16])
    pv_d = din("pv", [128, npv])
    ident_d = din("ident", [128, 128])
    ada_w = din("ada_w", [DEPTH, D, 6 * D])
    rg_w_in = din("rg_w_in", [2, D, 2 * D])
    rg_gate_w = din("rg_gate_w", [2, 2, 2, 16, 64, 64])
    rg_w_out = din("rg_w_out", [2, D, D])
    mla_wdn = din("mla_w_down", [D, 416])
    mla_wuq = din("mla_w_uq", [256, 1536])
    mla_wukv = din("mla_w_ukv", [128, 2048])
    mla_wo = din("mla_w_o", [D, D])
    ropeC_d = din("ropeC", [96, NT])
    ropeS_d = din("ropeS", [96, NT])
    ropeRT_d = din("ropeRT", [96, 96])
    ml_win = din("ml_w_in", [D, 3088])
    ml_wout = din("ml_w_out", [D, D])
    ml_gain = din("ml_gain", [128, D])
    ml_selc = din("ml_selc", [16, 2, 16, 64])
    ml_selh = din("ml_selh", [16, 2, 4])
    ml_maskc = din("ml_maskc", [64, 2, 4, 64])
    hdir = nc.dram_tensor("hdir_scratch", [2, NT, D], BF16).ap()
    moec_d = din("moec", [128, 128 + 64 + 1 + 36])
    NSLOT = 12800
    xslots = nc.dram_tensor("xslots_scratch", [NSLOT, D], BF16).ap()
    yslots = nc.dram_tensor("yslots_scratch", [NSLOT, D], BF16).ap()
    moe_wr = din("moe_wr", [DEPTH, D, 72])
    moe_br = din("moe_br", [DEPTH, 128, 72])
    moe_wgu = din("moe_w_gate_up", [DEPTH, 64, D, 512])
    moe_wd = din("moe_w_down", [DEPTH, 64, 256, D])
    out_d = nc.dram_tensor("out", [NLAT, D], F32, kind="ExternalOutput").ap()
    octx_d = nc.dram_tensor("octx", [NCTX, D], F32, kind="ExternalOutput").ap()

    with ExitStack() as st:
        P = Prog(nc, st)
        xT = P.sb("xT", [128, 8, NT], F32)
        hT = P.sb("hT", [128, 8, NT], BF16)
        mT = P.sb("mT", [128, 8, NT], BF16)
        pvt = P.sb("pvt", [128, npv], F32)
        ident = P.sb("ident_sb", [128, 128], F32)
        onesm = P.sb("onesm", [128, 128], F32)
        cst = P.sb("cst", [128, 4], F32)
        cct = P.sb("cct", [128, 16], F32)
        modt = P.sb("modt", [128, 48, 2], F32)
        mA = P.sb("mA", [128, 2, 8, 2], F32)
        tmp = [P.sb("tmp%d" % i, [128, 512], F32) for i in range(6)]
        rstd = P.sb("rstd", [128, 512], F32)
        SCRW = 11400
        scr = P.sb("scr", [128, SCRW], F32)
        mTflat = mT[:].rearrange("p c t -> p (c t)")
        hTflat = hT[:].rearrange("p c t -> p (c t)")
        PSB = [P.ps("psb%d" % i, [128, 512], F32) for i in range(8)]

        class Arena:
            def __init__(self, kind):
                self.kind = kind
                self.off = 0

            def reset(self):
                self.off = 0

            def alloc(self, shape, dt):
                n = 1
                for d_ in shape[1:]:
                    n *= d_
                nf32 = n if dt in (F32, I32) else (n + 1) // 2
                o = self.off
                self.off += nf32
                if self.kind == "A":
                    assert self.off <= SCRW, ("arena A overflow", self.off)
                    v = scr[0:shape[0], o:o + nf32]
                    if dt != F32:
                        v = v.bitcast(dt)[:, 0:n]
                else:
                    assert self.off * 2 <= 8 * NT, ("arena B/C overflow", self.off)
                    flat = mTflat if self.kind == "B" else hTflat
                    v = flat[0:shape[0], 2 * o:2 * o + 2 * nf32]
                    if dt == F32:
                        v = v.bitcast(F32)
                    else:
                        v = v[:, 0:n]
                if len(shape) == 3:
                    v = v.rearrange("p (a b) -> p a b", a=shape[1])
                elif len(shape) == 4:
                    v = v.rearrange("p (a b c) -> p a b c", a=shape[1], b=shape[2])
                return v

        arA, arB, arC = Arena("A"), Arena("B"), Arena("C")
        _regs = {}

        def getreg(e, v):
            if v not in _regs:
                _regs[v] = e.to_reg(v)
            return _regs[v]

        def phase():
            P.barrier()
            arA.reset()
            arB.reset()
            arC.reset()

        def pvc(name, k=0, n=1):
            o = pvoff[name] + k
            return pvt[:, o:o + n]

        P.op("sp", lambda e: e.dma_start(out=pvt[:], in_=pv_d), writes=["pvt"], dma=True)
        P.op("sp", lambda e: e.dma_start(out=ident[:], in_=ident_d), writes=["ident"], dma=True)
        P.op("sp", lambda e: e.dma_start(out=cct[:], in_=cc_d), writes=["cct"], dma=True)
        P.op("pool", lambda e: e.memset(onesm[:], 1.0 / 1024.0), writes=["onesm"])
        P.op("pool", lambda e: e.memset(cst[:, 0:1], 1e-6), writes=["cst0"])
        P.op("pool", lambda e: e.memset(cst[:, 1:2], 1.0), writes=["cst1"])
        P.op("act", lambda e: e.activation(out=cct[:], in_=cct[:], func=AF.Silu), reads=["cct"], writes=["cct"])

        stage = arA.alloc([128, 2, 1024], F32)
        for ti in range(18):
            src = ctx_d[ti * 128:(ti + 1) * 128, :] if ti < 2 else x_d[(ti - 2) * 128:(ti - 1) * 128, :]
            sbuf = ti % 2
            P.op("sp", lambda e, src=src, sbuf=sbuf: e.dma_start(out=stage[:, sbuf, :], in_=src), writes=[("stage", sbuf)], dma=True)
            jt = [j for j, (a, b) in enumerate(TT) if a <= ti * 128 < b][0]
            for half in range(2):
                pb = PSB[half]

                def trfn(e, pb=pb, half=half, sbuf=sbuf):
                    ins = None
                    for q in range(4):
                        c = half * 4 + q
                        ins = e.transpose(pb[:, q * 128:(q + 1) * 128], stage[:, sbuf, c * 128:(c + 1) * 128], ident[:])
                    return ins
                P.op("pe", trfn, reads=[("stage", sbuf), "ident"], writes=[("psb", half)])
                P.op("dve" if half == 0 else "act",
                     lambda e, pb=pb, half=half, ti=ti: (e.tensor_copy if half == 0 else e.copy)(
                         out=xT[:, half * 4:half * 4 + 4, ti * 128:(ti + 1) * 128], in_=pb[:].rearrange("p (q t) -> p q t", q=4)),
                     reads=[("psb", half)], writes=[("x", c, jt) for c in range(half * 4, half * 4 + 4)])

        def compute_mod(i):
            phase()
            adaw = arA.alloc([128, 5, 8, 256], F32)
            pm = PSB[7]
            for piece in range(24):
                bsel = piece % 5
                P.op("sp", lambda e, piece=piece, bsel=bsel: e.dma_start(
                    out=adaw[:, bsel, :, :], in_=ada_w[i, :, piece * 256:(piece + 1) * 256].rearrange("(k p) n -> p k n", p=128)),
                    writes=[("adaw", bsel)], dma=True)

                def mmfn(e, bsel=bsel, piece=piece):
                    ins = None
                    for sub in range(2):
                        oc = piece * 2 + sub
                        for k in range(8):
                            ins = e.matmul(pm[:, oc * 2:oc * 2 + 2], lhsT=adaw[:, bsel, k, sub * 128:(sub + 1) * 128],
                                           rhs=cct[:, k * 2:k * 2 + 2], start=(k == 0), stop=(k == 7))
                    return ins
                P.op("pe", mmfn, reads=[("adaw", bsel), "cct"], writes=[("psb", 7)])
            ab = pvt[:, pvoff["adab%d" % i]:pvoff["adab%d" % i] + 96]
            P.op("dve", lambda e: e.tensor_tensor(out=modt[:].rearrange("p a b -> p (a b)"), in0=pm[:, 0:96], in1=ab, op=ALU.add),
                 reads=[("psb", 7), "pvt"], writes=["modt"])
            for w, (nm, m) in enumerate((("nmix%d" % i, 1), ("nffn%d" % i, 4))):
                for j2 in range(2):
                    P.op("dve", lambda e, w=w, nm=nm, m=m, j2=j2: e.scalar_tensor_tensor(
                        out=mA[:, w, :, j2], in0=modt[:, m * 8:m * 8 + 8, j2], scalar=1.0, in1=pvc(nm, 0, 8),
                        op0=ALU.add, op1=ALU.mult),
                        reads=["modt", "pvt"], writes=["mA"])

        def modcol(m, c, j):
            jj = 1 if j == 0 else 0
            return modt[:, m * 8 + c, jj:jj + 1]

        def norm_mod(w, tiles, extra=None):
            mshift = 0 if w == 0 else 3
            for j in tiles:
                a, b = TT[j]
                n = b - a
                jj = 1 if j == 0 else 0
                pst = PSB[6]
                for c in range(8):
                    tb = tmp[c % 2]
                    P.op("act", lambda e, tb=tb, c=c, a=a, b=b, n=n: e.activation(out=tb[:, :n], in_=xT[:, c, a:b], func=AF.Square),
                         reads=[("x", c, j)], writes=[("tmp", c % 2)])
                    P.op("pe", lambda e, tb=tb, c=c, n=n: e.matmul(pst[:, :n], lhsT=onesm[:], rhs=tb[:, :n], start=(c == 0), stop=(c == 7)),
                         reads=[("tmp", c % 2), "onesm"], writes=[("psb", 6)])
                P.op("act", lambda e, n=n: e.activation(out=rstd[:, :n], in_=pst[:, :n], func=AF.Ln, bias=cst[:, 0:1], scale=1.0),
                     reads=[("psb", 6), "cst0"], writes=["rstd"])
                P.op("act", lambda e, n=n: e.activation(out=rstd[:, :n], in_=rstd[:, :n], func=AF.Exp, scale=-0.5), reads=["rstd"], writes=["rstd"])
                for c in range(8):
                    tb = tmp[2 + c % 2]
                    P.op("dve", lambda e, tb=tb, c=c, a=a, b=b, n=n: e.tensor_tensor(out=tb[:, :n], in0=xT[:, c, a:b], in1=rstd[:, :n], op=ALU.mult),
                         reads=[("x", c, j), "rstd"], writes=[("tmp", 2 + c % 2)])
                    P.op("act", lambda e, tb=tb, c=c, a=a, b=b, n=n, jj=jj: e.activation(
                        out=hT[:, c, a:b], in_=tb[:, :n], func=AF.Identity,
                        bias=modt[:, mshift * 8 + c, jj:jj + 1], scale=mA[:, w, c, jj:jj + 1]),
                        reads=[("tmp", 2 + c % 2), "modt", "mA"], writes=[("h", c, j)])
                    if extra is not None:
                        extra(j, c, tb)

        def rglru(jl, tiles_all):
            phase()
            k = "rg%d" % jl
            win = arA.alloc([128, 2, 8, 256], BF16)
            wout = arA.alloc([128, 2, 8, 128], BF16)
            gw = arA.alloc([128, 4, 128], BF16)
            gwf = arA.alloc([128, 4, 128], F32)
            clam = arA.alloc([128, 2, 2, 8], F32)
            uh = arA.alloc([128, NT], F32)
            ucv = arA.alloc([128, NT], F32)
            ub = arA.alloc([128, NT], BF16)
            hb = arA.alloc([128, 2, 512], F32)
            for dr in range(2):
                lam = pvc("rglam%d_%d" % (jl, dr), 0, 8)
                P.op("act", lambda e, dr=dr, lam=lam: e.activation(out=clam[:, dr, 0, :], in_=lam, func=AF.Exp, scale=-1.0),
                     reads=["pvt"], writes=[(k, "clam")])
                P.op("act", lambda e, dr=dr: e.activation(out=clam[:, dr, 0, :], in_=clam[:, dr, 0, :], func=AF.Ln, bias=cst[:, 1:2], scale=1.0),
                     reads=[(k, "clam"), "cst1"], writes=[(k, "clam")])
                P.op("dve", lambda e, dr=dr: e.tensor_scalar(out=clam[:, dr, 1, :], in0=clam[:, dr, 0, :], scalar1=-16.0, scalar2=None, op0=ALU.mult),
                     reads=[(k, "clam")], writes=[(k, "clam")])
                P.op("dve", lambda e, dr=dr: e.tensor_scalar(out=clam[:, dr, 0, :], in0=clam[:, dr, 0, :], scalar1=-8.0, scalar2=None, op0=ALU.mult),
                     reads=[(k, "clam")], writes=[(k, "clam")])
            for cc in range(8):
                wb_ = cc % 2
                for part in range(2):
                    P.op("pool", lambda e, part=part, wb_=wb_, cc=cc: e.dma_start(
                        out=win[:, wb_, :, part * 128:(part + 1) * 128],
                        in_=rg_w_in[jl, :, part * 1024 + cc * 128: part * 1024 + (cc + 1) * 128].rearrange("(k p) n -> p k n", p=128)),
                        writes=[(k, "win", wb_, part)], dma=True)
                subk = [(k, "gwf", q) for q in range(8)]
                P.op("pool", lambda e: e.memset(gwf[:, :, :], 0.0), writes=subk)
                for dr in range(2):
                    for g in range(2):
                        for blk in range(2):
                            P.op("sp", lambda e, dr=dr, g=g, blk=blk, cc=cc: e.dma_start(
                                out=gwf[blk * 64:(blk + 1) * 64, dr * 2 + g, blk * 64:(blk + 1) * 64],
                                in_=rg_gate_w[jl, dr, g, cc * 2 + blk, :, :]),
                                writes=[(k, "gwf", dr * 4 + g * 2 + blk)], dma=True)
                P.op("pool", lambda e: e.tensor_copy(out=gw[:, :, :], in_=gwf[:, :, :]), reads=subk, writes=[(k, "gw")])
                for j in tiles_all:
                    a, b = TT[j]
                    n = b - a
                    pg, pu = PSB[0 + j % 2], PSB[2 + j % 2]
                    for part, pp, pk in ((0, pg, ("psb", 0 + j % 2)), (1, pu, ("psb", 2 + j % 2))):
                        def mmfn(e, part=part, pp=pp, a=a, b=b, n=n, wb_=wb_):
                            ins = None
                            for kk in range(8):
                                ins = e.matmul(pp[:, :n], lhsT=win[:, wb_, kk, part * 128:(part + 1) * 128], rhs=hT[:, kk, a:b],
                                               start=(kk == 0), stop=(kk == 7))
                            return ins
                        P.op("pe", mmfn, reads=[(k, "win", wb_, part)] + [("h", kk, j) for kk in range(8)], writes=[pk])
                    t0, t1 = tmp[0], tmp[1]
                    pgk = ("psb", 0 + j % 2)
                    P.op("act", lambda e, pg=pg, n=n: e.activation(out=t0[:, :n], in_=pg[:, :n], func=AF.Square),
                         reads=[pgk], writes=[("tmp", 0)])
                    P.op("dve", lambda e, n=n: e.tensor_scalar(out=t0[:, :n], in0=t0[:, :n], scalar1=0.044715, scalar2=1.0, op0=ALU.mult, op1=ALU.add),
                         reads=[("tmp", 0)], writes=[("tmp", 0)])
                    P.op("dve", lambda e, pg=pg, n=n: e.tensor_tensor(out=t0[:, :n], in0=t0[:, :n], in1=pg[:, :n], op=ALU.mult),
                         reads=[("tmp", 0), pgk], writes=[("tmp", 0)])
                    P.op("act", lambda e, n=n: e.activation(out=t1[:, :n], in_=t0[:, :n], func=AF.Sigmoid, scale=1.5957691216),
                         reads=[("tmp", 0)], writes=[("tmp", 1)])
                    P.op("dve", lambda e, pg=pg, n=n, a=a, b=b, cc=cc: e.tensor_tensor(out=mT[:, cc, a:b], in0=t1[:, :n], in1=pg[:, :n], op=ALU.mult),
                         reads=[("tmp", 1), pgk], writes=[("m", cc, j)])
                    P.op("act", lambda e, pu=pu, n=n, a=a, b=b: e.copy(out=uh[:, a:b], in_=pu[:, :n]),
                         reads=[("psb", 2 + j % 2)], writes=[(k, "uh")])
                P.op("act", lambda e, cc=cc: e.activation(out=ucv[:, :], in_=uh[:, :], func=AF.Identity,
                                                           bias=pvc("rgcb%d" % jl, cc), scale=pvc("rgcw%d_2" % jl, cc)),
                     reads=[(k, "uh"), "pvt"], writes=[(k, "ucv")])
                for (s0, s1) in ((0, NCTX), (NCTX, NT)):
                    for tap, off in ((0, -2), (1, -1), (3, 1)):
                        lo = max(s0, s0 - off)
                        hi = min(s1, s1 - off)
                        P.op("dve", lambda e, lo=lo, hi=hi, off=off, tap=tap, cc=cc: e.scalar_tensor_tensor(
                            out=ucv[:, lo:hi], in0=uh[:, lo + off:hi + off], scalar=pvc("rgcw%d_%d" % (jl, tap), cc),
                            in1=ucv[:, lo:hi], op0=ALU.mult, op1=ALU.add),
                            reads=[(k, "uh"), (k, "ucv"), "pvt"], writes=[(k, "ucv")])
                P.op("pool", lambda e: e.tensor_copy(out=ub[:, :], in_=ucv[:, :]), reads=[(k, "ucv")], writes=[(k, "ub")])
                for dr in range(2):
                    order = tiles_all if dr == 0 else [tiles_all[0]] + list(reversed(tiles_all[1:]))
                    prev = None
                    groups = [order[g:g + 2] for g in range(0, len(order), 2)]
                    oi = 0
                    for grp in groups:
                        info = []
                        for gi, j in enumerate(grp):
                            a, b = TT[j]
                            n = b - a
                            tr, ta2, ti_ = tmp[3 * gi + 0], tmp[3 * gi + 1], tmp[3 * gi + 2]
                            kr, k2, ki_ = ("tmp", 3 * gi + 0), ("tmp", 3 * gi + 1), ("tmp", 3 * gi + 2)
                            pr = PSB[4 + gi]
                            pi = PSB[6 + gi]
                            prk = ("psb", 4 + gi)
                            pik = ("psb", 6 + gi)
                            P.op("pe", lambda e, pr=pr, dr=dr, a=a, b=b, n=n: e.matmul(pr[:, :n], lhsT=gw[:, dr * 2 + 0, :], rhs=ub[:, a:b], start=True, stop=True),
                                 reads=[(k, "gw"), (k, "ub")], writes=[prk])
                            P.op("pe", lambda e, pi=pi, dr=dr, a=a, b=b, n=n: e.matmul(pi[:, :n], lhsT=gw[:, dr * 2 + 1, :], rhs=ub[:, a:b], start=True, stop=True),
                                 reads=[(k, "gw"), (k, "ub")], writes=[pik])
                            P.op("act", lambda e, pr=pr, n=n, dr=dr, cc=cc, tr=tr: e.activation(out=tr[:, :n], in_=pr[:, :n], func=AF.Sigmoid, bias=pvc("rggb%d_%d_0" % (jl, dr), cc), scale=1.0),
                                 reads=[prk, "pvt"], writes=[kr])
                            P.op("act", lambda e, pi=pi, n=n, dr=dr, cc=cc, ti_=ti_: e.activation(out=ti_[:, :n], in_=pi[:, :n], func=AF.Sigmoid, bias=pvc("rggb%d_%d_1" % (jl, dr), cc), scale=1.0),
                                 reads=[pik, "pvt"], writes=[ki_])
                            info.append((j, a, b, n, tr, ta2, ti_, kr, k2, ki_))
                        for (j, a, b, n, tr, ta2, ti_, kr, k2, ki_) in info:
                            P.op("act", lambda e, n=n, dr=dr, cc=cc, tr=tr: e.activation(out=tr[:, :n], in_=tr[:, :n], func=AF.Exp, scale=clam[:, dr, 0, cc:cc + 1]),
                                 reads=[kr, (k, "clam")], writes=[kr])
                            P.op("act", lambda e, n=n, tr=tr, ta2=ta2: e.activation(out=ta2[:, :n], in_=tr[:, :n], func=AF.Square), reads=[kr], writes=[k2])
                            P.op("act", lambda e, n=n, ta2=ta2: e.activation(out=ta2[:, :n], in_=ta2[:, :n], func=AF.Ln, bias=cst[:, 1:2], scale=-1.0), reads=[k2, "cst1"], writes=[k2])
                            P.op("act", lambda e, n=n, ta2=ta2: e.activation(out=ta2[:, :n], in_=ta2[:, :n], func=AF.Exp, scale=0.5), reads=[k2], writes=[k2])
                        for (j, a, b, n, tr, ta2, ti_, kr, k2, ki_) in info:
                            P.op("dve", lambda e, n=n, a=a, b=b, ti_=ti_: e.tensor_tensor(out=ti_[:, :n], in0=ti_[:, :n], in1=ucv[:, a:b], op=ALU.mult),
                                 reads=[ki_, (k, "ucv")], writes=[ki_])
                            P.op("dve", lambda e, n=n, ti_=ti_, ta2=ta2: e.tensor_tensor(out=ti_[:, :n], in0=ti_[:, :n], in1=ta2[:, :n], op=ALU.mult),
                                 reads=[ki_, k2], writes=[ki_])
                            if dr == 0:
                                init = 0.0 if prev is None else uh[:, a - 1:a]
                                P.op("dve", lambda e, n=n, a=a, b=b, init=init, tr=tr, ti_=ti_: e.tensor_tensor_scan(out=uh[:, a:b], data0=tr[:, :n], data1=ti_[:, :n], initial=init, op0=ALU.mult, op1=ALU.add),
                                     reads=[kr, ki_, (k, "uh"), (k, "ucv")], writes=[(k, "uh")])
                            else:
                                hbb = oi % 2
                                init = 0.0 if prev is None else hb[:, 1 - hbb, 0:1]
                                P.op("dve", lambda e, n=n, hbb=hbb, init=init, tr=tr, ti_=ti_: e.tensor_tensor_scan(out=hb[:, hbb, 0:n][:, ::-1], data0=tr[:, 0:n][:, ::-1], data1=ti_[:, 0:n][:, ::-1], initial=init, op0=ALU.mult, op1=ALU.add),
                                     reads=[kr, ki_, (k, "hb", 1 - hbb)], writes=[(k, "hb", hbb)])
                                P.op("dve", lambda e, n=n, a=a, b=b, hbb=hbb, tr=tr: e.tensor_tensor(out=tr[:, :n], in0=hb[:, hbb, 0:n], in1=uh[:, a:b], op=ALU.add),
                                     reads=[(k, "hb", hbb), (k, "uh")], writes=[kr])
                                P.op("dve", lambda e, n=n, a=a, b=b, cc=cc, tr=tr: e.tensor_tensor(out=mT[:, cc, a:b], in0=tr[:, :n], in1=mT[:, cc, a:b], op=ALU.mult),
                                     reads=[kr, ("m", cc, j)], writes=[("m", cc, j)])
                            prev = j
                            oi += 1
            for oc in range(8):
                ob = oc % 2
                P.op("pool", lambda e, oc=oc, ob=ob: e.dma_start(out=wout[:, ob, :, :], in_=rg_w_out[jl, :, oc * 128:(oc + 1) * 128].rearrange("(k p) n -> p k n", p=128)),
                     writes=[(k, "wout", ob)], dma=True)
                for j in tiles_all:
                    a, b = TT[j]
                    n = b - a
                    py = PSB[j % 4]

                    def mmfn(e, py=py, ob=ob, a=a, b=b, n=n):
                        ins = None
                        for cc in range(8):
                            ins = e.matmul(py[:, :n], lhsT=wout[:, ob, cc, :], rhs=mT[:, cc, a:b], start=(cc == 0), stop=(cc == 7))
                        return ins
                    P.op("pe", mmfn, reads=[(k, "wout", ob)] + [("m", cc, j) for cc in range(8)], writes=[("psb", j % 4)])
                    P.op("dve", lambda e, py=py, oc=oc, a=a, b=b, n=n, j=j: e.scalar_tensor_tensor(
                        out=xT[:, oc, a:b], in0=py[:, :n], scalar=modcol(2, oc, j), in1=xT[:, oc, a:b], op0=ALU.mult, op1=ALU.add),
                        reads=[("psb", j % 4), "modt", ("x", oc, j)], writes=[("x", oc, j)])

        def mla(tiles_all):
            phase()
            k = "mla"
            SC = 96 ** -0.5
            cqn = arA.alloc([128, 2, NT], BF16)
            ckvn = arA.alloc([128, NT], BF16)
            krope = arA.alloc([96, NT], F32)
            wuq = arA.alloc([128, 2, 1536], BF16)
            wukv = arA.alloc([128, 2048], BF16)
            ones96 = arA.alloc([96, 96], BF16)
            RTf = arA.alloc([96, 96], F32)
            RT = arA.alloc([96, 96], BF16)
            ones1r = arA.alloc([65, 64], F32)
            rr = arA.alloc([65, 512], F32)
            onesb = arA.alloc([128, 64], BF16)
            wdn = arB.alloc([128, 8, 416], BF16)
            wo = arB.alloc([64, 2, 1024], BF16)
            tC = arB.alloc([96, NT], F32)
            tS = arB.alloc([96, NT], F32)
            P.op("pool", lambda e: e.dma_start(out=wdn, in_=mla_wdn.rearrange("(k p) n -> p k n", p=128)), writes=[(k, "wdn")], dma=True)
            P.op("pool", lambda e: e.dma_start(out=wuq, in_=mla_wuq.rearrange("(k p) n -> p k n", p=128)), writes=[(k, "wuq")], dma=True)
            P.op("pool", lambda e: e.dma_start(out=wukv, in_=mla_wukv), writes=[(k, "wukv")], dma=True)
            P.op("sp", lambda e: e.dma_start(out=tC, in_=ropeC_d), writes=[(k, "tC")], dma=True)
            P.op("sp", lambda e: e.dma_start(out=tS, in_=ropeS_d), writes=[(k, "tS")], dma=True)
            P.op("sp", lambda e: e.dma_start(out=RTf, in_=ropeRT_d), writes=[(k, "RTf")], dma=True)
            P.op("pool", lambda e: e.tensor_copy(out=RT, in_=RTf), reads=[(k, "RTf")], writes=[(k, "RT")])
            P.op("pool", lambda e: e.memset(ones1r, 1.0), writes=[(k, "ones1r")])
            P.op("pool", lambda e: e.memset(ones96, 1.0 / 96.0), writes=[(k, "ones96")])
            P.op("pool", lambda e: e.memset(onesb, 1.0), writes=[(k, "onesb")])
            for j in tiles_all:
                a, b = TT[j]
                n = b - a
                specs = ((0, 0, 128), (1, 128, 128), (2, 256, 128), (3, 320, 96))
                for bi, c0, m in specs:
                    def mmfn(e, bi=bi, c0=c0, m=m, a=a, b=b, n=n):
                        ins = None
                        for kk in range(8):
                            ins = e.matmul(PSB[bi][0:m, :n], lhsT=wdn[:, kk, c0:c0 + m], rhs=hT[:, kk, a:b], start=(kk == 0), stop=(kk == 7))
                        return ins
                    P.op("pe", mmfn, reads=[(k, "wdn")] + [("h", kk, j) for kk in range(8)], writes=[("psb", bi)])
                P.op("act", lambda e, a=a, b=b, n=n: e.copy(out=krope[64:96, a:b], in_=PSB[3][64:96, :n]), reads=[("psb", 3)], writes=[(k, "krope", j)])
                for c in range(2):
                    P.op("act", lambda e, c=c, n=n: e.activation(out=tmp[c][:, :n], in_=PSB[c][:, :n], func=AF.Square), reads=[("psb", c)], writes=[("tmp", c)])
                    P.op("pe", lambda e, c=c, n=n: e.matmul(PSB[4][:, :n], lhsT=onesm[:], rhs=tmp[c][:, :n], start=(c == 0), stop=(c == 1)),
                         reads=[("tmp", c), "onesm"], writes=[("psb", 4)])
                P.op("act", lambda e, n=n: e.activation(out=rstd[:, :n], in_=PSB[4][:, :n], func=AF.Ln, bias=cst[:, 0:1], scale=4.0), reads=[("psb", 4), "cst0"], writes=["rstd"])
                P.op("act", lambda e, n=n: e.activation(out=rstd[:, :n], in_=rstd[:, :n], func=AF.Exp, scale=-0.5), reads=["rstd"], writes=["rstd"])
                for c in range(2):
                    P.op("dve", lambda e, c=c, n=n: e.tensor_tensor(out=tmp[2 + c][:, :n], in0=PSB[c][:, :n], in1=rstd[:, :n], op=ALU.mult), reads=[("psb", c), "rstd"], writes=[("tmp", 2 + c)])
                    P.op("act", lambda e, c=c, a=a, b=b, n=n: e.activation(out=cqn[:, c, a:b], in_=tmp[2 + c][:, :n], func=AF.Identity, scale=pvc("mla_qn", c)), reads=[("tmp", 2 + c), "pvt"], writes=[(k, "cqn", j)])
                P.op("act", lambda e, n=n: e.activation(out=tmp[4][:, :n], in_=PSB[2][:, :n], func=AF.Square), reads=[("psb", 2)], writes=[("tmp", 4)])
                P.op("pe", lambda e, n=n: e.matmul(PSB[5][:, :n], lhsT=onesm[:], rhs=tmp[4][:, :n], start=True, stop=True), reads=[("tmp", 4), "onesm"], writes=[("psb", 5)])
                P.op("act", lambda e, n=n: e.activation(out=tmp[5][:, :n], in_=PSB[5][:, :n], func=AF.Ln, bias=cst[:, 0:1], scale=8.0), reads=[("psb", 5), "cst0"], writes=[("tmp", 5)])
                P.op("act", lambda e, n=n: e.activation(out=tmp[5][:, :n], in_=tmp[5][:, :n], func=AF.Exp, scale=-0.5), reads=[("tmp", 5)], writes=[("tmp", 5)])
                P.op("dve", lambda e, n=n: e.tensor_tensor(out=tmp[4][:, :n], in0=PSB[2][:, :n], in1=tmp[5][:, :n], op=ALU.mult), reads=[("psb", 2), ("tmp", 5)], writes=[("tmp", 4)])
                P.op("act", lambda e, a=a, b=b, n=n: e.activation(out=ckvn[:, a:b], in_=tmp[4][:, :n], func=AF.Identity, scale=pvc("mla_kvn", 0)), reads=[("tmp", 4), "pvt"], writes=[(k, "ckvn", j)])
            P.barrier()
            qTs = [arC.alloc([96, NT], BF16) for _ in range(2)]
            kTs = [arC.alloc([96, NT], BF16) for _ in range(2)]
            vhs = [arC.alloc([128, 18, 65], BF16) for _ in range(2)]
            for vv in range(2):
                P.op("pool", lambda e, vv=vv: e.memset(vhs[vv], 1.0), writes=[(k, "vh", vv)])
            PT = arC.alloc([128, 2, 512], BF16)
            xf = arC.alloc([96, 512], F32)
            xn = arC.alloc([96, 512], BF16)
            rs = arC.alloc([96, 512], F32)
            t1 = arC.alloc([96, 512], F32)
            t1b = arC.alloc([96, 512], BF16)
            t2 = arC.alloc([96, 512], F32)
            rden = tmp[0][0:64, :]
            oT = tmp[1][0:64, :].bitcast(BF16)[:, 0:512]

            def proj_gen(h):
                hp = h % 2
                qT, kT, vh = qTs[hp], kTs[hp], vhs[hp]
                P.op("pool", lambda e: e.dma_start(out=wo[:, hp, :], in_=mla_wo[h * 64:(h + 1) * 64, :]), writes=[(k, "wo", hp)], dma=True)
                for j in tiles_all:
                    a, b = TT[j]
                    n = b - a

                    def mmq(e, a=a, b=b, n=n):
                        ins = None
                        for kk in range(2):
                            ins = e.matmul(PSB[0][0:96, :n], lhsT=wuq[:, kk, h * 96:(h + 1) * 96], rhs=cqn[:, kk, a:b], start=(kk == 0), stop=(kk == 1))
                        return ins
                    P.op("pe", mmq, reads=[(k, "wuq"), (k, "cqn", j)], writes=[("psb", 0)])
                    P.op("pe", lambda e, a=a, b=b, n=n: e.matmul(PSB[1][0:64, :n], lhsT=wukv[:, h * 128:h * 128 + 64], rhs=ckvn[:, a:b], start=True, stop=True),
                         reads=[(k, "wukv"), (k, "ckvn", j)], writes=[("psb", 1)])
                    for which in range(2):
                        gname = "mla_qkn%d" % which
                        if which == 0:
                            P.op("act", lambda e, n=n: e.copy(out=xf[:, :n], in_=PSB[0][0:96, :n]), reads=[("psb", 0)], writes=[(k, "xf")])
                        else:
                            P.op("act", lambda e, n=n: e.copy(out=xf[0:64, :n], in_=PSB[1][0:64, :n]), reads=[("psb", 1)], writes=[(k, "xf")])
                            P.op("pool", lambda e, a=a, b=b, n=n: e.tensor_copy(out=xf[64:96, :n], in_=krope[64:96, a:b]), reads=[(k, "krope", j), (k, "xf")], writes=[(k, "xf")])
                        P.op("act", lambda e, n=n: e.activation(out=t1b[:, :n], in_=xf[:, :n], func=AF.Square), reads=[(k, "xf")], writes=[(k, "t1b")])
                        P.op("pe", lambda e, n=n: e.matmul(PSB[2][0:96, :n], lhsT=ones96, rhs=t1b[:, :n], start=True, stop=True), reads=[(k, "t1b"), (k, "ones96")], writes=[("psb", 2)])
                        yield
                        P.op("act", lambda e, n=n: e.activation(out=rs[:, :n], in_=PSB[2][0:96, :n], func=AF.Ln, bias=cst[0:96, 0:1], scale=1.0), reads=[("psb", 2), "cst0"], writes=[(k, "rs")])
                        P.op("act", lambda e, n=n: e.activation(out=rs[:, :n], in_=rs[:, :n], func=AF.Exp, scale=-0.5), reads=[(k, "rs")], writes=[(k, "rs")])
                        P.op("dve", lambda e, n=n, gname=gname: e.scalar_tensor_tensor(out=xn[:, :n], in0=xf[:, :n], scalar=pvt[0:96, pvoff[gname]:pvoff[gname] + 1], in1=rs[:, :n], op0=ALU.mult, op1=ALU.mult),
                             reads=[(k, "xf"), (k, "rs"), "pvt"], writes=[(k, "xn")])
                        P.op("pe", lambda e, n=n: e.matmul(PSB[3][0:96, :n], lhsT=RT, rhs=xn[:, :n], start=True, stop=True), reads=[(k, "xn"), (k, "RT")], writes=[("psb", 3)])
                        yield
                        P.op("pool", lambda e, a=a, b=b, n=n: e.tensor_tensor(out=t1[:, :n], in0=xn[:, :n], in1=tC[:, a:b], op=ALU.mult), reads=[(k, "xn"), (k, "tC")], writes=[(k, "t1")])
                        P.op("dve", lambda e, a=a, b=b, n=n: e.tensor_tensor(out=t2[:, :n], in0=PSB[3][0:96, :n], in1=tS[:, a:b], op=ALU.mult), reads=[("psb", 3), (k, "tS")], writes=[(k, "t2")])
                        dst = qT if which == 0 else kT
                        P.op("pool", lambda e, a=a, b=b, n=n, dst=dst: e.tensor_tensor(out=dst[:, a:b], in0=t1[:, :n], in1=t2[:, :n], op=ALU.add),
                             reads=[(k, "t1"), (k, "t2")], writes=[(k, "qk", hp, which, j)])
                        yield
                for g3 in range(3):
                    kts = list(range(g3 * 8, min(18, g3 * 8 + 8)))

                    def mmv(e, kts=kts):
                        ins = None
                        for qi, kt in enumerate(kts):
                            ins = e.matmul(PSB[3][:, qi * 64:(qi + 1) * 64], lhsT=ckvn[:, kt * 128:(kt + 1) * 128], rhs=wukv[:, h * 128 + 64:h * 128 + 128], start=True, stop=True)
                        return ins
                    P.op("pe", mmv, reads=[(k, "wukv")] + [(k, "ckvn", j) for j in tiles_all], writes=[("psb", 3)])
                    P.op("act", lambda e, kts=kts: e.copy(out=vh[:, kts[0]:kts[-1] + 1, 0:64], in_=PSB[3][:, 0:64 * len(kts)].rearrange("p (a b) -> p a b", b=64)),
                         reads=[("psb", 3)], writes=[(k, "vh", hp)])
                    yield

            def attn(h, gen):
                hp = h % 2
                qT, kT, vh = qTs[hp], kTs[hp], vhs[hp]

                def pump(cnt=1):
                    if gen is None:
                        return
                    for _ in range(cnt):
                        try:
                            next(gen)
                        except StopIteration:
                            return
                for j in tiles_all:
                    a, b = TT[j]
                    n = b - a
                    keyt = [0, 1] if j == 0 else list(range(18))

                    def emitS(ki, kt, a=a, b=b, n=n, j=j):
                        pb_ = ki % 2
                        jk = [jj for jj, (aa, bb) in enumerate(TT) if aa <= kt * 128 < bb][0]
                        P.op("pe", lambda e, kt=kt, pb_=pb_, n=n, a=a, b=b: e.matmul(PSB[4 + pb_][:, :n], lhsT=kT[:, kt * 128:(kt + 1) * 128], rhs=qT[:, a:b], start=True, stop=True),
                             reads=[(k, "qk", hp, 1, jk), (k, "qk", hp, 0, j)], writes=[("psb", 4 + pb_)])
                    emitS(0, keyt[0])
                    for ki, kt in enumerate(keyt):
                        pb_ = ki % 2
                        if ki + 1 < len(keyt):
                            emitS(ki + 1, keyt[ki + 1])
                        P.op("act", lambda e, n=n, pb_=pb_: e.activation(out=PT[:, pb_, :n], in_=PSB[4 + pb_][:, :n], func=AF.Exp, scale=SC), reads=[("psb", 4 + pb_)], writes=[(k, "PT", pb_)])
                        P.op("pe", lambda e, kt=kt, n=n, pb_=pb_, ki=ki, nk=len(keyt): e.matmul(PSB[6][0:65, :n], lhsT=vh[:, kt, :], rhs=PT[:, pb_, :n], start=(ki == 0), stop=(ki == nk - 1)),
                             reads=[(k, "vh", hp), (k, "PT", pb_)], writes=[("psb", 6)])
                        if ki % 2 == 1:
                            pump(1)
                    P.op("act", lambda e, n=n: e.activation(out=rr[64:65, :n], in_=PSB[6][64:65, :n], func=AF.Ln), reads=[("psb", 6)], writes=[(k, "rr")])
                    P.op("act", lambda e, n=n: e.activation(out=rr[64:65, :n], in_=rr[64:65, :n], func=AF.Exp, scale=-1.0), reads=[(k, "rr")], writes=[(k, "rr")])
                    P.op("pe", lambda e, n=n: e.matmul(PSB[7][0:64, :n], lhsT=ones1r[64:65, :], rhs=rr[64:65, :n], start=True, stop=True), reads=[(k, "rr"), (k, "ones1r")], writes=[("psb", 7)])
                    P.op("act", lambda e, n=n: e.copy(out=rden[:, :n], in_=PSB[7][0:64, :n]), reads=[("psb", 7)], writes=[("tmp", 0)])
                    P.op("dve", lambda e, n=n: e.tensor_tensor(out=oT[:, :n], in0=PSB[6][0:64, :n], in1=rden[:, :n], op=ALU.mult), reads=[("psb", 6), ("tmp", 0)], writes=[("tmp", 1)])
                    for oc in range(8):
                        pyb = 4 + oc % 2
                        P.op("pe", lambda e, oc=oc, n=n, pyb=pyb: e.matmul(PSB[pyb][:, :n], lhsT=wo[:, hp, oc * 128:(oc + 1) * 128], rhs=oT[:, :n], start=True, stop=True),
                             reads=[(k, "wo", hp), ("tmp", 1)], writes=[("psb", pyb)])
                        P.op("dve", lambda e, oc=oc, a=a, b=b, n=n, j=j, pyb=pyb: e.scalar_tensor_tensor(
                            out=xT[:, oc, a:b], in0=PSB[pyb][:, :n], scalar=modcol(2, oc, j), in1=xT[:, oc, a:b], op0=ALU.mult, op1=ALU.add),
                            reads=[("psb", pyb), "modt", ("x", oc, j)], writes=[("x", oc, j)])
                    pump(2)
                if gen is not None:
                    for _ in gen:
                        pass

            g0 = proj_gen(0)
            for _ in g0:
                pass
            for h in range(16):
                attn(h, proj_gen(h + 1) if h + 1 < 16 else None)

        def mlstm(tiles_all):
            phase()
            k = "ml"
            wqkv = arA.alloc([128, 8, 2048], BF16)
            selc = arA.alloc([16, 2, 16, 64], F32)
            maskc = arA.alloc([64, 2, 256], F32)
            selh = arA.alloc([16, 2, 4], F32)
            wg = arA.alloc([128, 8, 16], BF16)
            Cbf = arA.alloc([128, 4, 257], BF16)
            G = arB.alloc([16, NT], F32)
            Bd = [arB.alloc([16, NT], F32), arB.alloc([16, NT], F32)]
            Vp = arB.alloc([64, 4, 257], BF16)
            Cst = arB.alloc([128, 4, 257], F32)
            qkT = tmp[0][:, :].bitcast(BF16)[:, 0:512]
            Kw = tmp[1][0:64, :].bitcast(BF16)[:, 0:512].rearrange("p (a b) -> p a b", a=4)
            Em = tmp[2][0:64, 0:256]
            St = tmp[2][0:64, 256:384].bitcast(BF16)
            pis = tmp[3][0:64, 0:257]
            nd = tmp[4][0:64, 0:257]
            hout = tmp[5][0:64, :].bitcast(BF16)
            inter = rstd[0:64, 0:4]
            decay = rstd[:, 8:12]
            rdn = rstd[0:64, 16:17]
            P.op("pool", lambda e: e.dma_start(out=wqkv, in_=ml_win[:, 0:2048].rearrange("(k p) n -> p k n", p=128)), writes=[(k, "wqkv")], dma=True)
            P.op("pool", lambda e: e.dma_start(out=wg, in_=ml_win[:, 3072:3088].rearrange("(k p) n -> p k n", p=128)), writes=[(k, "wg")], dma=True)
            P.op("sp", lambda e: e.dma_start(out=selc, in_=ml_selc), writes=[(k, "selc")], dma=True)
            P.op("sp", lambda e: e.dma_start(out=selh, in_=ml_selh), writes=[(k, "selh")], dma=True)
            P.op("sp", lambda e: e.dma_start(out=maskc, in_=ml_maskc.rearrange("p a b c -> p a (b c)")), writes=[(k, "maskc")], dma=True)
            P.op("pool", lambda e: e.memset(Vp, 1.0), writes=[(k, "Vp")])
            for j in tiles_all:
                a, b = TT[j]
                n = b - a

                def mmg(e, a=a, b=b, n=n):
                    ins = None
                    for kk in range(8):
                        ins = e.matmul(PSB[0][0:16, :n], lhsT=wg[:, kk, :], rhs=hT[:, kk, a:b], start=(kk == 0), stop=(kk == 7))
                    return ins
                P.op("pe", mmg, reads=[(k, "wg")] + [("h", kk, j) for kk in range(8)], writes=[("psb", 0)])
                P.op("act", lambda e, a=a, b=b, n=n: e.activation(out=G[:, a:b], in_=PSB[0][0:16, :n], func=AF.Identity, bias=pvt[0:16, pvoff["ml_gb"]:pvoff["ml_gb"] + 1], scale=1.0),
                     reads=[("psb", 0), "pvt"], writes=[(k, "G")])
            lf = tmp[3][0:16, :]
            for j in tiles_all:
                a, b = TT[j]
                n = b - a
                P.op("act", lambda e, a=a, b=b, n=n: e.activation(out=lf[:, :n], in_=G[:, a:b], func=AF.Exp, scale=-1.0), reads=[(k, "G")], writes=[("tmp", 3)])
                P.op("act", lambda e, n=n: e.activation(out=lf[:, :n], in_=lf[:, :n], func=AF.Ln, bias=cst[0:16, 1:2], scale=1.0), reads=[("tmp", 3), "cst1"], writes=[("tmp", 3)])
                P.op("dve", lambda e, n=n: e.tensor_scalar(out=lf[:, :n], in0=lf[:, :n], scalar1=-1.0, scalar2=None, op0=ALU.mult), reads=[("tmp", 3)], writes=[("tmp", 3)])
                for ci in range(n // 64):
                    c0 = ci * 64
                    P.op("dve", lambda e, a=a, c0=c0: e.tensor_tensor_scan(out=Bd[0][:, a + c0:a + c0 + 64], data0=cst[0:16, 1:2].to_broadcast([16, 64]), data1=lf[:, c0:c0 + 64], initial=0.0, op0=ALU.mult, op1=ALU.add),
                         reads=[("tmp", 3), "cst1"], writes=[(k, "B0")])
                    P.op("dve", lambda e, a=a, c0=c0: e.tensor_tensor_scan(out=Bd[1][:, a + c0:a + c0 + 64][:, ::-1], data0=cst[0:16, 1:2].to_broadcast([16, 64]), data1=lf[:, c0:c0 + 64][:, ::-1], initial=0.0, op0=ALU.mult, op1=ALU.add),
                         reads=[("tmp", 3), "cst1"], writes=[(k, "B1")])
            SQ = 128 ** -0.5
            for d in range(2):
                P.op("pool", lambda e: e.memset(Cst, 0.0), reads=[(k, "C", h) for h in range(4)], writes=[(k, "C", h) for h in range(4)])
                P.op("pool", lambda e: e.memset(Cbf, 0.0), reads=[(k, "Cbf", h) for h in range(4)], writes=[(k, "Cbf", h) for h in range(4)])
                order = list(range(36)) if d == 0 else [3, 2, 1, 0] + list(range(35, 3, -1))
                for ch in order:
                    t0 = ch * 64
                    j = [jj for jj, (aa, bb) in enumerate(TT) if aa <= t0 < bb][0]
                    tend = t0 + 63 if d == 0 else t0
                    tl = 63 if d == 0 else 0
                    hr = [("h", kk, j) for kk in range(8)]

                    def mmk(e, t0=t0):
                        ins = None
                        for kk in range(8):
                            ins = e.matmul(PSB[0][0:64, 0:512], lhsT=hT[:, kk, t0:t0 + 64], rhs=wqkv[:, kk, 512:1024], start=(kk == 0), stop=(kk == 7))
                        return ins
                    P.op("pe", mmk, reads=hr + [(k, "wqkv")], writes=[("psb", 0)])
                    for half in range(2):
                        def mmv(e, t0=t0, half=half):
                            ins = None
                            for kk in range(8):
                                ins = e.matmul(PSB[1 + half][0:64, 0:512], lhsT=hT[:, kk, t0:t0 + 64], rhs=wqkv[:, kk, 1024 + half * 512:1536 + half * 512], start=(kk == 0), stop=(kk == 7))
                            return ins
                        P.op("pe", mmv, reads=hr + [(k, "wqkv")], writes=[("psb", 1 + half)])

                    def mmqk(e, t0=t0):
                        ins = None
                        for qk in range(2):
                            for h in range(4):
                                for kk in range(8):
                                    ins = e.matmul(PSB[3][:, qk * 256 + h * 64:qk * 256 + (h + 1) * 64], lhsT=wqkv[:, kk, qk * 512 + h * 128:qk * 512 + (h + 1) * 128],
                                                   rhs=hT[:, kk, t0:t0 + 64], start=(kk == 0), stop=(kk == 7))
                        return ins
                    P.op("pe", mmqk, reads=hr + [(k, "wqkv")], writes=[("psb", 3)])
                    P.op("act", lambda e: e.activation(out=qkT[:, 0:256], in_=PSB[3][:, 0:256], func=AF.Identity, scale=SQ), reads=[("psb", 3)], writes=[(k, "qT")])
                    P.op("act", lambda e: e.copy(out=qkT[:, 256:512], in_=PSB[3][:, 256:512]), reads=[("psb", 3)], writes=[(k, "kT")])
                    for half in range(2):
                        P.op("act", lambda e, half=half: e.copy(out=Vp[:, 2 * half:2 * half + 2, 0:256], in_=PSB[1 + half][0:64, :].rearrange("p (a b) -> p a b", a=2)),
                             reads=[("psb", 1 + half)], writes=[(k, "Vp")])
                    def mme(e, t0=t0, tend=tend, d=d):
                        ins = None
                        for h in range(4):
                            rb = (4 if d == 0 else 12) + h
                            ri = (0 if d == 0 else 8) + h
                            o = PSB[4][0:64, h * 64:(h + 1) * 64]
                            e.matmul(o, lhsT=selc[:, 0, rb, :], rhs=Bd[d][:, t0:t0 + 64], start=True, stop=False)
                            e.matmul(o, lhsT=Bd[d][:, t0:t0 + 64], rhs=selc[:, 1, rb, :], start=False, stop=False)
                            e.matmul(o, lhsT=G[:, t0:t0 + 64], rhs=selc[:, 0, ri, :], start=False, stop=True)
                        e.matmul(PSB[4][0:64, 256:260], lhsT=Bd[d][:, t0:t0 + 64], rhs=selh[:, d, :], start=True, stop=True)
                        ins = e.matmul(PSB[4][:, 260:264], lhsT=Bd[d][:, tend:tend + 1].to_broadcast([16, 128]), rhs=selh[:, d, :], start=True, stop=True)
                        return ins
                    P.op("pe", mme, reads=[(k, "selc"), (k, "selh"), (k, "B0"), (k, "B1"), (k, "G")], writes=[("psb", 4)])
                    P.op("dve", lambda e, d=d: e.tensor_tensor(out=Em, in0=PSB[4][0:64, 0:256], in1=maskc[:, d, :], op=ALU.add), reads=[("psb", 4), (k, "maskc")], writes=[(k, "Em")])
                    P.op("act", lambda e: e.activation(out=Em, in_=Em, func=AF.Exp), reads=[(k, "Em")], writes=[(k, "Em")])
                    P.op("act", lambda e: e.activation(out=inter, in_=PSB[4][0:64, 256:260], func=AF.Exp), reads=[("psb", 4)], writes=[(k, "inter")])
                    P.op("act", lambda e: e.activation(out=decay, in_=PSB[4][:, 260:264], func=AF.Exp), reads=[("psb", 4)], writes=[(k, "decay")])
                    def mms(e):
                        ins = None
                        for h in range(4):
                            ins = e.matmul(PSB[5][0:64, h * 64:(h + 1) * 64], lhsT=qkT[:, 256 + h * 64:256 + (h + 1) * 64], rhs=qkT[:, h * 64:(h + 1) * 64], start=True, stop=True)
                        return ins
                    P.op("pe", mms, reads=[(k, "qT"), (k, "kT")], writes=[("psb", 5)])
                    P.op("dve", lambda e: e.tensor_tensor(out=St, in0=PSB[5][0:64, 0:256], in1=Em, op=ALU.mult), reads=[("psb", 5), (k, "Em")], writes=[(k, "St")])
                    for h in range(4):
                        P.op("pe", lambda e, h=h: e.matmul(PSB[6][0:64, 0:257], lhsT=St[:, h * 64:(h + 1) * 64], rhs=Vp[:, h, :], start=True, stop=True),
                             reads=[(k, "St"), (k, "Vp")], writes=[("psb", 6)])
                        P.op("pe", lambda e, h=h: e.matmul(PSB[7][0:64, 0:257], lhsT=qkT[:, h * 64:(h + 1) * 64], rhs=Cbf[:, h, :], start=True, stop=True),
                             reads=[(k, "qT"), (k, "Cbf", h)], writes=[("psb", 7)])
                        P.op("act", lambda e: e.copy(out=pis, in_=PSB[6][0:64, 0:257]), reads=[("psb", 6)], writes=[(k, "pis")])
                        P.op("dve", lambda e, h=h: e.scalar_tensor_tensor(out=nd, in0=PSB[7][0:64, 0:257], scalar=inter[:, h:h + 1], in1=pis, op0=ALU.mult, op1=ALU.add),
                             reads=[("psb", 7), (k, "inter"), (k, "pis")], writes=[(k, "nd")])
                        P.op("dve", lambda e: e.tensor_scalar(out=rdn, in0=nd[:, 256:257], scalar1=-1.0, scalar2=None, op0=ALU.mult), reads=[(k, "nd")], writes=[(k, "rdn")])
                        P.op("dve", lambda e: e.tensor_tensor(out=rdn, in0=rdn, in1=nd[:, 256:257], op=ALU.max), reads=[(k, "nd"), (k, "rdn")], writes=[(k, "rdn")])
                        P.op("dve", lambda e: e.tensor_scalar(out=rdn, in0=rdn, scalar1=1.0, scalar2=None, op0=ALU.max), reads=[(k, "rdn")], writes=[(k, "rdn")])
                        P.op("dve", lambda e: e.reciprocal(out=rdn, in_=rdn), reads=[(k, "rdn")], writes=[(k, "rdn")])
                        P.op("dve", lambda e, h=h: e.tensor_scalar(out=hout[:, h * 256:(h + 1) * 256], in0=nd[:, 0:256], scalar1=rdn, scalar2=None, op0=ALU.mult),
                             reads=[(k, "nd"), (k, "rdn")], writes=[(k, "hout")])
                        P.op("dve", lambda e, h=h, tl=tl: e.tensor_scalar(out=Kw[:, h, :], in0=PSB[0][0:64, h * 128:(h + 1) * 128], scalar1=Em[:, h * 64 + tl:h * 64 + tl + 1], scalar2=None, op0=ALU.mult),
                             reads=[("psb", 0), (k, "Em")], writes=[(k, "Kw", h)])
                        ub_ = 1 + h % 2
                        P.op("pe", lambda e, h=h, ub_=ub_: e.matmul(PSB[ub_][:, 0:257], lhsT=Kw[:, h, :], rhs=Vp[:, h, :], start=True, stop=True),
                             reads=[(k, "Kw", h), (k, "Vp")], writes=[("psb", ub_)])
                        P.op("dve", lambda e, h=h, ub_=ub_: e.scalar_tensor_tensor(out=Cst[:, h, :], in0=Cst[:, h, :], scalar=decay[:, h:h + 1], in1=PSB[ub_][:, 0:257], op0=ALU.mult, op1=ALU.add),
                             reads=[(k, "C", h), (k, "decay"), ("psb", ub_)], writes=[(k, "C", h)])
                        P.op("act", lambda e, h=h: e.copy(out=Cbf[:, h, :], in_=Cst[:, h, :]), reads=[(k, "C", h)], writes=[(k, "Cbf", h)])
                    P.op("sp", lambda e, d=d, t0=t0: e.dma_start(out=hdir[d, t0:t0 + 64, :], in_=hout), reads=[(k, "hout")], writes=[(k, "hdir", d, ch // 2)], dma=True)
            phase()
            wog = arA.alloc([128, 8, 1024], BF16)
            gain = arA.alloc([128, 1024], F32)
            hfb = arA.alloc([128, 2, 1024], BF16)
            hs = arA.alloc([128, 1024], F32)
            sq = arA.alloc([128, 1024], F32)
            wout = arA.alloc([128, 2, 8, 128], BF16)
            ss = rstd[:, 0:4]
            P.op("pool", lambda e: e.dma_start(out=wog, in_=ml_win[:, 2048:3072].rearrange("(k p) n -> p k n", p=128)), writes=[(k, "wog")], dma=True)
            P.op("sp", lambda e: e.dma_start(out=gain, in_=ml_gain), writes=[(k, "gain")], dma=True)
            for ti in range(18):
                jt = [jj for jj, (aa, bb) in enumerate(TT) if aa <= ti * 128 < bb][0]
                for d in range(2):
                    P.op("sp", lambda e, d=d, ti=ti: e.dma_start(out=hfb[:, d, :], in_=hdir[d, ti * 128:(ti + 1) * 128, :]), writes=[(k, "hfb", d)], dma=True)
                for half in range(2):
                    def mmo(e, ti=ti, half=half):
                        ins = None
                        for kk in range(8):
                            ins = e.matmul(PSB[2 + half][:, :], lhsT=hT[:, kk, ti * 128:(ti + 1) * 128], rhs=wog[:, kk, half * 512:(half + 1) * 512], start=(kk == 0), stop=(kk == 7))
                        return ins
                    P.op("pe", mmo, reads=[("h", kk, jt) for kk in range(8)] + [(k, "wog")], writes=[("psb", 2 + half)])
                P.op("dve", lambda e: e.tensor_tensor(out=hs, in0=hfb[:, 0, :], in1=hfb[:, 1, :], op=ALU.add), reads=[(k, "hfb", 0), (k, "hfb", 1)], writes=[(k, "hs")])
                P.op("act", lambda e: e.activation(out=sq, in_=hs, func=AF.Square), reads=[(k, "hs")], writes=[(k, "sq")])
                P.op("dve", lambda e: e.tensor_reduce(out=ss, in_=sq[:, :].rearrange("p (a b) -> p a b", a=4), axis=AX.X, op=ALU.add), reads=[(k, "sq")], writes=[(k, "ss")])
                P.op("act", lambda e: e.activation(out=ss, in_=ss, func=AF.Sqrt, bias=cst[:, 0:1], scale=1.0 / 256.0), reads=[(k, "ss"), "cst0"], writes=[(k, "ss")])
                P.op("dve", lambda e: e.reciprocal(out=ss, in_=ss), reads=[(k, "ss")], writes=[(k, "ss")])
                for h in range(4):
                    P.op("dve", lambda e, h=h: e.scalar_tensor_tensor(out=hs[:, h * 256:(h + 1) * 256], in0=hs[:, h * 256:(h + 1) * 256], scalar=ss[:, h:h + 1], in1=gain[:, h * 256:(h + 1) * 256], op0=ALU.mult, op1=ALU.mult),
                         reads=[(k, "hs"), (k, "ss"), (k, "gain")], writes=[(k, "hs")])
                for half in range(2):
                    P.op("act", lambda e, half=half: e.activation(out=sq[:, half * 512:(half + 1) * 512], in_=PSB[2 + half][:, :], func=AF.Sigmoid), reads=[("psb", 2 + half), (k, "sq")], writes=[(k, "sq")])
                P.op("dve", lambda e: e.tensor_tensor(out=hs, in0=hs, in1=sq, op=ALU.mult), reads=[(k, "hs"), (k, "sq")], writes=[(k, "hs")])
                for half in range(2):
                    def trf(e, half=half):
                        ins = None
                        for q in range(4):
                            c = half * 4 + q
                            ins = e.transpose(PSB[half][:, q * 128:(q + 1) * 128], hs[:, c * 128:(c + 1) * 128], ident[:])
                        return ins
                    P.op("pe", trf, reads=[(k, "hs"), "ident"], writes=[("psb", half)])
                    P.op("act" if half else "dve", lambda e, half=half, ti=ti: (e.copy if half else e.tensor_copy)(out=mT[:, half * 4:half * 4 + 4, ti * 128:(ti + 1) * 128], in_=PSB[half][:, :].rearrange("p (q t) -> p q t", q=4)),
                         reads=[("psb", half)], writes=[("m", c, jt) for c in range(half * 4, half * 4 + 4)])
            for oc in range(8):
                ob = oc % 2
                P.op("pool", lambda e, oc=oc, ob=ob: e.dma_start(out=wout[:, ob, :, :], in_=ml_wout[:, oc * 128:(oc + 1) * 128].rearrange("(k p) n -> p k n", p=128)),
                     writes=[(k, "wout", ob)], dma=True)
                for j in tiles_all:
                    a, b = TT[j]
                    n = b - a
                    py = PSB[4 + j % 4]

                    def mmfn(e, py=py, ob=ob, a=a, b=b, n=n):
                        ins = None
                        for cc in range(8):
                            ins = e.matmul(py[:, :n], lhsT=wout[:, ob, cc, :], rhs=mT[:, cc, a:b], start=(cc == 0), stop=(cc == 7))
                        return ins
                    P.op("pe", mmfn, reads=[(k, "wout", ob)] + [("m", cc, j) for cc in range(8)], writes=[("psb", 4 + j % 4)])
                    P.op("dve", lambda e, py=py, oc=oc, a=a, b=b, n=n, j=j: e.scalar_tensor_tensor(
                        out=xT[:, oc, a:b], in0=py[:, :n], scalar=modcol(2, oc, j), in1=xT[:, oc, a:b], op0=ALU.mult, op1=ALU.add),
                        reads=[("psb", 4 + j % 4), "modt", ("x", oc, j)], writes=[("x", oc, j)])

        def moe_sparse(i, tiles):
            phase()
            k = "moe%d" % i
            subs = [t for j in tiles for t in range(TT[j][0] // 128, TT[j][1] // 128)]
            NOV = 36
            wr = arA.alloc([128, 8, 72], F32)
            brb = arA.alloc([128, 72], F32)
            f32t = arA.alloc([128, 8, 512], F32)
            o4 = arA.off
            lgs4 = arA.alloc([128, 4, 72], F32)
            sm4 = arA.alloc([128, 8, 4], F32)
            oh4 = arA.alloc([128, 4, 8], F32)
            eg4 = arA.alloc([128, 4, 8], F32)
            em4 = arA.alloc([128, 4, 64], F32)
            as4 = arA.alloc([128, 4, 64], F32)
            RK = arA.alloc([128, 18, 64], F32)
            assert arA.off - o4 >= 2048
            stg4 = scr[:, o4:o4 + 2048]
            OH = arA.alloc([128, 18, 2, 64], F32)
            WP = arA.alloc([128, 18, 2], F32)
            acum = arA.alloc([128, 64], F32)
            asum = arA.alloc([128, 64], F32)
            mc = arA.alloc([128, 229], F32)
            ones1 = arA.alloc([128, 128], F32)
            identb = arA.alloc([128, 128], BF16)
            cntb = arA.alloc([128, 64], F32)
            ovn = arA.alloc([128, 64], F32)
            ove = arA.alloc([128, 64], F32)
            ovsp = arA.alloc([128, 64], F32)
            dlt = arA.alloc([128, 64], F32)
            t64a = arA.alloc([128, 64], F32)
            t64b = arA.alloc([128, 64], F32)
            EB = arA.alloc([128, NOV], F32)
            DEST = arA.alloc([128, 18, 2], F32)
            DESTI = arA.alloc([128, 18, 2], I32)
            idxi = arA.alloc([128, NOV, 4], I32)
            idxf = arA.alloc([128, NOV, 4], F32)
            Ltri = mc[:, 0:128]
            e128 = mc[:, 128:192]
            iop = mc[:, 192:193]
            thr36 = mc[:, 193:229]
            P.op("sp", lambda e: e.dma_start(out=wr, in_=moe_wr[i].rearrange("(k p) n -> p k n", p=128)), writes=[(k, "wr")], dma=True)
            P.op("sp", lambda e: e.dma_start(out=brb, in_=moe_br[i]), writes=[(k, "br")], dma=True)
            P.op("sp", lambda e: e.dma_start(out=mc, in_=moec_d), writes=[(k, "mc")], dma=True)
            P.op("pool", lambda e: e.memset(ones1, 1.0), writes=[(k, "ones1")])
            P.op("pool", lambda e: e.memset(acum, 0.0), writes=[(k, "rk")])
            P.op("pool", lambda e: e.tensor_copy(out=identb, in_=ident[:]), reads=["ident"], writes=[(k, "identb")])
            RKK = [(k, "rk")]

            def extra(j, c, tb):
                a, b = TT[j]
                n = b - a
                jj = 1 if j == 0 else 0
                P.op("pool", lambda e, c=c, tb=tb, n=n, jj=jj: e.tensor_scalar(out=f32t[:, c, :n], in0=tb[:, :n], scalar1=mA[:, 1, c, jj:jj + 1],
                                                                               scalar2=modt[:, 3 * 8 + c, jj:jj + 1], op0=ALU.mult, op1=ALU.add),
                     reads=[("tmp", 2 + c % 2), "modt", "mA"], writes=[(k, "f32", c)])
                if c == 7:
                    S = n // 128
                    g0 = a // 128
                    pl = PSB[5]

                    def mmfn(e, S=S):
                        ins = None
                        for s_ in range(S):
                            for kk in range(8):
                                ins = e.matmul(pl[:, s_ * 72:(s_ + 1) * 72], lhsT=f32t[:, kk, s_ * 128:(s_ + 1) * 128], rhs=wr[:, kk, :], start=(kk == 0), stop=(kk == 7))
                        return ins
                    P.op("pe", mmfn, reads=[(k, "f32", cc) for cc in range(8)] + [(k, "wr")], writes=[("psb", 5)])
                    R = [(k, "rt")]
                    V = lambda fn, extra_r=(), extra_w=(): P.op("dve", fn, reads=R + list(extra_r), writes=R + list(extra_w))
                    L4 = lgs4[:, 0:S, :]
                    G4 = lgs4[:, 0:S, 0:8]
                    E4 = lgs4[:, 0:S, 8:72]
                    E44 = E4.rearrange("p s (g j) -> p s g j", g=8)
                    o8 = oh4[:, 0:S, :]
                    oh1 = OH[:, g0:g0 + S, 0, :]
                    oh2 = OH[:, g0:g0 + S, 1, :]
                    em_ = em4[:, 0:S, :]
                    sc = lambda i_: sm4[:, i_, 0:S]
                    bc8 = lambda v: v.unsqueeze(2).to_broadcast([128, S, 8])
                    bc64 = lambda v: v.unsqueeze(2).to_broadcast([128, S, 64])
                    V(lambda e: e.tensor_tensor(out=L4, in0=pl[:, 0:S * 72].rearrange("p (s c) -> p s c", s=S), in1=brb.unsqueeze(1).to_broadcast([128, S, 72]), op=ALU.add), [("psb", 5), (k, "br")])
                    V(lambda e: e.tensor_reduce(out=sc(0), in_=G4, axis=AX.X, op=ALU.max))
                    V(lambda e: e.tensor_tensor(out=o8, in0=G4, in1=bc8(sc(0)), op=ALU.is_equal))
                    V(lambda e: e.tensor_tensor(out=eg4[:, 0:S, :], in0=G4, in1=bc8(sc(0)), op=ALU.subtract))
                    P.op("act", lambda e: e.activation(out=eg4[:, 0:S, :], in_=eg4[:, 0:S, :], func=AF.Exp), reads=R, writes=R)
                    V(lambda e: e.tensor_reduce(out=sc(2), in_=eg4[:, 0:S, :], axis=AX.X, op=ALU.add))
                    V(lambda e: e.reciprocal(out=sc(3), in_=sc(2)))
                    V(lambda e: e.tensor_scalar(out=o8, in0=o8, scalar1=BIG, scalar2=-BIG, op0=ALU.mult, op1=ALU.add))
                    V(lambda e: e.tensor_tensor(out=em_.rearrange("p s (g j) -> p s g j", g=8), in0=E44, in1=o8.unsqueeze(3).to_broadcast([128, S, 8, 8]), op=ALU.add))
                    V(lambda e: e.tensor_reduce(out=sc(4), in_=em_, axis=AX.X, op=ALU.max))
                    V(lambda e: e.tensor_tensor(out=oh1, in0=em_, in1=bc64(sc(4)), op=ALU.is_equal), (), RKK)
                    V(lambda e: e.scalar_tensor_tensor(out=em_, in0=oh1, scalar=-BIG, in1=em_, op0=ALU.mult, op1=ALU.add))
                    V(lambda e: e.tensor_reduce(out=sc(5), in_=em_, axis=AX.X, op=ALU.max))
                    V(lambda e: e.tensor_tensor(out=oh2, in0=em_, in1=bc64(sc(5)), op=ALU.is_equal), (), RKK)
                    V(lambda e: e.tensor_tensor(out=sc(6), in0=sc(5), in1=sc(4), op=ALU.subtract))
                    P.op("act", lambda e: e.activation(out=sc(6), in_=sc(6), func=AF.Exp), reads=R, writes=R)
                    V(lambda e: e.tensor_scalar(out=sc(6), in0=sc(6), scalar1=1.0, scalar2=None, op0=ALU.add))
                    V(lambda e: e.reciprocal(out=sc(6), in_=sc(6)))
                    V(lambda e: e.tensor_tensor(out=WP[:, g0:g0 + S, 0], in0=sc(6), in1=sc(3), op=ALU.mult), (), RKK)
                    V(lambda e: e.tensor_tensor(out=WP[:, g0:g0 + S, 1], in0=sc(3), in1=WP[:, g0:g0 + S, 0], op=ALU.subtract), (), RKK)
                    V(lambda e: e.tensor_tensor(out=as4[:, 0:S, :], in0=oh1, in1=oh2, op=ALU.add), (), RKK)

                    def mmr(e, S=S):
                        ins = None
                        for s_ in range(S):
                            o = PSB[4][:, s_ * 64:(s_ + 1) * 64]
                            e.matmul(o, lhsT=Ltri, rhs=as4[:, s_, :], start=True, stop=False)
                            for sp_ in range(s_):
                                e.matmul(o, lhsT=ones1, rhs=as4[:, sp_, :], start=False, stop=False)
                            ins = e.matmul(o, lhsT=ones1, rhs=acum, start=False, stop=True)
                        return ins
                    P.op("pe", mmr, reads=RKK + R + [(k, "mc"), (k, "ones1")], writes=[("psb", 4)])
                    P.op("act", lambda e: e.copy(out=RK[:, g0:g0 + S, :], in_=PSB[4][:, 0:S * 64].rearrange("p (s c) -> p s c", s=S)), reads=[("psb", 4)] + RKK, writes=RKK)
                    V(lambda e: e.tensor_reduce(out=asum, in_=as4[:, 0:S, :].rearrange("p s c -> p c s"), axis=AX.X, op=ALU.add), [("psb", 4)], RKK)
                    V(lambda e: e.tensor_tensor(out=acum, in0=acum, in1=asum, op=ALU.add), [("psb", 4)], RKK)

            norm_mod(1, tiles, extra=extra)

            P.op("pe", lambda e: e.matmul(PSB[4][:, 0:64], lhsT=ones1, rhs=acum, start=True, stop=True), reads=RKK + [(k, "ones1")], writes=[("psb", 4)])
            V2 = lambda fn: P.op("dve", fn, reads=RKK + [("psb", 4), (k, "mc")], writes=RKK)
            V2(lambda e: e.tensor_scalar(out=cntb, in0=PSB[4][:, 0:64], scalar1=-128.0, scalar2=0.0, op0=ALU.add, op1=ALU.max))
            f32flat0 = f32t[:, :, :].rearrange("p a b -> p (a b)")
            tQ = f32flat0[:, 0:64 * NOV].rearrange("p (c q) -> p c q", q=NOV)
            F3a = [(k, "f32", cc) for cc in range(8)]
            V2b = lambda fn: P.op("dve", fn, reads=RKK + F3a + [("psb", 4), (k, "mc")], writes=RKK + F3a)
            V2b(lambda e: e.tensor_tensor(out=tQ, in0=cntb.unsqueeze(2).to_broadcast([128, 64, NOV]), in1=thr36.unsqueeze(1).to_broadcast([128, 64, NOV]), op=ALU.is_gt))
            V2b(lambda e: e.tensor_reduce(out=ovn, in_=tQ, axis=AX.X, op=ALU.add))
            V2(lambda e: e.tensor_scalar(out=ovn, in0=ovn, scalar1=128.0, scalar2=None, op0=ALU.mult))
            V2(lambda e: e.tensor_tensor_scan(out=ove, data0=ones1[:, 0:64], data1=ovn, initial=0.0, op0=ALU.mult, op1=ALU.add))
            V2(lambda e: e.tensor_tensor(out=ovsp, in0=ove, in1=ovn, op=ALU.subtract))
            V2(lambda e: e.tensor_scalar(out=ovsp, in0=ovsp, scalar1=8064.0, scalar2=None, op0=ALU.add))
            V2(lambda e: e.tensor_tensor(out=dlt, in0=e128, in1=ovsp, op=ALU.subtract))
            tQ2 = f32flat0[:, 0:64 * NOV].rearrange("p (q c) -> p q c", q=NOV)
            V2b(lambda e: e.tensor_tensor(out=tQ2, in0=ove.unsqueeze(1).to_broadcast([128, NOV, 64]), in1=thr36.unsqueeze(2).to_broadcast([128, NOV, 64]), op=ALU.is_le))
            V2b(lambda e: e.tensor_reduce(out=EB, in_=tQ2, axis=AX.X, op=ALU.add))
            V2(lambda e: e.tensor_scalar(out=t64a[:, 0:NOV], in0=EB, scalar1=64.0, scalar2=1.0e6, op0=ALU.is_ge, op1=ALU.mult))
            V2(lambda e: e.scalar_tensor_tensor(out=t64a[:, 0:NOV], in0=EB, scalar=256.0, in1=t64a[:, 0:NOV], op0=ALU.mult, op1=ALU.add))
            V2(lambda e: e.tensor_scalar(out=t64b[:, 0:1], in0=iop, scalar1=2.0, scalar2=float(i * 16384), op0=ALU.mult, op1=ALU.add))
            V2(lambda e: e.tensor_scalar(out=idxf[:, :, 0], in0=t64a[:, 0:NOV], scalar1=t64b[:, 0:1], scalar2=None, op0=ALU.add))
            V2(lambda e: e.tensor_scalar(out=idxf[:, :, 1], in0=idxf[:, :, 0], scalar1=1.0, scalar2=None, op0=ALU.add))
            V2(lambda e: e.tensor_scalar(out=t64a[:, 0:NOV], in0=t64a[:, 0:NOV], scalar1=0.5, scalar2=None, op0=ALU.mult))
            V2(lambda e: e.tensor_scalar(out=t64b[:, 1:2], in0=iop, scalar1=float(i * 8192), scalar2=None, op0=ALU.add))
            V2(lambda e: e.tensor_scalar(out=idxf[:, :, 2], in0=t64a[:, 0:NOV], scalar1=t64b[:, 1:2], scalar2=None, op0=ALU.add))
            V2(lambda e: e.tensor_copy(out=idxf[:, :, 3], in_=idxf[:, :, 2]))
            V2(lambda e: e.tensor_copy(out=idxi, in_=idxf))
            sg0, SG = subs[0], len(subs)
            f32flat = f32t[:, :, :].rearrange("p a b -> p (a b)")
            tA = f32flat[:, 0:SG * 64].rearrange("p (s c) -> p s c", s=SG)
            tB = f32flat[:, 2048:2048 + SG * 64].rearrange("p (s c) -> p s c", s=SG)
            RKs = RK[:, sg0:sg0 + SG, :]
            F3 = [(k, "f32", cc) for cc in range(8)]
            V3 = lambda fn: P.op("dve", fn, reads=RKK + F3 + [(k, "mc")], writes=RKK + F3)
            V3(lambda e: e.tensor_scalar(out=tA, in0=RKs, scalar1=128.0, scalar2=None, op0=ALU.is_lt))
            V3(lambda e: e.tensor_tensor(out=tA, in0=tA, in1=dlt.unsqueeze(1).to_broadcast([128, SG, 64]), op=ALU.mult))
            V3(lambda e: e.tensor_tensor(out=tB, in0=RKs, in1=ovsp.unsqueeze(1).to_broadcast([128, SG, 64]), op=ALU.add))
            V3(lambda e: e.tensor_tensor(out=tA, in0=tA, in1=tB, op=ALU.add))
            for kk in range(2):
                V3(lambda e, kk=kk: e.tensor_tensor(out=tB, in0=OH[:, sg0:sg0 + SG, kk, :], in1=tA, op=ALU.mult))
                V3(lambda e, kk=kk: e.tensor_reduce(out=DEST[:, sg0:sg0 + SG, kk], in_=tB, axis=AX.X, op=ALU.add))
            V2(lambda e: e.tensor_copy(out=DESTI, in_=DEST))
            P.barrier()
            ftok = arB.alloc([128, 2, 1024], BF16)
            for si, gt in enumerate(subs):
                fb = si % 2
                jt = [jj for jj, (aa, bb) in enumerate(TT) if aa <= gt * 128 < bb][0]
                pbf = PSB[fb][:, :].bitcast(BF16)

                def trf(e, gt=gt, pbf=pbf):
                    ins = None
                    for c in range(8):
                        ins = e.transpose(pbf[:, c * 128:(c + 1) * 128], hT[:, c, gt * 128:(gt + 1) * 128], identb)
                    return ins
                P.op("pe", trf, reads=[("h", c, jt) for c in range(8)] + [(k, "identb")], writes=[("psb", fb)])
                P.op("act", lambda e, fb=fb, pbf=pbf: e.copy(out=ftok[:, fb, :], in_=pbf), reads=[("psb", fb)], writes=[(k, "ftok", fb)])
                for kk in range(2):
                    P.op("pool", lambda e, fb=fb, gt=gt, kk=kk: e.indirect_dma_start(
                        out=xslots[:, :], out_offset=bass.IndirectOffsetOnAxis(ap=DESTI[:, gt, kk:kk + 1], axis=0), in_=ftok[:, fb, :], in_offset=None),
                        reads=[(k, "ftok", fb)] + RKK, writes=[(k, "xs", gt, kk)], dma=True)
            P.barrier()
            arB.reset()
            stg = f32t[:, :, :].rearrange("p a b -> p (a b)").rearrange("p (s n) -> p s n", s=2)
            xb = arB.alloc([128, 2, 1024], BF16)
            xbT = arB.alloc([128, 2, 8, 128], BF16)
            wgu = arB.alloc([128, 2, 8, 512], BF16)
            wd = arB.alloc([128, 2, 2, 1024], BF16)
            sg = arB.alloc([128, 256], F32)
            actb = arB.alloc([128, 2, 2, 128], BF16)
            wgu_rows = moe_wgu.rearrange("l e (p h q) n -> (l e p h) (q n)", p=128, h=2)
            wd_rows = moe_wd.rearrange("l e (p q) n -> (l e p) (q n)", p=128)
            OHflat = OH[:, :, :, :].rearrange("p a b c -> p (a b c)")
            stgs = [stg[:, 0, :], stg[:, 1, :], OHflat[:, 0:2048], stg4]
            nstg = [0]

            def emit_weights(b, si):
                wb = si % 2
                ov = b - 64
                for piece in range(3):
                    sb_ = nstg[0] % 4
                    nstg[0] += 1
                    sv = stgs[sb_]
                    if b < 64:
                        if piece < 2:
                            src = moe_wgu[i, b].rearrange("(p q) n -> p (q n)", p=128)[:, piece * 2048:(piece + 1) * 2048]
                        else:
                            src = moe_wd[i, b].rearrange("(p q) n -> p (q n)", p=128)
                        P.op("sp", lambda e, src=src, sv=sv: e.dma_start(out=sv, in_=src), writes=[(k, "stg", sb_)], dma=True)
                    else:
                        if piece < 2:
                            P.op("pool", lambda e, ov=ov, sv=sv, piece=piece: e.indirect_dma_start(
                                out=sv, out_offset=None, in_=wgu_rows[:, :],
                                in_offset=bass.IndirectOffsetOnAxis(ap=idxi[:, ov, piece:piece + 1], axis=0), bounds_check=getreg(e, 65535), oob_is_err=False),
                                reads=RKK, writes=[(k, "stg", sb_)], dma=True)
                        else:
                            P.op("pool", lambda e, ov=ov, sv=sv: e.indirect_dma_start(
                                out=sv, out_offset=None, in_=wd_rows[:, :],
                                in_offset=bass.IndirectOffsetOnAxis(ap=idxi[:, ov, 2:3], axis=0), bounds_check=getreg(e, 32767), oob_is_err=False),
                                reads=RKK, writes=[(k, "stg", sb_)], dma=True)
                    ceng = "act" if piece == 0 else "dve"
                    if piece < 2:
                        P.op(ceng, lambda e, sv=sv, wb=wb, piece=piece, ceng=ceng: (e.copy if ceng == "act" else e.tensor_copy)(out=wgu[:, wb, piece * 4:(piece + 1) * 4, :], in_=sv.rearrange("p (q n) -> p q n", q=4)),
                             reads=[(k, "stg", sb_)], writes=[(k, "wgu", wb, piece)])
                    else:
                        P.op(ceng, lambda e, sv=sv, wb=wb, ceng=ceng: (e.copy if ceng == "act" else e.tensor_copy)(out=wd[:, wb, :, :], in_=sv.rearrange("p (q n) -> p q n", q=2)),
                             reads=[(k, "stg", sb_)], writes=[(k, "wd", wb)])

            def emit_compute(b, si):
                wb = si % 2
                xbuf = si % 2
                P.op("sp", lambda e, b=b, xbuf=xbuf: e.dma_start(out=xb[:, xbuf, :], in_=xslots[b * 128:(b + 1) * 128, :]), writes=[(k, "xb", xbuf)], dma=True)
                pbf = PSB[xbuf][:, :].bitcast(BF16)

                def trx(e, xbuf=xbuf, pbf=pbf):
                    ins = None
                    for q in range(8):
                        ins = e.transpose(pbf[:, q * 128:(q + 1) * 128], xb[:, xbuf, q::8], identb)
                    return ins
                P.op("pe", trx, reads=[(k, "xb", xbuf), (k, "identb")], writes=[("psb", xbuf)])
                P.op("dve", lambda e, xbuf=xbuf, pbf=pbf: e.tensor_copy(out=xbT[:, xbuf, :, :], in_=pbf.rearrange("p (q s) -> p q s", q=8)), reads=[("psb", xbuf)], writes=[(k, "xbT", xbuf)])
                pgu = PSB[2 + xbuf]

                def mmgu(e, xbuf=xbuf, wb=wb, pgu=pgu):
                    ins = None
                    for oc in range(4):
                        for q in range(8):
                            ins = e.matmul(pgu[:, oc * 128:(oc + 1) * 128], lhsT=wgu[:, wb, q, (oc // 2) * 256 + (oc % 2):(oc // 2) * 256 + 256:2], rhs=xbT[:, xbuf, q, :], start=(q == 0), stop=(q == 7))
                    return ins
                P.op("pe", mmgu, reads=[(k, "wgu", wb, 0), (k, "wgu", wb, 1), (k, "xbT", xbuf)], writes=[("psb", 2 + xbuf)])
                P.op("act", lambda e, pgu=pgu: e.activation(out=sg, in_=pgu[:, 0:256], func=AF.Silu), reads=[("psb", 2 + xbuf)], writes=[(k, "sg")])
                P.op("dve", lambda e, pgu=pgu, xbuf=xbuf: e.tensor_tensor(out=actb[:, xbuf, :, :], in0=sg[:, :].rearrange("p (a b) -> p a b", a=2), in1=pgu[:, 256:512].rearrange("p (a b) -> p a b", a=2), op=ALU.mult),
                     reads=[(k, "sg"), ("psb", 2 + xbuf)], writes=[(k, "actb", xbuf)])
                for half in range(2):
                    pyb = 4 + 2 * xbuf + half
                    tb_ = 2 * xbuf + half

                    def mmd(e, half=half, pyb=pyb, xbuf=xbuf, wb=wb):
                        ins = None
                        for fc in range(2):
                            ins = e.matmul(PSB[pyb][:, :], lhsT=actb[:, xbuf, fc, :], rhs=wd[:, wb, fc, half * 512:(half + 1) * 512], start=(fc == 0), stop=(fc == 1))
                        return ins
                    P.op("pe", mmd, reads=[(k, "actb", xbuf), (k, "wd", wb)], writes=[("psb", pyb)])
                    P.op("act", lambda e, pyb=pyb, tb_=tb_: e.copy(out=tmp[tb_][:, :].bitcast(BF16)[:, 0:512], in_=PSB[pyb][:, :]),
                         reads=[("psb", pyb)], writes=[("tmp", tb_)])
                    P.op("act", lambda e, b=b, half=half, tb_=tb_: e.dma_start(out=yslots[b * 128:(b + 1) * 128, half * 512:(half + 1) * 512], in_=tmp[tb_][:, :].bitcast(BF16)[:, 0:512]),
                         reads=[("tmp", tb_)], writes=[(k, "ys", b, half)], dma=True)

            order = []
            ovl = list(range(64, 64 + NOV))
            for b in range(64):
                order.append(b)
                if b % 2 == 1 and ovl:
                    order.append(ovl.pop(0))
            order += ovl
            emit_weights(order[0], 0)
            for si, b in enumerate(order):
                if si + 1 < len(order):
                    emit_weights(order[si + 1], si + 1)
                emit_compute(b, si)
            P.barrier()
            arB.reset()
            yg = arB.alloc([128, 2, 2, 1024], BF16)
            yt = arB.alloc([128, 1024], F32)
            for si, gt in enumerate(subs):
                gb = si % 2
                jt = [jj for jj, (aa, bb) in enumerate(TT) if aa <= gt * 128 < bb][0]
                for kk in range(2):
                    P.op("pool", lambda e, gb=gb, gt=gt, kk=kk: e.indirect_dma_start(
                        out=yg[:, gb, kk, :], out_offset=None, in_=yslots[:, :], in_offset=bass.IndirectOffsetOnAxis(ap=DESTI[:, gt, kk:kk + 1], axis=0)),
                        reads=RKK, writes=[(k, "yg", gb, kk)], dma=True)
                P.op("dve", lambda e, gb=gb, gt=gt: e.tensor_scalar(out=yt, in0=yg[:, gb, 0, :], scalar1=WP[:, gt, 0:1], scalar2=None, op0=ALU.mult),
                     reads=[(k, "yg", gb, 0)] + RKK, writes=[(k, "yt")])
                P.op("dve", lambda e, gb=gb, gt=gt: e.scalar_tensor_tensor(out=yt, in0=yg[:, gb, 1, :], scalar=WP[:, gt, 1:2], in1=yt, op0=ALU.mult, op1=ALU.add),
                     reads=[(k, "yg", gb, 1), (k, "yt")] + RKK, writes=[(k, "yt")])
                for half in range(2):
                    pb = PSB[4 + half]

                    def trf(e, pb=pb, half=half):
                        ins = None
                        for q in range(4):
                            c = half * 4 + q
                            ins = e.transpose(pb[:, q * 128:(q + 1) * 128], yt[:, c * 128:(c + 1) * 128], ident[:])
                        return ins
                    P.op("pe", trf, reads=[(k, "yt"), "ident"], writes=[("psb", 4 + half)])
                    for q in range(4):
                        c = half * 4 + q
                        P.op("dve", lambda e, pb=pb, q=q, c=c, gt=gt, jt=jt: e.scalar_tensor_tensor(
                            out=xT[:, c, gt * 128:(gt + 1) * 128], in0=pb[:, q * 128:(q + 1) * 128], scalar=modcol(5, c, jt), in1=xT[:, c, gt * 128:(gt + 1) * 128], op0=ALU.mult, op1=ALU.add),
                            reads=[("psb", 4 + half), "modt", ("x", c, jt)], writes=[("x", c, jt)])

        def moe(i, tiles):
            phase()
            k = "moe%d" % i
            wr = arA.alloc([128, 8, 72], F32)
            brb = arA.alloc([128, 72], F32)
            f32t = arA.alloc([128, 8, 512], F32)
            lgs = arA.alloc([128, 72], F32)
            sm = arA.alloc([128, 16], F32)
            oh = arA.alloc([128, 8], F32)
            em = arA.alloc([128, 64], F32)
            oh1 = arA.alloc([128, 64], F32)
            oh2 = arA.alloc([128, 64], F32)
            wt = arA.alloc([128, 64], F32)
            wtT = arA.alloc([64, NT], F32)
            wgu = arB.alloc([128, 2, 8, 512], BF16)
            wd = arB.alloc([128, 2, 2, 1024], BF16)
            sgt = arB.alloc([128, 2, 512], F32)
            actt = arB.alloc([128, 2, 2, 512], BF16)
            wbc = arB.alloc([128, 512], F32)
            P.op("sp", lambda e: e.dma_start(out=wr, in_=moe_wr[i].rearrange("(k p) n -> p k n", p=128)), writes=[(k, "wr")], dma=True)
            P.op("sp", lambda e: e.dma_start(out=brb, in_=moe_br[i]), writes=[(k, "br")], dma=True)

            def extra(j, c, tb):
                a, b = TT[j]
                n = b - a
                jj = 1 if j == 0 else 0
                P.op("pool", lambda e, c=c, tb=tb, n=n, jj=jj: e.tensor_scalar(out=f32t[:, c, :n], in0=tb[:, :n], scalar1=mA[:, 1, c, jj:jj + 1],
                                                                               scalar2=modt[:, 3 * 8 + c, jj:jj + 1], op0=ALU.mult, op1=ALU.add),
                     reads=[("tmp", 2 + c % 2), "modt", "mA"], writes=[(k, "f32", c)])
                if c == 7:
                    for s in range(n // 128):
                        gt = a // 128 + s
                        pl = PSB[5]

                        def mmfn(e, s=s):
                            ins = None
                            for kk in range(8):
                                ins = e.matmul(pl[:, 0:72], lhsT=f32t[:, kk, s * 128:(s + 1) * 128], rhs=wr[:, kk, :], start=(kk == 0), stop=(kk == 7))
                            return ins
                        P.op("pe", mmfn, reads=[(k, "f32", cc) for cc in range(8)] + [(k, "wr")], writes=[("psb", 5)])
                        R = [(k, "rt")]
                        V = lambda fn, extra_r=(): P.op("dve", fn, reads=R + list(extra_r), writes=R)
                        V(lambda e: e.tensor_tensor(out=lgs, in0=pl[:, 0:72], in1=brb, op=ALU.add), [("psb", 5), (k, "br")])
                        V(lambda e: e.tensor_reduce(out=sm[:, 0:1], in_=lgs[:, 0:8], axis=AX.X, op=ALU.max))
                        V(lambda e: e.tensor_scalar(out=oh, in0=lgs[:, 0:8], scalar1=sm[:, 0:1], scalar2=None, op0=ALU.is_equal))
                        V(lambda e: e.tensor_scalar(out=sm[:, 1:2], in0=sm[:, 0:1], scalar1=-1.0, scalar2=None, op0=ALU.mult))
                        P.op("act", lambda e: e.activation(out=sm[:, 8:16], in_=lgs[:, 0:8], func=AF.Exp, bias=sm[:, 1:2], scale=1.0), reads=R, writes=R)
                        V(lambda e: e.tensor_reduce(out=sm[:, 2:3], in_=sm[:, 8:16], axis=AX.X, op=ALU.add))
                        V(lambda e: e.reciprocal(out=sm[:, 3:4], in_=sm[:, 2:3]))
                        V(lambda e: e.tensor_scalar(out=oh, in0=oh, scalar1=BIG, scalar2=-BIG, op0=ALU.mult, op1=ALU.add))
                        for g in range(8):
                            V(lambda e, g=g: e.tensor_scalar(out=em[:, g * 8:(g + 1) * 8], in0=lgs[:, 8 + g * 8:16 + g * 8], scalar1=oh[:, g:g + 1], scalar2=None, op0=ALU.add))
                        V(lambda e: e.tensor_reduce(out=sm[:, 4:5], in_=em, axis=AX.X, op=ALU.max))
                        V(lambda e: e.tensor_scalar(out=oh1, in0=em, scalar1=sm[:, 4:5], scalar2=None, op0=ALU.is_equal))
                        V(lambda e: e.scalar_tensor_tensor(out=em, in0=oh1, scalar=-BIG, in1=em, op0=ALU.mult, op1=ALU.add))
                        V(lambda e: e.tensor_reduce(out=sm[:, 5:6], in_=em, axis=AX.X, op=ALU.max))
                        V(lambda e: e.tensor_scalar(out=oh2, in0=em, scalar1=sm[:, 5:6], scalar2=None, op0=ALU.is_equal))
                        V(lambda e: e.tensor_tensor(out=sm[:, 6:7], in0=sm[:, 5:6], in1=sm[:, 4:5], op=ALU.subtract))
                        P.op("act", lambda e: e.activation(out=sm[:, 6:7], in_=sm[:, 6:7], func=AF.Exp), reads=R, writes=R)
                        V(lambda e: e.tensor_scalar(out=sm[:, 6:7], in0=sm[:, 6:7], scalar1=1.0, scalar2=None, op0=ALU.add))
                        V(lambda e: e.reciprocal(out=sm[:, 6:7], in_=sm[:, 6:7]))
                        V(lambda e: e.tensor_tensor(out=sm[:, 6:7], in0=sm[:, 6:7], in1=sm[:, 3:4], op=ALU.mult))
                        V(lambda e: e.tensor_tensor(out=sm[:, 7:8], in0=sm[:, 3:4], in1=sm[:, 6:7], op=ALU.subtract))
                        V(lambda e: e.tensor_scalar(out=wt, in0=oh1, scalar1=sm[:, 6:7], scalar2=None, op0=ALU.mult))
                        V(lambda e: e.scalar_tensor_tensor(out=wt, in0=oh2, scalar=sm[:, 7:8], in1=wt, op0=ALU.mult, op1=ALU.add))
                        pt = PSB[4]
                        P.op("pe", lambda e: e.transpose(pt[0:64, 0:128], wt, ident[:]), reads=R + ["ident"], writes=[("psb", 4)])
                        P.op("act", lambda e, gt=gt: e.copy(out=wtT[:, gt * 128:(gt + 1) * 128], in_=pt[0:64, 0:128]), reads=[("psb", 4)], writes=[(k, "wtT", j)])

            norm_mod(1, tiles, extra=extra)

            for ex in range(64):
                eb = ex % 2
                P.op("pool", lambda e, ex=ex, eb=eb: e.dma_start(out=wgu[:, eb, :, :], in_=moe_wgu[i, ex].rearrange("(k p) n -> p k n", p=128)),
                     writes=[(k, "wgu", eb)], dma=True)
                P.op("pool", lambda e, ex=ex, eb=eb: e.dma_start(out=wd[:, eb, :, :], in_=moe_wd[i, ex].rearrange("(k p) n -> p k n", p=128)),
                     writes=[(k, "wd", eb)], dma=True)
                for j in tiles:
                    a, b = TT[j]
                    n = b - a
                    pw = PSB[6]
                    P.op("pe", lambda e, ex=ex, a=a, b=b, n=n: e.matmul(pw[:, :n], lhsT=ident[0:64, ex:ex + 1].to_broadcast([64, 128]), rhs=wtT[:, a:b], start=True, stop=True),
                         reads=["ident", (k, "wtT", j)], writes=[("psb", 6)])
                    P.op("act", lambda e, n=n: e.copy(out=wbc[:, :n], in_=pw[:, :n]), reads=[("psb", 6)], writes=[(k, "wbc")])
                    for fc in range(2):
                        pg, pu = PSB[0 + fc], PSB[2 + fc]
                        for part, pp, pk in ((0, pg, ("psb", 0 + fc)), (1, pu, ("psb", 2 + fc))):
                            def mmfn(e, part=part, pp=pp, a=a, b=b, n=n, eb=eb, fc=fc):
                                ins = None
                                for kk in range(8):
                                    ins = e.matmul(pp[:, :n], lhsT=wgu[:, eb, kk, part * 256 + fc * 128: part * 256 + (fc + 1) * 128], rhs=hT[:, kk, a:b],
                                                   start=(kk == 0), stop=(kk == 7))
                                return ins
                            P.op("pe", mmfn, reads=[(k, "wgu", eb)] + [("h", kk, j) for kk in range(8)], writes=[pk])
                        P.op("act", lambda e, pg=pg, fc=fc, n=n: e.activation(out=sgt[:, fc, :n], in_=pg[:, :n], func=AF.Silu), reads=[("psb", 0 + fc)], writes=[(k, "sgt", fc)])
                        P.op("dve", lambda e, pu=pu, fc=fc, n=n: e.tensor_tensor(out=sgt[:, fc, :n], in0=sgt[:, fc, :n], in1=pu[:, :n], op=ALU.mult), reads=[(k, "sgt", fc), ("psb", 2 + fc)], writes=[(k, "sgt", fc)])
                        P.op("pool", lambda e, fc=fc, n=n, eb=eb: e.tensor_tensor(out=actt[:, eb, fc, :n], in0=sgt[:, fc, :n], in1=wbc[:, :n], op=ALU.mult), reads=[(k, "sgt", fc), (k, "wbc")], writes=[(k, "actt", eb, fc)])
                    for oc in range(8):
                        py = PSB[4 + oc % 2]

                        def mmfn(e, py=py, oc=oc, n=n, eb=eb):
                            ins = None
                            for fc in range(2):
                                ins = e.matmul(py[:, :n], lhsT=wd[:, eb, fc, oc * 128:(oc + 1) * 128], rhs=actt[:, eb, fc, :n], start=(fc == 0), stop=(fc == 1))
                            return ins
                        P.op("pe", mmfn, reads=[(k, "wd", eb), (k, "actt", eb, 0), (k, "actt", eb, 1)], writes=[("psb", 4 + oc % 2)])
                        P.op("dve", lambda e, py=py, oc=oc, a=a, b=b, n=n, j=j: e.scalar_tensor_tensor(
                            out=xT[:, oc, a:b], in0=py[:, :n], scalar=modcol(5, oc, j), in1=xT[:, oc, a:b], op0=ALU.mult, op1=ALU.add),
                            reads=[("psb", 4 + oc % 2), "modt", ("x", oc, j)], writes=[("x", oc, j)])

        all_tiles = [0, 1, 2, 3, 4]
        for i in range(n_layers):
            last = i == DEPTH - 1
            compute_mod(i)
            phase()
            norm_mod(0, all_tiles)
            kind, jl = i % 3, i // 3
            if kind == 0:
                rglru(jl, all_tiles)
            elif kind == 1:
                mla(all_tiles)
            else:
                mlstm(all_tiles)
            if stop_after_mixer and i == n_layers - 1:
                break
            moe_sparse(i, [1, 2, 3, 4] if last else all_tiles)

        phase()
        stage = arA.alloc([128, 2, 1024], F32)
        outkeys = []
        for ti in range(18):
            jt = [j for j, (a, b) in enumerate(TT) if a <= ti * 128 < b][0]
            dst = octx_d[ti * 128:(ti + 1) * 128, :] if ti < 2 else out_d[(ti - 2) * 128:(ti - 1) * 128, :]
            sbuf = ti % 2
            for half in range(2):
                pb = PSB[half]

                def trfn(e, pb=pb, half=half, ti=ti):
                    ins = None
                    for q in range(4):
                        c = half * 4 + q
                        ins = e.transpose(pb[:, q * 128:(q + 1) * 128], xT[:, c, ti * 128:(ti + 1) * 128], ident[:])
                    return ins
                P.op("pe", trfn, reads=[("x", c, jt) for c in range(half * 4, half * 4 + 4)] + ["ident"], writes=[("psb", half)])
                P.op("dve" if half == 0 else "act",
                     lambda e, pb=pb, half=half, sbuf=sbuf: (e.tensor_copy if half == 0 else e.copy)(out=stage[:, sbuf, half * 512:(half + 1) * 512], in_=pb[:, :]),
                     reads=[("psb", half)], writes=[("ostage", sbuf, half)])
            P.op("sp", lambda e, dst=dst, sbuf=sbuf: e.dma_start(out=dst, in_=stage[:, sbuf, :]),
                 reads=[("ostage", sbuf, 0), ("ostage", sbuf, 1)], writes=[("out", ti)], dma=True)
            outkeys.append(("out", ti))
        P.finish(outkeys)
        P.emit()
    return nc


_CACHE = {}


def host_prep(inp, n_layers=DEPTH, stop_after_mixer=False):
    pv = pv_layout(inp)
    pvt = pv.table()
    key = (pvt.shape[1], n_layers, stop_after_mixer)
    if key not in _CACHE:
        _CACHE[key] = build(pv.off, pvt.shape[1], n_layers, stop_after_mixer)
    nc = _CACHE[key]
    f32 = lambda a: np.ascontiguousarray(np.asarray(a, np.float32))
    rC, rS, rRT = rope_tables()
    moec = np.zeros((128, 229), np.float32)
    moec[:, 193:229] = (np.arange(36, dtype=np.float32) * 128.0)[None, :]
    tp_, tt_ = np.meshgrid(np.arange(128), np.arange(128), indexing="ij")
    moec[:, 0:128] = (tp_ < tt_).astype(np.float32)
    moec[:, 128:192] = (np.arange(64, dtype=np.float32) * 128.0)[None, :]
    moec[:, 192] = np.arange(128, dtype=np.float32)
    selc = np.zeros((16, 2, 16, 64), np.float32)
    for r in range(16):
        selc[r, 0, r, :] = 1.0
        selc[r, 1, r, :] = -1.0
    selh = np.zeros((16, 2, 4), np.float32)
    for h in range(4):
        selh[4 + h, 0, h] = 1.0
        selh[12 + h, 1, h] = 1.0
    maskc = np.zeros((64, 2, 4, 64), np.float32)
    si, ti_ = np.meshgrid(np.arange(64), np.arange(64), indexing="ij")
    maskc[:, 0, :, :] = np.where(si <= ti_, 0.0, -30000.0)[:, None, :]
    maskc[:, 1, :, :] = np.where(si >= ti_, 0.0, -30000.0)[:, None, :]
    moe_wr = f32(np.concatenate([inp["moe_w_group"], inp["moe_w_expert"]], axis=2))
    br = np.concatenate([inp["moe_b_group"], inp["moe_b_expert"]], axis=1)
    moe_br = f32(np.repeat(br[:, None, :], 128, axis=1))
    shared = {
        "pv": pvt, "ident": np.eye(128, dtype=np.float32), "ada_w": f32(inp["ada_w"]),
        "rg_w_in": f32(inp["rg_w_in"]), "rg_gate_w": f32(inp["rg_gate_w"]), "rg_w_out": f32(inp["rg_w_out"]),
        "moe_wr": moe_wr, "moe_br": moe_br, "moec": moec,
        "mla_w_down": f32(inp["mla_w_down"][0]), "mla_w_uq": f32(inp["mla_w_uq"][0]), "mla_w_ukv": f32(inp["mla_w_ukv"][0]),
        "ml_w_in": f32(inp["ml_w_in"][0]), "ml_w_out": f32(inp["ml_w_out"][0]),
        "ml_gain": f32(np.repeat(np.asarray(inp["ml_out_norm"][0], np.float32)[None, :], 128, axis=0)),
        "ml_selc": selc, "ml_selh": selh, "ml_maskc": maskc,
        "mla_w_o": f32(inp["mla_w_o"][0]), "ropeC": rC, "ropeS": rS, "ropeRT": rRT,
        "moe_w_gate_up": f32(inp["moe_w_gate_up"]), "moe_w_down": f32(inp["moe_w_down"]),
    }
    in_maps = []
    for b in range(8):
        cc = np.zeros((128, 16), np.float32)
        cc[:, 0::2] = np.asarray(inp["c"][b], np.float32).reshape(8, 128).T
        cc[:, 1::2] = np.asarray(inp["c_ctx"], np.float32).reshape(8, 128).T
        m = dict(shared)
        m["x"] = f32(inp["x"][b])
        m["ctx"] = f32(inp["ctx"][b])
        m["cc"] = cc
        in_maps.append(m)
    return nc, in_maps


def kernel(**inputs):
    nc, in_maps = host_prep(inputs)
    res = run_bass_kernel_spmd(nc, in_maps, core_ids=list(range(8)))
    return np.stack([np.asarray(r["out"]) for r in res.results], axis=0).astype(np.float32)
```

```python
import numpy as np
from contextlib import ExitStack
import concourse.bass as bass
import concourse.mybir as mybir
from concourse.bass_utils import run_bass_kernel_spmd

F32 = mybir.dt.float32
BF16 = mybir.dt.bfloat16
I32 = mybir.dt.int32
AF = mybir.ActivationFunctionType
ALU = mybir.AluOpType
AX = mybir.AxisListType

NDMA_SEM = 32
D = 1024
NT = 2304
NCTX = 256
NLAT = 2048
TT = [(0, 256), (256, 768), (768, 1280), (1280, 1792), (1792, 2304)]
DEPTH = 4
BIG = 1.0e30


class Prog:
    ENGS = ("pe", "act", "dve", "pool", "sp")

    def __init__(self, nc, stack):
        self.nc = nc
        self.stack = stack
        self.sem = {e: stack.enter_context(nc.semaphore("s_" + e)) for e in self.ENGS}
        self.dsem = [stack.enter_context(nc.semaphore("d%d" % i)) for i in range(NDMA_SEM)]
        self.ops = {e: [] for e in self.ENGS}
        self.cnt = {e: 0 for e in self.ENGS}
        self.ndma = 0
        self.lw = {}
        self.rd = {}
        self.known = {e: {} for e in self.ENGS}
        self.final_waits = []
        self.pending = {e: [] for e in self.ENGS}

    def barrier(self):
        toks = [(e, self.cnt[e]) for e in self.ENGS if self.cnt[e] > 0]
        for j in range(NDMA_SEM):
            n = (self.ndma - j + NDMA_SEM - 1) // NDMA_SEM
            if n > 0:
                toks.append((("d", j), 16 * n))
        for e in self.ENGS:
            self.pending[e] = list(toks)

    def _semobj(self, sk):
        return self.sem[sk] if isinstance(sk, str) else self.dsem[sk[1]]

    def _need(self, eng, tok, waits):
        sk, v = tok
        if sk == "pe" and eng == "pe":
            return
        if self.known[eng].get(sk, 0) >= v:
            return
        if waits.get(sk, 0) < v:
            waits[sk] = v

    def op(self, eng, fn, reads=(), writes=(), dma=False):
        waits = {}
        for k in reads:
            t = self.lw.get(k)
            if t is not None:
                self._need(eng, t, waits)
        for k in writes:
            t = self.lw.get(k)
            if t is not None:
                self._need(eng, t, waits)
            for t in self.rd.get(k, ()):
                self._need(eng, t, waits)
        for tok in self.pending[eng]:
            self._need(eng, tok, waits)
        self.pending[eng] = []
        if dma:
            j = self.ndma % NDMA_SEM
            rnd = self.ndma // NDMA_SEM
            self.ndma += 1
            sk = ("d", j)
            if rnd > 0:
                self._need(eng, (sk, 16 * rnd), waits)
            tok = (sk, 16 * (rnd + 1))
        else:
            self.cnt[eng] += 1
            tok = (eng, self.cnt[eng])
        for sk, v in waits.items():
            self.known[eng][sk] = v
        self.ops[eng].append((list(waits.items()), fn, tok))
        for k in writes:
            self.lw[k] = tok
            self.rd[k] = []
        for k in reads:
            if k in writes:
                continue
            self.rd.setdefault(k, []).append(tok)
        return tok

    def finish(self, keys):
        waits = {}
        for k in keys:
            t = self.lw.get(k)
            if t is not None:
                self._need("sp", t, waits)
        self.final_waits = list(waits.items())

    def emit(self):
        nc = self.nc
        with nc.Block() as block:
            def mk(e):
                def body(engobj):
                    for waits, fn, tok in self.ops[e]:
                        for sk, v in waits:
                            engobj.wait_ge(self._semobj(sk), v)
                        ins = fn(engobj)
                        ins.then_inc(self._semobj(tok[0]), 1 if isinstance(tok[0], str) else 16)
                    if e == "sp":
                        for sk, v in self.final_waits:
                            engobj.wait_ge(self._semobj(sk), v)
                return body
            block.tensor(mk("pe"))
            block.scalar(mk("act"))
            block.vector(mk("dve"))
            block.gpsimd(mk("pool"))
            block.sync(mk("sp"))

    def sb(self, name, shape, dt):
        return self.stack.enter_context(self.nc.sbuf_tensor(name, shape, dt))

    def ps(self, name, shape, dt):
        return self.stack.enter_context(self.nc.psum_tensor(name, shape, dt))


class PV:
    def __init__(self):
        self.cols = []
        self.off = {}
        self.n = 0

    def add(self, name, v):
        v = np.asarray(v, np.float32).reshape(-1)
        assert v.size % 128 == 0
        a = v.reshape(-1, 128).T
        self.off[name] = self.n
        self.cols.append(a)
        self.n += a.shape[1]

    def table(self):
        return np.ascontiguousarray(np.concatenate(self.cols, axis=1))


def pv_layout(inp):
    pv = PV()
    for i in range(DEPTH):
        pv.add("nmix%d" % i, inp["norm_mix"][i])
        pv.add("nffn%d" % i, inp["norm_ffn"][i])
        ab = inp["ada_b"][i].reshape(48, 128)
        ab2 = np.repeat(ab[:, None, :], 2, axis=1)
        pv.add("adab%d" % i, ab2.reshape(-1))
    for j in range(2):
        for k in range(4):
            pv.add("rgcw%d_%d" % (j, k), inp["rg_conv_w"][j, k])
        pv.add("rgcb%d" % j, inp["rg_conv_b"][j])
        for dr in range(2):
            for g in range(2):
                pv.add("rggb%d_%d_%d" % (j, dr, g), inp["rg_gate_b"][j, dr, g])
            pv.add("rglam%d_%d" % (j, dr), inp["rg_lambda"][j, dr])
    pad = lambda v: np.concatenate([np.asarray(v, np.float32).reshape(-1), np.zeros(128 - np.asarray(v).size % 128 if np.asarray(v).size % 128 else 0, np.float32)])
    pv.add("mla_qn", inp["mla_q_norm"][0])
    pv.add("mla_kvn", inp["mla_kv_norm"][0])
    pv.add("mla_qkn0", pad(inp["mla_qk_norm"][0, 0]))
    pv.add("mla_qkn1", pad(inp["mla_qk_norm"][0, 1]))
    pv.add("ml_gb", pad(inp["ml_gate_b"][0].reshape(-1)))
    return pv


def rope_tables():
    L = NLAT
    rows = L // 64
    row = np.broadcast_to(np.arange(rows, dtype=np.float32)[:, None], (rows, 64)).reshape(L)
    col = np.broadcast_to(np.arange(64, dtype=np.float32)[None, :], (rows, 64)).reshape(L)
    inv_freq = (np.float32(10000.0) ** (-np.arange(0, 16, 2, dtype=np.float32) / np.float32(16))).astype(np.float32)
    ar = (row[:, None] * inv_freq).astype(np.float32)
    ac = (col[:, None] * inv_freq).astype(np.float32)
    C = np.ones((96, NT), np.float32)
    S = np.zeros((96, NT), np.float32)
    for base, ang in ((64, ar), (80, ac)):
        C[base:base + 8, NCTX:] = np.cos(ang).T
        C[base + 8:base + 16, NCTX:] = np.cos(ang).T
        S[base:base + 8, NCTX:] = np.sin(ang).T
        S[base + 8:base + 16, NCTX:] = np.sin(ang).T
    R = np.zeros((96, 96), np.float32)
    for base in (64, 80):
        for j in range(8):
            R[base + j, base + 8 + j] = -1.0
            R[base + 8 + j, base + j] = 1.0
    return C, S, np.ascontiguousarray(R.T)


def build(pvoff, npv, n_layers=DEPTH, stop_after_mixer=False):
    nc = bass.Bass("TRN2", target_bir_lowering=False)

    def din(name, shape, dt=F32):
        return nc.dram_tensor(name, list(shape), dt, kind="ExternalInput").ap()

    x_d = din("x", [NLAT, D])
    ctx_d = din("ctx", [NCTX, D])
    cc_d = din("cc", [128, 16])
    pv_d = din("pv", [128, npv])
    ident_d = din("ident", [128, 128])
    ada_w = din("ada_w", [DEPTH, D, 6 * D])
    rg_w_in = din("rg_w_in", [2, D, 2 * D])
    rg_gate_w = din("rg_gate_w", [2, 2, 2, 16, 64, 64])
    rg_w_out = din("rg_w_out", [2, D, D])
    mla_wdn = din("mla_w_down", [D, 416])
    mla_wuq = din("mla_w_uq", [256, 1536])
    mla_wukv = din("mla_w_ukv", [128, 2048])
    mla_wo = din("mla_w_o", [D, D])
    ropeC_d = din("ropeC", [96, NT])
    ropeS_d = din("ropeS", [96, NT])
    ropeRT_d = din("ropeRT", [96, 96])
    ml_win = din("ml_w_in", [D, 3088])
    ml_wout = din("ml_w_out", [D, D])
    ml_gain = din("ml_gain", [128, D])
    ml_selc = din("ml_selc", [16, 2, 16, 64])
    ml_selh = din("ml_selh", [16, 2, 4])
    ml_maskc = din("ml_maskc", [64, 2, 4, 64])
    hdir = nc.dram_tensor("hdir_scratch", [2, NT, D], BF16).ap()
    moec_d = din("moec", [128, 128 + 64 + 1 + 36])
    NSLOT = 12800
    xslots = nc.dram_tensor("xslots_scratch", [NSLOT, D], BF16).ap()
    yslots = nc.dram_tensor("yslots_scratch", [NSLOT, D], BF16).ap()
    moe_wr = din("moe_wr", [DEPTH, D, 72])
    moe_br = din("moe_br", [DEPTH, 128, 72])
    moe_wgu = din("moe_w_gate_up", [DEPTH, 64, D, 512])
    moe_wd = din("moe_w_down", [DEPTH, 64, 256, D])
    out_d = nc.dram_tensor("out", [NLAT, D], F32, kind="ExternalOutput").ap()
    octx_d = nc.dram_tensor("octx", [NCTX, D], F32, kind="ExternalOutput").ap()

    with ExitStack() as st:
        P = Prog(nc, st)
        xT = P.sb("xT", [128, 8, NT], F32)
        hT = P.sb("hT", [128, 8, NT], BF16)
        mT = P.sb("mT", [128, 8, NT], BF16)
        pvt = P.sb("pvt", [128, npv], F32)
        ident = P.sb("ident_sb", [128, 128], F32)
        onesm = P.sb("onesm", [128, 128], F32)
        cst = P.sb("cst", [128, 4], F32)
        cct = P.sb("cct", [128, 16], F32)
        modt = P.sb("modt", [128, 48, 2], F32)
        mA = P.sb("mA", [128, 2, 8, 2], F32)
        tmp = [P.sb("tmp%d" % i, [128, 512], F32) for i in range(6)]
        rstd = P.sb("rstd", [128, 512], F32)
        SCRW = 11400
        scr = P.sb("scr", [128, SCRW], F32)
        mTflat = mT[:].rearrange("p c t -> p (c t)")
        hTflat = hT[:].rearrange("p c t -> p (c t)")
        PSB = [P.ps("psb%d" % i, [128, 512], F32) for i in range(8)]

        class Arena:
            def __init__(self, kind):
                self.kind = kind
                self.off = 0

            def reset(self):
                self.off = 0

            def alloc(self, shape, dt):
                n = 1
                for d_ in shape[1:]:
                    n *= d_
                nf32 = n if dt in (F32, I32) else (n + 1) // 2
                o = self.off
                self.off += nf32
                if self.kind == "A":
                    assert self.off <= SCRW, ("arena A overflow", self.off)
                    v = scr[0:shape[0], o:o + nf32]
                    if dt != F32:
                        v = v.bitcast(dt)[:, 0:n]
                else:
                    assert self.off * 2 <= 8 * NT, ("arena B/C overflow", self.off)
                    flat = mTflat if self.kind == "B" else hTflat
                    v = flat[0:shape[0], 2 * o:2 * o + 2 * nf32]
                    if dt == F32:
                        v = v.bitcast(F32)
                    else:
                        v = v[:, 0:n]
                if len(shape) == 3:
                    v = v.rearrange("p (a b) -> p a b", a=shape[1])
                elif len(shape) == 4:
                    v = v.rearrange("p (a b c) -> p a b c", a=shape[1], b=shape[2])
                return v

        arA, arB, arC = Arena("A"), Arena("B"), Arena("C")
        _regs = {}

        def getreg(e, v):
            if v not in _regs:
                _regs[v] = e.to_reg(v)
            return _regs[v]

        def phase():
            P.barrier()
            arA.reset()
            arB.reset()
            arC.reset()

        def pvc(name, k=0, n=1):
            o = pvoff[name] + k
            return pvt[:, o:o + n]

        P.op("sp", lambda e: e.dma_start(out=pvt[:], in_=pv_d), writes=["pvt"], dma=True)
        P.op("sp", lambda e: e.dma_start(out=ident[:], in_=ident_d), writes=["ident"], dma=True)
        P.op("sp", lambda e: e.dma_start(out=cct[:], in_=cc_d), writes=["cct"], dma=True)
        P.op("pool", lambda e: e.memset(onesm[:], 1.0 / 1024.0), writes=["onesm"])
        P.op("pool", lambda e: e.memset(cst[:, 0:1], 1e-6), writes=["cst0"])
        P.op("pool", lambda e: e.memset(cst[:, 1:2], 1.0), writes=["cst1"])
        P.op("act", lambda e: e.activation(out=cct[:], in_=cct[:], func=AF.Silu), reads=["cct"], writes=["cct"])

        stage = arA.alloc([128, 2, 1024], F32)
        for ti in range(18):
            src = ctx_d[ti * 128:(ti + 1) * 128, :] if ti < 2 else x_d[(ti - 2) * 128:(ti - 1) * 128, :]
            sbuf = ti % 2
            P.op("sp", lambda e, src=src, sbuf=sbuf: e.dma_start(out=stage[:, sbuf, :], in_=src), writes=[("stage", sbuf)], dma=True)
            jt = [j for j, (a, b) in enumerate(TT) if a <= ti * 128 < b][0]
            for half in range(2):
                pb = PSB[half]

                def trfn(e, pb=pb, half=half, sbuf=sbuf):
                    ins = None
                    for q in range(4):
                        c = half * 4 + q
                        ins = e.transpose(pb[:, q * 128:(q + 1) * 128], stage[:, sbuf, c * 128:(c + 1) * 128], ident[:])
                    return ins
                P.op("pe", trfn, reads=[("stage", sbuf), "ident"], writes=[("psb", half)])
                P.op("dve" if half == 0 else "act",
                     lambda e, pb=pb, half=half, ti=ti: (e.tensor_copy if half == 0 else e.copy)(
                         out=xT[:, half * 4:half * 4 + 4, ti * 128:(ti + 1) * 128], in_=pb[:].rearrange("p (q t) -> p q t", q=4)),
                     reads=[("psb", half)], writes=[("x", c, jt) for c in range(half * 4, half * 4 + 4)])

        def compute_mod(i):
            phase()
            adaw = arA.alloc([128, 5, 8, 256], F32)
            pm = PSB[7]
            for piece in range(24):
                bsel = piece % 5
                P.op("sp", lambda e, piece=piece, bsel=bsel: e.dma_start(
                    out=adaw[:, bsel, :, :], in_=ada_w[i, :, piece * 256:(piece + 1) * 256].rearrange("(k p) n -> p k n", p=128)),
                    writes=[("adaw", bsel)], dma=True)

                def mmfn(e, bsel=bsel, piece=piece):
                    ins = None
                    for sub in range(2):
                        oc = piece * 2 + sub
                        for k in range(8):
                            ins = e.matmul(pm[:, oc * 2:oc * 2 + 2], lhsT=adaw[:, bsel, k, sub * 128:(sub + 1) * 128],
                                           rhs=cct[:, k * 2:k * 2 + 2], start=(k == 0), stop=(k == 7))
                    return ins
                P.op("pe", mmfn, reads=[("adaw", bsel), "cct"], writes=[("psb", 7)])
            ab = pvt[:, pvoff["adab%d" % i]:pvoff["adab%d" % i] + 96]
            P.op("dve", lambda e: e.tensor_tensor(out=modt[:].rearrange("p a b -> p (a b)"), in0=pm[:, 0:96], in1=ab, op=ALU.add),
                 reads=[("psb", 7), "pvt"], writes=["modt"])
            for w, (nm, m) in enumerate((("nmix%d" % i, 1), ("nffn%d" % i, 4))):
                for j2 in range(2):
                    P.op("dve", lambda e, w=w, nm=nm, m=m, j2=j2: e.scalar_tensor_tensor(
                        out=mA[:, w, :, j2], in0=modt[:, m * 8:m * 8 + 8, j2], scalar=1.0, in1=pvc(nm, 0, 8),
                        op0=ALU.add, op1=ALU.mult),
                        reads=["modt", "pvt"], writes=["mA"])

        def modcol(m, c, j):
            jj = 1 if j == 0 else 0
            return modt[:, m * 8 + c, jj:jj + 1]

        def norm_mod(w, tiles, extra=None):
            mshift = 0 if w == 0 else 3
            for j in tiles:
                a, b = TT[j]
                n = b - a
                jj = 1 if j == 0 else 0
                pst = PSB[6]
                for c in range(8):
                    tb = tmp[c % 2]
                    P.op("act", lambda e, tb=tb, c=c, a=a, b=b, n=n: e.activation(out=tb[:, :n], in_=xT[:, c, a:b], func=AF.Square),
                         reads=[("x", c, j)], writes=[("tmp", c % 2)])
                    P.op("pe", lambda e, tb=tb, c=c, n=n: e.matmul(pst[:, :n], lhsT=onesm[:], rhs=tb[:, :n], start=(c == 0), stop=(c == 7)),
                         reads=[("tmp", c % 2), "onesm"], writes=[("psb", 6)])
                P.op("act", lambda e, n=n: e.activation(out=rstd[:, :n], in_=pst[:, :n], func=AF.Ln, bias=cst[:, 0:1], scale=1.0),
                     reads=[("psb", 6), "cst0"], writes=["rstd"])
                P.op("act", lambda e, n=n: e.activation(out=rstd[:, :n], in_=rstd[:, :n], func=AF.Exp, scale=-0.5), reads=["rstd"], writes=["rstd"])
                for c in range(8):
                    tb = tmp[2 + c % 2]
                    P.op("dve", lambda e, tb=tb, c=c, a=a, b=b, n=n: e.tensor_tensor(out=tb[:, :n], in0=xT[:, c, a:b], in1=rstd[:, :n], op=ALU.mult),
                         reads=[("x", c, j), "rstd"], writes=[("tmp", 2 + c % 2)])
                    P.op("act", lambda e, tb=tb, c=c, a=a, b=b, n=n, jj=jj: e.activation(
                        out=hT[:, c, a:b], in_=tb[:, :n], func=AF.Identity,
                        bias=modt[:, mshift * 8 + c, jj:jj + 1], scale=mA[:, w, c, jj:jj + 1]),
                        reads=[("tmp", 2 + c % 2), "modt", "mA"], writes=[("h", c, j)])
                    if extra is not None:
                        extra(j, c, tb)

        def rglru(jl, tiles_all):
            phase()
            k = "rg%d" % jl
            win = arA.alloc([128, 2, 8, 256], BF16)
            wout = arA.alloc([128, 2, 8, 128], BF16)
            gw = arA.alloc([128, 4, 128], BF16)
            gwf = arA.alloc([128, 4, 128], F32)
            clam = arA.alloc([128, 2, 2, 8], F32)
            uh = arA.alloc([128, NT], F32)
            ucv = arA.alloc([128, NT], F32)
            ub = arA.alloc([128, NT], BF16)
            hb = arA.alloc([128, 2, 512], F32)
            for dr in range(2):
                lam = pvc("rglam%d_%d" % (jl, dr), 0, 8)
                P.op("act", lambda e, dr=dr, lam=lam: e.activation(out=clam[:, dr, 0, :], in_=lam, func=AF.Exp, scale=-1.0),
                     reads=["pvt"], writes=[(k, "clam")])
                P.op("act", lambda e, dr=dr: e.activation(out=clam[:, dr, 0, :], in_=clam[:, dr, 0, :], func=AF.Ln, bias=cst[:, 1:2], scale=1.0),
                     reads=[(k, "clam"), "cst1"], writes=[(k, "clam")])
                P.op("dve", lambda e, dr=dr: e.tensor_scalar(out=clam[:, dr, 1, :], in0=clam[:, dr, 0, :], scalar1=-16.0, scalar2=None, op0=ALU.mult),
                     reads=[(k, "clam")], writes=[(k, "clam")])
                P.op("dve", lambda e, dr=dr: e.tensor_scalar(out=clam[:, dr, 0, :], in0=clam[:, dr, 0, :], scalar1=-8.0, scalar2=None, op0=ALU.mult),
                     reads=[(k, "clam")], writes=[(k, "clam")])
            for cc in range(8):
                wb_ = cc % 2
                for part in range(2):
                    P.op("pool", lambda e, part=part, wb_=wb_, cc=cc: e.dma_start(
                        out=win[:, wb_, :, part * 128:(part + 1) * 128],
                        in_=rg_w_in[jl, :, part * 1024 + cc * 128: part * 1024 + (cc + 1) * 128].rearrange("(k p) n -> p k n", p=128)),
                        writes=[(k, "win", wb_, part)], dma=True)
                subk = [(k, "gwf", q) for q in range(8)]
                P.op("pool", lambda e: e.memset(gwf[:, :, :], 0.0), writes=subk)
                for dr in range(2):
                    for g in range(2):
                        for blk in range(2):
                            P.op("sp", lambda e, dr=dr, g=g, blk=blk, cc=cc: e.dma_start(
                                out=gwf[blk * 64:(blk + 1) * 64, dr * 2 + g, blk * 64:(blk + 1) * 64],
                                in_=rg_gate_w[jl, dr, g, cc * 2 + blk, :, :]),
                                writes=[(k, "gwf", dr * 4 + g * 2 + blk)], dma=True)
                P.op("pool", lambda e: e.tensor_copy(out=gw[:, :, :], in_=gwf[:, :, :]), reads=subk, writes=[(k, "gw")])
                for j in tiles_all:
                    a, b = TT[j]
                    n = b - a
                    pg, pu = PSB[0 + j % 2], PSB[2 + j % 2]
                    for part, pp, pk in ((0, pg, ("psb", 0 + j % 2)), (1, pu, ("psb", 2 + j % 2))):
                        def mmfn(e, part=part, pp=pp, a=a, b=b, n=n, wb_=wb_):
                            ins = None
                            for kk in range(8):
                                ins = e.matmul(pp[:, :n], lhsT=win[:, wb_, kk, part * 128:(part + 1) * 128], rhs=hT[:, kk, a:b],
                                               start=(kk == 0), stop=(kk == 7))
                            return ins
                        P.op("pe", mmfn, reads=[(k, "win", wb_, part)] + [("h", kk, j) for kk in range(8)], writes=[pk])
                    t0, t1 = tmp[0], tmp[1]
                    pgk = ("psb", 0 + j % 2)
                    P.op("act", lambda e, pg=pg, n=n: e.activation(out=t0[:, :n], in_=pg[:, :n], func=AF.Square),
                         reads=[pgk], writes=[("tmp", 0)])
                    P.op("dve", lambda e, n=n: e.tensor_scalar(out=t0[:, :n], in0=t0[:, :n], scalar1=0.044715, scalar2=1.0, op0=ALU.mult, op1=ALU.add),
                         reads=[("tmp", 0)], writes=[("tmp", 0)])
                    P.op("dve", lambda e, pg=pg, n=n: e.tensor_tensor(out=t0[:, :n], in0=t0[:, :n], in1=pg[:, :n], op=ALU.mult),
                         reads=[("tmp", 0), pgk], writes=[("tmp", 0)])
                    P.op("act", lambda e, n=n: e.activation(out=t1[:, :n], in_=t0[:, :n], func=AF.Sigmoid, scale=1.5957691216),
                         reads=[("tmp", 0)], writes=[("tmp", 1)])
                    P.op("dve", lambda e, pg=pg, n=n, a=a, b=b, cc=cc: e.tensor_tensor(out=mT[:, cc, a:b], in0=t1[:, :n], in1=pg[:, :n], op=ALU.mult),
                         reads=[("tmp", 1), pgk], writes=[("m", cc, j)])
                    P.op("act", lambda e, pu=pu, n=n, a=a, b=b: e.copy(out=uh[:, a:b], in_=pu[:, :n]),
                         reads=[("psb", 2 + j % 2)], writes=[(k, "uh")])
                P.op("act", lambda e, cc=cc: e.activation(out=ucv[:, :], in_=uh[:, :], func=AF.Identity,
                                                           bias=pvc("rgcb%d" % jl, cc), scale=pvc("rgcw%d_2" % jl, cc)),
                     reads=[(k, "uh"), "pvt"], writes=[(k, "ucv")])
                for (s0, s1) in ((0, NCTX), (NCTX, NT)):
                    for tap, off in ((0, -2), (1, -1), (3, 1)):
                        lo = max(s0, s0 - off)
                        hi = min(s1, s1 - off)
                        P.op("dve", lambda e, lo=lo, hi=hi, off=off, tap=tap, cc=cc: e.scalar_tensor_tensor(
                            out=ucv[:, lo:hi], in0=uh[:, lo + off:hi + off], scalar=pvc("rgcw%d_%d" % (jl, tap), cc),
                            in1=ucv[:, lo:hi], op0=ALU.mult, op1=ALU.add),
                            reads=[(k, "uh"), (k, "ucv"), "pvt"], writes=[(k, "ucv")])
                P.op("pool", lambda e: e.tensor_copy(out=ub[:, :], in_=ucv[:, :]), reads=[(k, "ucv")], writes=[(k, "ub")])
                for dr in range(2):
                    order = tiles_all if dr == 0 else [tiles_all[0]] + list(reversed(tiles_all[1:]))
                    prev = None
                    groups = [order[g:g + 2] for g in range(0, len(order), 2)]
                    oi = 0
                    for grp in groups:
                        info = []
                        for gi, j in enumerate(grp):
                            a, b = TT[j]
                            n = b - a
                            tr, ta2, ti_ = tmp[3 * gi + 0], tmp[3 * gi + 1], tmp[3 * gi + 2]
                            kr, k2, ki_ = ("tmp", 3 * gi + 0), ("tmp", 3 * gi + 1), ("tmp", 3 * gi + 2)
                            pr = PSB[4 + gi]
                            pi = PSB[6 + gi]
                            prk = ("psb", 4 + gi)
                            pik = ("psb", 6 + gi)
                            P.op("pe", lambda e, pr=pr, dr=dr, a=a, b=b, n=n: e.matmul(pr[:, :n], lhsT=gw[:, dr * 2 + 0, :], rhs=ub[:, a:b], start=True, stop=True),
                                 reads=[(k, "gw"), (k, "ub")], writes=[prk])
                            P.op("pe", lambda e, pi=pi, dr=dr, a=a, b=b, n=n: e.matmul(pi[:, :n], lhsT=gw[:, dr * 2 + 1, :], rhs=ub[:, a:b], start=True, stop=True),
                                 reads=[(k, "gw"), (k, "ub")], writes=[pik])
                            P.op("act", lambda e, pr=pr, n=n, dr=dr, cc=cc, tr=tr: e.activation(out=tr[:, :n], in_=pr[:, :n], func=AF.Sigmoid, bias=pvc("rggb%d_%d_0" % (jl, dr), cc), scale=1.0),
                                 reads=[prk, "pvt"], writes=[kr])
                            P.op("act", lambda e, pi=pi, n=n, dr=dr, cc=cc, ti_=ti_: e.activation(out=ti_[:, :n], in_=pi[:, :n], func=AF.Sigmoid, bias=pvc("rggb%d_%d_1" % (jl, dr), cc), scale=1.0),
                                 reads=[pik, "pvt"], writes=[ki_])
                            info.append((j, a, b, n, tr, ta2, ti_, kr, k2, ki_))
                        for (j, a, b, n, tr, ta2, ti_, kr, k2, ki_) in info:
                            P.op("act", lambda e, n=n, dr=dr, cc=cc, tr=tr: e.activation(out=tr[:, :n], in_=tr[:, :n], func=AF.Exp, scale=clam[:, dr, 0, cc:cc + 1]),
                                 reads=[kr, (k, "clam")], writes=[kr])
                            P.op("act", lambda e, n=n, tr=tr, ta2=ta2: e.activation(out=ta2[:, :n], in_=tr[:, :n], func=AF.Square), reads=[kr], writes=[k2])
                            P.op("act", lambda e, n=n, ta2=ta2: e.activation(out=ta2[:, :n], in_=ta2[:, :n], func=AF.Ln, bias=cst[:, 1:2], scale=-1.0), reads=[k2, "cst1"], writes=[k2])
                            P.op("act", lambda e, n=n, ta2=ta2: e.activation(out=ta2[:, :n], in_=ta2[:, :n], func=AF.Exp, scale=0.5), reads=[k2], writes=[k2])
                        for (j, a, b, n, tr, ta2, ti_, kr, k2, ki_) in info:
                            P.op("dve", lambda e, n=n, a=a, b=b, ti_=ti_: e.tensor_tensor(out=ti_[:, :n], in0=ti_[:, :n], in1=ucv[:, a:b], op=ALU.mult),
                                 reads=[ki_, (k, "ucv")], writes=[ki_])
                            P.op("dve", lambda e, n=n, ti_=ti_, ta2=ta2: e.tensor_tensor(out=ti_[:, :n], in0=ti_[:, :n], in1=ta2[:, :n], op=ALU.mult),
                                 reads=[ki_, k2], writes=[ki_])
                            if dr == 0:
                                init = 0.0 if prev is None else uh[:, a - 1:a]
                                P.op("dve", lambda e, n=n, a=a, b=b, init=init, tr=tr, ti_=ti_: e.tensor_tensor_scan(out=uh[:, a:b], data0=tr[:, :n], data1=ti_[:, :n], initial=init, op0=ALU.mult, op1=ALU.add),
                                     reads=[kr, ki_, (k, "uh"), (k, "ucv")], writes=[(k, "uh")])
                            else:
                                hbb = oi % 2
                                init = 0.0 if prev is None else hb[:, 1 - hbb, 0:1]
                                P.op("dve", lambda e, n=n, hbb=hbb, init=init, tr=tr, ti_=ti_: e.tensor_tensor_scan(out=hb[:, hbb, 0:n][:, ::-1], data0=tr[:, 0:n][:, ::-1], data1=ti_[:, 0:n][:, ::-1], initial=init, op0=ALU.mult, op1=ALU.add),
                                     reads=[kr, ki_, (k, "hb", 1 - hbb)], writes=[(k, "hb", hbb)])
                                P.op("dve", lambda e, n=n, a=a, b=b, hbb=hbb, tr=tr: e.tensor_tensor(out=tr[:, :n], in0=hb[:, hbb, 0:n], in1=uh[:, a:b], op=ALU.add),
                                     reads=[(k, "hb", hbb), (k, "uh")], writes=[kr])
                                P.op("dve", lambda e, n=n, a=a, b=b, cc=cc, tr=tr: e.tensor_tensor(out=mT[:, cc, a:b], in0=tr[:, :n], in1=mT[:, cc, a:b], op=ALU.mult),
                                     reads=[kr, ("m", cc, j)], writes=[("m", cc, j)])
                            prev = j
                            oi += 1
            for oc in range(8):
                ob = oc % 2
                P.op("pool", lambda e, oc=oc, ob=ob: e.dma_start(out=wout[:, ob, :, :], in_=rg_w_out[jl, :, oc * 128:(oc + 1) * 128].rearrange("(k p) n -> p k n", p=128)),
                     writes=[(k, "wout", ob)], dma=True)
                for j in tiles_all:
                    a, b = TT[j]
                    n = b - a
                    py = PSB[j % 4]

                    def mmfn(e, py=py, ob=ob, a=a, b=b, n=n):
                        ins = None
                        for cc in range(8):
                            ins = e.matmul(py[:, :n], lhsT=wout[:, ob, cc, :], rhs=mT[:, cc, a:b], start=(cc == 0), stop=(cc == 7))
                        return ins
                    P.op("pe", mmfn, reads=[(k, "wout", ob)] + [("m", cc, j) for cc in range(8)], writes=[("psb", j % 4)])
                    P.op("dve", lambda e, py=py, oc=oc, a=a, b=b, n=n, j=j: e.scalar_tensor_tensor(
                        out=xT[:, oc, a:b], in0=py[:, :n], scalar=modcol(2, oc, j), in1=xT[:, oc, a:b], op0=ALU.mult, op1=ALU.add),
                        reads=[("psb", j % 4), "modt", ("x", oc, j)], writes=[("x", oc, j)])

        def mla(tiles_all):
            phase()
            k = "mla"
            SC = 96 ** -0.5
            cqn = arA.alloc([128, 2, NT], BF16)
            ckvn = arA.alloc([128, NT], BF16)
            krope = arA.alloc([96, NT], F32)
            wuq = arA.alloc([128, 2, 1536], BF16)
            wukv = arA.alloc([128, 2048], BF16)
            ones96 = arA.alloc([96, 96], BF16)
            RTf = arA.alloc([96, 96], F32)
            RT = arA.alloc([96, 96], BF16)
            ones1r = arA.alloc([65, 64], F32)
            rr = arA.alloc([65, 512], F32)
            onesb = arA.alloc([128, 64], BF16)
            wdn = arB.alloc([128, 8, 416], BF16)
            wo = arB.alloc([64, 2, 1024], BF16)
            tC = arB.alloc([96, NT], F32)
            tS = arB.alloc([96, NT], F32)
            P.op("pool", lambda e: e.dma_start(out=wdn, in_=mla_wdn.rearrange("(k p) n -> p k n", p=128)), writes=[(k, "wdn")], dma=True)
            P.op("pool", lambda e: e.dma_start(out=wuq, in_=mla_wuq.rearrange("(k p) n -> p k n", p=128)), writes=[(k, "wuq")], dma=True)
            P.op("pool", lambda e: e.dma_start(out=wukv, in_=mla_wukv), writes=[(k, "wukv")], dma=True)
            P.op("sp", lambda e: e.dma_start(out=tC, in_=ropeC_d), writes=[(k, "tC")], dma=True)
            P.op("sp", lambda e: e.dma_start(out=tS, in_=ropeS_d), writes=[(k, "tS")], dma=True)
            P.op("sp", lambda e: e.dma_start(out=RTf, in_=ropeRT_d), writes=[(k, "RTf")], dma=True)
            P.op("pool", lambda e: e.tensor_copy(out=RT, in_=RTf), reads=[(k, "RTf")], writes=[(k, "RT")])
            P.op("pool", lambda e: e.memset(ones1r, 1.0), writes=[(k, "ones1r")])
            P.op("pool", lambda e: e.memset(ones96, 1.0 / 96.0), writes=[(k, "ones96")])
            P.op("pool", lambda e: e.memset(onesb, 1.0), writes=[(k, "onesb")])
            for j in tiles_all:
                a, b = TT[j]
                n = b - a
                specs = ((0, 0, 128), (1, 128, 128), (2, 256, 128), (3, 320, 96))
                for bi, c0, m in specs:
                    def mmfn(e, bi=bi, c0=c0, m=m, a=a, b=b, n=n):
                        ins = None
                        for kk in range(8):
                            ins = e.matmul(PSB[bi][0:m, :n], lhsT=wdn[:, kk, c0:c0 + m], rhs=hT[:, kk, a:b], start=(kk == 0), stop=(kk == 7))
                        return ins
                    P.op("pe", mmfn, reads=[(k, "wdn")] + [("h", kk, j) for kk in range(8)], writes=[("psb", bi)])
                P.op("act", lambda e, a=a, b=b, n=n: e.copy(out=krope[64:96, a:b], in_=PSB[3][64:96, :n]), reads=[("psb", 3)], writes=[(k, "krope", j)])
                for c in range(2):
                    P.op("act", lambda e, c=c, n=n: e.activation(out=tmp[c][:, :n], in_=PSB[c][:, :n], func=AF.Square), reads=[("psb", c)], writes=[("tmp", c)])
                    P.op("pe", lambda e, c=c, n=n: e.matmul(PSB[4][:, :n], lhsT=onesm[:], rhs=tmp[c][:, :n], start=(c == 0), stop=(c == 1)),
                         reads=[("tmp", c), "onesm"], writes=[("psb", 4)])
                P.op("act", lambda e, n=n: e.activation(out=rstd[:, :n], in_=PSB[4][:, :n], func=AF.Ln, bias=cst[:, 0:1], scale=4.0), reads=[("psb", 4), "cst0"], writes=["rstd"])
                P.op("act", lambda e, n=n: e.activation(out=rstd[:, :n], in_=rstd[:, :n], func=AF.Exp, scale=-0.5), reads=["rstd"], writes=["rstd"])
                for c in range(2):
                    P.op("dve", lambda e, c=c, n=n: e.tensor_tensor(out=tmp[2 + c][:, :n], in0=PSB[c][:, :n], in1=rstd[:, :n], op=ALU.mult), reads=[("psb", c), "rstd"], writes=[("tmp", 2 + c)])
                    P.op("act", lambda e, c=c, a=a, b=b, n=n: e.activation(out=cqn[:, c, a:b], in_=tmp[2 + c][:, :n], func=AF.Identity, scale=pvc("mla_qn", c)), reads=[("tmp", 2 + c), "pvt"], writes=[(k, "cqn", j)])
                P.op("act", lambda e, n=n: e.activation(out=tmp[4][:, :n], in_=PSB[2][:, :n], func=AF.Square), reads=[("psb", 2)], writes=[("tmp", 4)])
                P.op("pe", lambda e, n=n: e.matmul(PSB[5][:, :n], lhsT=onesm[:], rhs=tmp[4][:, :n], start=True, stop=True), reads=[("tmp", 4), "onesm"], writes=[("psb", 5)])
                P.op("act", lambda e, n=n: e.activation(out=tmp[5][:, :n], in_=PSB[5][:, :n], func=AF.Ln, bias=cst[:, 0:1], scale=8.0), reads=[("psb", 5), "cst0"], writes=[("tmp", 5)])
                P.op("act", lambda e, n=n: e.activation(out=tmp[5][:, :n], in_=tmp[5][:, :n], func=AF.Exp, scale=-0.5), reads=[("tmp", 5)], writes=[("tmp", 5)])
                P.op("dve", lambda e, n=n: e.tensor_tensor(out=tmp[4][:, :n], in0=PSB[2][:, :n], in1=tmp[5][:, :n], op=ALU.mult), reads=[("psb", 2), ("tmp", 5)], writes=[("tmp", 4)])
                P.op("act", lambda e, a=a, b=b, n=n: e.activation(out=ckvn[:, a:b], in_=tmp[4][:, :n], func=AF.Identity, scale=pvc("mla_kvn", 0)), reads=[("tmp", 4), "pvt"], writes=[(k, "ckvn", j)])
            P.barrier()
            qTs = [arC.alloc([96, NT], BF16) for _ in range(2)]
            kTs = [arC.alloc([96, NT], BF16) for _ in range(2)]
            vhs = [arC.alloc([128, 18, 65], BF16) for _ in range(2)]
            for vv in range(2):
                P.op("pool", lambda e, vv=vv: e.memset(vhs[vv], 1.0), writes=[(k, "vh", vv)])
            PT = arC.alloc([128, 2, 512], BF16)
            xf = arC.alloc([96, 512], F32)
            xn = arC.alloc([96, 512], BF16)
            rs = arC.alloc([96, 512], F32)
            t1 = arC.alloc([96, 512], F32)
            t1b = arC.alloc([96, 512], BF16)
            t2 = arC.alloc([96, 512], F32)
            rden = tmp[0][0:64, :]
            oT = tmp[1][0:64, :].bitcast(BF16)[:, 0:512]

            def proj_gen(h):
                hp = h % 2
                qT, kT, vh = qTs[hp], kTs[hp], vhs[hp]
                P.op("pool", lambda e: e.dma_start(out=wo[:, hp, :], in_=mla_wo[h * 64:(h + 1) * 64, :]), writes=[(k, "wo", hp)], dma=True)
                for j in tiles_all:
                    a, b = TT[j]
                    n = b - a

                    def mmq(e, a=a, b=b, n=n):
                        ins = None
                        for kk in range(2):
                            ins = e.matmul(PSB[0][0:96, :n], lhsT=wuq[:, kk, h * 96:(h + 1) * 96], rhs=cqn[:, kk, a:b], start=(kk == 0), stop=(kk == 1))
                        return ins
                    P.op("pe", mmq, reads=[(k, "wuq"), (k, "cqn", j)], writes=[("psb", 0)])
                    P.op("pe", lambda e, a=a, b=b, n=n: e.matmul(PSB[1][0:64, :n], lhsT=wukv[:, h * 128:h * 128 + 64], rhs=ckvn[:, a:b], start=True, stop=True),
                         reads=[(k, "wukv"), (k, "ckvn", j)], writes=[("psb", 1)])
                    for which in range(2):
                        gname = "mla_qkn%d" % which
                        if which == 0:
                            P.op("act", lambda e, n=n: e.copy(out=xf[:, :n], in_=PSB[0][0:96, :n]), reads=[("psb", 0)], writes=[(k, "xf")])
                        else:
                            P.op("act", lambda e, n=n: e.copy(out=xf[0:64, :n], in_=PSB[1][0:64, :n]), reads=[("psb", 1)], writes=[(k, "xf")])
                            P.op("pool", lambda e, a=a, b=b, n=n: e.tensor_copy(out=xf[64:96, :n], in_=krope[64:96, a:b]), reads=[(k, "krope", j), (k, "xf")], writes=[(k, "xf")])
                        P.op("act", lambda e, n=n: e.activation(out=t1b[:, :n], in_=xf[:, :n], func=AF.Square), reads=[(k, "xf")], writes=[(k, "t1b")])
                        P.op("pe", lambda e, n=n: e.matmul(PSB[2][0:96, :n], lhsT=ones96, rhs=t1b[:, :n], start=True, stop=True), reads=[(k, "t1b"), (k, "ones96")], writes=[("psb", 2)])
                        yield
                        P.op("act", lambda e, n=n: e.activation(out=rs[:, :n], in_=PSB[2][0:96, :n], func=AF.Ln, bias=cst[0:96, 0:1], scale=1.0), reads=[("psb", 2), "cst0"], writes=[(k, "rs")])
                        P.op("act", lambda e, n=n: e.activation(out=rs[:, :n], in_=rs[:, :n], func=AF.Exp, scale=-0.5), reads=[(k, "rs")], writes=[(k, "rs")])
                        P.op("dve", lambda e, n=n, gname=gname: e.scalar_tensor_tensor(out=xn[:, :n], in0=xf[:, :n], scalar=pvt[0:96, pvoff[gname]:pvoff[gname] + 1], in1=rs[:, :n], op0=ALU.mult, op1=ALU.mult),
                             reads=[(k, "xf"), (k, "rs"), "pvt"], writes=[(k, "xn")])
                        P.op("pe", lambda e, n=n: e.matmul(PSB[3][0:96, :n], lhsT=RT, rhs=xn[:, :n], start=True, stop=True), reads=[(k, "xn"), (k, "RT")], writes=[("psb", 3)])
                        yield
                        P.op("pool", lambda e, a=a, b=b, n=n: e.tensor_tensor(out=t1[:, :n], in0=xn[:, :n], in1=tC[:, a:b], op=ALU.mult), reads=[(k, "xn"), (k, "tC")], writes=[(k, "t1")])
                        P.op("dve", lambda e, a=a, b=b, n=n: e.tensor_tensor(out=t2[:, :n], in0=PSB[3][0:96, :n], in1=tS[:, a:b], op=ALU.mult), reads=[("psb", 3), (k, "tS")], writes=[(k, "t2")])
                        dst = qT if which == 0 else kT
                        P.op("pool", lambda e, a=a, b=b, n=n, dst=dst: e.tensor_tensor(out=dst[:, a:b], in0=t1[:, :n], in1=t2[:, :n], op=ALU.add),
                             reads=[(k, "t1"), (k, "t2")], writes=[(k, "qk", hp, which, j)])
                        yield
                for g3 in range(3):
                    kts = list(range(g3 * 8, min(18, g3 * 8 + 8)))

                    def mmv(e, kts=kts):
                        ins = None
                        for qi, kt in enumerate(kts):
                            ins = e.matmul(PSB[3][:, qi * 64:(qi + 1) * 64], lhsT=ckvn[:, kt * 128:(kt + 1) * 128], rhs=wukv[:, h * 128 + 64:h * 128 + 128], start=True, stop=True)
                        return ins
                    P.op("pe", mmv, reads=[(k, "wukv")] + [(k, "ckvn", j) for j in tiles_all], writes=[("psb", 3)])
                    P.op("act", lambda e, kts=kts: e.copy(out=vh[:, kts[0]:kts[-1] + 1, 0:64], in_=PSB[3][:, 0:64 * len(kts)].rearrange("p (a b) -> p a b", b=64)),
                         reads=[("psb", 3)], writes=[(k, "vh", hp)])
                    yield

            def attn(h, gen):
                hp = h % 2
                qT, kT, vh = qTs[hp], kTs[hp], vhs[hp]

                def pump(cnt=1):
                    if gen is None:
                        return
                    for _ in range(cnt):
                        try:
                            next(gen)
                        except StopIteration:
                            return
                for j in tiles_all:
                    a, b = TT[j]
                    n = b - a
                    keyt = [0, 1] if j == 0 else list(range(18))

                    def emitS(ki, kt, a=a, b=b, n=n, j=j):
                        pb_ = ki % 2
                        jk = [jj for jj, (aa, bb) in enumerate(TT) if aa <= kt * 128 < bb][0]
                        P.op("pe", lambda e, kt=kt, pb_=pb_, n=n, a=a, b=b: e.matmul(PSB[4 + pb_][:, :n], lhsT=kT[:, kt * 128:(kt + 1) * 128], rhs=qT[:, a:b], start=True, stop=True),
                             reads=[(k, "qk", hp, 1, jk), (k, "qk", hp, 0, j)], writes=[("psb", 4 + pb_)])
                    emitS(0, keyt[0])
                    for ki, kt in enumerate(keyt):
                        pb_ = ki % 2
                        if ki + 1 < len(keyt):
                            emitS(ki + 1, keyt[ki + 1])
                        P.op("act", lambda e, n=n, pb_=pb_: e.activation(out=PT[:, pb_, :n], in_=PSB[4 + pb_][:, :n], func=AF.Exp, scale=SC), reads=[("psb", 4 + pb_)], writes=[(k, "PT", pb_)])
                        P.op("pe", lambda e, kt=kt, n=n, pb_=pb_, ki=ki, nk=len(keyt): e.matmul(PSB[6][0:65, :n], lhsT=vh[:, kt, :], rhs=PT[:, pb_, :n], start=(ki == 0), stop=(ki == nk - 1)),
                             reads=[(k, "vh", hp), (k, "PT", pb_)], writes=[("psb", 6)])
                        if ki % 2 == 1:
                            pump(1)
                    P.op("act", lambda e, n=n: e.activation(out=rr[64:65, :n], in_=PSB[6][64:65, :n], func=AF.Ln), reads=[("psb", 6)], writes=[(k, "rr")])
                    P.op("act", lambda e, n=n: e.activation(out=rr[64:65, :n], in_=rr[64:65, :n], func=AF.Exp, scale=-1.0), reads=[(k, "rr")], writes=[(k, "rr")])
                    P.op("pe", lambda e, n=n: e.matmul(PSB[7][0:64, :n], lhsT=ones1r[64:65, :], rhs=rr[64:65, :n], start=True, stop=True), reads=[(k, "rr"), (k, "ones1r")], writes=[("psb", 7)])
                    P.op("act", lambda e, n=n: e.copy(out=rden[:, :n], in_=PSB[7][0:64, :n]), reads=[("psb", 7)], writes=[("tmp", 0)])
                    P.op("dve", lambda e, n=n: e.tensor_tensor(out=oT[:, :n], in0=PSB[6][0:64, :n], in1=rden[:, :n], op=ALU.mult), reads=[("psb", 6), ("tmp", 0)], writes=[("tmp", 1)])
                    for oc in range(8):
                        pyb = 4 + oc % 2
                        P.op("pe", lambda e, oc=oc, n=n, pyb=pyb: e.matmul(PSB[pyb][:, :n], lhsT=wo[:, hp, oc * 128:(oc + 1) * 128], rhs=oT[:, :n], start=True, stop=True),
                             reads=[(k, "wo", hp), ("tmp", 1)], writes=[("psb", pyb)])
                        P.op("dve", lambda e, oc=oc, a=a, b=b, n=n, j=j, pyb=pyb: e.scalar_tensor_tensor(
                            out=xT[:, oc, a:b], in0=PSB[pyb][:, :n], scalar=modcol(2, oc, j), in1=xT[:, oc, a:b], op0=ALU.mult, op1=ALU.add),
                            reads=[("psb", pyb), "modt", ("x", oc, j)], writes=[("x", oc, j)])
                    pump(2)
                if gen is not None:
                    for _ in gen:
                        pass

            g0 = proj_gen(0)
            for _ in g0:
                pass
            for h in range(16):
                attn(h, proj_gen(h + 1) if h + 1 < 16 else None)

        def mlstm(tiles_all):
            phase()
            k = "ml"
            wqkv = arA.alloc([128, 8, 2048], BF16)
            selc = arA.alloc([16, 2, 16, 64], F32)
            maskc = arA.alloc([64, 2, 256], F32)
            selh = arA.alloc([16, 2, 4], F32)
            wg = arA.alloc([128, 8, 16], BF16)
            Cbf = arA.alloc([128, 4, 257], BF16)
            G = arB.alloc([16, NT], F32)
            Bd = [arB.alloc([16, NT], F32), arB.alloc([16, NT], F32)]
            Vp = arB.alloc([64, 4, 257], BF16)
            Cst = arB.alloc([128, 4, 257], F32)
            qkT = tmp[0][:, :].bitcast(BF16)[:, 0:512]
            Kw = tmp[1][0:64, :].bitcast(BF16)[:, 0:512].rearrange("p (a b) -> p a b", a=4)
            Em = tmp[2][0:64, 0:256]
            St = tmp[2][0:64, 256:384].bitcast(BF16)
            pis = tmp[3][0:64, 0:257]
            nd = tmp[4][0:64, 0:257]
            hout = tmp[5][0:64, :].bitcast(BF16)
            inter = rstd[0:64, 0:4]
            decay = rstd[:, 8:12]
            rdn = rstd[0:64, 16:17]
            P.op("pool", lambda e: e.dma_start(out=wqkv, in_=ml_win[:, 0:2048].rearrange("(k p) n -> p k n", p=128)), writes=[(k, "wqkv")], dma=True)
            P.op("pool", lambda e: e.dma_start(out=wg, in_=ml_win[:, 3072:3088].rearrange("(k p) n -> p k n", p=128)), writes=[(k, "wg")], dma=True)
            P.op("sp", lambda e: e.dma_start(out=selc, in_=ml_selc), writes=[(k, "selc")], dma=True)
            P.op("sp", lambda e: e.dma_start(out=selh, in_=ml_selh), writes=[(k, "selh")], dma=True)
            P.op("sp", lambda e: e.dma_start(out=maskc, in_=ml_maskc.rearrange("p a b c -> p a (b c)")), writes=[(k, "maskc")], dma=True)
            P.op("pool", lambda e: e.memset(Vp, 1.0), writes=[(k, "Vp")])
            for j in tiles_all:
                a, b = TT[j]
                n = b - a

                def mmg(e, a=a, b=b, n=n):
                    ins = None
                    for kk in range(8):
                        ins = e.matmul(PSB[0][0:16, :n], lhsT=wg[:, kk, :], rhs=hT[:, kk, a:b], start=(kk == 0), stop=(kk == 7))
                    return ins
                P.op("pe", mmg, reads=[(k, "wg")] + [("h", kk, j) for kk in range(8)], writes=[("psb", 0)])
                P.op("act", lambda e, a=a, b=b, n=n: e.activation(out=G[:, a:b], in_=PSB[0][0:16, :n], func=AF.Identity, bias=pvt[0:16, pvoff["ml_gb"]:pvoff["ml_gb"] + 1], scale=1.0),
                     reads=[("psb", 0), "pvt"], writes=[(k, "G")])
            lf = tmp[3][0:16, :]
            for j in tiles_all:
                a, b = TT[j]
                n = b - a
                P.op("act", lambda e, a=a, b=b, n=n: e.activation(out=lf[:, :n], in_=G[:, a:b], func=AF.Exp, scale=-1.0), reads=[(k, "G")], writes=[("tmp", 3)])
                P.op("act", lambda e, n=n: e.activation(out=lf[:, :n], in_=lf[:, :n], func=AF.Ln, bias=cst[0:16, 1:2], scale=1.0), reads=[("tmp", 3), "cst1"], writes=[("tmp", 3)])
                P.op("dve", lambda e, n=n: e.tensor_scalar(out=lf[:, :n], in0=lf[:, :n], scalar1=-1.0, scalar2=None, op0=ALU.mult), reads=[("tmp", 3)], writes=[("tmp", 3)])
                for ci in range(n // 64):
                    c0 = ci * 64
                    P.op("dve", lambda e, a=a, c0=c0: e.tensor_tensor_scan(out=Bd[0][:, a + c0:a + c0 + 64], data0=cst[0:16, 1:2].to_broadcast([16, 64]), data1=lf[:, c0:c0 + 64], initial=0.0, op0=ALU.mult, op1=ALU.add),
                         reads=[("tmp", 3), "cst1"], writes=[(k, "B0")])
                    P.op("dve", lambda e, a=a, c0=c0: e.tensor_tensor_scan(out=Bd[1][:, a + c0:a + c0 + 64][:, ::-1], data0=cst[0:16, 1:2].to_broadcast([16, 64]), data1=lf[:, c0:c0 + 64][:, ::-1], initial=0.0, op0=ALU.mult, op1=ALU.add),
                         reads=[("tmp", 3), "cst1"], writes=[(k, "B1")])
            SQ = 128 ** -0.5
            for d in range(2):
                P.op("pool", lambda e: e.memset(Cst, 0.0), reads=[(k, "C", h) for h in range(4)], writes=[(k, "C", h) for h in range(4)])
                P.op("pool", lambda e: e.memset(Cbf, 0.0), reads=[(k, "Cbf", h) for h in range(4)], writes=[(k, "Cbf", h) for h in range(4)])
                order = list(range(36)) if d == 0 else [3, 2, 1, 0] + list(range(35, 3, -1))
                for ch in order:
                    t0 = ch * 64
                    j = [jj for jj, (aa, bb) in enumerate(TT) if aa <= t0 < bb][0]
                    tend = t0 + 63 if d == 0 else t0
                    tl = 63 if d == 0 else 0
                    hr = [("h", kk, j) for kk in range(8)]

                    def mmk(e, t0=t0):
                        ins = None
                        for kk in range(8):
                            ins = e.matmul(PSB[0][0:64, 0:512], lhsT=hT[:, kk, t0:t0 + 64], rhs=wqkv[:, kk, 512:1024], start=(kk == 0), stop=(kk == 7))
                        return ins
                    P.op("pe", mmk, reads=hr + [(k, "wqkv")], writes=[("psb", 0)])
                    for half in range(2):
                        def mmv(e, t0=t0, half=half):
                            ins = None
                            for kk in range(8):
                                ins = e.matmul(PSB[1 + half][0:64, 0:512], lhsT=hT[:, kk, t0:t0 + 64], rhs=wqkv[:, kk, 1024 + half * 512:1536 + half * 512], start=(kk == 0), stop=(kk == 7))
                            return ins
                        P.op("pe", mmv, reads=hr + [(k, "wqkv")], writes=[("psb", 1 + half)])

                    def mmqk(e, t0=t0):
                        ins = None
                        for qk in range(2):
                            for h in range(4):
                                for kk in range(8):
                                    ins = e.matmul(PSB[3][:, qk * 256 + h * 64:qk * 256 + (h + 1) * 64], lhsT=wqkv[:, kk, qk * 512 + h * 128:qk * 512 + (h + 1) * 128],
                                                   rhs=hT[:, kk, t0:t0 + 64], start=(kk == 0), stop=(kk == 7))
                        return ins
                    P.op("pe", mmqk, reads=hr + [(k, "wqkv")], writes=[("psb", 3)])
                    P.op("act", lambda e: e.activation(out=qkT[:, 0:256], in_=PSB[3][:, 0:256], func=AF.Identity, scale=SQ), reads=[("psb", 3)], writes=[(k, "qT")])
                    P.op("act", lambda e: e.copy(out=qkT[:, 256:512], in_=PSB[3][:, 256:512]), reads=[("psb", 3)], writes=[(k, "kT")])
                    for half in range(2):
                        P.op("act", lambda e, half=half: e.copy(out=Vp[:, 2 * half:2 * half + 2, 0:256], in_=PSB[1 + half][0:64, :].rearrange("p (a b) -> p a b", a=2)),
                             reads=[("psb", 1 + half)], writes=[(k, "Vp")])
                    def mme(e, t0=t0, tend=tend, d=d):
                        ins = None
                        for h in range(4):
                            rb = (4 if d == 0 else 12) + h
                            ri = (0 if d == 0 else 8) + h
                            o = PSB[4][0:64, h * 64:(h + 1) * 64]
                            e.matmul(o, lhsT=selc[:, 0, rb, :], rhs=Bd[d][:, t0:t0 + 64], start=True, stop=False)
                            e.matmul(o, lhsT=Bd[d][:, t0:t0 + 64], rhs=selc[:, 1, rb, :], start=False, stop=False)
                            e.matmul(o, lhsT=G[:, t0:t0 + 64], rhs=selc[:, 0, ri, :], start=False, stop=True)
                        e.matmul(PSB[4][0:64, 256:260], lhsT=Bd[d][:, t0:t0 + 64], rhs=selh[:, d, :], start=True, stop=True)
                        ins = e.matmul(PSB[4][:, 260:264], lhsT=Bd[d][:, tend:tend + 1].to_broadcast([16, 128]), rhs=selh[:, d, :], start=True, stop=True)
                        return ins
                    P.op("pe", mme, reads=[(k, "selc"), (k, "selh"), (k, "B0"), (k, "B1"), (k, "G")], writes=[("psb", 4)])
                    P.op("dve", lambda e, d=d: e.tensor_tensor(out=Em, in0=PSB[4][0:64, 0:256], in1=maskc[:, d, :], op=ALU.add), reads=[("psb", 4), (k, "maskc")], writes=[(k, "Em")])
                    P.op("act", lambda e: e.activation(out=Em, in_=Em, func=AF.Exp), reads=[(k, "Em")], writes=[(k, "Em")])
                    P.op("act", lambda e: e.activation(out=inter, in_=PSB[4][0:64, 256:260], func=AF.Exp), reads=[("psb", 4)], writes=[(k, "inter")])
                    P.op("act", lambda e: e.activation(out=decay, in_=PSB[4][:, 260:264], func=AF.Exp), reads=[("psb", 4)], writes=[(k, "decay")])
                    def mms(e):
                        ins = None
                        for h in range(4):
                            ins = e.matmul(PSB[5][0:64, h * 64:(h + 1) * 64], lhsT=qkT[:, 256 + h * 64:256 + (h + 1) * 64], rhs=qkT[:, h * 64:(h + 1) * 64], start=True, stop=True)
                        return ins
                    P.op("pe", mms, reads=[(k, "qT"), (k, "kT")], writes=[("psb", 5)])
                    P.op("dve", lambda e: e.tensor_tensor(out=St, in0=PSB[5][0:64, 0:256], in1=Em, op=ALU.mult), reads=[("psb", 5), (k, "Em")], writes=[(k, "St")])
                    for h in range(4):
                        P.op("pe", lambda e, h=h: e.matmul(PSB[6][0:64, 0:257], lhsT=St[:, h * 64:(h + 1) * 64], rhs=Vp[:, h, :], start=True, stop=True),
                             reads=[(k, "St"), (k, "Vp")], writes=[("psb", 6)])
                        P.op("pe", lambda e, h=h: e.matmul(PSB[7][0:64, 0:257], lhsT=qkT[:, h * 64:(h + 1) * 64], rhs=Cbf[:, h, :], start=True, stop=True),
                             reads=[(k, "qT"), (k, "Cbf", h)], writes=[("psb", 7)])
                        P.op("act", lambda e: e.copy(out=pis, in_=PSB[6][0:64, 0:257]), reads=[("psb", 6)], writes=[(k, "pis")])
                        P.op("dve", lambda e, h=h: e.scalar_tensor_tensor(out=nd, in0=PSB[7][0:64, 0:257], scalar=inter[:, h:h + 1], in1=pis, op0=ALU.mult, op1=ALU.add),
                             reads=[("psb", 7), (k, "inter"), (k, "pis")], writes=[(k, "nd")])
                        P.op("dve", lambda e: e.tensor_scalar(out=rdn, in0=nd[:, 256:257], scalar1=-1.0, scalar2=None, op0=ALU.mult), reads=[(k, "nd")], writes=[(k, "rdn")])
                        P.op("dve", lambda e: e.tensor_tensor(out=rdn, in0=rdn, in1=nd[:, 256:257], op=ALU.max), reads=[(k, "nd"), (k, "rdn")], writes=[(k, "rdn")])
                        P.op("dve", lambda e: e.tensor_scalar(out=rdn, in0=rdn, scalar1=1.0, scalar2=None, op0=ALU.max), reads=[(k, "rdn")], writes=[(k, "rdn")])
                        P.op("dve", lambda e: e.reciprocal(out=rdn, in_=rdn), reads=[(k, "rdn")], writes=[(k, "rdn")])
                        P.op("dve", lambda e, h=h: e.tensor_scalar(out=hout[:, h * 256:(h + 1) * 256], in0=nd[:, 0:256], scalar1=rdn, scalar2=None, op0=ALU.mult),
                             reads=[(k, "nd"), (k, "rdn")], writes=[(k, "hout")])
                        P.op("dve", lambda e, h=h, tl=tl: e.tensor_scalar(out=Kw[:, h, :], in0=PSB[0][0:64, h * 128:(h + 1) * 128], scalar1=Em[:, h * 64 + tl:h * 64 + tl + 1], scalar2=None, op0=ALU.mult),
                             reads=[("psb", 0), (k, "Em")], writes=[(k, "Kw", h)])
                        ub_ = 1 + h % 2
                        P.op("pe", lambda e, h=h, ub_=ub_: e.matmul(PSB[ub_][:, 0:257], lhsT=Kw[:, h, :], rhs=Vp[:, h, :], start=True, stop=True),
                             reads=[(k, "Kw", h), (k, "Vp")], writes=[("psb", ub_)])
                        P.op("dve", lambda e, h=h, ub_=ub_: e.scalar_tensor_tensor(out=Cst[:, h, :], in0=Cst[:, h, :], scalar=decay[:, h:h + 1], in1=PSB[ub_][:, 0:257], op0=ALU.mult, op1=ALU.add),
                             reads=[(k, "C", h), (k, "decay"), ("psb", ub_)], writes=[(k, "C", h)])
                        P.op("act", lambda e, h=h: e.copy(out=Cbf[:, h, :], in_=Cst[:, h, :]), reads=[(k, "C", h)], writes=[(k, "Cbf", h)])
                    P.op("sp", lambda e, d=d, t0=t0: e.dma_start(out=hdir[d, t0:t0 + 64, :], in_=hout), reads=[(k, "hout")], writes=[(k, "hdir", d, ch // 2)], dma=True)
            phase()
            wog = arA.alloc([128, 8, 1024], BF16)
            gain = arA.alloc([128, 1024], F32)
            hfb = arA.alloc([128, 2, 1024], BF16)
            hs = arA.alloc([128, 1024], F32)
            sq = arA.alloc([128, 1024], F32)
            wout = arA.alloc([128, 2, 8, 128], BF16)
            ss = rstd[:, 0:4]
            P.op("pool", lambda e: e.dma_start(out=wog, in_=ml_win[:, 2048:3072].rearrange("(k p) n -> p k n", p=128)), writes=[(k, "wog")], dma=True)
            P.op("sp", lambda e: e.dma_start(out=gain, in_=ml_gain), writes=[(k, "gain")], dma=True)
            for ti in range(18):
                jt = [jj for jj, (aa, bb) in enumerate(TT) if aa <= ti * 128 < bb][0]
                for d in range(2):
                    P.op("sp", lambda e, d=d, ti=ti: e.dma_start(out=hfb[:, d, :], in_=hdir[d, ti * 128:(ti + 1) * 128, :]), writes=[(k, "hfb", d)], dma=True)
                for half in range(2):
                    def mmo(e, ti=ti, half=half):
                        ins = None
                        for kk in range(8):
                            ins = e.matmul(PSB[2 + half][:, :], lhsT=hT[:, kk, ti * 128:(ti + 1) * 128], rhs=wog[:, kk, half * 512:(half + 1) * 512], start=(kk == 0), stop=(kk == 7))
                        return ins
                    P.op("pe", mmo, reads=[("h", kk, jt) for kk in range(8)] + [(k, "wog")], writes=[("psb", 2 + half)])
                P.op("dve", lambda e: e.tensor_tensor(out=hs, in0=hfb[:, 0, :], in1=hfb[:, 1, :], op=ALU.add), reads=[(k, "hfb", 0), (k, "hfb", 1)], writes=[(k, "hs")])
                P.op("act", lambda e: e.activation(out=sq, in_=hs, func=AF.Square), reads=[(k, "hs")], writes=[(k, "sq")])
                P.op("dve", lambda e: e.tensor_reduce(out=ss, in_=sq[:, :].rearrange("p (a b) -> p a b", a=4), axis=AX.X, op=ALU.add), reads=[(k, "sq")], writes=[(k, "ss")])
                P.op("act", lambda e: e.activation(out=ss, in_=ss, func=AF.Sqrt, bias=cst[:, 0:1], scale=1.0 / 256.0), reads=[(k, "ss"), "cst0"], writes=[(k, "ss")])
                P.op("dve", lambda e: e.reciprocal(out=ss, in_=ss), reads=[(k, "ss")], writes=[(k, "ss")])
                for h in range(4):
                    P.op("dve", lambda e, h=h: e.scalar_tensor_tensor(out=hs[:, h * 256:(h + 1) * 256], in0=hs[:, h * 256:(h + 1) * 256], scalar=ss[:, h:h + 1], in1=gain[:, h * 256:(h + 1) * 256], op0=ALU.mult, op1=ALU.mult),
                         reads=[(k, "hs"), (k, "ss"), (k, "gain")], writes=[(k, "hs")])
                for half in range(2):
                    P.op("act", lambda e, half=half: e.activation(out=sq[:, half * 512:(half + 1) * 512], in_=PSB[2 + half][:, :], func=AF.Sigmoid), reads=[("psb", 2 + half), (k, "sq")], writes=[(k, "sq")])
                P.op("dve", lambda e: e.tensor_tensor(out=hs, in0=hs, in1=sq, op=ALU.mult), reads=[(k, "hs"), (k, "sq")], writes=[(k, "hs")])
                for half in range(2):
                    def trf(e, half=half):
                        ins = None
                        for q in range(4):
                            c = half * 4 + q
                            ins = e.transpose(PSB[half][:, q * 128:(q + 1) * 128], hs[:, c * 128:(c + 1) * 128], ident[:])
                        return ins
                    P.op("pe", trf, reads=[(k, "hs"), "ident"], writes=[("psb", half)])
                    P.op("act" if half else "dve", lambda e, half=half, ti=ti: (e.copy if half else e.tensor_copy)(out=mT[:, half * 4:half * 4 + 4, ti * 128:(ti + 1) * 128], in_=PSB[half][:, :].rearrange("p (q t) -> p q t", q=4)),
                         reads=[("psb", half)], writes=[("m", c, jt) for c in range(half * 4, half * 4 + 4)])
            for oc in range(8):
                ob = oc % 2
                P.op("pool", lambda e, oc=oc, ob=ob: e.dma_start(out=wout[:, ob, :, :], in_=ml_wout[:, oc * 128:(oc + 1) * 128].rearrange("(k p) n -> p k n", p=128)),
                     writes=[(k, "wout", ob)], dma=True)
                for j in tiles_all:
                    a, b = TT[j]
                    n = b - a
                    py = PSB[4 + j % 4]

                    def mmfn(e, py=py, ob=ob, a=a, b=b, n=n):
                        ins = None
                        for cc in range(8):
                            ins = e.matmul(py[:, :n], lhsT=wout[:, ob, cc, :], rhs=mT[:, cc, a:b], start=(cc == 0), stop=(cc == 7))
                        return ins
                    P.op("pe", mmfn, reads=[(k, "wout", ob)] + [("m", cc, j) for cc in range(8)], writes=[("psb", 4 + j % 4)])
                    P.op("dve", lambda e, py=py, oc=oc, a=a, b=b, n=n, j=j: e.scalar_tensor_tensor(
                        out=xT[:, oc, a:b], in0=py[:, :n], scalar=modcol(2, oc, j), in1=xT[:, oc, a:b], op0=ALU.mult, op1=ALU.add),
                        reads=[("psb", 4 + j % 4), "modt", ("x", oc, j)], writes=[("x", oc, j)])

        def moe_sparse(i, tiles):
            phase()
            k = "moe%d" % i
            subs = [t for j in tiles for t in range(TT[j][0] // 128, TT[j][1] // 128)]
            NOV = 36
            wr = arA.alloc([128, 8, 72], F32)
            brb = arA.alloc([128, 72], F32)
            f32t = arA.alloc([128, 8, 512], F32)
            o4 = arA.off
            lgs4 = arA.alloc([128, 4, 72], F32)
            sm4 = arA.alloc([128, 8, 4], F32)
            oh4 = arA.alloc([128, 4, 8], F32)
            eg4 = arA.alloc([128, 4, 8], F32)
            em4 = arA.alloc([128, 4, 64], F32)
            as4 = arA.alloc([128, 4, 64], F32)
            RK = arA.alloc([128, 18, 64], F32)
            assert arA.off - o4 >= 2048
            stg4 = scr[:, o4:o4 + 2048]
            OH = arA.alloc([128, 18, 2, 64], F32)
            WP = arA.alloc([128, 18, 2], F32)
            acum = arA.alloc([128, 64], F32)
            asum = arA.alloc([128, 64], F32)
            mc = arA.alloc([128, 229], F32)
            ones1 = arA.alloc([128, 128], F32)
            identb = arA.alloc([128, 128], BF16)
            cntb = arA.alloc([128, 64], F32)
            ovn = arA.alloc([128, 64], F32)
            ove = arA.alloc([128, 64], F32)
            ovsp = arA.alloc([128, 64], F32)
            dlt = arA.alloc([128, 64], F32)
            t64a = arA.alloc([128, 64], F32)
            t64b = arA.alloc([128, 64], F32)
            EB = arA.alloc([128, NOV], F32)
            DEST = arA.alloc([128, 18, 2], F32)
            DESTI = arA.alloc([128, 18, 2], I32)
            idxi = arA.alloc([128, NOV, 4], I32)
            idxs = arA.alloc([128, NOV], I32)
            idxsf = arA.alloc([128, NOV], F32)
            idxf = arA.alloc([128, NOV, 4], F32)
            Ltri = mc[:, 0:128]
            e128 = mc[:, 128:192]
            iop = mc[:, 192:193]
            thr36 = mc[:, 193:229]
            P.op("sp", lambda e: e.dma_start(out=wr, in_=moe_wr[i].rearrange("(k p) n -> p k n", p=128)), writes=[(k, "wr")], dma=True)
            P.op("sp", lambda e: e.dma_start(out=brb, in_=moe_br[i]), writes=[(k, "br")], dma=True)
            P.op("sp", lambda e: e.dma_start(out=mc, in_=moec_d), writes=[(k, "mc")], dma=True)
            P.op("pool", lambda e: e.memset(ones1, 1.0), writes=[(k, "ones1")])
            P.op("pool", lambda e: e.memset(acum, 0.0), writes=[(k, "rk")])
            P.op("pool", lambda e: e.tensor_copy(out=identb, in_=ident[:]), reads=["ident"], writes=[(k, "identb")])
            RKK = [(k, "rk")]

            def extra(j, c, tb):
                a, b = TT[j]
                n = b - a
                jj = 1 if j == 0 else 0
                P.op("pool", lambda e, c=c, tb=tb, n=n, jj=jj: e.tensor_scalar(out=f32t[:, c, :n], in0=tb[:, :n], scalar1=mA[:, 1, c, jj:jj + 1],
                                                                               scalar2=modt[:, 3 * 8 + c, jj:jj + 1], op0=ALU.mult, op1=ALU.add),
                     reads=[("tmp", 2 + c % 2), "modt", "mA"], writes=[(k, "f32", c)])
                if c == 7:
                    S = n // 128
                    g0 = a // 128
                    pl = PSB[5]

                    def mmfn(e, S=S):
                        ins = None
                        for s_ in range(S):
                            for kk in range(8):
                                ins = e.matmul(pl[:, s_ * 72:(s_ + 1) * 72], lhsT=f32t[:, kk, s_ * 128:(s_ + 1) * 128], rhs=wr[:, kk, :], start=(kk == 0), stop=(kk == 7))
                        return ins
                    P.op("pe", mmfn, reads=[(k, "f32", cc) for cc in range(8)] + [(k, "wr")], writes=[("psb", 5)])
                    R = [(k, "rt")]
                    V = lambda fn, extra_r=(), extra_w=(): P.op("dve", fn, reads=R + list(extra_r), writes=R + list(extra_w))
                    L4 = lgs4[:, 0:S, :]
                    G4 = lgs4[:, 0:S, 0:8]
                    E4 = lgs4[:, 0:S, 8:72]
                    E44 = E4.rearrange("p s (g j) -> p s g j", g=8)
                    o8 = oh4[:, 0:S, :]
                    oh1 = OH[:, g0:g0 + S, 0, :]
                    oh2 = OH[:, g0:g0 + S, 1, :]
                    em_ = em4[:, 0:S, :]
                    sc = lambda i_: sm4[:, i_, 0:S]
                    bc8 = lambda v: v.unsqueeze(2).to_broadcast([128, S, 8])
                    bc64 = lambda v: v.unsqueeze(2).to_broadcast([128, S, 64])
                    V(lambda e: e.tensor_tensor(out=L4, in0=pl[:, 0:S * 72].rearrange("p (s c) -> p s c", s=S), in1=brb.unsqueeze(1).to_broadcast([128, S, 72]), op=ALU.add), [("psb", 5), (k, "br")])
                    V(lambda e: e.tensor_reduce(out=sc(0), in_=G4, axis=AX.X, op=ALU.max))
                    V(lambda e: e.tensor_tensor(out=o8, in0=G4, in1=bc8(sc(0)), op=ALU.is_equal))
                    V(lambda e: e.tensor_tensor(out=eg4[:, 0:S, :], in0=G4, in1=bc8(sc(0)), op=ALU.subtract))
                    P.op("act", lambda e: e.activation(out=eg4[:, 0:S, :], in_=eg4[:, 0:S, :], func=AF.Exp), reads=R, writes=R)
                    V(lambda e: e.tensor_reduce(out=sc(2), in_=eg4[:, 0:S, :], axis=AX.X, op=ALU.add))
                    V(lambda e: e.reciprocal(out=sc(3), in_=sc(2)))
                    V(lambda e: e.tensor_scalar(out=o8, in0=o8, scalar1=BIG, scalar2=-BIG, op0=ALU.mult, op1=ALU.add))
                    V(lambda e: e.tensor_tensor(out=em_.rearrange("p s (g j) -> p s g j", g=8), in0=E44, in1=o8.unsqueeze(3).to_broadcast([128, S, 8, 8]), op=ALU.add))
                    V(lambda e: e.tensor_reduce(out=sc(4), in_=em_, axis=AX.X, op=ALU.max))
                    V(lambda e: e.tensor_tensor(out=oh1, in0=em_, in1=bc64(sc(4)), op=ALU.is_equal), (), RKK)
                    V(lambda e: e.scalar_tensor_tensor(out=em_, in0=oh1, scalar=-BIG, in1=em_, op0=ALU.mult, op1=ALU.add))
                    V(lambda e: e.tensor_reduce(out=sc(5), in_=em_, axis=AX.X, op=ALU.max))
                    V(lambda e: e.tensor_tensor(out=oh2, in0=em_, in1=bc64(sc(5)), op=ALU.is_equal), (), RKK)
                    V(lambda e: e.tensor_tensor(out=sc(6), in0=sc(5), in1=sc(4), op=ALU.subtract))
                    P.op("act", lambda e: e.activation(out=sc(6), in_=sc(6), func=AF.Exp), reads=R, writes=R)
                    V(lambda e: e.tensor_scalar(out=sc(6), in0=sc(6), scalar1=1.0, scalar2=None, op0=ALU.add))
                    V(lambda e: e.reciprocal(out=sc(6), in_=sc(6)))
                    V(lambda e: e.tensor_tensor(out=WP[:, g0:g0 + S, 0], in0=sc(6), in1=sc(3), op=ALU.mult), (), RKK)
                    V(lambda e: e.tensor_tensor(out=WP[:, g0:g0 + S, 1], in0=sc(3), in1=WP[:, g0:g0 + S, 0], op=ALU.subtract), (), RKK)
                    V(lambda e: e.tensor_tensor(out=as4[:, 0:S, :], in0=oh1, in1=oh2, op=ALU.add), (), RKK)

                    def mmr(e, S=S):
                        ins = None
                        for s_ in range(S):
                            o = PSB[4][:, s_ * 64:(s_ + 1) * 64]
                            e.matmul(o, lhsT=Ltri, rhs=as4[:, s_, :], start=True, stop=False)
                            for sp_ in range(s_):
                                e.matmul(o, lhsT=ones1, rhs=as4[:, sp_, :], start=False, stop=False)
                            ins = e.matmul(o, lhsT=ones1, rhs=acum, start=False, stop=True)
                        return ins
                    P.op("pe", mmr, reads=RKK + R + [(k, "mc"), (k, "ones1")], writes=[("psb", 4)])
                    P.op("act", lambda e: e.copy(out=RK[:, g0:g0 + S, :], in_=PSB[4][:, 0:S * 64].rearrange("p (s c) -> p s c", s=S)), reads=[("psb", 4)] + RKK, writes=RKK)
                    V(lambda e: e.tensor_reduce(out=asum, in_=as4[:, 0:S, :].rearrange("p s c -> p c s"), axis=AX.X, op=ALU.add), [("psb", 4)], RKK)
                    V(lambda e: e.tensor_tensor(out=acum, in0=acum, in1=asum, op=ALU.add), [("psb", 4)], RKK)

            norm_mod(1, tiles, extra=extra)

            P.op("pe", lambda e: e.matmul(PSB[4][:, 0:64], lhsT=ones1, rhs=acum, start=True, stop=True), reads=RKK + [(k, "ones1")], writes=[("psb", 4)])
            V2 = lambda fn: P.op("dve", fn, reads=RKK + [("psb", 4), (k, "mc")], writes=RKK)
            V2(lambda e: e.tensor_scalar(out=cntb, in0=PSB[4][:, 0:64], scalar1=-128.0, scalar2=0.0, op0=ALU.add, op1=ALU.max))
            f32flat0 = f32t[:, :, :].rearrange("p a b -> p (a b)")
            tQ = f32flat0[:, 0:64 * NOV].rearrange("p (c q) -> p c q", q=NOV)
            F3a = [(k, "f32", cc) for cc in range(8)]
            V2b = lambda fn: P.op("dve", fn, reads=RKK + F3a + [("psb", 4), (k, "mc")], writes=RKK + F3a)
            V2b(lambda e: e.tensor_tensor(out=tQ, in0=cntb.unsqueeze(2).to_broadcast([128, 64, NOV]), in1=thr36.unsqueeze(1).to_broadcast([128, 64, NOV]), op=ALU.is_gt))
            V2b(lambda e: e.tensor_reduce(out=ovn, in_=tQ, axis=AX.X, op=ALU.add))
            V2(lambda e: e.tensor_scalar(out=ovn, in0=ovn, scalar1=128.0, scalar2=None, op0=ALU.mult))
            V2(lambda e: e.tensor_tensor_scan(out=ove, data0=ones1[:, 0:64], data1=ovn, initial=0.0, op0=ALU.mult, op1=ALU.add))
            V2(lambda e: e.tensor_tensor(out=ovsp, in0=ove, in1=ovn, op=ALU.subtract))
            V2(lambda e: e.tensor_scalar(out=ovsp, in0=ovsp, scalar1=8064.0, scalar2=None, op0=ALU.add))
            V2(lambda e: e.tensor_tensor(out=dlt, in0=e128, in1=ovsp, op=ALU.subtract))
            tQ2 = f32flat0[:, 0:64 * NOV].rearrange("p (q c) -> p q c", q=NOV)
            V2b(lambda e: e.tensor_tensor(out=tQ2, in0=ove.unsqueeze(1).to_broadcast([128, NOV, 64]), in1=thr36.unsqueeze(2).to_broadcast([128, NOV, 64]), op=ALU.is_le))
            V2b(lambda e: e.tensor_reduce(out=EB, in_=tQ2, axis=AX.X, op=ALU.add))
            V2(lambda e: e.tensor_scalar(out=t64a[:, 0:NOV], in0=EB, scalar1=64.0, scalar2=1.0e6, op0=ALU.is_ge, op1=ALU.mult))
            V2(lambda e: e.scalar_tensor_tensor(out=t64a[:, 0:NOV], in0=EB, scalar=256.0, in1=t64a[:, 0:NOV], op0=ALU.mult, op1=ALU.add))
            V2(lambda e: e.tensor_scalar(out=t64b[:, 0:1], in0=iop, scalar1=2.0, scalar2=float(i * 16384), op0=ALU.mult, op1=ALU.add))
            V2(lambda e: e.tensor_scalar(out=idxf[:, :, 0], in0=t64a[:, 0:NOV], scalar1=t64b[:, 0:1], scalar2=None, op0=ALU.add))
            V2(lambda e: e.tensor_scalar(out=idxf[:, :, 1], in0=idxf[:, :, 0], scalar1=1.0, scalar2=None, op0=ALU.add))
            V2(lambda e: e.tensor_scalar(out=t64a[:, 0:NOV], in0=t64a[:, 0:NOV], scalar1=0.5, scalar2=None, op0=ALU.mult))
            V2(lambda e: e.tensor_scalar(out=t64b[:, 1:2], in0=iop, scalar1=float(i * 8192), scalar2=None, op0=ALU.add))
            V2(lambda e: e.tensor_scalar(out=idxf[:, :, 2], in0=t64a[:, 0:NOV], scalar1=t64b[:, 1:2], scalar2=None, op0=ALU.add))
            V2(lambda e: e.tensor_copy(out=idxf[:, :, 3], in_=idxf[:, :, 2]))
            V2(lambda e: e.tensor_copy(out=idxi, in_=idxf))
            V2(lambda e: e.tensor_scalar(out=idxsf, in0=EB, scalar1=64.0, scalar2=1.0e6, op0=ALU.is_ge, op1=ALU.mult))
            V2(lambda e: e.tensor_tensor(out=idxsf, in0=idxsf, in1=thr36, op=ALU.add))
            V2(lambda e: e.tensor_scalar(out=idxsf, in0=idxsf, scalar1=iop, scalar2=8192.0, op0=ALU.add, op1=ALU.add))
            V2(lambda e: e.tensor_copy(out=idxs, in_=idxsf))
            sg0, SG = subs[0], len(subs)
            f32flat = f32t[:, :, :].rearrange("p a b -> p (a b)")
            tA = f32flat[:, 0:SG * 64].rearrange("p (s c) -> p s c", s=SG)
            tB = f32flat[:, 2048:2048 + SG * 64].rearrange("p (s c) -> p s c", s=SG)
            RKs = RK[:, sg0:sg0 + SG, :]
            F3 = [(k, "f32", cc) for cc in range(8)]
            V3 = lambda fn: P.op("dve", fn, reads=RKK + F3 + [(k, "mc")], writes=RKK + F3)
            V3(lambda e: e.tensor_scalar(out=tA, in0=RKs, scalar1=128.0, scalar2=None, op0=ALU.is_lt))
            V3(lambda e: e.tensor_tensor(out=tA, in0=tA, in1=dlt.unsqueeze(1).to_broadcast([128, SG, 64]), op=ALU.mult))
            V3(lambda e: e.tensor_tensor(out=tB, in0=RKs, in1=ovsp.unsqueeze(1).to_broadcast([128, SG, 64]), op=ALU.add))
            V3(lambda e: e.tensor_tensor(out=tA, in0=tA, in1=tB, op=ALU.add))
            for kk in range(2):
                V3(lambda e, kk=kk: e.tensor_tensor(out=tB, in0=OH[:, sg0:sg0 + SG, kk, :], in1=tA, op=ALU.mult))
                V3(lambda e, kk=kk: e.tensor_reduce(out=DEST[:, sg0:sg0 + SG, kk], in_=tB, axis=AX.X, op=ALU.add))
            V2(lambda e: e.tensor_copy(out=DESTI, in_=DEST))
            P.barrier()
            ftok = arB.alloc([128, 2, 1024], BF16)
            for si, gt in enumerate(subs):
                fb = si % 2
                jt = [jj for jj, (aa, bb) in enumerate(TT) if aa <= gt * 128 < bb][0]
                pbf = PSB[fb][:, :].bitcast(BF16)

                def trf(e, gt=gt, pbf=pbf):
                    ins = None
                    for c in range(8):
                        ins = e.transpose(pbf[:, c * 128:(c + 1) * 128], hT[:, c, gt * 128:(gt + 1) * 128], identb)
                    return ins
                P.op("pe", trf, reads=[("h", c, jt) for c in range(8)] + [(k, "identb")], writes=[("psb", fb)])
                P.op("act", lambda e, fb=fb, pbf=pbf: e.copy(out=ftok[:, fb, :], in_=pbf), reads=[("psb", fb)], writes=[(k, "ftok", fb)])
                for kk in range(2):
                    P.op("pool", lambda e, fb=fb, gt=gt, kk=kk: e.indirect_dma_start(
                        out=xslots[:, :], out_offset=bass.IndirectOffsetOnAxis(ap=DESTI[:, gt, kk:kk + 1], axis=0), in_=ftok[:, fb, :], in_offset=None),
                        reads=[(k, "ftok", fb)] + RKK, writes=[(k, "xs", gt, kk)], dma=True)
            P.barrier()
            arB.reset()
            stg = f32t[:, :, :].rearrange("p a b -> p (a b)").rearrange("p (s n) -> p s n", s=2)
            xb = arB.alloc([128, 2, 1024], BF16)
            xbT = arB.alloc([128, 2, 8, 128], BF16)
            wgu = arB.alloc([128, 2, 8, 512], BF16)
            wd = arB.alloc([128, 2, 2, 1024], BF16)
            sg = arB.alloc([128, 256], F32)
            actb = arB.alloc([128, 2, 2, 128], BF16)
            wgu_rows = moe_wgu.rearrange("l e (p h q) n -> (l e p h) (q n)", p=128, h=2)
            wd_rows = moe_wd.rearrange("l e (p q) n -> (l e p) (q n)", p=128)
            OHflat = OH[:, :, :, :].rearrange("p a b c -> p (a b c)")
            stgs = [stg[:, 0, :], stg[:, 1, :], OHflat[:, 0:2048], stg4]
            nstg = [0]

            def emit_weights(b, si):
                wb = si % 2
                ov = b - 64
                for piece in range(3):
                    sb_ = nstg[0] % 4
                    nstg[0] += 1
                    sv = stgs[sb_]
                    if b < 64:
                        if piece < 2:
                            src = moe_wgu[i, b].rearrange("(p q) n -> p (q n)", p=128)[:, piece * 2048:(piece + 1) * 2048]
                        else:
                            src = moe_wd[i, b].rearrange("(p q) n -> p (q n)", p=128)
                        P.op("sp", lambda e, src=src, sv=sv: e.dma_start(out=sv, in_=src), writes=[(k, "stg", sb_)], dma=True)
                    else:
                        if piece < 2:
                            P.op("pool", lambda e, ov=ov, sv=sv, piece=piece: e.indirect_dma_start(
                                out=sv, out_offset=None, in_=wgu_rows[:, :],
                                in_offset=bass.IndirectOffsetOnAxis(ap=idxi[:, ov, piece:piece + 1], axis=0), bounds_check=getreg(e, 65535), oob_is_err=False),
                                reads=RKK, writes=[(k, "stg", sb_)], dma=True)
                        else:
                            P.op("pool", lambda e, ov=ov, sv=sv: e.indirect_dma_start(
                                out=sv, out_offset=None, in_=wd_rows[:, :],
                                in_offset=bass.IndirectOffsetOnAxis(ap=idxi[:, ov, 2:3], axis=0), bounds_check=getreg(e, 32767), oob_is_err=False),
                                reads=RKK, writes=[(k, "stg", sb_)], dma=True)
                    ceng = "act" if piece == 0 else "dve"
                    if piece < 2:
                        P.op(ceng, lambda e, sv=sv, wb=wb, piece=piece, ceng=ceng: (e.copy if ceng == "act" else e.tensor_copy)(out=wgu[:, wb, piece * 4:(piece + 1) * 4, :], in_=sv.rearrange("p (q n) -> p q n", q=4)),
                             reads=[(k, "stg", sb_)], writes=[(k, "wgu", wb, piece)])
                    else:
                        P.op(ceng, lambda e, sv=sv, wb=wb, ceng=ceng: (e.copy if ceng == "act" else e.tensor_copy)(out=wd[:, wb, :, :], in_=sv.rearrange("p (q n) -> p q n", q=2)),
                             reads=[(k, "stg", sb_)], writes=[(k, "wd", wb)])

            def emit_compute(b, si):
                wb = si % 2
                xbuf = si % 2
                if b < 64:
                    P.op("sp", lambda e, b=b, xbuf=xbuf: e.dma_start(out=xb[:, xbuf, :], in_=xslots[b * 128:(b + 1) * 128, :]), writes=[(k, "xb", xbuf)], dma=True)
                else:
                    P.op("pool", lambda e, b=b, xbuf=xbuf: e.indirect_dma_start(
                        out=xb[:, xbuf, :], out_offset=None, in_=xslots[:, :],
                        in_offset=bass.IndirectOffsetOnAxis(ap=idxs[:, b - 64:b - 63], axis=0), bounds_check=getreg(e, NSLOT - 1), oob_is_err=False),
                        reads=RKK, writes=[(k, "xb", xbuf)], dma=True)
                pbf = PSB[xbuf][:, :].bitcast(BF16)

                def trx(e, xbuf=xbuf, pbf=pbf):
                    ins = None
                    for q in range(8):
                        ins = e.transpose(pbf[:, q * 128:(q + 1) * 128], xb[:, xbuf, q::8], identb)
                    return ins
                P.op("pe", trx, reads=[(k, "xb", xbuf), (k, "identb")], writes=[("psb", xbuf)])
                P.op("dve", lambda e, xbuf=xbuf, pbf=pbf: e.tensor_copy(out=xbT[:, xbuf, :, :], in_=pbf.rearrange("p (q s) -> p q s", q=8)), reads=[("psb", xbuf)], writes=[(k, "xbT", xbuf)])
                pgu = PSB[2 + xbuf]

                def mmgu(e, xbuf=xbuf, wb=wb, pgu=pgu):
                    ins = None
                    for oc in range(4):
                        for q in range(8):
                            ins = e.matmul(pgu[:, oc * 128:(oc + 1) * 128], lhsT=wgu[:, wb, q, (oc // 2) * 256 + (oc % 2):(oc // 2) * 256 + 256:2], rhs=xbT[:, xbuf, q, :], start=(q == 0), stop=(q == 7))
                    return ins
                P.op("pe", mmgu, reads=[(k, "wgu", wb, 0), (k, "wgu", wb, 1), (k, "xbT", xbuf)], writes=[("psb", 2 + xbuf)])
                P.op("act", lambda e, pgu=pgu: e.activation(out=sg, in_=pgu[:, 0:256], func=AF.Silu), reads=[("psb", 2 + xbuf)], writes=[(k, "sg")])
                P.op("dve", lambda e, pgu=pgu, xbuf=xbuf: e.tensor_tensor(out=actb[:, xbuf, :, :], in0=sg[:, :].rearrange("p (a b) -> p a b", a=2), in1=pgu[:, 256:512].rearrange("p (a b) -> p a b", a=2), op=ALU.mult),
                     reads=[(k, "sg"), ("psb", 2 + xbuf)], writes=[(k, "actb", xbuf)])
                tb_ = 2 * xbuf
                ybf = tmp[tb_][:, :].bitcast(BF16)
                for half in range(2):
                    pyb = 4 + 2 * xbuf + half

                    def mmd(e, half=half, pyb=pyb, xbuf=xbuf, wb=wb):
                        ins = None
                        for fc in range(2):
                            ins = e.matmul(PSB[pyb][:, :], lhsT=actb[:, xbuf, fc, :], rhs=wd[:, wb, fc, half * 512:(half + 1) * 512], start=(fc == 0), stop=(fc == 1))
                        return ins
                    P.op("pe", mmd, reads=[(k, "actb", xbuf), (k, "wd", wb)], writes=[("psb", pyb)])
                    P.op("act", lambda e, pyb=pyb, ybf=ybf, half=half: e.copy(out=ybf[:, half * 512:(half + 1) * 512], in_=PSB[pyb][:, :]),
                         reads=[("psb", pyb)], writes=[("tmp", tb_)])
                if b < 64:
                    P.op("act", lambda e, b=b, ybf=ybf: e.dma_start(out=yslots[b * 128:(b + 1) * 128, :], in_=ybf),
                         reads=[("tmp", tb_)], writes=[(k, "ys", b)], dma=True)
                else:
                    P.op("pool", lambda e, b=b, ybf=ybf: e.indirect_dma_start(
                        out=yslots[:, :], out_offset=bass.IndirectOffsetOnAxis(ap=idxs[:, b - 64:b - 63], axis=0), in_=ybf, in_offset=None,
                        bounds_check=getreg(e, NSLOT - 1), oob_is_err=False),
                        reads=[("tmp", tb_)] + RKK, writes=[(k, "ys", b)], dma=True)

            order = []
            ovl = list(range(64, 64 + NOV))
            for b in range(64):
                order.append(b)
                if b % 2 == 1 and ovl:
                    order.append(ovl.pop(0))
            order += ovl
            emit_weights(order[0], 0)
            for si, b in enumerate(order):
                if si + 1 < len(order):
                    emit_weights(order[si + 1], si + 1)
                emit_compute(b, si)
            P.barrier()
            arB.reset()
            yg = arB.alloc([128, 2, 2, 1024], BF16)
            yt = arB.alloc([128, 1024], F32)
            for si, gt in enumerate(subs):
                gb = si % 2
                jt = [jj for jj, (aa, bb) in enumerate(TT) if aa <= gt * 128 < bb][0]
                for kk in range(2):
                    P.op("pool", lambda e, gb=gb, gt=gt, kk=kk: e.indirect_dma_start(
                        out=yg[:, gb, kk, :], out_offset=None, in_=yslots[:, :], in_offset=bass.IndirectOffsetOnAxis(ap=DESTI[:, gt, kk:kk + 1], axis=0)),
                        reads=RKK, writes=[(k, "yg", gb, kk)], dma=True)
                P.op("dve", lambda e, gb=gb, gt=gt: e.tensor_scalar(out=yt, in0=yg[:, gb, 0, :], scalar1=WP[:, gt, 0:1], scalar2=None, op0=ALU.mult),
                     reads=[(k, "yg", gb, 0)] + RKK, writes=[(k, "yt")])
                P.op("dve", lambda e, gb=gb, gt=gt: e.scalar_tensor_tensor(out=yt, in0=yg[:, gb, 1, :], scalar=WP[:, gt, 1:2], in1=yt, op0=ALU.mult, op1=ALU.add),
                     reads=[(k, "yg", gb, 1), (k, "yt")] + RKK, writes=[(k, "yt")])
                for half in range(2):
                    pb = PSB[4 + half]

                    def trf(e, pb=pb, half=half):
                        ins = None
                        for q in range(4):
                            c = half * 4 + q
                            ins = e.transpose(pb[:, q * 128:(q + 1) * 128], yt[:, c * 128:(c + 1) * 128], ident[:])
                        return ins
                    P.op("pe", trf, reads=[(k, "yt"), "ident"], writes=[("psb", 4 + half)])
                    for q in range(4):
                        c = half * 4 + q
                        P.op("dve", lambda e, pb=pb, q=q, c=c, gt=gt, jt=jt: e.scalar_tensor_tensor(
                            out=xT[:, c, gt * 128:(gt + 1) * 128], in0=pb[:, q * 128:(q + 1) * 128], scalar=modcol(5, c, jt), in1=xT[:, c, gt * 128:(gt + 1) * 128], op0=ALU.mult, op1=ALU.add),
                            reads=[("psb", 4 + half), "modt", ("x", c, jt)], writes=[("x", c, jt)])

        def moe(i, tiles):
            phase()
            k = "moe%d" % i
            wr = arA.alloc([128, 8, 72], F32)
            brb = arA.alloc([128, 72], F32)
            f32t = arA.alloc([128, 8, 512], F32)
            lgs = arA.alloc([128, 72], F32)
            sm = arA.alloc([128, 16], F32)
            oh = arA.alloc([128, 8], F32)
            em = arA.alloc([128, 64], F32)
            oh1 = arA.alloc([128, 64], F32)
            oh2 = arA.alloc([128, 64], F32)
            wt = arA.alloc([128, 64], F32)
            wtT = arA.alloc([64, NT], F32)
            wgu = arB.alloc([128, 2, 8, 512], BF16)
            wd = arB.alloc([128, 2, 2, 1024], BF16)
            sgt = arB.alloc([128, 2, 512], F32)
            actt = arB.alloc([128, 2, 2, 512], BF16)
            wbc = arB.alloc([128, 512], F32)
            P.op("sp", lambda e: e.dma_start(out=wr, in_=moe_wr[i].rearrange("(k p) n -> p k n", p=128)), writes=[(k, "wr")], dma=True)
            P.op("sp", lambda e: e.dma_start(out=brb, in_=moe_br[i]), writes=[(k, "br")], dma=True)

            def extra(j, c, tb):
                a, b = TT[j]
                n = b - a
                jj = 1 if j == 0 else 0
                P.op("pool", lambda e, c=c, tb=tb, n=n, jj=jj: e.tensor_scalar(out=f32t[:, c, :n], in0=tb[:, :n], scalar1=mA[:, 1, c, jj:jj + 1],
                                                                               scalar2=modt[:, 3 * 8 + c, jj:jj + 1], op0=ALU.mult, op1=ALU.add),
                     reads=[("tmp", 2 + c % 2), "modt", "mA"], writes=[(k, "f32", c)])
                if c == 7:
                    for s in range(n // 128):
                        gt = a // 128 + s
                        pl = PSB[5]

                        def mmfn(e, s=s):
                            ins = None
                            for kk in range(8):
                                ins = e.matmul(pl[:, 0:72], lhsT=f32t[:, kk, s * 128:(s + 1) * 128], rhs=wr[:, kk, :], start=(kk == 0), stop=(kk == 7))
                            return ins
                        P.op("pe", mmfn, reads=[(k, "f32", cc) for cc in range(8)] + [(k, "wr")], writes=[("psb", 5)])
                        R = [(k, "rt")]
                        V = lambda fn, extra_r=(): P.op("dve", fn, reads=R + list(extra_r), writes=R)
                        V(lambda e: e.tensor_tensor(out=lgs, in0=pl[:, 0:72], in1=brb, op=ALU.add), [("psb", 5), (k, "br")])
                        V(lambda e: e.tensor_reduce(out=sm[:, 0:1], in_=lgs[:, 0:8], axis=AX.X, op=ALU.max))
                        V(lambda e: e.tensor_scalar(out=oh, in0=lgs[:, 0:8], scalar1=sm[:, 0:1], scalar2=None, op0=ALU.is_equal))
                        V(lambda e: e.tensor_scalar(out=sm[:, 1:2], in0=sm[:, 0:1], scalar1=-1.0, scalar2=None, op0=ALU.mult))
                        P.op("act", lambda e: e.activation(out=sm[:, 8:16], in_=lgs[:, 0:8], func=AF.Exp, bias=sm[:, 1:2], scale=1.0), reads=R, writes=R)
                        V(lambda e: e.tensor_reduce(out=sm[:, 2:3], in_=sm[:, 8:16], axis=AX.X, op=ALU.add))
                        V(lambda e: e.reciprocal(out=sm[:, 3:4], in_=sm[:, 2:3]))
                        V(lambda e: e.tensor_scalar(out=oh, in0=oh, scalar1=BIG, scalar2=-BIG, op0=ALU.mult, op1=ALU.add))
                        for g in range(8):
                            V(lambda e, g=g: e.tensor_scalar(out=em[:, g * 8:(g + 1) * 8], in0=lgs[:, 8 + g * 8:16 + g * 8], scalar1=oh[:, g:g + 1], scalar2=None, op0=ALU.add))
                        V(lambda e: e.tensor_reduce(out=sm[:, 4:5], in_=em, axis=AX.X, op=ALU.max))
                        V(lambda e: e.tensor_scalar(out=oh1, in0=em, scalar1=sm[:, 4:5], scalar2=None, op0=ALU.is_equal))
                        V(lambda e: e.scalar_tensor_tensor(out=em, in0=oh1, scalar=-BIG, in1=em, op0=ALU.mult, op1=ALU.add))
                        V(lambda e: e.tensor_reduce(out=sm[:, 5:6], in_=em, axis=AX.X, op=ALU.max))
                        V(lambda e: e.tensor_scalar(out=oh2, in0=em, scalar1=sm[:, 5:6], scalar2=None, op0=ALU.is_equal))
                        V(lambda e: e.tensor_tensor(out=sm[:, 6:7], in0=sm[:, 5:6], in1=sm[:, 4:5], op=ALU.subtract))
                        P.op("act", lambda e: e.activation(out=sm[:, 6:7], in_=sm[:, 6:7], func=AF.Exp), reads=R, writes=R)
                        V(lambda e: e.tensor_scalar(out=sm[:, 6:7], in0=sm[:, 6:7], scalar1=1.0, scalar2=None, op0=ALU.add))
                        V(lambda e: e.reciprocal(out=sm[:, 6:7], in_=sm[:, 6:7]))
                        V(lambda e: e.tensor_tensor(out=sm[:, 6:7], in0=sm[:, 6:7], in1=sm[:, 3:4], op=ALU.mult))
                        V(lambda e: e.tensor_tensor(out=sm[:, 7:8], in0=sm[:, 3:4], in1=sm[:, 6:7], op=ALU.subtract))
                        V(lambda e: e.tensor_scalar(out=wt, in0=oh1, scalar1=sm[:, 6:7], scalar2=None, op0=ALU.mult))
                        V(lambda e: e.scalar_tensor_tensor(out=wt, in0=oh2, scalar=sm[:, 7:8], in1=wt, op0=ALU.mult, op1=ALU.add))
                        pt = PSB[4]
                        P.op("pe", lambda e: e.transpose(pt[0:64, 0:128], wt, ident[:]), reads=R + ["ident"], writes=[("psb", 4)])
                        P.op("act", lambda e, gt=gt: e.copy(out=wtT[:, gt * 128:(gt + 1) * 128], in_=pt[0:64, 0:128]), reads=[("psb", 4)], writes=[(k, "wtT", j)])

            norm_mod(1, tiles, extra=extra)

            for ex in range(64):
                eb = ex % 2
                P.op("pool", lambda e, ex=ex, eb=eb: e.dma_start(out=wgu[:, eb, :, :], in_=moe_wgu[i, ex].rearrange("(k p) n -> p k n", p=128)),
                     writes=[(k, "wgu", eb)], dma=True)
                P.op("pool", lambda e, ex=ex, eb=eb: e.dma_start(out=wd[:, eb, :, :], in_=moe_wd[i, ex].rearrange("(k p) n -> p k n", p=128)),
                     writes=[(k, "wd", eb)], dma=True)
                for j in tiles:
                    a, b = TT[j]
                    n = b - a
                    pw = PSB[6]
                    P.op("pe", lambda e, ex=ex, a=a, b=b, n=n: e.matmul(pw[:, :n], lhsT=ident[0:64, ex:ex + 1].to_broadcast([64, 128]), rhs=wtT[:, a:b], start=True, stop=True),
                         reads=["ident", (k, "wtT", j)], writes=[("psb", 6)])
                    P.op("act", lambda e, n=n: e.copy(out=wbc[:, :n], in_=pw[:, :n]), reads=[("psb", 6)], writes=[(k, "wbc")])
                    for fc in range(2):
                        pg, pu = PSB[0 + fc], PSB[2 + fc]
                        for part, pp, pk in ((0, pg, ("psb", 0 + fc)), (1, pu, ("psb", 2 + fc))):
                            def mmfn(e, part=part, pp=pp, a=a, b=b, n=n, eb=eb, fc=fc):
                                ins = None
                                for kk in range(8):
                                    ins = e.matmul(pp[:, :n], lhsT=wgu[:, eb, kk, part * 256 + fc * 128: part * 256 + (fc + 1) * 128], rhs=hT[:, kk, a:b],
                                                   start=(kk == 0), stop=(kk == 7))
                                return ins
                            P.op("pe", mmfn, reads=[(k, "wgu", eb)] + [("h", kk, j) for kk in range(8)], writes=[pk])
                        P.op("act", lambda e, pg=pg, fc=fc, n=n: e.activation(out=sgt[:, fc, :n], in_=pg[:, :n], func=AF.Silu), reads=[("psb", 0 + fc)], writes=[(k, "sgt", fc)])
                        P.op("dve", lambda e, pu=pu, fc=fc, n=n: e.tensor_tensor(out=sgt[:, fc, :n], in0=sgt[:, fc, :n], in1=pu[:, :n], op=ALU.mult), reads=[(k, "sgt", fc), ("psb", 2 + fc)], writes=[(k, "sgt", fc)])
                        P.op("pool", lambda e, fc=fc, n=n, eb=eb: e.tensor_tensor(out=actt[:, eb, fc, :n], in0=sgt[:, fc, :n], in1=wbc[:, :n], op=ALU.mult), reads=[(k, "sgt", fc), (k, "wbc")], writes=[(k, "actt", eb, fc)])
                    for oc in range(8):
                        py = PSB[4 + oc % 2]

                        def mmfn(e, py=py, oc=oc, n=n, eb=eb):
                            ins = None
                            for fc in range(2):
                                ins = e.matmul(py[:, :n], lhsT=wd[:, eb, fc, oc * 128:(oc + 1) * 128], rhs=actt[:, eb, fc, :n], start=(fc == 0), stop=(fc == 1))
                            return ins
                        P.op("pe", mmfn, reads=[(k, "wd", eb), (k, "actt", eb, 0), (k, "actt", eb, 1)], writes=[("psb", 4 + oc % 2)])
                        P.op("dve", lambda e, py=py, oc=oc, a=a, b=b, n=n, j=j: e.scalar_tensor_tensor(
                            out=xT[:, oc, a:b], in0=py[:, :n], scalar=modcol(5, oc, j), in1=xT[:, oc, a:b], op0=ALU.mult, op1=ALU.add),
                            reads=[("psb", 4 + oc % 2), "modt", ("x", oc, j)], writes=[("x", oc, j)])

        all_tiles = [0, 1, 2, 3, 4]
        for i in range(n_layers):
            last = i == DEPTH - 1
            compute_mod(i)
            phase()
            norm_mod(0, all_tiles)
            kind, jl = i % 3, i // 3
            if kind == 0:
                rglru(jl, all_tiles)
            elif kind == 1:
                mla(all_tiles)
            else:
                mlstm(all_tiles)
            if stop_after_mixer and i == n_layers - 1:
                break
            moe_sparse(i, [1, 2, 3, 4] if last else all_tiles)

        phase()
        stage = arA.alloc([128, 2, 1024], F32)
        outkeys = []
        for ti in range(18):
            jt = [j for j, (a, b) in enumerate(TT) if a <= ti * 128 < b][0]
            dst = octx_d[ti * 128:(ti + 1) * 128, :] if ti < 2 else out_d[(ti - 2) * 128:(ti - 1) * 128, :]
            sbuf = ti % 2
            for half in range(2):
                pb = PSB[half]

                def trfn(e, pb=pb, half=half, ti=ti):
                    ins = None
                    for q in range(4):
                        c = half * 4 + q
                        ins = e.transpose(pb[:, q * 128:(q + 1) * 128], xT[:, c, ti * 128:(ti + 1) * 128], ident[:])
                    return ins
                P.op("pe", trfn, reads=[("x", c, jt) for c in range(half * 4, half * 4 + 4)] + ["ident"], writes=[("psb", half)])
                P.op("dve" if half == 0 else "act",
                     lambda e, pb=pb, half=half, sbuf=sbuf: (e.tensor_copy if half == 0 else e.copy)(out=stage[:, sbuf, half * 512:(half + 1) * 512], in_=pb[:, :]),
                     reads=[("psb", half)], writes=[("ostage", sbuf, half)])
            P.op("sp", lambda e, dst=dst, sbuf=sbuf: e.dma_start(out=dst, in_=stage[:, sbuf, :]),
                 reads=[("ostage", sbuf, 0), ("ostage", sbuf, 1)], writes=[("out", ti)], dma=True)
            outkeys.append(("out", ti))
        P.finish(outkeys)
        P.emit()
    return nc


_CACHE = {}


def host_prep(inp, n_layers=DEPTH, stop_after_mixer=False):
    pv = pv_layout(inp)
    pvt = pv.table()
    key = (pvt.shape[1], n_layers, stop_after_mixer)
    if key not in _CACHE:
        _CACHE[key] = build(pv.off, pvt.shape[1], n_layers, stop_after_mixer)
    nc = _CACHE[key]
    f32 = lambda a: np.ascontiguousarray(np.asarray(a, np.float32))
    rC, rS, rRT = rope_tables()
    moec = np.zeros((128, 229), np.float32)
    moec[:, 193:229] = (np.arange(36, dtype=np.float32) * 128.0)[None, :]
    tp_, tt_ = np.meshgrid(np.arange(128), np.arange(128), indexing="ij")
    moec[:, 0:128] = (tp_ < tt_).astype(np.float32)
    moec[:, 128:192] = (np.arange(64, dtype=np.float32) * 128.0)[None, :]
    moec[:, 192] = np.arange(128, dtype=np.float32)
    selc = np.zeros((16, 2, 16, 64), np.float32)
    for r in range(16):
        selc[r, 0, r, :] = 1.0
        selc[r, 1, r, :] = -1.0
    selh = np.zeros((16, 2, 4), np.float32)
    for h in range(4):
        selh[4 + h, 0, h] = 1.0
        selh[12 + h, 1, h] = 1.0
    maskc = np.zeros((64, 2, 4, 64), np.float32)
    si, ti_ = np.meshgrid(np.arange(64), np.arange(64), indexing="ij")
    maskc[:, 0, :, :] = np.where(si <= ti_, 0.0, -30000.0)[:, None, :]
    maskc[:, 1, :, :] = np.where(si >= ti_, 0.0, -30000.0)[:, None, :]
    moe_wr = f32(np.concatenate([inp["moe_w_group"], inp["moe_w_expert"]], axis=2))
    br = np.concatenate([inp["moe_b_group"], inp["moe_b_expert"]], axis=1)
    moe_br = f32(np.repeat(br[:, None, :], 128, axis=1))
    shared = {
        "pv": pvt, "ident": np.eye(128, dtype=np.float32), "ada_w": f32(inp["ada_w"]),
        "rg_w_in": f32(inp["rg_w_in"]), "rg_gate_w": f32(inp["rg_gate_w"]), "rg_w_out": f32(inp["rg_w_out"]),
        "moe_wr": moe_wr, "moe_br": moe_br, "moec": moec,
        "mla_w_down": f32(inp["mla_w_down"][0]), "mla_w_uq": f32(inp["mla_w_uq"][0]), "mla_w_ukv": f32(inp["mla_w_ukv"][0]),
        "ml_w_in": f32(inp["ml_w_in"][0]), "ml_w_out": f32(inp["ml_w_out"][0]),
        "ml_gain": f32(np.repeat(np.asarray(inp["ml_out_norm"][0], np.float32)[None, :], 128, axis=0)),
        "ml_selc": selc, "ml_selh": selh, "ml_maskc": maskc,
        "mla_w_o": f32(inp["mla_w_o"][0]), "ropeC": rC, "ropeS": rS, "ropeRT": rRT,
        "moe_w_gate_up": f32(inp["moe_w_gate_up"]), "moe_w_down": f32(inp["moe_w_down"]),
    }
    in_maps = []
    for b in range(8):
        cc = np.zeros((128, 16), np.float32)
        cc[:, 0::2] = np.asarray(inp["c"][b], np.float32).reshape(8, 128).T
        cc[:, 1::2] = np.asarray(inp["c_ctx"], np.float32).reshape(8, 128).T
        m = dict(shared)
        m["x"] = f32(inp["x"][b])
        m["ctx"] = f32(inp["ctx"][b])
        m["cc"] = cc
        in_maps.append(m)
    return nc, in_maps


def kernel(**inputs):
    nc, in_maps = host_prep(inputs)
    res = run_bass_kernel_spmd(nc, in_maps, core_ids=list(range(8)))
    return np.stack([np.asarray(r["out"]) for r in res.results], axis=0).astype(np.float32)
```

```python
import numpy as np
from contextlib import ExitStack
import concourse.bass as bass
import concourse.mybir as mybir
from concourse.bass_utils import run_bass_kernel_spmd

F32 = mybir.dt.float32
BF16 = mybir.dt.bfloat16
I32 = mybir.dt.int32
AF = mybir.ActivationFunctionType
ALU = mybir.AluOpType
AX = mybir.AxisListType

NDMA_SEM = 32
D = 1024
NT = 2304
NCTX = 256
NLAT = 2048
TT = [(0, 256), (256, 768), (768, 1280), (1280, 1792), (1792, 2304)]
DEPTH = 4
BIG = 1.0e30


class Prog:
    ENGS = ("pe", "act", "dve", "pool", "sp")

    def __init__(self, nc, stack):
        self.nc = nc
        self.stack = stack
        self.sem = {e: stack.enter_context(nc.semaphore("s_" + e)) for e in self.ENGS}
        self.dsem = [stack.enter_context(nc.semaphore("d%d" % i)) for i in range(NDMA_SEM)]
        self.ops = {e: [] for e in self.ENGS}
        self.cnt = {e: 0 for e in self.ENGS}
        self.ndma = 0
        self.lw = {}
        self.rd = {}
        self.known = {e: {} for e in self.ENGS}
        self.final_waits = []
        self.pending = {e: [] for e in self.ENGS}

    def barrier(self):
        toks = [(e, self.cnt[e]) for e in self.ENGS if self.cnt[e] > 0]
        for j in range(NDMA_SEM):
            n = (self.ndma - j + NDMA_SEM - 1) // NDMA_SEM
            if n > 0:
                toks.append((("d", j), 16 * n))
        for e in self.ENGS:
            self.pending[e] = list(toks)

    def _semobj(self, sk):
        return self.sem[sk] if isinstance(sk, str) else self.dsem[sk[1]]

    def _need(self, eng, tok, waits):
        sk, v = tok
        if sk == "pe" and eng == "pe":
            return
        if self.known[eng].get(sk, 0) >= v:
            return
        if waits.get(sk, 0) < v:
            waits[sk] = v

    def op(self, eng, fn, reads=(), writes=(), dma=False):
        waits = {}
        for k in reads:
            t = self.lw.get(k)
            if t is not None:
                self._need(eng, t, waits)
        for k in writes:
            t = self.lw.get(k)
            if t is not None:
                self._need(eng, t, waits)
            for t in self.rd.get(k, ()):
                self._need(eng, t, waits)
        for tok in self.pending[eng]:
            self._need(eng, tok, waits)
        self.pending[eng] = []
        if dma:
            j = self.ndma % NDMA_SEM
            rnd = self.ndma // NDMA_SEM
            self.ndma += 1
            sk = ("d", j)
            if rnd > 0:
                self._need(eng, (sk, 16 * rnd), waits)
            tok = (sk, 16 * (rnd + 1))
        else:
            self.cnt[eng] += 1
            tok = (eng, self.cnt[eng])
        for sk, v in waits.items():
            self.known[eng][sk] = v
        self.ops[eng].append((list(waits.items()), fn, tok))
        for k in writes:
            self.lw[k] = tok
            self.rd[k] = []
        for k in reads:
            if k in writes:
                continue
            self.rd.setdefault(k, []).append(tok)
        return tok

    def finish(self, keys):
        waits = {}
        for k in keys:
            t = self.lw.get(k)
            if t is not None:
                self._need("sp", t, waits)
        self.final_waits = list(waits.items())

    def emit(self):
        nc = self.nc
        with nc.Block() as block:
            def mk(e):
                def body(engobj):
                    for waits, fn, tok in self.ops[e]:
                        for sk, v in waits:
                            engobj.wait_ge(self._semobj(sk), v)
                        ins = fn(engobj)
                        ins.then_inc(self._semobj(tok[0]), 1 if isinstance(tok[0], str) else 16)
                    if e == "sp":
                        for sk, v in self.final_waits:
                            engobj.wait_ge(self._semobj(sk), v)
                return body
            block.tensor(mk("pe"))
            block.scalar(mk("act"))
            block.vector(mk("dve"))
            block.gpsimd(mk("pool"))
            block.sync(mk("sp"))

    def sb(self, name, shape, dt):
        return self.stack.enter_context(self.nc.sbuf_tensor(name, shape, dt))

    def ps(self, name, shape, dt):
        return self.stack.enter_context(self.nc.psum_tensor(name, shape, dt))


class PV:
    def __init__(self):
        self.cols = []
        self.off = {}
        self.n = 0

    def add(self, name, v):
        v = np.asarray(v, np.float32).reshape(-1)
        assert v.size % 128 == 0
        a = v.reshape(-1, 128).T
        self.off[name] = self.n
        self.cols.append(a)
        self.n += a.shape[1]

    def table(self):
        return np.ascontiguousarray(np.concatenate(self.cols, axis=1))


def pv_layout(inp):
    pv = PV()
    for i in range(DEPTH):
        pv.add("nmix%d" % i, inp["norm_mix"][i])
        pv.add("nffn%d" % i, inp["norm_ffn"][i])
        ab = inp["ada_b"][i].reshape(48, 128)
        ab2 = np.repeat(ab[:, None, :], 2, axis=1)
        pv.add("adab%d" % i, ab2.reshape(-1))
    for j in range(2):
        for k in range(4):
            pv.add("rgcw%d_%d" % (j, k), inp["rg_conv_w"][j, k])
        pv.add("rgcb%d" % j, inp["rg_conv_b"][j])
        for dr in range(2):
            for g in range(2):
                pv.add("rggb%d_%d_%d" % (j, dr, g), inp["rg_gate_b"][j, dr, g])
            pv.add("rglam%d_%d" % (j, dr), inp["rg_lambda"][j, dr])
    pad = lambda v: np.concatenate([np.asarray(v, np.float32).reshape(-1), np.zeros(128 - np.asarray(v).size % 128 if np.asarray(v).size % 128 else 0, np.float32)])
    pv.add("mla_qn", inp["mla_q_norm"][0])
    pv.add("mla_kvn", inp["mla_kv_norm"][0])
    pv.add("mla_qkn0", pad(inp["mla_qk_norm"][0, 0]))
    pv.add("mla_qkn1", pad(inp["mla_qk_norm"][0, 1]))
    pv.add("ml_gb", pad(inp["ml_gate_b"][0].reshape(-1)))
    return pv


def rope_tables():
    L = NLAT
    rows = L // 64
    row = np.broadcast_to(np.arange(rows, dtype=np.float32)[:, None], (rows, 64)).reshape(L)
    col = np.broadcast_to(np.arange(64, dtype=np.float32)[None, :], (rows, 64)).reshape(L)
    inv_freq = (np.float32(10000.0) ** (-np.arange(0, 16, 2, dtype=np.float32) / np.float32(16))).astype(np.float32)
    ar = (row[:, None] * inv_freq).astype(np.float32)
    ac = (col[:, None] * inv_freq).astype(np.float32)
    C = np.ones((96, NT), np.float32)
    S = np.zeros((96, NT), np.float32)
    for base, ang in ((64, ar), (80, ac)):
        C[base:base + 8, NCTX:] = np.cos(ang).T
        C[base + 8:base + 16, NCTX:] = np.cos(ang).T
        S[base:base + 8, NCTX:] = np.sin(ang).T
        S[base + 8:base + 16, NCTX:] = np.sin(ang).T
    R = np.zeros((96, 96), np.float32)
    for base in (64, 80):
        for j in range(8):
            R[base + j, base + 8 + j] = -1.0
            R[base + 8 + j, base + j] = 1.0
    return C, S, np.ascontiguousarray(R.T)


def build(pvoff, npv, n_layers=DEPTH, stop_after_mixer=False):
    nc = bass.Bass("TRN2", target_bir_lowering=False)

    def din(name, shape, dt=F32):
        return nc.dram_tensor(name, list(shape), dt, kind="ExternalInput").ap()

    x_d = din("x", [NLAT, D])
    ctx_d = din("ctx", [NCTX, D])
    cc_d = din("cc", [128, 16])
    pv_d = din("pv", [128, npv])
    ident_d = din("ident", [128, 128])
    ada_w = din("ada_w", [DEPTH, D, 6 * D])
    rg_w_in = din("rg_w_in", [2, D, 2 * D])
    rg_gate_w = din("rg_gate_w", [2, 2, 2, 16, 64, 64])
    rg_w_out = din("rg_w_out", [2, D, D])
    mla_wdn = din("mla_w_down", [D, 416])
    mla_wuq = din("mla_w_uq", [256, 1536])
    mla_wukv = din("mla_w_ukv", [128, 2048])
    mla_wo = din("mla_w_o", [D, D])
    ropeC_d = din("ropeC", [96, NT])
    ropeS_d = din("ropeS", [96, NT])
    ropeRT_d = din("ropeRT", [96, 96])
    ml_win = din("ml_w_in", [D, 3088])
    ml_wout = din("ml_w_out", [D, D])
    ml_gain = din("ml_gain", [128, D])
    ml_selc = din("ml_selc", [16, 2, 16, 64])
    ml_selh = din("ml_selh", [16, 2, 4])
    ml_maskc = din("ml_maskc", [64, 2, 4, 64])
    hdir = nc.dram_tensor("hdir_scratch", [2, NT, D], BF16).ap()
    moec_d = din("moec", [128, 128 + 64 + 1 + 36])
    NSLOT = 12800
    xslots = nc.dram_tensor("xslots_scratch", [NSLOT, D], BF16).ap()
    yslots = nc.dram_tensor("yslots_scratch", [NSLOT, D], BF16).ap()
    moe_wr = din("moe_wr", [DEPTH, D, 72])
    moe_br = din("moe_br", [DEPTH, 128, 72])
    moe_wgu = din("moe_w_gate_up", [DEPTH, 64, D, 512])
    moe_wd = din("moe_w_down", [DEPTH, 64, 256, D])
    out_d = nc.dram_tensor("out", [NLAT, D], F32, kind="ExternalOutput").ap()
    octx_d = nc.dram_tensor("octx", [NCTX, D], F32, kind="ExternalOutput").ap()

    with ExitStack() as st:
        P = Prog(nc, st)
        xT = P.sb("xT", [128, 8, NT], F32)
        hT = P.sb("hT", [128, 8, NT], BF16)
        mT = P.sb("mT", [128, 8, NT], BF16)
        pvt = P.sb("pvt", [128, npv], F32)
        ident = P.sb("ident_sb", [128, 128], F32)
        onesm = P.sb("onesm", [128, 128], F32)
        cst = P.sb("cst", [128, 4], F32)
        cct = P.sb("cct", [128, 16], F32)
        modt = P.sb("modt", [128, 48, 2], F32)
        mA = P.sb("mA", [128, 2, 8, 2], F32)
        tmp = [P.sb("tmp%d" % i, [128, 512], F32) for i in range(6)]
        rstd = P.sb("rstd", [128, 512], F32)
        SCRW = 11400
        scr = P.sb("scr", [128, SCRW], F32)
        mTflat = mT[:].rearrange("p c t -> p (c t)")
        hTflat = hT[:].rearrange("p c t -> p (c t)")
        PSB = [P.ps("psb%d" % i, [128, 512], F32) for i in range(8)]

        class Arena:
            def __init__(self, kind):
                self.kind = kind
                self.off = 0

            def reset(self):
                self.off = 0

            def alloc(self, shape, dt):
                n = 1
                for d_ in shape[1:]:
                    n *= d_
                nf32 = n if dt in (F32, I32) else (n + 1) // 2
                o = self.off
                self.off += nf32
                if self.kind == "A":
                    assert self.off <= SCRW, ("arena A overflow", self.off)
                    v = scr[0:shape[0], o:o + nf32]
                    if dt != F32:
                        v = v.bitcast(dt)[:, 0:n]
                else:
                    assert self.off * 2 <= 8 * NT, ("arena B/C overflow", self.off)
                    flat = mTflat if self.kind == "B" else hTflat
                    v = flat[0:shape[0], 2 * o:2 * o + 2 * nf32]
                    if dt == F32:
                        v = v.bitcast(F32)
                    else:
                        v = v[:, 0:n]
                if len(shape) == 3:
                    v = v.rearrange("p (a b) -> p a b", a=shape[1])
                elif len(shape) == 4:
                    v = v.rearrange("p (a b c) -> p a b c", a=shape[1], b=shape[2])
                return v

        arA, arB, arC = Arena("A"), Arena("B"), Arena("C")
        _regs = {}

        def getreg(e, v):
            if v not in _regs:
                _regs[v] = e.to_reg(v)
            return _regs[v]

        def phase():
            P.barrier()
            arA.reset()
            arB.reset()
            arC.reset()

        def pvc(name, k=0, n=1):
            o = pvoff[name] + k
            return pvt[:, o:o + n]

        P.op("sp", lambda e: e.dma_start(out=pvt[:], in_=pv_d), writes=["pvt"], dma=True)
        P.op("sp", lambda e: e.dma_start(out=ident[:], in_=ident_d), writes=["ident"], dma=True)
        P.op("sp", lambda e: e.dma_start(out=cct[:], in_=cc_d), writes=["cct"], dma=True)
        P.op("pool", lambda e: e.memset(onesm[:], 1.0 / 1024.0), writes=["onesm"])
        P.op("pool", lambda e: e.memset(cst[:, 0:1], 1e-6), writes=["cst0"])
        P.op("pool", lambda e: e.memset(cst[:, 1:2], 1.0), writes=["cst1"])
        P.op("act", lambda e: e.activation(out=cct[:], in_=cct[:], func=AF.Silu), reads=["cct"], writes=["cct"])

        stage = arA.alloc([128, 2, 1024], F32)
        for ti in range(18):
            src = ctx_d[ti * 128:(ti + 1) * 128, :] if ti < 2 else x_d[(ti - 2) * 128:(ti - 1) * 128, :]
            sbuf = ti % 2
            P.op("sp", lambda e, src=src, sbuf=sbuf: e.dma_start(out=stage[:, sbuf, :], in_=src), writes=[("stage", sbuf)], dma=True)
            jt = [j for j, (a, b) in enumerate(TT) if a <= ti * 128 < b][0]
            for half in range(2):
                pb = PSB[half]

                def trfn(e, pb=pb, half=half, sbuf=sbuf):
                    ins = None
                    for q in range(4):
                        c = half * 4 + q
                        ins = e.transpose(pb[:, q * 128:(q + 1) * 128], stage[:, sbuf, c * 128:(c + 1) * 128], ident[:])
                    return ins
                P.op("pe", trfn, reads=[("stage", sbuf), "ident"], writes=[("psb", half)])
                P.op("dve" if half == 0 else "act",
                     lambda e, pb=pb, half=half, ti=ti: (e.tensor_copy if half == 0 else e.copy)(
                         out=xT[:, half * 4:half * 4 + 4, ti * 128:(ti + 1) * 128], in_=pb[:].rearrange("p (q t) -> p q t", q=4)),
                     reads=[("psb", half)], writes=[("x", c, jt) for c in range(half * 4, half * 4 + 4)])

        def compute_mod(i):
            phase()
            adaw = arA.alloc([128, 5, 8, 256], F32)
            pm = PSB[7]
            for piece in range(24):
                bsel = piece % 5
                P.op("sp", lambda e, piece=piece, bsel=bsel: e.dma_start(
                    out=adaw[:, bsel, :, :], in_=ada_w[i, :, piece * 256:(piece + 1) * 256].rearrange("(k p) n -> p k n", p=128)),
                    writes=[("adaw", bsel)], dma=True)

                def mmfn(e, bsel=bsel, piece=piece):
                    ins = None
                    for sub in range(2):
                        oc = piece * 2 + sub
                        for k in range(8):
                            ins = e.matmul(pm[:, oc * 2:oc * 2 + 2], lhsT=adaw[:, bsel, k, sub * 128:(sub + 1) * 128],
                                           rhs=cct[:, k * 2:k * 2 + 2], start=(k == 0), stop=(k == 7))
                    return ins
                P.op("pe", mmfn, reads=[("adaw", bsel), "cct"], writes=[("psb", 7)])
            ab = pvt[:, pvoff["adab%d" % i]:pvoff["adab%d" % i] + 96]
            P.op("dve", lambda e: e.tensor_tensor(out=modt[:].rearrange("p a b -> p (a b)"), in0=pm[:, 0:96], in1=ab, op=ALU.add),
                 reads=[("psb", 7), "pvt"], writes=["modt"])
            for w, (nm, m) in enumerate((("nmix%d" % i, 1), ("nffn%d" % i, 4))):
                for j2 in range(2):
                    P.op("dve", lambda e, w=w, nm=nm, m=m, j2=j2: e.scalar_tensor_tensor(
                        out=mA[:, w, :, j2], in0=modt[:, m * 8:m * 8 + 8, j2], scalar=1.0, in1=pvc(nm, 0, 8),
                        op0=ALU.add, op1=ALU.mult),
                        reads=["modt", "pvt"], writes=["mA"])

        def modcol(m, c, j):
            jj = 1 if j == 0 else 0
            return modt[:, m * 8 + c, jj:jj + 1]

        def norm_mod(w, tiles, extra=None):
            mshift = 0 if w == 0 else 3
            for j in tiles:
                a, b = TT[j]
                n = b - a
                jj = 1 if j == 0 else 0
                pst = PSB[6]
                for c in range(8):
                    tb = tmp[c % 2]
                    P.op("act", lambda e, tb=tb, c=c, a=a, b=b, n=n: e.activation(out=tb[:, :n], in_=xT[:, c, a:b], func=AF.Square),
                         reads=[("x", c, j)], writes=[("tmp", c % 2)])
                    P.op("pe", lambda e, tb=tb, c=c, n=n: e.matmul(pst[:, :n], lhsT=onesm[:], rhs=tb[:, :n], start=(c == 0), stop=(c == 7)),
                         reads=[("tmp", c % 2), "onesm"], writes=[("psb", 6)])
                P.op("act", lambda e, n=n: e.activation(out=rstd[:, :n], in_=pst[:, :n], func=AF.Ln, bias=cst[:, 0:1], scale=1.0),
                     reads=[("psb", 6), "cst0"], writes=["rstd"])
                P.op("act", lambda e, n=n: e.activation(out=rstd[:, :n], in_=rstd[:, :n], func=AF.Exp, scale=-0.5), reads=["rstd"], writes=["rstd"])
                for c in range(8):
                    tb = tmp[2 + c % 2]
                    P.op("dve", lambda e, tb=tb, c=c, a=a, b=b, n=n: e.tensor_tensor(out=tb[:, :n], in0=xT[:, c, a:b], in1=rstd[:, :n], op=ALU.mult),
                         reads=[("x", c, j), "rstd"], writes=[("tmp", 2 + c % 2)])
                    P.op("act", lambda e, tb=tb, c=c, a=a, b=b, n=n, jj=jj: e.activation(
                        out=hT[:, c, a:b], in_=tb[:, :n], func=AF.Identity,
                        bias=modt[:, mshift * 8 + c, jj:jj + 1], scale=mA[:, w, c, jj:jj + 1]),
                        reads=[("tmp", 2 + c % 2), "modt", "mA"], writes=[("h", c, j)])
                    if extra is not None:
                        extra(j, c, tb)

        def rglru(jl, tiles_all):
            phase()
            k = "rg%d" % jl
            win = arA.alloc([128, 2, 8, 256], BF16)
            wout = arA.alloc([128, 2, 8, 128], BF16)
            gw = arA.alloc([128, 4, 128], BF16)
            gwf = arA.alloc([128, 4, 128], F32)
            clam = arA.alloc([128, 2, 2, 8], F32)
            uh = arA.alloc([128, NT], F32)
            ucv = arA.alloc([128, NT], F32)
            ub = arA.alloc([128, NT], BF16)
            hb = arA.alloc([128, 2, 512], F32)
            for dr in range(2):
                lam = pvc("rglam%d_%d" % (jl, dr), 0, 8)
                P.op("act", lambda e, dr=dr, lam=lam: e.activation(out=clam[:, dr, 0, :], in_=lam, func=AF.Exp, scale=-1.0),
                     reads=["pvt"], writes=[(k, "clam")])
                P.op("act", lambda e, dr=dr: e.activation(out=clam[:, dr, 0, :], in_=clam[:, dr, 0, :], func=AF.Ln, bias=cst[:, 1:2], scale=1.0),
                     reads=[(k, "clam"), "cst1"], writes=[(k, "clam")])
                P.op("dve", lambda e, dr=dr: e.tensor_scalar(out=clam[:, dr, 1, :], in0=clam[:, dr, 0, :], scalar1=-16.0, scalar2=None, op0=ALU.mult),
                     reads=[(k, "clam")], writes=[(k, "clam")])
                P.op("dve", lambda e, dr=dr: e.tensor_scalar(out=clam[:, dr, 0, :], in0=clam[:, dr, 0, :], scalar1=-8.0, scalar2=None, op0=ALU.mult),
                     reads=[(k, "clam")], writes=[(k, "clam")])
            for cc in range(8):
                wb_ = cc % 2
                for part in range(2):
                    P.op("pool", lambda e, part=part, wb_=wb_, cc=cc: e.dma_start(
                        out=win[:, wb_, :, part * 128:(part + 1) * 128],
                        in_=rg_w_in[jl, :, part * 1024 + cc * 128: part * 1024 + (cc + 1) * 128].rearrange("(k p) n -> p k n", p=128)),
                        writes=[(k, "win", wb_, part)], dma=True)
                subk = [(k, "gwf", q) for q in range(8)]
                P.op("pool", lambda e: e.memset(gwf[:, :, :], 0.0), writes=subk)
                for dr in range(2):
                    for g in range(2):
                        for blk in range(2):
                            P.op("sp", lambda e, dr=dr, g=g, blk=blk, cc=cc: e.dma_start(
                                out=gwf[blk * 64:(blk + 1) * 64, dr * 2 + g, blk * 64:(blk + 1) * 64],
                                in_=rg_gate_w[jl, dr, g, cc * 2 + blk, :, :]),
                                writes=[(k, "gwf", dr * 4 + g * 2 + blk)], dma=True)
                P.op("pool", lambda e: e.tensor_copy(out=gw[:, :, :], in_=gwf[:, :, :]), reads=subk, writes=[(k, "gw")])
                for j in tiles_all:
                    a, b = TT[j]
                    n = b - a
                    pg, pu = PSB[0 + j % 2], PSB[2 + j % 2]
                    for part, pp, pk in ((0, pg, ("psb", 0 + j % 2)), (1, pu, ("psb", 2 + j % 2))):
                        def mmfn(e, part=part, pp=pp, a=a, b=b, n=n, wb_=wb_):
                            ins = None
                            for kk in range(8):
                                ins = e.matmul(pp[:, :n], lhsT=win[:, wb_, kk, part * 128:(part + 1) * 128], rhs=hT[:, kk, a:b],
                                               start=(kk == 0), stop=(kk == 7))
                            return ins
                        P.op("pe", mmfn, reads=[(k, "win", wb_, part)] + [("h", kk, j) for kk in range(8)], writes=[pk])
                    t0, t1 = tmp[0], tmp[1]
                    pgk = ("psb", 0 + j % 2)
                    P.op("act", lambda e, pg=pg, n=n: e.activation(out=t0[:, :n], in_=pg[:, :n], func=AF.Square),
                         reads=[pgk], writes=[("tmp", 0)])
                    P.op("dve", lambda e, n=n: e.tensor_scalar(out=t0[:, :n], in0=t0[:, :n], scalar1=0.044715, scalar2=1.0, op0=ALU.mult, op1=ALU.add),
                         reads=[("tmp", 0)], writes=[("tmp", 0)])
                    P.op("dve", lambda e, pg=pg, n=n: e.tensor_tensor(out=t0[:, :n], in0=t0[:, :n], in1=pg[:, :n], op=ALU.mult),
                         reads=[("tmp", 0), pgk], writes=[("tmp", 0)])
                    P.op("act", lambda e, n=n: e.activation(out=t1[:, :n], in_=t0[:, :n], func=AF.Sigmoid, scale=1.5957691216),
                         reads=[("tmp", 0)], writes=[("tmp", 1)])
                    P.op("dve", lambda e, pg=pg, n=n, a=a, b=b, cc=cc: e.tensor_tensor(out=mT[:, cc, a:b], in0=t1[:, :n], in1=pg[:, :n], op=ALU.mult),
                         reads=[("tmp", 1), pgk], writes=[("m", cc, j)])
                    P.op("act", lambda e, pu=pu, n=n, a=a, b=b: e.copy(out=uh[:, a:b], in_=pu[:, :n]),
                         reads=[("psb", 2 + j % 2)], writes=[(k, "uh")])
                P.op("act", lambda e, cc=cc: e.activation(out=ucv[:, :], in_=uh[:, :], func=AF.Identity,
                                                           bias=pvc("rgcb%d" % jl, cc), scale=pvc("rgcw%d_2" % jl, cc)),
                     reads=[(k, "uh"), "pvt"], writes=[(k, "ucv")])
                for (s0, s1) in ((0, NCTX), (NCTX, NT)):
                    for tap, off in ((0, -2), (1, -1), (3, 1)):
                        lo = max(s0, s0 - off)
                        hi = min(s1, s1 - off)
                        P.op("dve", lambda e, lo=lo, hi=hi, off=off, tap=tap, cc=cc: e.scalar_tensor_tensor(
                            out=ucv[:, lo:hi], in0=uh[:, lo + off:hi + off], scalar=pvc("rgcw%d_%d" % (jl, tap), cc),
                            in1=ucv[:, lo:hi], op0=ALU.mult, op1=ALU.add),
                            reads=[(k, "uh"), (k, "ucv"), "pvt"], writes=[(k, "ucv")])
                P.op("pool", lambda e: e.tensor_copy(out=ub[:, :], in_=ucv[:, :]), reads=[(k, "ucv")], writes=[(k, "ub")])
                for dr in range(2):
                    order = tiles_all if dr == 0 else [tiles_all[0]] + list(reversed(tiles_all[1:]))
                    prev = None
                    groups = [order[g:g + 2] for g in range(0, len(order), 2)]
                    oi = 0
                    for grp in groups:
                        info = []
                        for gi, j in enumerate(grp):
                            a, b = TT[j]
                            n = b - a
                            tr, ta2, ti_ = tmp[3 * gi + 0], tmp[3 * gi + 1], tmp[3 * gi + 2]
                            kr, k2, ki_ = ("tmp", 3 * gi + 0), ("tmp", 3 * gi + 1), ("tmp", 3 * gi + 2)
                            pr = PSB[4 + gi]
                            pi = PSB[6 + gi]
                            prk = ("psb", 4 + gi)
                            pik = ("psb", 6 + gi)
                            P.op("pe", lambda e, pr=pr, dr=dr, a=a, b=b, n=n: e.matmul(pr[:, :n], lhsT=gw[:, dr * 2 + 0, :], rhs=ub[:, a:b], start=True, stop=True),
                                 reads=[(k, "gw"), (k, "ub")], writes=[prk])
                            P.op("pe", lambda e, pi=pi, dr=dr, a=a, b=b, n=n: e.matmul(pi[:, :n], lhsT=gw[:, dr * 2 + 1, :], rhs=ub[:, a:b], start=True, stop=True),
                                 reads=[(k, "gw"), (k, "ub")], writes=[pik])
                            P.op("act", lambda e, pr=pr, n=n, dr=dr, cc=cc, tr=tr: e.activation(out=tr[:, :n], in_=pr[:, :n], func=AF.Sigmoid, bias=pvc("rggb%d_%d_0" % (jl, dr), cc), scale=1.0),
                                 reads=[prk, "pvt"], writes=[kr])
                            P.op("act", lambda e, pi=pi, n=n, dr=dr, cc=cc, ti_=ti_: e.activation(out=ti_[:, :n], in_=pi[:, :n], func=AF.Sigmoid, bias=pvc("rggb%d_%d_1" % (jl, dr), cc), scale=1.0),
                                 reads=[pik, "pvt"], writes=[ki_])
                            info.append((j, a, b, n, tr, ta2, ti_, kr, k2, ki_))
                        for (j, a, b, n, tr, ta2, ti_, kr, k2, ki_) in info:
                            P.op("act", lambda e, n=n, dr=dr, cc=cc, tr=tr: e.activation(out=tr[:, :n], in_=tr[:, :n], func=AF.Exp, scale=clam[:, dr, 0, cc:cc + 1]),
                                 reads=[kr, (k, "clam")], writes=[kr])
                            P.op("act", lambda e, n=n, tr=tr, ta2=ta2: e.activation(out=ta2[:, :n], in_=tr[:, :n], func=AF.Square), reads=[kr], writes=[k2])
                            P.op("act", lambda e, n=n, ta2=ta2: e.activation(out=ta2[:, :n], in_=ta2[:, :n], func=AF.Ln, bias=cst[:, 1:2], scale=-1.0), reads=[k2, "cst1"], writes=[k2])
                            P.op("act", lambda e, n=n, ta2=ta2: e.activation(out=ta2[:, :n], in_=ta2[:, :n], func=AF.Exp, scale=0.5), reads=[k2], writes=[k2])
                        for (j, a, b, n, tr, ta2, ti_, kr, k2, ki_) in info:
                            P.op("dve", lambda e, n=n, a=a, b=b, ti_=ti_: e.tensor_tensor(out=ti_[:, :n], in0=ti_[:, :n], in1=ucv[:, a:b], op=ALU.mult),
                                 reads=[ki_, (k, "ucv")], writes=[ki_])
                            P.op("dve", lambda e, n=n, ti_=ti_, ta2=ta2: e.tensor_tensor(out=ti_[:, :n], in0=ti_[:, :n], in1=ta2[:, :n], op=ALU.mult),
                                 reads=[ki_, k2], writes=[ki_])
                            if dr == 0:
                                init = 0.0 if prev is None else uh[:, a - 1:a]
                                P.op("dve", lambda e, n=n, a=a, b=b, init=init, tr=tr, ti_=ti_: e.tensor_tensor_scan(out=uh[:, a:b], data0=tr[:, :n], data1=ti_[:, :n], initial=init, op0=ALU.mult, op1=ALU.add),
                                     reads=[kr, ki_, (k, "uh"), (k, "ucv")], writes=[(k, "uh")])
                            else:
                                hbb = oi % 2
                                init = 0.0 if prev is None else hb[:, 1 - hbb, 0:1]
                                P.op("dve", lambda e, n=n, hbb=hbb, init=init, tr=tr, ti_=ti_: e.tensor_tensor_scan(out=hb[:, hbb, 0:n][:, ::-1], data0=tr[:, 0:n][:, ::-1], data1=ti_[:, 0:n][:, ::-1], initial=init, op0=ALU.mult, op1=ALU.add),
                                     reads=[kr, ki_, (k, "hb", 1 - hbb)], writes=[(k, "hb", hbb)])
                                P.op("dve", lambda e, n=n, a=a, b=b, hbb=hbb, tr=tr: e.tensor_tensor(out=tr[:, :n], in0=hb[:, hbb, 0:n], in1=uh[:, a:b], op=ALU.add),
                                     reads=[(k, "hb", hbb), (k, "uh")], writes=[kr])
                                P.op("dve", lambda e, n=n, a=a, b=b, cc=cc, tr=tr: e.tensor_tensor(out=mT[:, cc, a:b], in0=tr[:, :n], in1=mT[:, cc, a:b], op=ALU.mult),
                                     reads=[kr, ("m", cc, j)], writes=[("m", cc, j)])
                            prev = j
                            oi += 1
            for oc in range(8):
                ob = oc % 2
                P.op("pool", lambda e, oc=oc, ob=ob: e.dma_start(out=wout[:, ob, :, :], in_=rg_w_out[jl, :, oc * 128:(oc + 1) * 128].rearrange("(k p) n -> p k n", p=128)),
                     writes=[(k, "wout", ob)], dma=True)
                for j in tiles_all:
                    a, b = TT[j]
                    n = b - a
                    py = PSB[j % 4]

                    def mmfn(e, py=py, ob=ob, a=a, b=b, n=n):
                        ins = None
                        for cc in range(8):
                            ins = e.matmul(py[:, :n], lhsT=wout[:, ob, cc, :], rhs=mT[:, cc, a:b], start=(cc == 0), stop=(cc == 7))
                        return ins
                    P.op("pe", mmfn, reads=[(k, "wout", ob)] + [("m", cc, j) for cc in range(8)], writes=[("psb", j % 4)])
                    P.op("dve", lambda e, py=py, oc=oc, a=a, b=b, n=n, j=j: e.scalar_tensor_tensor(
                        out=xT[:, oc, a:b], in0=py[:, :n], scalar=modcol(2, oc, j), in1=xT[:, oc, a:b], op0=ALU.mult, op1=ALU.add),
                        reads=[("psb", j % 4), "modt", ("x", oc, j)], writes=[("x", oc, j)])

        def mla(tiles_all):
            phase()
            k = "mla"
            SC = 96 ** -0.5
            cqn = arA.alloc([128, 2, NT], BF16)
            ckvn = arA.alloc([128, NT], BF16)
            krope = arA.alloc([96, NT], F32)
            wuq = arA.alloc([128, 2, 1536], BF16)
            wukv = arA.alloc([128, 2048], BF16)
            ones96 = arA.alloc([96, 96], BF16)
            RTf = arA.alloc([96, 96], F32)
            RT = arA.alloc([96, 96], BF16)
            ones1r = arA.alloc([65, 64], F32)
            rr = arA.alloc([65, 512], F32)
            onesb = arA.alloc([128, 64], BF16)
            wdn = arB.alloc([128, 8, 416], BF16)
            wo = arB.alloc([64, 2, 1024], BF16)
            tC = arB.alloc([96, NT], F32)
            tS = arB.alloc([96, NT], F32)
            P.op("pool", lambda e: e.dma_start(out=wdn, in_=mla_wdn.rearrange("(k p) n -> p k n", p=128)), writes=[(k, "wdn")], dma=True)
            P.op("pool", lambda e: e.dma_start(out=wuq, in_=mla_wuq.rearrange("(k p) n -> p k n", p=128)), writes=[(k, "wuq")], dma=True)
            P.op("pool", lambda e: e.dma_start(out=wukv, in_=mla_wukv), writes=[(k, "wukv")], dma=True)
            P.op("sp", lambda e: e.dma_start(out=tC, in_=ropeC_d), writes=[(k, "tC")], dma=True)
            P.op("sp", lambda e: e.dma_start(out=tS, in_=ropeS_d), writes=[(k, "tS")], dma=True)
            P.op("sp", lambda e: e.dma_start(out=RTf, in_=ropeRT_d), writes=[(k, "RTf")], dma=True)
            P.op("pool", lambda e: e.tensor_copy(out=RT, in_=RTf), reads=[(k, "RTf")], writes=[(k, "RT")])
            P.op("pool", lambda e: e.memset(ones1r, 1.0), writes=[(k, "ones1r")])
            P.op("pool", lambda e: e.memset(ones96, 1.0 / 96.0), writes=[(k, "ones96")])
            P.op("pool", lambda e: e.memset(onesb, 1.0), writes=[(k, "onesb")])
            for j in tiles_all:
                a, b = TT[j]
                n = b - a
                specs = ((0, 0, 128), (1, 128, 128), (2, 256, 128), (3, 320, 96))
                for bi, c0, m in specs:
                    def mmfn(e, bi=bi, c0=c0, m=m, a=a, b=b, n=n):
                        ins = None
                        for kk in range(8):
                            ins = e.matmul(PSB[bi][0:m, :n], lhsT=wdn[:, kk, c0:c0 + m], rhs=hT[:, kk, a:b], start=(kk == 0), stop=(kk == 7))
                        return ins
                    P.op("pe", mmfn, reads=[(k, "wdn")] + [("h", kk, j) for kk in range(8)], writes=[("psb", bi)])
                P.op("act", lambda e, a=a, b=b, n=n: e.copy(out=krope[64:96, a:b], in_=PSB[3][64:96, :n]), reads=[("psb", 3)], writes=[(k, "krope", j)])
                for c in range(2):
                    P.op("act", lambda e, c=c, n=n: e.activation(out=tmp[c][:, :n], in_=PSB[c][:, :n], func=AF.Square), reads=[("psb", c)], writes=[("tmp", c)])
                    P.op("pe", lambda e, c=c, n=n: e.matmul(PSB[4][:, :n], lhsT=onesm[:], rhs=tmp[c][:, :n], start=(c == 0), stop=(c == 1)),
                         reads=[("tmp", c), "onesm"], writes=[("psb", 4)])
                P.op("act", lambda e, n=n: e.activation(out=rstd[:, :n], in_=PSB[4][:, :n], func=AF.Ln, bias=cst[:, 0:1], scale=4.0), reads=[("psb", 4), "cst0"], writes=["rstd"])
                P.op("act", lambda e, n=n: e.activation(out=rstd[:, :n], in_=rstd[:, :n], func=AF.Exp, scale=-0.5), reads=["rstd"], writes=["rstd"])
                for c in range(2):
                    P.op("dve", lambda e, c=c, n=n: e.tensor_tensor(out=tmp[2 + c][:, :n], in0=PSB[c][:, :n], in1=rstd[:, :n], op=ALU.mult), reads=[("psb", c), "rstd"], writes=[("tmp", 2 + c)])
                    P.op("act", lambda e, c=c, a=a, b=b, n=n: e.activation(out=cqn[:, c, a:b], in_=tmp[2 + c][:, :n], func=AF.Identity, scale=pvc("mla_qn", c)), reads=[("tmp", 2 + c), "pvt"], writes=[(k, "cqn", j)])
                P.op("act", lambda e, n=n: e.activation(out=tmp[4][:, :n], in_=PSB[2][:, :n], func=AF.Square), reads=[("psb", 2)], writes=[("tmp", 4)])
                P.op("pe", lambda e, n=n: e.matmul(PSB[5][:, :n], lhsT=onesm[:], rhs=tmp[4][:, :n], start=True, stop=True), reads=[("tmp", 4), "onesm"], writes=[("psb", 5)])
                P.op("act", lambda e, n=n: e.activation(out=tmp[5][:, :n], in_=PSB[5][:, :n], func=AF.Ln, bias=cst[:, 0:1], scale=8.0), reads=[("psb", 5), "cst0"], writes=[("tmp", 5)])
                P.op("act", lambda e, n=n: e.activation(out=tmp[5][:, :n], in_=tmp[5][:, :n], func=AF.Exp, scale=-0.5), reads=[("tmp", 5)], writes=[("tmp", 5)])
                P.op("dve", lambda e, n=n: e.tensor_tensor(out=tmp[4][:, :n], in0=PSB[2][:, :n], in1=tmp[5][:, :n], op=ALU.mult), reads=[("psb", 2), ("tmp", 5)], writes=[("tmp", 4)])
                P.op("act", lambda e, a=a, b=b, n=n: e.activation(out=ckvn[:, a:b], in_=tmp[4][:, :n], func=AF.Identity, scale=pvc("mla_kvn", 0)), reads=[("tmp", 4), "pvt"], writes=[(k, "ckvn", j)])
            P.barrier()
            qTs = [arC.alloc([96, NT], BF16) for _ in range(2)]
            kTs = [arC.alloc([96, NT], BF16) for _ in range(2)]
            vhs = [arC.alloc([128, 18, 65], BF16) for _ in range(2)]
            for vv in range(2):
                P.op("pool", lambda e, vv=vv: e.memset(vhs[vv], 1.0), writes=[(k, "vh", vv)])
            PT = arC.alloc([128, 2, 512], BF16)
            xf = arC.alloc([96, 512], F32)
            xn = arC.alloc([96, 512], BF16)
            rs = arC.alloc([96, 512], F32)
            t1 = arC.alloc([96, 512], F32)
            t1b = arC.alloc([96, 512], BF16)
            t2 = arC.alloc([96, 512], F32)
            rden = tmp[0][0:64, :]
            oT = tmp[1][0:64, :].bitcast(BF16)[:, 0:512]

            def proj_gen(h):
                hp = h % 2
                qT, kT, vh = qTs[hp], kTs[hp], vhs[hp]
                P.op("pool", lambda e: e.dma_start(out=wo[:, hp, :], in_=mla_wo[h * 64:(h + 1) * 64, :]), writes=[(k, "wo", hp)], dma=True)
                for j in tiles_all:
                    a, b = TT[j]
                    n = b - a

                    def mmq(e, a=a, b=b, n=n):
                        ins = None
                        for kk in range(2):
                            ins = e.matmul(PSB[0][0:96, :n], lhsT=wuq[:, kk, h * 96:(h + 1) * 96], rhs=cqn[:, kk, a:b], start=(kk == 0), stop=(kk == 1))
                        return ins
                    P.op("pe", mmq, reads=[(k, "wuq"), (k, "cqn", j)], writes=[("psb", 0)])
                    P.op("pe", lambda e, a=a, b=b, n=n: e.matmul(PSB[1][0:64, :n], lhsT=wukv[:, h * 128:h * 128 + 64], rhs=ckvn[:, a:b], start=True, stop=True),
                         reads=[(k, "wukv"), (k, "ckvn", j)], writes=[("psb", 1)])
                    for which in range(2):
                        gname = "mla_qkn%d" % which
                        if which == 0:
                            P.op("act", lambda e, n=n: e.copy(out=xf[:, :n], in_=PSB[0][0:96, :n]), reads=[("psb", 0)], writes=[(k, "xf")])
                        else:
                            P.op("act", lambda e, n=n: e.copy(out=xf[0:64, :n], in_=PSB[1][0:64, :n]), reads=[("psb", 1)], writes=[(k, "xf")])
                            P.op("pool", lambda e, a=a, b=b, n=n: e.tensor_copy(out=xf[64:96, :n], in_=krope[64:96, a:b]), reads=[(k, "krope", j), (k, "xf")], writes=[(k, "xf")])
                        P.op("act", lambda e, n=n: e.activation(out=t1b[:, :n], in_=xf[:, :n], func=AF.Square), reads=[(k, "xf")], writes=[(k, "t1b")])
                        P.op("pe", lambda e, n=n: e.matmul(PSB[2][0:96, :n], lhsT=ones96, rhs=t1b[:, :n], start=True, stop=True), reads=[(k, "t1b"), (k, "ones96")], writes=[("psb", 2)])
                        yield
                        P.op("act", lambda e, n=n: e.activation(out=rs[:, :n], in_=PSB[2][0:96, :n], func=AF.Ln, bias=cst[0:96, 0:1], scale=1.0), reads=[("psb", 2), "cst0"], writes=[(k, "rs")])
                        P.op("act", lambda e, n=n: e.activation(out=rs[:, :n], in_=rs[:, :n], func=AF.Exp, scale=-0.5), reads=[(k, "rs")], writes=[(k, "rs")])
                        P.op("dve", lambda e, n=n, gname=gname: e.scalar_tensor_tensor(out=xn[:, :n], in0=xf[:, :n], scalar=pvt[0:96, pvoff[gname]:pvoff[gname] + 1], in1=rs[:, :n], op0=ALU.mult, op1=ALU.mult),
                             reads=[(k, "xf"), (k, "rs"), "pvt"], writes=[(k, "xn")])
                        P.op("pe", lambda e, n=n: e.matmul(PSB[3][0:96, :n], lhsT=RT, rhs=xn[:, :n], start=True, stop=True), reads=[(k, "xn"), (k, "RT")], writes=[("psb", 3)])
                        yield
                        P.op("pool", lambda e, a=a, b=b, n=n: e.tensor_tensor(out=t1[:, :n], in0=xn[:, :n], in1=tC[:, a:b], op=ALU.mult), reads=[(k, "xn"), (k, "tC")], writes=[(k, "t1")])
                        P.op("dve", lambda e, a=a, b=b, n=n: e.tensor_tensor(out=t2[:, :n], in0=PSB[3][0:96, :n], in1=tS[:, a:b], op=ALU.mult), reads=[("psb", 3), (k, "tS")], writes=[(k, "t2")])
                        dst = qT if which == 0 else kT
                        P.op("pool", lambda e, a=a, b=b, n=n, dst=dst: e.tensor_tensor(out=dst[:, a:b], in0=t1[:, :n], in1=t2[:, :n], op=ALU.add),
                             reads=[(k, "t1"), (k, "t2")], writes=[(k, "qk", hp, which, j)])
                        yield
                for g3 in range(3):
                    kts = list(range(g3 * 8, min(18, g3 * 8 + 8)))

                    def mmv(e, kts=kts):
                        ins = None
                        for qi, kt in enumerate(kts):
                            ins = e.matmul(PSB[3][:, qi * 64:(qi + 1) * 64], lhsT=ckvn[:, kt * 128:(kt + 1) * 128], rhs=wukv[:, h * 128 + 64:h * 128 + 128], start=True, stop=True)
                        return ins
                    P.op("pe", mmv, reads=[(k, "wukv")] + [(k, "ckvn", j) for j in tiles_all], writes=[("psb", 3)])
                    P.op("act", lambda e, kts=kts: e.copy(out=vh[:, kts[0]:kts[-1] + 1, 0:64], in_=PSB[3][:, 0:64 * len(kts)].rearrange("p (a b) -> p a b", b=64)),
                         reads=[("psb", 3)], writes=[(k, "vh", hp)])
                    yield

            def attn(h, gen):
                hp = h % 2
                qT, kT, vh = qTs[hp], kTs[hp], vhs[hp]

                def pump(cnt=1):
                    if gen is None:
                        return
                    for _ in range(cnt):
                        try:
                            next(gen)
                        except StopIteration:
                            return
                for j in tiles_all:
                    a, b = TT[j]
                    n = b - a
                    keyt = [0, 1] if j == 0 else list(range(18))

                    def emitS(ki, kt, a=a, b=b, n=n, j=j):
                        pb_ = ki % 2
                        jk = [jj for jj, (aa, bb) in enumerate(TT) if aa <= kt * 128 < bb][0]
                        P.op("pe", lambda e, kt=kt, pb_=pb_, n=n, a=a, b=b: e.matmul(PSB[4 + pb_][:, :n], lhsT=kT[:, kt * 128:(kt + 1) * 128], rhs=qT[:, a:b], start=True, stop=True),
                             reads=[(k, "qk", hp, 1, jk), (k, "qk", hp, 0, j)], writes=[("psb", 4 + pb_)])
                    emitS(0, keyt[0])
                    for ki, kt in enumerate(keyt):
                        pb_ = ki % 2
                        if ki + 1 < len(keyt):
                            emitS(ki + 1, keyt[ki + 1])
                        P.op("act", lambda e, n=n, pb_=pb_: e.activation(out=PT[:, pb_, :n], in_=PSB[4 + pb_][:, :n], func=AF.Exp, scale=SC), reads=[("psb", 4 + pb_)], writes=[(k, "PT", pb_)])
                        P.op("pe", lambda e, kt=kt, n=n, pb_=pb_, ki=ki, nk=len(keyt): e.matmul(PSB[6][0:65, :n], lhsT=vh[:, kt, :], rhs=PT[:, pb_, :n], start=(ki == 0), stop=(ki == nk - 1)),
                             reads=[(k, "vh", hp), (k, "PT", pb_)], writes=[("psb", 6)])
                        if ki % 2 == 1:
                            pump(1)
                    P.op("act", lambda e, n=n: e.activation(out=rr[64:65, :n], in_=PSB[6][64:65, :n], func=AF.Ln), reads=[("psb", 6)], writes=[(k, "rr")])
                    P.op("act", lambda e, n=n: e.activation(out=rr[64:65, :n], in_=rr[64:65, :n], func=AF.Exp, scale=-1.0), reads=[(k, "rr")], writes=[(k, "rr")])
                    P.op("pe", lambda e, n=n: e.matmul(PSB[7][0:64, :n], lhsT=ones1r[64:65, :], rhs=rr[64:65, :n], start=True, stop=True), reads=[(k, "rr"), (k, "ones1r")], writes=[("psb", 7)])
                    P.op("act", lambda e, n=n: e.copy(out=rden[:, :n], in_=PSB[7][0:64, :n]), reads=[("psb", 7)], writes=[("tmp", 0)])
                    P.op("dve", lambda e, n=n: e.tensor_tensor(out=oT[:, :n], in0=PSB[6][0:64, :n], in1=rden[:, :n], op=ALU.mult), reads=[("psb", 6), ("tmp", 0)], writes=[("tmp", 1)])
                    for oc in range(8):
                        pyb = 4 + oc % 2
                        P.op("pe", lambda e, oc=oc, n=n, pyb=pyb: e.matmul(PSB[pyb][:, :n], lhsT=wo[:, hp, oc * 128:(oc + 1) * 128], rhs=oT[:, :n], start=True, stop=True),
                             reads=[(k, "wo", hp), ("tmp", 1)], writes=[("psb", pyb)])
                        P.op("dve", lambda e, oc=oc, a=a, b=b, n=n, j=j, pyb=pyb: e.scalar_tensor_tensor(
                            out=xT[:, oc, a:b], in0=PSB[pyb][:, :n], scalar=modcol(2, oc, j), in1=xT[:, oc, a:b], op0=ALU.mult, op1=ALU.add),
                            reads=[("psb", pyb), "modt", ("x", oc, j)], writes=[("x", oc, j)])
                    pump(2)
                if gen is not None:
                    for _ in gen:
                        pass

            g0 = proj_gen(0)
            for _ in g0:
                pass
            for h in range(16):
                attn(h, proj_gen(h + 1) if h + 1 < 16 else None)

        def mlstm(tiles_all):
            phase()
            k = "ml"
            wqkv = arA.alloc([128, 8, 2048], BF16)
            selc = arA.alloc([16, 2, 16, 64], F32)
            maskc = arA.alloc([64, 2, 256], F32)
            selh = arA.alloc([16, 2, 4], F32)
            wg = arA.alloc([128, 8, 16], BF16)
            Cbf = arA.alloc([128, 4, 257], BF16)
            G = arB.alloc([16, NT], F32)
            Bd = [arB.alloc([16, NT], F32), arB.alloc([16, NT], F32)]
            Vp = arB.alloc([64, 4, 257], BF16)
            Cst = arB.alloc([128, 4, 257], F32)
            qkT = tmp[0][:, :].bitcast(BF16)[:, 0:512]
            Kw = tmp[1][0:64, :].bitcast(BF16)[:, 0:512].rearrange("p (a b) -> p a b", a=4)
            Em = tmp[2][0:64, 0:256]
            St = tmp[2][0:64, 256:384].bitcast(BF16)
            pis = tmp[3][0:64, 0:257]
            nd = tmp[4][0:64, 0:257]
            hout = tmp[5][0:64, :].bitcast(BF16)
            inter = rstd[0:64, 0:4]
            decay = rstd[:, 8:12]
            rdn = rstd[0:64, 16:17]
            P.op("pool", lambda e: e.dma_start(out=wqkv, in_=ml_win[:, 0:2048].rearrange("(k p) n -> p k n", p=128)), writes=[(k, "wqkv")], dma=True)
            P.op("pool", lambda e: e.dma_start(out=wg, in_=ml_win[:, 3072:3088].rearrange("(k p) n -> p k n", p=128)), writes=[(k, "wg")], dma=True)
            P.op("sp", lambda e: e.dma_start(out=selc, in_=ml_selc), writes=[(k, "selc")], dma=True)
            P.op("sp", lambda e: e.dma_start(out=selh, in_=ml_selh), writes=[(k, "selh")], dma=True)
            P.op("sp", lambda e: e.dma_start(out=maskc, in_=ml_maskc.rearrange("p a b c -> p a (b c)")), writes=[(k, "maskc")], dma=True)
            P.op("pool", lambda e: e.memset(Vp, 1.0), writes=[(k, "Vp")])
            for j in tiles_all:
                a, b = TT[j]
                n = b - a

                def mmg(e, a=a, b=b, n=n):
                    ins = None
                    for kk in range(8):
                        ins = e.matmul(PSB[0][0:16, :n], lhsT=wg[:, kk, :], rhs=hT[:, kk, a:b], start=(kk == 0), stop=(kk == 7))
                    return ins
                P.op("pe", mmg, reads=[(k, "wg")] + [("h", kk, j) for kk in range(8)], writes=[("psb", 0)])
                P.op("act", lambda e, a=a, b=b, n=n: e.activation(out=G[:, a:b], in_=PSB[0][0:16, :n], func=AF.Identity, bias=pvt[0:16, pvoff["ml_gb"]:pvoff["ml_gb"] + 1], scale=1.0),
                     reads=[("psb", 0), "pvt"], writes=[(k, "G")])
            lf = tmp[3][0:16, :]
            for j in tiles_all:
                a, b = TT[j]
                n = b - a
                P.op("act", lambda e, a=a, b=b, n=n: e.activation(out=lf[:, :n], in_=G[:, a:b], func=AF.Exp, scale=-1.0), reads=[(k, "G")], writes=[("tmp", 3)])
                P.op("act", lambda e, n=n: e.activation(out=lf[:, :n], in_=lf[:, :n], func=AF.Ln, bias=cst[0:16, 1:2], scale=1.0), reads=[("tmp", 3), "cst1"], writes=[("tmp", 3)])
                P.op("dve", lambda e, n=n: e.tensor_scalar(out=lf[:, :n], in0=lf[:, :n], scalar1=-1.0, scalar2=None, op0=ALU.mult), reads=[("tmp", 3)], writes=[("tmp", 3)])
                for ci in range(n // 64):
                    c0 = ci * 64
                    P.op("dve", lambda e, a=a, c0=c0: e.tensor_tensor_scan(out=Bd[0][:, a + c0:a + c0 + 64], data0=cst[0:16, 1:2].to_broadcast([16, 64]), data1=lf[:, c0:c0 + 64], initial=0.0, op0=ALU.mult, op1=ALU.add),
                         reads=[("tmp", 3), "cst1"], writes=[(k, "B0")])
                    P.op("dve", lambda e, a=a, c0=c0: e.tensor_tensor_scan(out=Bd[1][:, a + c0:a + c0 + 64][:, ::-1], data0=cst[0:16, 1:2].to_broadcast([16, 64]), data1=lf[:, c0:c0 + 64][:, ::-1], initial=0.0, op0=ALU.mult, op1=ALU.add),
                         reads=[("tmp", 3), "cst1"], writes=[(k, "B1")])
            SQ = 128 ** -0.5
            for d in range(2):
                P.op("pool", lambda e: e.memset(Cst, 0.0), reads=[(k, "C", h) for h in range(4)], writes=[(k, "C", h) for h in range(4)])
                P.op("pool", lambda e: e.memset(Cbf, 0.0), reads=[(k, "Cbf", h) for h in range(4)], writes=[(k, "Cbf", h) for h in range(4)])
                order = list(range(36)) if d == 0 else [3, 2, 1, 0] + list(range(35, 3, -1))
                for ch in order:
                    t0 = ch * 64
                    j = [jj for jj, (aa, bb) in enumerate(TT) if aa <= t0 < bb][0]
                    tend = t0 + 63 if d == 0 else t0
                    tl = 63 if d == 0 else 0
                    hr = [("h", kk, j) for kk in range(8)]

                    def mmk(e, t0=t0):
                        ins = None
                        for kk in range(8):
                            ins = e.matmul(PSB[0][0:64, 0:512], lhsT=hT[:, kk, t0:t0 + 64], rhs=wqkv[:, kk, 512:1024], start=(kk == 0), stop=(kk == 7))
                        return ins
                    P.op("pe", mmk, reads=hr + [(k, "wqkv")], writes=[("psb", 0)])
                    for half in range(2):
                        def mmv(e, t0=t0, half=half):
                            ins = None
                            for kk in range(8):
                                ins = e.matmul(PSB[1 + half][0:64, 0:512], lhsT=hT[:, kk, t0:t0 + 64], rhs=wqkv[:, kk, 1024 + half * 512:1536 + half * 512], start=(kk == 0), stop=(kk == 7))
                            return ins
                        P.op("pe", mmv, reads=hr + [(k, "wqkv")], writes=[("psb", 1 + half)])

                    def mmqk(e, t0=t0):
                        ins = None
                        for qk in range(2):
                            for h in range(4):
                                for kk in range(8):
                                    ins = e.matmul(PSB[3][:, qk * 256 + h * 64:qk * 256 + (h + 1) * 64], lhsT=wqkv[:, kk, qk * 512 + h * 128:qk * 512 + (h + 1) * 128],
                                                   rhs=hT[:, kk, t0:t0 + 64], start=(kk == 0), stop=(kk == 7))
                        return ins
                    P.op("pe", mmqk, reads=hr + [(k, "wqkv")], writes=[("psb", 3)])
                    P.op("act", lambda e: e.activation(out=qkT[:, 0:256], in_=PSB[3][:, 0:256], func=AF.Identity, scale=SQ), reads=[("psb", 3)], writes=[(k, "qT")])
                    P.op("act", lambda e: e.copy(out=qkT[:, 256:512], in_=PSB[3][:, 256:512]), reads=[("psb", 3)], writes=[(k, "kT")])
                    for half in range(2):
                        P.op("act", lambda e, half=half: e.copy(out=Vp[:, 2 * half:2 * half + 2, 0:256], in_=PSB[1 + half][0:64, :].rearrange("p (a b) -> p a b", a=2)),
                             reads=[("psb", 1 + half)], writes=[(k, "Vp")])
                    def mme(e, t0=t0, tend=tend, d=d):
                        ins = None
                        for h in range(4):
                            rb = (4 if d == 0 else 12) + h
                            ri = (0 if d == 0 else 8) + h
                            o = PSB[4][0:64, h * 64:(h + 1) * 64]
                            e.matmul(o, lhsT=selc[:, 0, rb, :], rhs=Bd[d][:, t0:t0 + 64], start=True, stop=False)
                            e.matmul(o, lhsT=Bd[d][:, t0:t0 + 64], rhs=selc[:, 1, rb, :], start=False, stop=False)
                            e.matmul(o, lhsT=G[:, t0:t0 + 64], rhs=selc[:, 0, ri, :], start=False, stop=True)
                        e.matmul(PSB[4][0:64, 256:260], lhsT=Bd[d][:, t0:t0 + 64], rhs=selh[:, d, :], start=True, stop=True)
                        ins = e.matmul(PSB[4][:, 260:264], lhsT=Bd[d][:, tend:tend + 1].to_broadcast([16, 128]), rhs=selh[:, d, :], start=True, stop=True)
                        return ins
                    P.op("pe", mme, reads=[(k, "selc"), (k, "selh"), (k, "B0"), (k, "B1"), (k, "G")], writes=[("psb", 4)])
                    P.op("dve", lambda e, d=d: e.tensor_tensor(out=Em, in0=PSB[4][0:64, 0:256], in1=maskc[:, d, :], op=ALU.add), reads=[("psb", 4), (k, "maskc")], writes=[(k, "Em")])
                    P.op("act", lambda e: e.activation(out=Em, in_=Em, func=AF.Exp), reads=[(k, "Em")], writes=[(k, "Em")])
                    P.op("act", lambda e: e.activation(out=inter, in_=PSB[4][0:64, 256:260], func=AF.Exp), reads=[("psb", 4)], writes=[(k, "inter")])
                    P.op("act", lambda e: e.activation(out=decay, in_=PSB[4][:, 260:264], func=AF.Exp), reads=[("psb", 4)], writes=[(k, "decay")])
                    def mms(e):
                        ins = None
                        for h in range(4):
                            ins = e.matmul(PSB[5][0:64, h * 64:(h + 1) * 64], lhsT=qkT[:, 256 + h * 64:256 + (h + 1) * 64], rhs=qkT[:, h * 64:(h + 1) * 64], start=True, stop=True)
                        return ins
                    P.op("pe", mms, reads=[(k, "qT"), (k, "kT")], writes=[("psb", 5)])
                    P.op("dve", lambda e: e.tensor_tensor(out=St, in0=PSB[5][0:64, 0:256], in1=Em, op=ALU.mult), reads=[("psb", 5), (k, "Em")], writes=[(k, "St")])
                    for h in range(4):
                        P.op("pe", lambda e, h=h: e.matmul(PSB[6][0:64, 0:257], lhsT=St[:, h * 64:(h + 1) * 64], rhs=Vp[:, h, :], start=True, stop=True),
                             reads=[(k, "St"), (k, "Vp")], writes=[("psb", 6)])
                        P.op("pe", lambda e, h=h: e.matmul(PSB[7][0:64, 0:257], lhsT=qkT[:, h * 64:(h + 1) * 64], rhs=Cbf[:, h, :], start=True, stop=True),
                             reads=[(k, "qT"), (k, "Cbf", h)], writes=[("psb", 7)])
                        P.op("act", lambda e: e.copy(out=pis, in_=PSB[6][0:64, 0:257]), reads=[("psb", 6)], writes=[(k, "pis")])
                        P.op("dve", lambda e, h=h: e.scalar_tensor_tensor(out=nd, in0=PSB[7][0:64, 0:257], scalar=inter[:, h:h + 1], in1=pis, op0=ALU.mult, op1=ALU.add),
                             reads=[("psb", 7), (k, "inter"), (k, "pis")], writes=[(k, "nd")])
                        P.op("dve", lambda e: e.tensor_scalar(out=rdn, in0=nd[:, 256:257], scalar1=-1.0, scalar2=None, op0=ALU.mult), reads=[(k, "nd")], writes=[(k, "rdn")])
                        P.op("dve", lambda e: e.tensor_tensor(out=rdn, in0=rdn, in1=nd[:, 256:257], op=ALU.max), reads=[(k, "nd"), (k, "rdn")], writes=[(k, "rdn")])
                        P.op("dve", lambda e: e.tensor_scalar(out=rdn, in0=rdn, scalar1=1.0, scalar2=None, op0=ALU.max), reads=[(k, "rdn")], writes=[(k, "rdn")])
                        P.op("dve", lambda e: e.reciprocal(out=rdn, in_=rdn), reads=[(k, "rdn")], writes=[(k, "rdn")])
                        P.op("dve", lambda e, h=h: e.tensor_scalar(out=hout[:, h * 256:(h + 1) * 256], in0=nd[:, 0:256], scalar1=rdn, scalar2=None, op0=ALU.mult),
                             reads=[(k, "nd"), (k, "rdn")], writes=[(k, "hout")])
                        P.op("dve", lambda e, h=h, tl=tl: e.tensor_scalar(out=Kw[:, h, :], in0=PSB[0][0:64, h * 128:(h + 1) * 128], scalar1=Em[:, h * 64 + tl:h * 64 + tl + 1], scalar2=None, op0=ALU.mult),
                             reads=[("psb", 0), (k, "Em")], writes=[(k, "Kw", h)])
                        ub_ = 1 + h % 2
                        P.op("pe", lambda e, h=h, ub_=ub_: e.matmul(PSB[ub_][:, 0:257], lhsT=Kw[:, h, :], rhs=Vp[:, h, :], start=True, stop=True),
                             reads=[(k, "Kw", h), (k, "Vp")], writes=[("psb", ub_)])
                        P.op("dve", lambda e, h=h, ub_=ub_: e.scalar_tensor_tensor(out=Cst[:, h, :], in0=Cst[:, h, :], scalar=decay[:, h:h + 1], in1=PSB[ub_][:, 0:257], op0=ALU.mult, op1=ALU.add),
                             reads=[(k, "C", h), (k, "decay"), ("psb", ub_)], writes=[(k, "C", h)])
                        P.op("act", lambda e, h=h: e.copy(out=Cbf[:, h, :], in_=Cst[:, h, :]), reads=[(k, "C", h)], writes=[(k, "Cbf", h)])
                    P.op("sp", lambda e, d=d, t0=t0: e.dma_start(out=hdir[d, t0:t0 + 64, :], in_=hout), reads=[(k, "hout")], writes=[(k, "hdir", d, ch // 2)], dma=True)
            phase()
            wog = arA.alloc([128, 8, 1024], BF16)
            gain = arA.alloc([128, 1024], F32)
            hfb = arA.alloc([128, 2, 1024], BF16)
            hs = arA.alloc([128, 1024], F32)
            sq = arA.alloc([128, 1024], F32)
            wout = arA.alloc([128, 2, 8, 128], BF16)
            ss = rstd[:, 0:4]
            P.op("pool", lambda e: e.dma_start(out=wog, in_=ml_win[:, 2048:3072].rearrange("(k p) n -> p k n", p=128)), writes=[(k, "wog")], dma=True)
            P.op("sp", lambda e: e.dma_start(out=gain, in_=ml_gain), writes=[(k, "gain")], dma=True)
            for ti in range(18):
                jt = [jj for jj, (aa, bb) in enumerate(TT) if aa <= ti * 128 < bb][0]
                for d in range(2):
                    P.op("sp", lambda e, d=d, ti=ti: e.dma_start(out=hfb[:, d, :], in_=hdir[d, ti * 128:(ti + 1) * 128, :]), writes=[(k, "hfb", d)], dma=True)
                for half in range(2):
                    def mmo(e, ti=ti, half=half):
                        ins = None
                        for kk in range(8):
                            ins = e.matmul(PSB[2 + half][:, :], lhsT=hT[:, kk, ti * 128:(ti + 1) * 128], rhs=wog[:, kk, half * 512:(half + 1) * 512], start=(kk == 0), stop=(kk == 7))
                        return ins
                    P.op("pe", mmo, reads=[("h", kk, jt) for kk in range(8)] + [(k, "wog")], writes=[("psb", 2 + half)])
                P.op("dve", lambda e: e.tensor_tensor(out=hs, in0=hfb[:, 0, :], in1=hfb[:, 1, :], op=ALU.add), reads=[(k, "hfb", 0), (k, "hfb", 1)], writes=[(k, "hs")])
                P.op("act", lambda e: e.activation(out=sq, in_=hs, func=AF.Square), reads=[(k, "hs")], writes=[(k, "sq")])
                P.op("dve", lambda e: e.tensor_reduce(out=ss, in_=sq[:, :].rearrange("p (a b) -> p a b", a=4), axis=AX.X, op=ALU.add), reads=[(k, "sq")], writes=[(k, "ss")])
                P.op("act", lambda e: e.activation(out=ss, in_=ss, func=AF.Sqrt, bias=cst[:, 0:1], scale=1.0 / 256.0), reads=[(k, "ss"), "cst0"], writes=[(k, "ss")])
                P.op("dve", lambda e: e.reciprocal(out=ss, in_=ss), reads=[(k, "ss")], writes=[(k, "ss")])
                for h in range(4):
                    P.op("dve", lambda e, h=h: e.scalar_tensor_tensor(out=hs[:, h * 256:(h + 1) * 256], in0=hs[:, h * 256:(h + 1) * 256], scalar=ss[:, h:h + 1], in1=gain[:, h * 256:(h + 1) * 256], op0=ALU.mult, op1=ALU.mult),
                         reads=[(k, "hs"), (k, "ss"), (k, "gain")], writes=[(k, "hs")])
                for half in range(2):
                    P.op("act", lambda e, half=half: e.activation(out=sq[:, half * 512:(half + 1) * 512], in_=PSB[2 + half][:, :], func=AF.Sigmoid), reads=[("psb", 2 + half), (k, "sq")], writes=[(k, "sq")])
                P.op("dve", lambda e: e.tensor_tensor(out=hs, in0=hs, in1=sq, op=ALU.mult), reads=[(k, "hs"), (k, "sq")], writes=[(k, "hs")])
                for half in range(2):
                    def trf(e, half=half):
                        ins = None
                        for q in range(4):
                            c = half * 4 + q
                            ins = e.transpose(PSB[half][:, q * 128:(q + 1) * 128], hs[:, c * 128:(c + 1) * 128], ident[:])
                        return ins
                    P.op("pe", trf, reads=[(k, "hs"), "ident"], writes=[("psb", half)])
                    P.op("act" if half else "dve", lambda e, half=half, ti=ti: (e.copy if half else e.tensor_copy)(out=mT[:, half * 4:half * 4 + 4, ti * 128:(ti + 1) * 128], in_=PSB[half][:, :].rearrange("p (q t) -> p q t", q=4)),
                         reads=[("psb", half)], writes=[("m", c, jt) for c in range(half * 4, half * 4 + 4)])
            for oc in range(8):
                ob = oc % 2
                P.op("pool", lambda e, oc=oc, ob=ob: e.dma_start(out=wout[:, ob, :, :], in_=ml_wout[:, oc * 128:(oc + 1) * 128].rearrange("(k p) n -> p k n", p=128)),
                     writes=[(k, "wout", ob)], dma=True)
                for j in tiles_all:
                    a, b = TT[j]
                    n = b - a
                    py = PSB[4 + j % 4]

                    def mmfn(e, py=py, ob=ob, a=a, b=b, n=n):
                        ins = None
                        for cc in range(8):
                            ins = e.matmul(py[:, :n], lhsT=wout[:, ob, cc, :], rhs=mT[:, cc, a:b], start=(cc == 0), stop=(cc == 7))
                        return ins
                    P.op("pe", mmfn, reads=[(k, "wout", ob)] + [("m", cc, j) for cc in range(8)], writes=[("psb", 4 + j % 4)])
                    P.op("dve", lambda e, py=py, oc=oc, a=a, b=b, n=n, j=j: e.scalar_tensor_tensor(
                        out=xT[:, oc, a:b], in0=py[:, :n], scalar=modcol(2, oc, j), in1=xT[:, oc, a:b], op0=ALU.mult, op1=ALU.add),
                        reads=[("psb", 4 + j % 4), "modt", ("x", oc, j)], writes=[("x", oc, j)])

        def moe_sparse(i, tiles):
            phase()
            k = "moe%d" % i
            subs = [t for j in tiles for t in range(TT[j][0] // 128, TT[j][1] // 128)]
            NOV = 36
            wr = arA.alloc([128, 8, 72], F32)
            brb = arA.alloc([128, 72], F32)
            f32t = arA.alloc([128, 8, 512], F32)
            o4 = arA.off
            lgs4 = arA.alloc([128, 4, 72], F32)
            sm4 = arA.alloc([128, 8, 4], F32)
            oh4 = arA.alloc([128, 4, 8], F32)
            eg4 = arA.alloc([128, 4, 8], F32)
            em4 = arA.alloc([128, 4, 64], F32)
            as4 = arA.alloc([128, 4, 64], F32)
            RK = arA.alloc([128, 18, 64], F32)
            assert arA.off - o4 >= 2048
            stg4 = scr[:, o4:o4 + 2048]
            OH = arA.alloc([128, 18, 2, 64], F32)
            WP = arA.alloc([128, 18, 2], F32)
            acum = arA.alloc([128, 64], F32)
            asum = arA.alloc([128, 64], F32)
            mc = arA.alloc([128, 229], F32)
            ones1 = arA.alloc([128, 128], F32)
            identb = arA.alloc([128, 128], BF16)
            cntb = arA.alloc([128, 64], F32)
            ovn = arA.alloc([128, 64], F32)
            ove = arA.alloc([128, 64], F32)
            ovsp = arA.alloc([128, 64], F32)
            dlt = arA.alloc([128, 64], F32)
            t64a = arA.alloc([128, 64], F32)
            t64b = arA.alloc([128, 64], F32)
            EB = arA.alloc([128, NOV], F32)
            DEST = arA.alloc([128, 18, 2], F32)
            DESTI = arA.alloc([128, 18, 2], I32)
            idxi = arA.alloc([128, NOV, 4], I32)
            idxf = arA.alloc([128, NOV, 4], F32)
            Ltri = mc[:, 0:128]
            e128 = mc[:, 128:192]
            iop = mc[:, 192:193]
            thr36 = mc[:, 193:229]
            P.op("sp", lambda e: e.dma_start(out=wr, in_=moe_wr[i].rearrange("(k p) n -> p k n", p=128)), writes=[(k, "wr")], dma=True)
            P.op("sp", lambda e: e.dma_start(out=brb, in_=moe_br[i]), writes=[(k, "br")], dma=True)
            P.op("sp", lambda e: e.dma_start(out=mc, in_=moec_d), writes=[(k, "mc")], dma=True)
            P.op("pool", lambda e: e.memset(ones1, 1.0), writes=[(k, "ones1")])
            P.op("pool", lambda e: e.memset(acum, 0.0), writes=[(k, "rk")])
            P.op("pool", lambda e: e.tensor_copy(out=identb, in_=ident[:]), reads=["ident"], writes=[(k, "identb")])
            RKK = [(k, "rk")]

            def extra(j, c, tb):
                a, b = TT[j]
                n = b - a
                jj = 1 if j == 0 else 0
                P.op("pool", lambda e, c=c, tb=tb, n=n, jj=jj: e.tensor_scalar(out=f32t[:, c, :n], in0=tb[:, :n], scalar1=mA[:, 1, c, jj:jj + 1],
                                                                               scalar2=modt[:, 3 * 8 + c, jj:jj + 1], op0=ALU.mult, op1=ALU.add),
                     reads=[("tmp", 2 + c % 2), "modt", "mA"], writes=[(k, "f32", c)])
                if c == 7:
                    S = n // 128
                    g0 = a // 128
                    pl = PSB[5]

                    def mmfn(e, S=S):
                        ins = None
                        for s_ in range(S):
                            for kk in range(8):
                                ins = e.matmul(pl[:, s_ * 72:(s_ + 1) * 72], lhsT=f32t[:, kk, s_ * 128:(s_ + 1) * 128], rhs=wr[:, kk, :], start=(kk == 0), stop=(kk == 7))
                        return ins
                    P.op("pe", mmfn, reads=[(k, "f32", cc) for cc in range(8)] + [(k, "wr")], writes=[("psb", 5)])
                    R = [(k, "rt")]
                    V = lambda fn, extra_r=(), extra_w=(): P.op("dve", fn, reads=R + list(extra_r), writes=R + list(extra_w))
                    L4 = lgs4[:, 0:S, :]
                    G4 = lgs4[:, 0:S, 0:8]
                    E4 = lgs4[:, 0:S, 8:72]
                    E44 = E4.rearrange("p s (g j) -> p s g j", g=8)
                    o8 = oh4[:, 0:S, :]
                    oh1 = OH[:, g0:g0 + S, 0, :]
                    oh2 = OH[:, g0:g0 + S, 1, :]
                    em_ = em4[:, 0:S, :]
                    sc = lambda i_: sm4[:, i_, 0:S]
                    bc8 = lambda v: v.unsqueeze(2).to_broadcast([128, S, 8])
                    bc64 = lambda v: v.unsqueeze(2).to_broadcast([128, S, 64])
                    V(lambda e: e.tensor_tensor(out=L4, in0=pl[:, 0:S * 72].rearrange("p (s c) -> p s c", s=S), in1=brb.unsqueeze(1).to_broadcast([128, S, 72]), op=ALU.add), [("psb", 5), (k, "br")])
                    V(lambda e: e.tensor_reduce(out=sc(0), in_=G4, axis=AX.X, op=ALU.max))
                    V(lambda e: e.tensor_tensor(out=o8, in0=G4, in1=bc8(sc(0)), op=ALU.is_equal))
                    V(lambda e: e.tensor_tensor(out=eg4[:, 0:S, :], in0=G4, in1=bc8(sc(0)), op=ALU.subtract))
                    P.op("act", lambda e: e.activation(out=eg4[:, 0:S, :], in_=eg4[:, 0:S, :], func=AF.Exp), reads=R, writes=R)
                    V(lambda e: e.tensor_reduce(out=sc(2), in_=eg4[:, 0:S, :], axis=AX.X, op=ALU.add))
                    V(lambda e: e.reciprocal(out=sc(3), in_=sc(2)))
                    V(lambda e: e.tensor_scalar(out=o8, in0=o8, scalar1=BIG, scalar2=-BIG, op0=ALU.mult, op1=ALU.add))
                    V(lambda e: e.tensor_tensor(out=em_.rearrange("p s (g j) -> p s g j", g=8), in0=E44, in1=o8.unsqueeze(3).to_broadcast([128, S, 8, 8]), op=ALU.add))
                    V(lambda e: e.tensor_reduce(out=sc(4), in_=em_, axis=AX.X, op=ALU.max))
                    V(lambda e: e.tensor_tensor(out=oh1, in0=em_, in1=bc64(sc(4)), op=ALU.is_equal), (), RKK)
                    V(lambda e: e.scalar_tensor_tensor(out=em_, in0=oh1, scalar=-BIG, in1=em_, op0=ALU.mult, op1=ALU.add))
                    V(lambda e: e.tensor_reduce(out=sc(5), in_=em_, axis=AX.X, op=ALU.max))
                    V(lambda e: e.tensor_tensor(out=oh2, in0=em_, in1=bc64(sc(5)), op=ALU.is_equal), (), RKK)
                    V(lambda e: e.tensor_tensor(out=sc(6), in0=sc(5), in1=sc(4), op=ALU.subtract))
                    P.op("act", lambda e: e.activation(out=sc(6), in_=sc(6), func=AF.Exp), reads=R, writes=R)
                    V(lambda e: e.tensor_scalar(out=sc(6), in0=sc(6), scalar1=1.0, scalar2=None, op0=ALU.add))
                    V(lambda e: e.reciprocal(out=sc(6), in_=sc(6)))
                    V(lambda e: e.tensor_tensor(out=WP[:, g0:g0 + S, 0], in0=sc(6), in1=sc(3), op=ALU.mult), (), RKK)
                    V(lambda e: e.tensor_tensor(out=WP[:, g0:g0 + S, 1], in0=sc(3), in1=WP[:, g0:g0 + S, 0], op=ALU.subtract), (), RKK)
                    V(lambda e: e.tensor_tensor(out=as4[:, 0:S, :], in0=oh1, in1=oh2, op=ALU.add), (), RKK)

                    def mmr(e, S=S):
                        ins = None
                        for s_ in range(S):
                            o = PSB[4][:, s_ * 64:(s_ + 1) * 64]
                            e.matmul(o, lhsT=Ltri, rhs=as4[:, s_, :], start=True, stop=False)
                            for sp_ in range(s_):
                                e.matmul(o, lhsT=ones1, rhs=as4[:, sp_, :], start=False, stop=False)
                            ins = e.matmul(o, lhsT=ones1, rhs=acum, start=False, stop=True)
                        return ins
                    P.op("pe", mmr, reads=RKK + R + [(k, "mc"), (k, "ones1")], writes=[("psb", 4)])
                    P.op("act", lambda e: e.copy(out=RK[:, g0:g0 + S, :], in_=PSB[4][:, 0:S * 64].rearrange("p (s c) -> p s c", s=S)), reads=[("psb", 4)] + RKK, writes=RKK)
                    V(lambda e: e.tensor_reduce(out=asum, in_=as4[:, 0:S, :].rearrange("p s c -> p c s"), axis=AX.X, op=ALU.add), [("psb", 4)], RKK)
                    V(lambda e: e.tensor_tensor(out=acum, in0=acum, in1=asum, op=ALU.add), [("psb", 4)], RKK)

            norm_mod(1, tiles, extra=extra)

            P.op("pe", lambda e: e.matmul(PSB[4][:, 0:64], lhsT=ones1, rhs=acum, start=True, stop=True), reads=RKK + [(k, "ones1")], writes=[("psb", 4)])
            V2 = lambda fn: P.op("dve", fn, reads=RKK + [("psb", 4), (k, "mc")], writes=RKK)
            V2(lambda e: e.tensor_scalar(out=cntb, in0=PSB[4][:, 0:64], scalar1=-128.0, scalar2=0.0, op0=ALU.add, op1=ALU.max))
            f32flat0 = f32t[:, :, :].rearrange("p a b -> p (a b)")
            tQ = f32flat0[:, 0:64 * NOV].rearrange("p (c q) -> p c q", q=NOV)
            F3a = [(k, "f32", cc) for cc in range(8)]
            V2b = lambda fn: P.op("dve", fn, reads=RKK + F3a + [("psb", 4), (k, "mc")], writes=RKK + F3a)
            V2b(lambda e: e.tensor_tensor(out=tQ, in0=cntb.unsqueeze(2).to_broadcast([128, 64, NOV]), in1=thr36.unsqueeze(1).to_broadcast([128, 64, NOV]), op=ALU.is_gt))
            V2b(lambda e: e.tensor_reduce(out=ovn, in_=tQ, axis=AX.X, op=ALU.add))
            V2(lambda e: e.tensor_scalar(out=ovn, in0=ovn, scalar1=128.0, scalar2=None, op0=ALU.mult))
            V2(lambda e: e.tensor_tensor_scan(out=ove, data0=ones1[:, 0:64], data1=ovn, initial=0.0, op0=ALU.mult, op1=ALU.add))
            V2(lambda e: e.tensor_tensor(out=ovsp, in0=ove, in1=ovn, op=ALU.subtract))
            V2(lambda e: e.tensor_scalar(out=ovsp, in0=ovsp, scalar1=8064.0, scalar2=None, op0=ALU.add))
            V2(lambda e: e.tensor_tensor(out=dlt, in0=e128, in1=ovsp, op=ALU.subtract))
            tQ2 = f32flat0[:, 0:64 * NOV].rearrange("p (q c) -> p q c", q=NOV)
            V2b(lambda e: e.tensor_tensor(out=tQ2, in0=ove.unsqueeze(1).to_broadcast([128, NOV, 64]), in1=thr36.unsqueeze(2).to_broadcast([128, NOV, 64]), op=ALU.is_le))
            V2b(lambda e: e.tensor_reduce(out=EB, in_=tQ2, axis=AX.X, op=ALU.add))
            V2(lambda e: e.tensor_scalar(out=t64a[:, 0:NOV], in0=EB, scalar1=64.0, scalar2=1.0e6, op0=ALU.is_ge, op1=ALU.mult))
            V2(lambda e: e.scalar_tensor_tensor(out=t64a[:, 0:NOV], in0=EB, scalar=256.0, in1=t64a[:, 0:NOV], op0=ALU.mult, op1=ALU.add))
            V2(lambda e: e.tensor_scalar(out=t64b[:, 0:1], in0=iop, scalar1=2.0, scalar2=float(i * 16384), op0=ALU.mult, op1=ALU.add))
            V2(lambda e: e.tensor_scalar(out=idxf[:, :, 0], in0=t64a[:, 0:NOV], scalar1=t64b[:, 0:1], scalar2=None, op0=ALU.add))
            V2(lambda e: e.tensor_scalar(out=idxf[:, :, 1], in0=idxf[:, :, 0], scalar1=1.0, scalar2=None, op0=ALU.add))
            V2(lambda e: e.tensor_scalar(out=t64a[:, 0:NOV], in0=t64a[:, 0:NOV], scalar1=0.5, scalar2=None, op0=ALU.mult))
            V2(lambda e: e.tensor_scalar(out=t64b[:, 1:2], in0=iop, scalar1=float(i * 8192), scalar2=None, op0=ALU.add))
            V2(lambda e: e.tensor_scalar(out=idxf[:, :, 2], in0=t64a[:, 0:NOV], scalar1=t64b[:, 1:2], scalar2=None, op0=ALU.add))
            V2(lambda e: e.tensor_copy(out=idxf[:, :, 3], in_=idxf[:, :, 2]))
            V2(lambda e: e.tensor_copy(out=idxi, in_=idxf))
            sg0, SG = subs[0], len(subs)
            f32flat = f32t[:, :, :].rearrange("p a b -> p (a b)")
            tA = f32flat[:, 0:SG * 64].rearrange("p (s c) -> p s c", s=SG)
            tB = f32flat[:, 2048:2048 + SG * 64].rearrange("p (s c) -> p s c", s=SG)
            RKs = RK[:, sg0:sg0 + SG, :]
            F3 = [(k, "f32", cc) for cc in range(8)]
            V3 = lambda fn: P.op("dve", fn, reads=RKK + F3 + [(k, "mc")], writes=RKK + F3)
            V3(lambda e: e.tensor_scalar(out=tA, in0=RKs, scalar1=128.0, scalar2=None, op0=ALU.is_lt))
            V3(lambda e: e.tensor_tensor(out=tA, in0=tA, in1=dlt.unsqueeze(1).to_broadcast([128, SG, 64]), op=ALU.mult))
            V3(lambda e: e.tensor_tensor(out=tB, in0=RKs, in1=ovsp.unsqueeze(1).to_broadcast([128, SG, 64]), op=ALU.add))
            V3(lambda e: e.tensor_tensor(out=tA, in0=tA, in1=tB, op=ALU.add))
            for kk in range(2):
                V3(lambda e, kk=kk: e.tensor_tensor(out=tB, in0=OH[:, sg0:sg0 + SG, kk, :], in1=tA, op=ALU.mult))
                V3(lambda e, kk=kk: e.tensor_reduce(out=DEST[:, sg0:sg0 + SG, kk], in_=tB, axis=AX.X, op=ALU.add))
            V2(lambda e: e.tensor_copy(out=DESTI, in_=DEST))
            P.barrier()
            ftok = arB.alloc([128, 2, 1024], BF16)
            for si, gt in enumerate(subs):
                fb = si % 2
                jt = [jj for jj, (aa, bb) in enumerate(TT) if aa <= gt * 128 < bb][0]
                pbf = PSB[fb][:, :].bitcast(BF16)

                def trf(e, gt=gt, pbf=pbf):
                    ins = None
                    for c in range(8):
                        ins = e.transpose(pbf[:, c * 128:(c + 1) * 128], hT[:, c, gt * 128:(gt + 1) * 128], identb)
                    return ins
                P.op("pe", trf, reads=[("h", c, jt) for c in range(8)] + [(k, "identb")], writes=[("psb", fb)])
                P.op("act", lambda e, fb=fb, pbf=pbf: e.copy(out=ftok[:, fb, :], in_=pbf), reads=[("psb", fb)], writes=[(k, "ftok", fb)])
                for kk in range(2):
                    P.op("pool", lambda e, fb=fb, gt=gt, kk=kk: e.indirect_dma_start(
                        out=xslots[:, :], out_offset=bass.IndirectOffsetOnAxis(ap=DESTI[:, gt, kk:kk + 1], axis=0), in_=ftok[:, fb, :], in_offset=None),
                        reads=[(k, "ftok", fb)] + RKK, writes=[(k, "xs", gt, kk)], dma=True)
            P.barrier()
            arB.reset()
            stg = f32t[:, :, :].rearrange("p a b -> p (a b)").rearrange("p (s n) -> p s n", s=2)
            xb = arB.alloc([128, 2, 1024], BF16)
            xbT = arB.alloc([128, 2, 8, 128], BF16)
            wgu = arB.alloc([128, 2, 8, 512], BF16)
            wd = arB.alloc([128, 2, 2, 1024], BF16)
            sg = arB.alloc([128, 256], F32)
            actb = arB.alloc([128, 2, 2, 128], BF16)
            wgu_rows = moe_wgu.rearrange("l e (p h q) n -> (l e p h) (q n)", p=128, h=2)
            wd_rows = moe_wd.rearrange("l e (p q) n -> (l e p) (q n)", p=128)
            OHflat = OH[:, :, :, :].rearrange("p a b c -> p (a b c)")
            stgs = [stg[:, 0, :], stg[:, 1, :], OHflat[:, 0:2048], stg4]
            nstg = [0]

            def emit_weights(b, si):
                wb = si % 2
                ov = b - 64
                if b >= 64:
                    for piece in range(2):
                        P.op("pool", lambda e, ov=ov, wb=wb, piece=piece: e.indirect_dma_start(
                            out=wgu[:, wb, piece * 4:(piece + 1) * 4, :].rearrange("p q n -> p (q n)"), out_offset=None, in_=wgu_rows[:, :],
                            in_offset=bass.IndirectOffsetOnAxis(ap=idxi[:, ov, piece:piece + 1], axis=0), bounds_check=getreg(e, 65535), oob_is_err=False),
                            reads=RKK, writes=[(k, "wgu", wb, piece)], dma=True)
                    P.op("pool", lambda e, ov=ov, wb=wb: e.indirect_dma_start(
                        out=wd[:, wb, :, :].rearrange("p q n -> p (q n)"), out_offset=None, in_=wd_rows[:, :],
                        in_offset=bass.IndirectOffsetOnAxis(ap=idxi[:, ov, 2:3], axis=0), bounds_check=getreg(e, 32767), oob_is_err=False),
                        reads=RKK, writes=[(k, "wd", wb)], dma=True)
                    return
                for piece in range(3):
                    sb_ = nstg[0] % 4
                    nstg[0] += 1
                    sv = stgs[sb_]
                    if piece < 2:
                        src = moe_wgu[i, b].rearrange("(p q) n -> p (q n)", p=128)[:, piece * 2048:(piece + 1) * 2048]
                    else:
                        src = moe_wd[i, b].rearrange("(p q) n -> p (q n)", p=128)
                    P.op("sp", lambda e, src=src, sv=sv: e.dma_start(out=sv, in_=src), writes=[(k, "stg", sb_)], dma=True)
                    ceng = "act" if piece == 0 else "dve"
                    if piece < 2:
                        P.op(ceng, lambda e, sv=sv, wb=wb, piece=piece, ceng=ceng: (e.copy if ceng == "act" else e.tensor_copy)(out=wgu[:, wb, piece * 4:(piece + 1) * 4, :], in_=sv.rearrange("p (q n) -> p q n", q=4)),
                             reads=[(k, "stg", sb_)], writes=[(k, "wgu", wb, piece)])
                    else:
                        P.op(ceng, lambda e, sv=sv, wb=wb, ceng=ceng: (e.copy if ceng == "act" else e.tensor_copy)(out=wd[:, wb, :, :], in_=sv.rearrange("p (q n) -> p q n", q=2)),
                             reads=[(k, "stg", sb_)], writes=[(k, "wd", wb)])

            def emit_compute(b, si):
                wb = si % 2
                xbuf = si % 2
                P.op("sp", lambda e, b=b, xbuf=xbuf: e.dma_start(out=xb[:, xbuf, :], in_=xslots[b * 128:(b + 1) * 128, :]), writes=[(k, "xb", xbuf)], dma=True)
                pbf = PSB[xbuf][:, :].bitcast(BF16)

                def trx(e, xbuf=xbuf, pbf=pbf):
                    ins = None
                    for q in range(8):
                        ins = e.transpose(pbf[:, q * 128:(q + 1) * 128], xb[:, xbuf, q::8], identb)
                    return ins
                P.op("pe", trx, reads=[(k, "xb", xbuf), (k, "identb")], writes=[("psb", xbuf)])
                P.op("dve", lambda e, xbuf=xbuf, pbf=pbf: e.tensor_copy(out=xbT[:, xbuf, :, :], in_=pbf.rearrange("p (q s) -> p q s", q=8)), reads=[("psb", xbuf)], writes=[(k, "xbT", xbuf)])
                pgu = PSB[2 + xbuf]

                def mmgu(e, xbuf=xbuf, wb=wb, pgu=pgu):
                    ins = None
                    for oc in range(4):
                        for q in range(8):
                            ins = e.matmul(pgu[:, oc * 128:(oc + 1) * 128], lhsT=wgu[:, wb, q, (oc // 2) * 256 + (oc % 2):(oc // 2) * 256 + 256:2], rhs=xbT[:, xbuf, q, :], start=(q == 0), stop=(q == 7))
                    return ins
                P.op("pe", mmgu, reads=[(k, "wgu", wb, 0), (k, "wgu", wb, 1), (k, "xbT", xbuf)], writes=[("psb", 2 + xbuf)])
                P.op("act", lambda e, pgu=pgu: e.activation(out=sg, in_=pgu[:, 0:256], func=AF.Silu), reads=[("psb", 2 + xbuf)], writes=[(k, "sg")])
                P.op("dve", lambda e, pgu=pgu, xbuf=xbuf: e.tensor_tensor(out=actb[:, xbuf, :, :], in0=sg[:, :].rearrange("p (a b) -> p a b", a=2), in1=pgu[:, 256:512].rearrange("p (a b) -> p a b", a=2), op=ALU.mult),
                     reads=[(k, "sg"), ("psb", 2 + xbuf)], writes=[(k, "actb", xbuf)])
                for half in range(2):
                    pyb = 4 + 2 * xbuf + half
                    tb_ = 2 * xbuf + half

                    def mmd(e, half=half, pyb=pyb, xbuf=xbuf, wb=wb):
                        ins = None
                        for fc in range(2):
                            ins = e.matmul(PSB[pyb][:, :], lhsT=actb[:, xbuf, fc, :], rhs=wd[:, wb, fc, half * 512:(half + 1) * 512], start=(fc == 0), stop=(fc == 1))
                        return ins
                    P.op("pe", mmd, reads=[(k, "actb", xbuf), (k, "wd", wb)], writes=[("psb", pyb)])
                    P.op("act", lambda e, pyb=pyb, tb_=tb_: e.copy(out=tmp[tb_][:, :].bitcast(BF16)[:, 0:512], in_=PSB[pyb][:, :]),
                         reads=[("psb", pyb)], writes=[("tmp", tb_)])
                    P.op("act", lambda e, b=b, half=half, tb_=tb_: e.dma_start(out=yslots[b * 128:(b + 1) * 128, half * 512:(half + 1) * 512], in_=tmp[tb_][:, :].bitcast(BF16)[:, 0:512]),
                         reads=[("tmp", tb_)], writes=[(k, "ys", b, half)], dma=True)

            order = []
            ovl = list(range(64, 64 + NOV))
            for b in range(64):
                order.append(b)
                if b % 2 == 1 and ovl:
                    order.append(ovl.pop(0))
            order += ovl
            emit_weights(order[0], 0)
            for si, b in enumerate(order):
                if si + 1 < len(order):
                    emit_weights(order[si + 1], si + 1)
                emit_compute(b, si)
            P.barrier()
            arB.reset()
            yg = arB.alloc([128, 2, 2, 1024], BF16)
            yt = arB.alloc([128, 1024], F32)
            for si, gt in enumerate(subs):
                gb = si % 2
                jt = [jj for jj, (aa, bb) in enumerate(TT) if aa <= gt * 128 < bb][0]
                for kk in range(2):
                    P.op("pool", lambda e, gb=gb, gt=gt, kk=kk: e.indirect_dma_start(
                        out=yg[:, gb, kk, :], out_offset=None, in_=yslots[:, :], in_offset=bass.IndirectOffsetOnAxis(ap=DESTI[:, gt, kk:kk + 1], axis=0)),
                        reads=RKK, writes=[(k, "yg", gb, kk)], dma=True)
                P.op("dve", lambda e, gb=gb, gt=gt: e.tensor_scalar(out=yt, in0=yg[:, gb, 0, :], scalar1=WP[:, gt, 0:1], scalar2=None, op0=ALU.mult),
                     reads=[(k, "yg", gb, 0)] + RKK, writes=[(k, "yt")])
                P.op("dve", lambda e, gb=gb, gt=gt: e.scalar_tensor_tensor(out=yt, in0=yg[:, gb, 1, :], scalar=WP[:, gt, 1:2], in1=yt, op0=ALU.mult, op1=ALU.add),
                     reads=[(k, "yg", gb, 1), (k, "yt")] + RKK, writes=[(k, "yt")])
                for half in range(2):
                    pb = PSB[4 + half]

                    def trf(e, pb=pb, half=half):
                        ins = None
                        for q in range(4):
                            c = half * 4 + q
                            ins = e.transpose(pb[:, q * 128:(q + 1) * 128], yt[:, c * 128:(c + 1) * 128], ident[:])
                        return ins
                    P.op("pe", trf, reads=[(k, "yt"), "ident"], writes=[("psb", 4 + half)])
                    for q in range(4):
                        c = half * 4 + q
                        P.op("dve", lambda e, pb=pb, q=q, c=c, gt=gt, jt=jt: e.scalar_tensor_tensor(
                            out=xT[:, c, gt * 128:(gt + 1) * 128], in0=pb[:, q * 128:(q + 1) * 128], scalar=modcol(5, c, jt), in1=xT[:, c, gt * 128:(gt + 1) * 128], op0=ALU.mult, op1=ALU.add),
                            reads=[("psb", 4 + half), "modt", ("x", c, jt)], writes=[("x", c, jt)])

        def moe(i, tiles):
            phase()
            k = "moe%d" % i
            wr = arA.alloc([128, 8, 72], F32)
            brb = arA.alloc([128, 72], F32)
            f32t = arA.alloc([128, 8, 512], F32)
            lgs = arA.alloc([128, 72], F32)
            sm = arA.alloc([128, 16], F32)
            oh = arA.alloc([128, 8], F32)
            em = arA.alloc([128, 64], F32)
            oh1 = arA.alloc([128, 64], F32)
            oh2 = arA.alloc([128, 64], F32)
            wt = arA.alloc([128, 64], F32)
            wtT = arA.alloc([64, NT], F32)
            wgu = arB.alloc([128, 2, 8, 512], BF16)
            wd = arB.alloc([128, 2, 2, 1024], BF16)
            sgt = arB.alloc([128, 2, 512], F32)
            actt = arB.alloc([128, 2, 2, 512], BF16)
            wbc = arB.alloc([128, 512], F32)
            P.op("sp", lambda e: e.dma_start(out=wr, in_=moe_wr[i].rearrange("(k p) n -> p k n", p=128)), writes=[(k, "wr")], dma=True)
            P.op("sp", lambda e: e.dma_start(out=brb, in_=moe_br[i]), writes=[(k, "br")], dma=True)

            def extra(j, c, tb):
                a, b = TT[j]
                n = b - a
                jj = 1 if j == 0 else 0
                P.op("pool", lambda e, c=c, tb=tb, n=n, jj=jj: e.tensor_scalar(out=f32t[:, c, :n], in0=tb[:, :n], scalar1=mA[:, 1, c, jj:jj + 1],
                                                                               scalar2=modt[:, 3 * 8 + c, jj:jj + 1], op0=ALU.mult, op1=ALU.add),
                     reads=[("tmp", 2 + c % 2), "modt", "mA"], writes=[(k, "f32", c)])
                if c == 7:
                    for s in range(n // 128):
                        gt = a // 128 + s
                        pl = PSB[5]

                        def mmfn(e, s=s):
                            ins = None
                            for kk in range(8):
                                ins = e.matmul(pl[:, 0:72], lhsT=f32t[:, kk, s * 128:(s + 1) * 128], rhs=wr[:, kk, :], start=(kk == 0), stop=(kk == 7))
                            return ins
                        P.op("pe", mmfn, reads=[(k, "f32", cc) for cc in range(8)] + [(k, "wr")], writes=[("psb", 5)])
                        R = [(k, "rt")]
                        V = lambda fn, extra_r=(): P.op("dve", fn, reads=R + list(extra_r), writes=R)
                        V(lambda e: e.tensor_tensor(out=lgs, in0=pl[:, 0:72], in1=brb, op=ALU.add), [("psb", 5), (k, "br")])
                        V(lambda e: e.tensor_reduce(out=sm[:, 0:1], in_=lgs[:, 0:8], axis=AX.X, op=ALU.max))
                        V(lambda e: e.tensor_scalar(out=oh, in0=lgs[:, 0:8], scalar1=sm[:, 0:1], scalar2=None, op0=ALU.is_equal))
                        V(lambda e: e.tensor_scalar(out=sm[:, 1:2], in0=sm[:, 0:1], scalar1=-1.0, scalar2=None, op0=ALU.mult))
                        P.op("act", lambda e: e.activation(out=sm[:, 8:16], in_=lgs[:, 0:8], func=AF.Exp, bias=sm[:, 1:2], scale=1.0), reads=R, writes=R)
                        V(lambda e: e.tensor_reduce(out=sm[:, 2:3], in_=sm[:, 8:16], axis=AX.X, op=ALU.add))
                        V(lambda e: e.reciprocal(out=sm[:, 3:4], in_=sm[:, 2:3]))
                        V(lambda e: e.tensor_scalar(out=oh, in0=oh, scalar1=BIG, scalar2=-BIG, op0=ALU.mult, op1=ALU.add))
                        for g in range(8):
                            V(lambda e, g=g: e.tensor_scalar(out=em[:, g * 8:(g + 1) * 8], in0=lgs[:, 8 + g * 8:16 + g * 8], scalar1=oh[:, g:g + 1], scalar2=None, op0=ALU.add))
                        V(lambda e: e.tensor_reduce(out=sm[:, 4:5], in_=em, axis=AX.X, op=ALU.max))
                        V(lambda e: e.tensor_scalar(out=oh1, in0=em, scalar1=sm[:, 4:5], scalar2=None, op0=ALU.is_equal))
                        V(lambda e: e.scalar_tensor_tensor(out=em, in0=oh1, scalar=-BIG, in1=em, op0=ALU.mult, op1=ALU.add))
                        V(lambda e: e.tensor_reduce(out=sm[:, 5:6], in_=em, axis=AX.X, op=ALU.max))
                        V(lambda e: e.tensor_scalar(out=oh2, in0=em, scalar1=sm[:, 5:6], scalar2=None, op0=ALU.is_equal))
                        V(lambda e: e.tensor_tensor(out=sm[:, 6:7], in0=sm[:, 5:6], in1=sm[:, 4:5], op=ALU.subtract))
                        P.op("act", lambda e: e.activation(out=sm[:, 6:7], in_=sm[:, 6:7], func=AF.Exp), reads=R, writes=R)
                        V(lambda e: e.tensor_scalar(out=sm[:, 6:7], in0=sm[:, 6:7], scalar1=1.0, scalar2=None, op0=ALU.add))
                        V(lambda e: e.reciprocal(out=sm[:, 6:7], in_=sm[:, 6:7]))
                        V(lambda e: e.tensor_tensor(out=sm[:, 6:7], in0=sm[:, 6:7], in1=sm[:, 3:4], op=ALU.mult))
                        V(lambda e: e.tensor_tensor(out=sm[:, 7:8], in0=sm[:, 3:4], in1=sm[:, 6:7], op=ALU.subtract))
                        V(lambda e: e.tensor_scalar(out=wt, in0=oh1, scalar1=sm[:, 6:7], scalar2=None, op0=ALU.mult))
                        V(lambda e: e.scalar_tensor_tensor(out=wt, in0=oh2, scalar=sm[:, 7:8], in1=wt, op0=ALU.mult, op1=ALU.add))
                        pt = PSB[4]
                        P.op("pe", lambda e: e.transpose(pt[0:64, 0:128], wt, ident[:]), reads=R + ["ident"], writes=[("psb", 4)])
                        P.op("act", lambda e, gt=gt: e.copy(out=wtT[:, gt * 128:(gt + 1) * 128], in_=pt[0:64, 0:128]), reads=[("psb", 4)], writes=[(k, "wtT", j)])

            norm_mod(1, tiles, extra=extra)

            for ex in range(64):
                eb = ex % 2
                P.op("pool", lambda e, ex=ex, eb=eb: e.dma_start(out=wgu[:, eb, :, :], in_=moe_wgu[i, ex].rearrange("(k p) n -> p k n", p=128)),
                     writes=[(k, "wgu", eb)], dma=True)
                P.op("pool", lambda e, ex=ex, eb=eb: e.dma_start(out=wd[:, eb, :, :], in_=moe_wd[i, ex].rearrange("(k p) n -> p k n", p=128)),
                     writes=[(k, "wd", eb)], dma=True)
                for j in tiles:
                    a, b = TT[j]
                    n = b - a
                    pw = PSB[6]
                    P.op("pe", lambda e, ex=ex, a=a, b=b, n=n: e.matmul(pw[:, :n], lhsT=ident[0:64, ex:ex + 1].to_broadcast([64, 128]), rhs=wtT[:, a:b], start=True, stop=True),
                         reads=["ident", (k, "wtT", j)], writes=[("psb", 6)])
                    P.op("act", lambda e, n=n: e.copy(out=wbc[:, :n], in_=pw[:, :n]), reads=[("psb", 6)], writes=[(k, "wbc")])
                    for fc in range(2):
                        pg, pu = PSB[0 + fc], PSB[2 + fc]
                        for part, pp, pk in ((0, pg, ("psb", 0 + fc)), (1, pu, ("psb", 2 + fc))):
                            def mmfn(e, part=part, pp=pp, a=a, b=b, n=n, eb=eb, fc=fc):
                                ins = None
                                for kk in range(8):
                                    ins = e.matmul(pp[:, :n], lhsT=wgu[:, eb, kk, part * 256 + fc * 128: part * 256 + (fc + 1) * 128], rhs=hT[:, kk, a:b],
                                                   start=(kk == 0), stop=(kk == 7))
                                return ins
                            P.op("pe", mmfn, reads=[(k, "wgu", eb)] + [("h", kk, j) for kk in range(8)], writes=[pk])
                        P.op("act", lambda e, pg=pg, fc=fc, n=n: e.activation(out=sgt[:, fc, :n], in_=pg[:, :n], func=AF.Silu), reads=[("psb", 0 + fc)], writes=[(k, "sgt", fc)])
                        P.op("dve", lambda e, pu=pu, fc=fc, n=n: e.tensor_tensor(out=sgt[:, fc, :n], in0=sgt[:, fc, :n], in1=pu[:, :n], op=ALU.mult), reads=[(k, "sgt", fc), ("psb", 2 + fc)], writes=[(k, "sgt", fc)])
                        P.op("pool", lambda e, fc=fc, n=n, eb=eb: e.tensor_tensor(out=actt[:, eb, fc, :n], in0=sgt[:, fc, :n], in1=wbc[:, :n], op=ALU.mult), reads=[(k, "sgt", fc), (k, "wbc")], writes=[(k, "actt", eb, fc)])
                    for oc in range(8):
                        py = PSB[4 + oc % 2]

                        def mmfn(e, py=py, oc=oc, n=n, eb=eb):
                            ins = None
                            for fc in range(2):
                                ins = e.matmul(py[:, :n], lhsT=wd[:, eb, fc, oc * 128:(oc + 1) * 128], rhs=actt[:, eb, fc, :n], start=(fc == 0), stop=(fc == 1))
                            return ins
                        P.op("pe", mmfn, reads=[(k, "wd", eb), (k, "actt", eb, 0), (k, "actt", eb, 1)], writes=[("psb", 4 + oc % 2)])
                        P.op("dve", lambda e, py=py, oc=oc, a=a, b=b, n=n, j=j: e.scalar_tensor_tensor(
                            out=xT[:, oc, a:b], in0=py[:, :n], scalar=modcol(5, oc, j), in1=xT[:, oc, a:b], op0=ALU.mult, op1=ALU.add),
                            reads=[("psb", 4 + oc % 2), "modt", ("x", oc, j)], writes=[("x", oc, j)])

        all_tiles = [0, 1, 2, 3, 4]
        for i in range(n_layers):
            last = i == DEPTH - 1
            compute_mod(i)
            phase()
            norm_mod(0, all_tiles)
            kind, jl = i % 3, i // 3
            if kind == 0:
                rglru(jl, all_tiles)
            elif kind == 1:
                mla(all_tiles)
            else:
                mlstm(all_tiles)
            if stop_after_mixer and i == n_layers - 1:
                break
            moe_sparse(i, [1, 2, 3, 4] if last else all_tiles)

        phase()
        stage = arA.alloc([128, 2, 1024], F32)
        outkeys = []
        for ti in range(18):
            jt = [j for j, (a, b) in enumerate(TT) if a <= ti * 128 < b][0]
            dst = octx_d[ti * 128:(ti + 1) * 128, :] if ti < 2 else out_d[(ti - 2) * 128:(ti - 1) * 128, :]
            sbuf = ti % 2
            for half in range(2):
                pb = PSB[half]

                def trfn(e, pb=pb, half=half, ti=ti):
                    ins = None
                    for q in range(4):
                        c = half * 4 + q
                        ins = e.transpose(pb[:, q * 128:(q + 1) * 128], xT[:, c, ti * 128:(ti + 1) * 128], ident[:])
                    return ins
                P.op("pe", trfn, reads=[("x", c, jt) for c in range(half * 4, half * 4 + 4)] + ["ident"], writes=[("psb", half)])
                P.op("dve" if half == 0 else "act",
                     lambda e, pb=pb, half=half, sbuf=sbuf: (e.tensor_copy if half == 0 else e.copy)(out=stage[:, sbuf, half * 512:(half + 1) * 512], in_=pb[:, :]),
                     reads=[("psb", half)], writes=[("ostage", sbuf, half)])
            P.op("sp", lambda e, dst=dst, sbuf=sbuf: e.dma_start(out=dst, in_=stage[:, sbuf, :]),
                 reads=[("ostage", sbuf, 0), ("ostage", sbuf, 1)], writes=[("out", ti)], dma=True)
            outkeys.append(("out", ti))
        P.finish(outkeys)
        P.emit()
    return nc


_CACHE = {}


def host_prep(inp, n_layers=DEPTH, stop_after_mixer=False):
    pv = pv_layout(inp)
    pvt = pv.table()
    key = (pvt.shape[1], n_layers, stop_after_mixer)
    if key not in _CACHE:
        _CACHE[key] = build(pv.off, pvt.shape[1], n_layers, stop_after_mixer)
    nc = _CACHE[key]
    f32 = lambda a: np.ascontiguousarray(np.asarray(a, np.float32))
    rC, rS, rRT = rope_tables()
    moec = np.zeros((128, 229), np.float32)
    moec[:, 193:229] = (np.arange(36, dtype=np.float32) * 128.0)[None, :]
    tp_, tt_ = np.meshgrid(np.arange(128), np.arange(128), indexing="ij")
    moec[:, 0:128] = (tp_ < tt_).astype(np.float32)
    moec[:, 128:192] = (np.arange(64, dtype=np.float32) * 128.0)[None, :]
    moec[:, 192] = np.arange(128, dtype=np.float32)
    selc = np.zeros((16, 2, 16, 64), np.float32)
    for r in range(16):
        selc[r, 0, r, :] = 1.0
        selc[r, 1, r, :] = -1.0
    selh = np.zeros((16, 2, 4), np.float32)
    for h in range(4):
        selh[4 + h, 0, h] = 1.0
        selh[12 + h, 1, h] = 1.0
    maskc = np.zeros((64, 2, 4, 64), np.float32)
    si, ti_ = np.meshgrid(np.arange(64), np.arange(64), indexing="ij")
    maskc[:, 0, :, :] = np.where(si <= ti_, 0.0, -30000.0)[:, None, :]
    maskc[:, 1, :, :] = np.where(si >= ti_, 0.0, -30000.0)[:, None, :]
    moe_wr = f32(np.concatenate([inp["moe_w_group"], inp["moe_w_expert"]], axis=2))
    br = np.concatenate([inp["moe_b_group"], inp["moe_b_expert"]], axis=1)
    moe_br = f32(np.repeat(br[:, None, :], 128, axis=1))
    shared = {
        "pv": pvt, "ident": np.eye(128, dtype=np.float32), "ada_w": f32(inp["ada_w"]),
        "rg_w_in": f32(inp["rg_w_in"]), "rg_gate_w": f32(inp["rg_gate_w"]), "rg_w_out": f32(inp["rg_w_out"]),
        "moe_wr": moe_wr, "moe_br": moe_br, "moec": moec,
        "mla_w_down": f32(inp["mla_w_down"][0]), "mla_w_uq": f32(inp["mla_w_uq"][0]), "mla_w_ukv": f32(inp["mla_w_ukv"][0]),
        "ml_w_in": f32(inp["ml_w_in"][0]), "ml_w_out": f32(inp["ml_w_out"][0]),
        "ml_gain": f32(np.repeat(np.asarray(inp["ml_out_norm"][0], np.float32)[None, :], 128, axis=0)),
        "ml_selc": selc, "ml_selh": selh, "ml_maskc": maskc,
        "mla_w_o": f32(inp["mla_w_o"][0]), "ropeC": rC, "ropeS": rS, "ropeRT": rRT,
        "moe_w_gate_up": f32(inp["moe_w_gate_up"]), "moe_w_down": f32(inp["moe_w_down"]),
    }
    in_maps = []
    for b in range(8):
        cc = np.zeros((128, 16), np.float32)
        cc[:, 0::2] = np.asarray(inp["c"][b], np.float32).reshape(8, 128).T
        cc[:, 1::2] = np.asarray(inp["c_ctx"], np.float32).reshape(8, 128).T
        m = dict(shared)
        m["x"] = f32(inp["x"][b])
        m["ctx"] = f32(inp["ctx"][b])
        m["cc"] = cc
        in_maps.append(m)
    return nc, in_maps


def kernel(**inputs):
    nc, in_maps = host_prep(inputs)
    res = run_bass_kernel_spmd(nc, in_maps, core_ids=list(range(8)))
    return np.stack([np.asarray(r["out"]) for r in res.results], axis=0).astype(np.float32)
```
